# Optimizing a Trainium2 kernel written in Bass

```python
import math
import jax, jax.numpy as jnp
from jax import lax
import numpy as np

D_MODEL = 2048
BATCH = 8
SEQ = 4096
DEPTH = 2

EPS = 1e-6
N_Q_HEADS_A = 12
N_KV_HEADS_A = 4
HEAD_DIM_A = 64
WINDOW = 128
BLOCK_A = 128
N_HEADS_B = 4
Q_LORA_B = 448
KV_LORA_B = 128
D_NOPE_B = 128
D_ROPE_B = 64
D_V_B = 128
ROPE_THETA = 10000.0
Q_BLOCK_B = 128
N_HEADS_C = 4
DQK_C = 96
DV_C = 192
CHUNK_C = 64
W_A = N_Q_HEADS_A * HEAD_DIM_A
W_B = N_HEADS_B * D_V_B
W_C = N_HEADS_C * DV_C
MIX_WIDTH = W_A + W_B + W_C
IN_SIZES = (N_Q_HEADS_A * HEAD_DIM_A, N_KV_HEADS_A * HEAD_DIM_A, N_KV_HEADS_A * HEAD_DIM_A,
            Q_LORA_B, KV_LORA_B, D_ROPE_B,
            N_HEADS_C * DQK_C, N_HEADS_C * DQK_C, N_HEADS_C * DV_C, 4 * N_HEADS_C, N_HEADS_C * DV_C)
IN_WIDTH = sum(IN_SIZES)
D_FF = -(-8 * D_MODEL // (3 * 256)) * 256

kernel_name = "hybrid_swa_mla_mlstm_encoder"


def _split_points(sizes):
    pts, acc = [], 0
    for s in sizes[:-1]:
        acc += s
        pts.append(acc)
    return pts


def rms_norm(x, g):
    xf = x.astype(jnp.float32)
    y = xf * lax.rsqrt(jnp.mean(xf * xf, axis=-1, keepdims=True) + EPS)
    return (y * g.astype(jnp.float32)).astype(x.dtype)


def alibi_slopes(n):
    def pow2(m):
        start = 2.0 ** (-8.0 / m)
        return [start ** (i + 1) for i in range(m)]
    if math.log2(n).is_integer():
        s = pow2(n)
    else:
        p = 2 ** math.floor(math.log2(n))
        s = pow2(p) + pow2(2 * p)[0::2][: n - p]
    return np.array(s, dtype=np.float32)


def rope_tables(positions):
    inv = 1.0 / (ROPE_THETA ** (jnp.arange(0, D_ROPE_B, 2, dtype=jnp.float32) / D_ROPE_B))
    ang = positions.astype(jnp.float32)[..., None] * inv
    return jnp.cos(ang), jnp.sin(ang)


def apply_rope(x, cos, sin):
    xf = x.astype(jnp.float32)
    x1, x2 = jnp.split(xf, 2, axis=-1)
    return jnp.concatenate([x1 * cos - x2 * sin, x1 * sin + x2 * cos], axis=-1).astype(x.dtype)


def window_attention(q, k, v, sinks):
    B, S, _ = q.shape
    nb = S // BLOCK_A
    G = N_Q_HEADS_A // N_KV_HEADS_A
    qb = q.reshape(B, nb, BLOCK_A, N_KV_HEADS_A, G, HEAD_DIM_A)
    pad = ((0, 0), (BLOCK_A, BLOCK_A), (0, 0))
    kp = jnp.pad(k, pad).reshape(B, nb + 2, BLOCK_A, N_KV_HEADS_A, HEAD_DIM_A)
    vp = jnp.pad(v, pad).reshape(B, nb + 2, BLOCK_A, N_KV_HEADS_A, HEAD_DIM_A)
    kb = jnp.concatenate([kp[:, :-2], kp[:, 1:-1], kp[:, 2:]], axis=2)
    vb = jnp.concatenate([vp[:, :-2], vp[:, 1:-1], vp[:, 2:]], axis=2)
    qpos = jnp.arange(S).reshape(nb, BLOCK_A)
    kpos = jnp.arange(-BLOCK_A, S + BLOCK_A).reshape(nb + 2, BLOCK_A)
    kpos = jnp.concatenate([kpos[:-2], kpos[1:-1], kpos[2:]], axis=1)
    dist = jnp.abs(qpos[:, :, None] - kpos[:, None, :])
    valid = (dist <= WINDOW) & (kpos[:, None, :] >= 0) & (kpos[:, None, :] < S)
    slopes = jnp.asarray(alibi_slopes(N_Q_HEADS_A)).reshape(N_KV_HEADS_A, G)
    bias = -slopes[None, :, :, None, None] * dist.astype(jnp.float32)[:, None, None, :, :]
    s = jnp.einsum('bnqhgd,bnkhd->bnhgqk', qb, kb, preferred_element_type=jnp.float32)
    s = jnp.where(valid[None, :, None, None], s * (HEAD_DIM_A ** -0.5) + bias[None], -jnp.inf)
    sink = sinks.astype(jnp.float32).reshape(N_KV_HEADS_A, G)[None, None, :, :, None]
    lse = jnp.logaddexp(jax.nn.logsumexp(s, axis=-1), sink)
    p = jnp.exp(s - lse[..., None])
    o = jnp.einsum('bnhgqk,bnkhd->bnqhgd', p.astype(v.dtype), vb)
    return o.reshape(B, S, W_A)


def mla_attention(c_q, c_kv, k_rope_in, q_norm_g, w_uq, kv_norm_g, w_ukv, cos, sin):
    B, S, _ = c_q.shape
    q = (rms_norm(c_q, q_norm_g) @ w_uq).reshape(B, S, N_HEADS_B, D_NOPE_B + D_ROPE_B)
    q_nope = q[..., :D_NOPE_B]
    q_rope = apply_rope(q[..., D_NOPE_B:], cos[:, :, None, :], sin[:, :, None, :])
    kv = (rms_norm(c_kv, kv_norm_g) @ w_ukv).reshape(B, S, N_HEADS_B, D_NOPE_B + D_V_B)
    k_nope, v = kv[..., :D_NOPE_B], kv[..., D_NOPE_B:]
    k_rope = apply_rope(k_rope_in, cos, sin)
    nb = S // Q_BLOCK_B
    qn = q_nope.reshape(B, nb, Q_BLOCK_B, N_HEADS_B, D_NOPE_B).transpose(1, 0, 2, 3, 4)
    qr = q_rope.reshape(B, nb, Q_BLOCK_B, N_HEADS_B, D_ROPE_B).transpose(1, 0, 2, 3, 4)
    scale = (D_NOPE_B + D_ROPE_B) ** -0.5

    def block(args):
        qn_b, qr_b = args
        s = (jnp.einsum('bqhd,bkhd->bhqk', qn_b, k_nope, preferred_element_type=jnp.float32)
             + jnp.einsum('bqhd,bkd->bhqk', qr_b, k_rope, preferred_element_type=jnp.float32)) * scale
        p = jax.nn.softmax(s, axis=-1)
        return jnp.einsum('bhqk,bkhd->bqhd', p.astype(v.dtype), v)

    o = lax.map(block, (qn, qr))
    return o.transpose(1, 0, 2, 3, 4).reshape(B, S, W_B)


def mlstm_direction(q, k, v, log_i, log_f):
    B, H, S, _ = q.shape
    nc, L = S // CHUNK_C, CHUNK_C
    qc = q.reshape(B, H, nc, L, DQK_C)
    kc = k.reshape(B, H, nc, L, DQK_C)
    vc = v.reshape(B, H, nc, L, DV_C)
    ic = log_i.reshape(B, H, nc, L)
    b = jnp.cumsum(log_f.reshape(B, H, nc, L), axis=-1)
    b_last = b[..., -1]
    tri = jnp.tril(jnp.ones((L, L), dtype=bool))
    dmat = jnp.where(tri, b[..., :, None] - b[..., None, :] + ic[..., None, :], -jnp.inf)
    a = b_last[..., None] - b + ic
    m_loc = jnp.max(a, axis=-1)
    w = jnp.exp(a - m_loc[..., None])
    C_loc = jnp.einsum('bhcl,bhcld,bhcle->bhcde', w, kc, vc)
    n_loc = jnp.einsum('bhcl,bhcld->bhcd', w, kc)

    def step(carry, inp):
        C, n, m = carry
        Cl, nl, ml, bl = inp
        m_new = jnp.maximum(bl + m, ml)
        sp = jnp.exp(bl + m - m_new)
        sl = jnp.exp(ml - m_new)
        C_new = sp[..., None, None] * C + sl[..., None, None] * Cl
        n_new = sp[..., None] * n + sl[..., None] * nl
        return (C_new, n_new, m_new), (C, n, m)

    init = (jnp.zeros((B, H, DQK_C, DV_C), jnp.float32), jnp.zeros((B, H, DQK_C), jnp.float32),
            jnp.full((B, H), -jnp.inf, jnp.float32))
    xs = (jnp.moveaxis(C_loc, 2, 0), jnp.moveaxis(n_loc, 2, 0),
          jnp.moveaxis(m_loc, 2, 0), jnp.moveaxis(b_last, 2, 0))
    _, (C_prev, n_prev, m_prev) = lax.scan(step, init, xs)
    C_prev = jnp.moveaxis(C_prev, 0, 2)
    n_prev = jnp.moveaxis(n_prev, 0, 2)
    m_prev = jnp.moveaxis(m_prev, 0, 2)
    inter_log = b + m_prev[..., None]
    m_t = jnp.maximum(inter_log, jnp.max(dmat, axis=-1))
    inter_w = jnp.exp(inter_log - m_t)
    qk = jnp.einsum('bhcld,bhcsd->bhcls', qc, kc) * jnp.exp(dmat - m_t[..., None])
    num = (inter_w[..., None] * jnp.einsum('bhcld,bhcde->bhcle', qc, C_prev)
           + jnp.einsum('bhcls,bhcse->bhcle', qk, vc))
    den = inter_w * jnp.einsum('bhcld,bhcd->bhcl', qc, n_prev) + jnp.sum(qk, axis=-1)
    h = num / jnp.maximum(jnp.abs(den), jnp.exp(-m_t))[..., None]
    return h.reshape(B, H, S, DV_C)


def mlstm_mixer(q, k, v, gates, o_pre, gate_b, head_g):
    B, S, _ = q.shape
    f32 = jnp.float32

    def heads(t, d):
        return t.astype(f32).reshape(B, S, N_HEADS_C, d).transpose(0, 2, 1, 3)

    qh = heads(q, DQK_C) * (DQK_C ** -0.5)
    kh = heads(k, DQK_C)
    vh = heads(v, DV_C)
    g = (gates.astype(f32) + gate_b.astype(f32)).reshape(B, S, 4, N_HEADS_C).transpose(2, 0, 3, 1)
    log_i_f, log_i_b = g[0], g[1]
    log_f_f, log_f_b = jax.nn.log_sigmoid(g[2]), jax.nn.log_sigmoid(g[3])
    h_f = mlstm_direction(qh, kh, vh, log_i_f, log_f_f)
    fl = lambda t: jnp.flip(t, axis=2)
    h_b = fl(mlstm_direction(fl(qh), fl(kh), fl(vh), fl(log_i_b), fl(log_f_b)))
    h = (h_f + h_b).transpose(0, 2, 1, 3)
    h = h * lax.rsqrt(jnp.mean(h * h, axis=-1, keepdims=True) + EPS)
    h = h * head_g.astype(f32).reshape(N_HEADS_C, DV_C)
    out = jax.nn.sigmoid(o_pre.astype(f32)) * h.reshape(B, S, W_C)
    return out.astype(q.dtype)


def setup_inputs(seed: int = 0) -> dict:
    key = jax.random.key(seed)
    ks = jax.random.split(key, 24)
    f32 = jnp.float32

    def dense(k, shape, fan_in, scale=1.0):
        return jax.random.normal(k, shape, f32) * (scale * fan_in ** -0.5)

    def gain(k, shape):
        return 1.0 + 0.05 * jax.random.normal(k, shape, f32)

    x = jax.random.normal(ks[0], (BATCH, SEQ, D_MODEL), f32)
    c = jax.random.normal(ks[1], (BATCH, D_MODEL), f32)
    offs = jax.random.randint(ks[2], (BATCH, 1), 0, 1024, dtype=jnp.int32)
    positions = offs + jnp.arange(SEQ, dtype=jnp.int32)[None, :]
    gate_b = jnp.concatenate([
        0.5 * jax.random.normal(ks[3], (DEPTH, 2 * N_HEADS_C), f32),
        3.0 + 3.0 * jax.random.uniform(ks[4], (DEPTH, 2 * N_HEADS_C), f32)], axis=-1)
    return {
        "x": x,
        "c": c,
        "positions": positions,
        "mod_w": dense(ks[5], (DEPTH, D_MODEL, 6 * D_MODEL), D_MODEL, 0.5),
        "mod_b": 0.02 * jax.random.normal(ks[6], (DEPTH, 6 * D_MODEL), f32),
        "pre_mix_g": gain(ks[7], (DEPTH, D_MODEL)),
        "post_mix_g": gain(ks[8], (DEPTH, D_MODEL)),
        "pre_ffn_g": gain(ks[9], (DEPTH, D_MODEL)),
        "post_ffn_g": gain(ks[10], (DEPTH, D_MODEL)),
        "w_in": dense(ks[11], (DEPTH, D_MODEL, IN_WIDTH), D_MODEL),
        "attn_sink": jax.random.normal(ks[12], (DEPTH, N_Q_HEADS_A), f32),
        "mla_q_norm_g": gain(ks[13], (DEPTH, Q_LORA_B)),
        "mla_w_uq": dense(ks[14], (DEPTH, Q_LORA_B, N_HEADS_B * (D_NOPE_B + D_ROPE_B)), Q_LORA_B),
        "mla_kv_norm_g": gain(ks[15], (DEPTH, KV_LORA_B)),
        "mla_w_ukv": dense(ks[16], (DEPTH, KV_LORA_B, N_HEADS_B * (D_NOPE_B + D_V_B)), KV_LORA_B),
        "mlstm_gate_b": gate_b,
        "mlstm_head_g": gain(ks[17], (DEPTH, W_C)),
        "w_out": dense(ks[18], (DEPTH, MIX_WIDTH, D_MODEL), MIX_WIDTH),
        "ffn_w_gate": dense(ks[19], (DEPTH, D_MODEL, D_FF), D_MODEL),
        "ffn_w_up": dense(ks[20], (DEPTH, D_MODEL, D_FF), D_MODEL),
        "ffn_w_down": dense(ks[21], (DEPTH, D_FF, D_MODEL), D_FF),
    }


def reference(x, c, positions, mod_w, mod_b, pre_mix_g, post_mix_g, pre_ffn_g, post_ffn_g,
              w_in, attn_sink, mla_q_norm_g, mla_w_uq, mla_kv_norm_g, mla_w_ukv,
              mlstm_gate_b, mlstm_head_g, w_out, ffn_w_gate, ffn_w_up, ffn_w_down):
    cos, sin = rope_tables(positions)
    split_pts = _split_points(IN_SIZES)
    for l in range(DEPTH):
        mod = jax.nn.silu(c) @ mod_w[l] + mod_b[l]
        shift1, scale1, gate1, shift2, scale2, gate2 = jnp.split(mod, 6, axis=-1)
        h = rms_norm(x, pre_mix_g[l]) * (1.0 + scale1[:, None, :]) + shift1[:, None, :]
        proj = h @ w_in[l]
        (aq, ak, av, bcq, bckv, bkr, cq, ck, cv, cg, co) = jnp.split(proj, split_pts, axis=-1)
        y_a = window_attention(aq, ak, av, attn_sink[l])
        y_b = mla_attention(bcq, bckv, bkr, mla_q_norm_g[l], mla_w_uq[l],
                            mla_kv_norm_g[l], mla_w_ukv[l], cos, sin)
        y_c = mlstm_mixer(cq, ck, cv, cg, co, mlstm_gate_b[l], mlstm_head_g[l])
        y = jnp.concatenate([y_a, y_b.astype(y_a.dtype), y_c.astype(y_a.dtype)], axis=-1) @ w_out[l]
        x = x + gate1[:, None, :] * rms_norm(y, post_mix_g[l])
        h = rms_norm(x, pre_ffn_g[l]) * (1.0 + scale2[:, None, :]) + shift2[:, None, :]
        f = (jax.nn.silu(h @ ffn_w_gate[l]) * (h @ ffn_w_up[l])) @ ffn_w_down[l]
        x = x + gate2[:, None, :] * rms_norm(f, post_ffn_g[l])
    return x
```

```python
import numpy as np
import concourse.bass as bass
import concourse.mybir as mybir
from concourse.bass_utils import run_bass_kernel_spmd
F32 = mybir.dt.float32
BF16 = mybir.dt.bfloat16
I32 = mybir.dt.int32
AF = mybir.ActivationFunctionType
ALU = mybir.AluOpType
AX = mybir.AxisListType


class Buf:
    __slots__ = ("name", "st", "sem", "excl")

    def __init__(self, name, excl=False):
        self.name = name
        self.st = {}
        self.sem = None
        self.excl = excl


def PB(name):
    return Buf(name, excl=True)


class Op:
    __slots__ = ("eng", "fn", "idx", "waits", "is_dma", "needs_inc", "clock", "sem", "semval")

    def __init__(self, eng, fn, is_dma):
        self.eng = eng
        self.fn = fn
        self.is_dma = is_dma
        self.waits = []
        self.needs_inc = False
        self.clock = None
        self.sem = None
        self.semval = None


class Prog:
    ENGS = ("sp", "act", "dve", "pool", "pe")

    def __init__(self, nc):
        self.nc = nc
        self.ops = []
        self.known_idx = {}
        self.known_dma = {}
        self.dma_issued = {}
        self.sems = {}
        self.ctx = []
        self.free_pool = {}
        self.npool = 0
        self.phase_bufs = []
        self.phase_start = 0

    def _sem(self, key):
        s = self.sems.get(key)
        if s is None:
            cm = self.nc.semaphore("s_" + str(key))
            s = cm.__enter__()
            self.ctx.append(cm)
            self.sems[key] = s
        return s

    @staticmethod
    def _states(buf, key, create):
        st = buf.st
        if key is None:
            if create and None not in st:
                st[None] = {"w": {}, "r": {}}
            return list(st.values())
        out = []
        if None in st:
            out.append(st[None])
        if key not in st and create:
            st[key] = {"w": {}, "r": {}}
        if key in st:
            out.append(st[key])
        return out

    def _record(self, op, reads, writes):
        op.idx = len(self.ops)
        self.ops.append(op)
        ex = [rk for rk in reads if rk[0].excl]
        if ex:
            reads = [rk for rk in reads if not rk[0].excl]
            writes = list(writes) + [rk for rk in ex if rk not in writes]
        deps = []
        for (buf, key) in reads:
            for s in self._states(buf, key, True):
                deps += [(p, "RAW") for p in s["w"].values()]
        for (buf, key) in writes:
            for s in self._states(buf, key, True):
                deps += [(p, "WAW") for p in s["w"].values()]
                deps += [(p, "WAR") for p in s["r"].values()]
        for (p, kind) in deps:
            if p is op or p.idx < self.phase_start:
                continue
            if p.is_dma:
                val = self.dma_issued[p.sem]
                if op.is_dma and op.sem == p.sem:
                    val -= 16
                k = (op.eng, p.sem)
                if self.known_dma.get(k, 0) >= val:
                    continue
                self.known_dma[k] = val
                op.waits.append(("sem", p.sem, val))
            else:
                if p.eng == op.eng:
                    if op.eng == "pe":
                        continue
                k = (op.eng, p.eng)
                if self.known_idx.get(k, -1) >= p.idx:
                    continue
                self.known_idx[k] = p.idx
                p.needs_inc = True
                op.waits.append(("op", p))
        clk = ("dma", op.sem) if op.is_dma else op.eng
        for (buf, key) in reads:
            if key is None:
                if None not in buf.st:
                    buf.st[None] = {"w": {}, "r": {}}
                buf.st[None]["r"][clk] = op
            else:
                buf.st[key]["r"][clk] = op
        for (buf, key) in writes:
            if key is None:
                buf.st.clear()
                buf.st[None] = {"w": {clk: op}, "r": {}}
            else:
                buf.st[key] = {"w": {clk: op}, "r": {}}

    def op(self, eng, fn, reads=(), writes=()):
        o = Op(eng, fn, False)
        self._record(o, reads, writes)
        return o

    def dma(self, queue, out_ap, in_ap, reads=(), writes=(), sembuf=None, **kw):
        assert sembuf is not None
        qt = "sw" if queue == "pool" else "hw"
        if sembuf.sem is None:
            sembuf.sem = {}
            self.phase_bufs.append(sembuf)
        if qt not in sembuf.sem:
            fp = self.free_pool.setdefault(qt, [])
            if fp:
                sembuf.sem[qt] = fp.pop()
            else:
                sembuf.sem[qt] = "%s_%d" % (qt, self.npool)
                self.npool += 1
        o = Op(queue, lambda e: e.dma_start(out=out_ap, in_=in_ap, **kw), True)
        o.sem = sembuf.sem[qt]
        self.dma_issued[o.sem] = self.dma_issued.get(o.sem, 0) + 16
        o.semval = self.dma_issued[o.sem]
        self._record(o, reads, writes)
        return o

    def sb(self, name, shape, dtype):
        cm = self.nc.sbuf_tensor(name, list(shape), dtype)
        t = cm.__enter__()
        self.ctx.append(cm)
        return t

    def ps(self, name, shape, dtype):
        cm = self.nc.psum_tensor(name, list(shape), dtype)
        t = cm.__enter__()
        self.ctx.append(cm)
        return t

    def emit_phase(self):
        start = getattr(self, "_emitted", 0)
        ops = self.ops[start:]
        self._emitted = len(self.ops)
        if not hasattr(self, "cnt"):
            self.cnt = {e: 0 for e in self.ENGS}
            self.tot_stats = {e: 0 for e in self.ENGS}
            self.nwaits = 0
        nc = self.nc
        cnt = self.cnt
        per = {e: [o for o in ops if o.eng == e] for e in self.ENGS}
        for e in self.ENGS:
            for o in reversed(per[e]):
                if not o.is_dma:
                    o.needs_inc = True
                    break
        for o in ops:
            if (not o.is_dma) and o.needs_inc:
                cnt[o.eng] += 1
                o.clock = cnt[o.eng]
        for e in self.ENGS:
            self._sem("eng_" + e)
            self.tot_stats[e] += len(per[e])
        for o in ops:
            if o.is_dma:
                self._sem(o.sem)
            self.nwaits += len(o.waits)

        def run(e, name):
            for o in per[name]:
                for w in o.waits:
                    if w[0] == "sem":
                        e.wait_ge(self.sems[w[1]], w[2])
                    else:
                        p = w[1]
                        e.wait_ge(self.sems["eng_" + p.eng], p.clock)
                ins = o.fn(e)
                if o.is_dma:
                    ins.then_inc(self.sems[o.sem], 16)
                elif o.needs_inc:
                    ins.then_inc(self.sems["eng_" + o.eng], 1)
            for sem, tot in self.dma_issued.items():
                if sem in self.sems:
                    e.wait_ge(self.sems[sem], tot)
            for en in self.ENGS:
                if en != name and cnt[en] > 0:
                    e.wait_ge(self.sems["eng_" + en], cnt[en])

        with nc.Block() as block:
            @block.sync
            def _(e):
                run(e, "sp")

            @block.scalar
            def _(e):
                run(e, "act")

            @block.vector
            def _(e):
                run(e, "dve")

            @block.gpsimd
            def _(e):
                run(e, "pool")

            @block.tensor
            def _(e):
                run(e, "pe")
        for b in self.phase_bufs:
            for qt, sm in b.sem.items():
                self.free_pool.setdefault(qt, []).append(sm)
            b.sem = None
        self.phase_bufs = []
        self.phase_start = len(self.ops)
        last = len(self.ops)
        for a in self.ENGS:
            for b in self.ENGS:
                self.known_idx[(a, b)] = last - 1
            for sem, tot in self.dma_issued.items():
                self.known_dma[(a, sem)] = tot

    def finish(self):
        self.stats = dict(self.tot_stats)
        self.stats["waits"] = self.nwaits
        self.stats["clock"] = dict(self.cnt)
        self.stats["maxdma"] = max(self.dma_issued.values()) if self.dma_issued else 0
        self.stats["nsem"] = len(self.sems)

    def emit(self, final_wait_bufs=()):
        nc = self.nc
        cnt = {e: 0 for e in self.ENGS}
        for o in self.ops:
            if (not o.is_dma) and o.needs_inc:
                cnt[o.eng] += 1
                o.clock = cnt[o.eng]
        per = {e: [o for o in self.ops if o.eng == e] for e in self.ENGS}
        for e in self.ENGS:
            self._sem("eng_" + e)
        for o in self.ops:
            if o.is_dma:
                self._sem(o.sem)
        self.stats = {e: len(per[e]) for e in self.ENGS}
        self.stats["waits"] = sum(len(o.waits) for o in self.ops)
        self.stats["maxclock"] = dict(cnt)
        self.stats["maxdma"] = max(self.dma_issued.values()) if self.dma_issued else 0
        self.stats["nsem"] = len(self.sems)

        def run(e, name):
            for o in per[name]:
                for w in o.waits:
                    if w[0] == "sem":
                        e.wait_ge(self.sems[w[1]], w[2])
                    else:
                        p = w[1]
                        e.wait_ge(self.sems["eng_" + p.eng], p.clock)
                ins = o.fn(e)
                if o.is_dma:
                    ins.then_inc(self.sems[o.sem], 16)
                elif o.needs_inc:
                    ins.then_inc(self.sems["eng_" + o.eng], 1)
            if name == "sp":
                for sem, tot in self.dma_issued.items():
                    e.wait_ge(self.sems[sem], tot)
                for en in self.ENGS:
                    if en != "sp" and cnt[en] > 0:
                        e.wait_ge(self.sems["eng_" + en], cnt[en])

        with nc.Block() as block:
            @block.sync
            def _(e):
                run(e, "sp")

            @block.scalar
            def _(e):
                run(e, "act")

            @block.vector
            def _(e):
                run(e, "dve")

            @block.gpsimd
            def _(e):
                run(e, "pool")

            @block.tensor
            def _(e):
                run(e, "pe")

    def close(self):
        for cm in reversed(self.ctx):
            cm.__exit__(None, None, None)
        self.ctx = []


import math

S_ = 4096
D_ = 2048
NT = 32
DFF = 5632
EPS = 1e-6
L_ = 2
NFM = 24
NTM = 1808
FM_AQ, FM_AK, FM_BCQ, FM_BCKV, FM_BKR, FM_CQ, FM_CK = 0, 6, 10, 14, 15, 16, 20


def MM(P, out, lhsT, rhs, start, stop, R, W):
    return P.op("pe", lambda e: e.matmul(out, lhsT=lhsT, rhs=rhs, start=start, stop=stop), R, W)


def TR(P, out, in_, ident, R, W):
    return P.op("pe", lambda e: e.transpose(out, in_, ident), R, W)


def ACTF(P, out, in_, func, R, W, bias=None, scale=None, accum=None):
    kw = {}
    if bias is not None:
        kw["bias"] = bias
    if scale is not None:
        kw["scale"] = scale
    if accum is not None:
        kw["accum_out"] = accum
    return P.op("act", lambda e: e.activation(out=out, in_=in_, func=func, **kw), R, W)


def TS(P, eng, out, in0, s1, s2, op0, op1, R, W):
    if op1 is None:
        return P.op(eng, lambda e: e.tensor_scalar(out=out, in0=in0, scalar1=s1, scalar2=None, op0=op0), R, W)
    return P.op(eng, lambda e: e.tensor_scalar(out=out, in0=in0, scalar1=s1, scalar2=s2, op0=op0, op1=op1), R, W)


def TT(P, eng, out, in0, in1, op, R, W):
    return P.op(eng, lambda e: e.tensor_tensor(out=out, in0=in0, in1=in1, op=op), R, W)


def STT(P, out, in0, scalar, in1, op0, op1, R, W):
    return P.op("dve", lambda e: e.scalar_tensor_tensor(out=out, in0=in0, scalar=scalar, in1=in1, op0=op0, op1=op1), R, W)


def CP(P, eng, out, in_, R, W):
    if eng == "act":
        return P.op("act", lambda e: e.activation(out=out, in_=in_, func=AF.Copy), R, W)
    return P.op(eng, lambda e: e.tensor_copy(out=out, in_=in_), R, W)


def RECIP(P, out, in_, R, W):
    return P.op("dve", lambda e: e.reciprocal(out=out, in_=in_), R, W)


def MEMSET(P, eng, ap, val, R, W):
    return P.op(eng, lambda e: e.memset(ap, val), R, W)


class Ctx:
    pass


KNOB = {}


def build_program(dbg=False, phases=None, nlayers=L_):
    nc = bass.Bass("TRN2", target_bir_lowering=False)
    P = Prog(nc)
    C = Ctx()
    C.nc, C.P, C.dbg = nc, P, dbg

    def din(name, shape, dt=F32):
        return nc.dram_tensor(name, list(shape), dt, kind="ExternalInput").ap()

    def dscr(name, shape, dt):
        return nc.dram_tensor(name, list(shape), dt, kind=("ExternalOutput" if (dbg and name in dbg) else "Internal")).ap()

    I = C.I = {}
    I["x"] = din("x", [S_, D_])
    I["cT"] = din("cT", [128, 16])
    I["pos"] = din("pos", [1, S_], I32)
    I["mod_w"] = din("mod_w", [L_, D_, 6 * D_])
    I["mod_bT"] = din("mod_bT", [L_, 128, 96])
    for g in ("pre_mix_gT", "post_mix_gT", "pre_ffn_gT", "post_ffn_gT"):
        I[g] = din(g, [L_, 128, 16])
    I["w_in_fm"] = din("w_in_fm", [L_, D_, NFM * 128])
    I["w_in_tm"] = din("w_in_tm", [L_, D_, NTM])
    I["attn_sink"] = din("attn_sink", [L_, 1, 12])
    I["q_norm_gT"] = din("q_norm_gT", [L_, 128, 4])
    I["w_uq"] = din("w_uq", [L_, 512, 768])
    I["kv_norm_gT"] = din("kv_norm_gT", [L_, 128, 1])
    I["w_ukv"] = din("w_ukv", [L_, 128, 1024])
    I["gate_b"] = din("gate_b", [L_, 1, 16])
    I["head_g"] = din("head_g", [L_, 1, 768])
    I["w_out"] = din("w_out", [L_, D_, D_])
    I["w_gate"] = din("w_gate", [L_, D_, DFF])
    I["w_up"] = din("w_up", [L_, D_, DFF])
    I["w_down"] = din("w_down", [L_, DFF, D_])
    I["c_ident"] = din("c_ident", [128, 128])
    I["c_ones"] = din("c_ones", [128, 128])
    I["c_U"] = din("c_U", [128, 128])
    I["c_L"] = din("c_L", [128, 128])
    I["c_R"] = din("c_R", [64, 64])
    I["c_E"] = din("c_E", [128, 12 * 384])
    I["c_invf"] = din("c_invf", [64, 1])
    C.out = nc.dram_tensor("out", [S_, D_], F32, kind="ExternalOutput").ap()

    D = C.D = {}
    D["XR"] = dscr("XR", [S_, D_], F32)
    D["WFM"] = dscr("WFM", [L_, D_, NFM * 128], BF16)
    D["WTM"] = dscr("WTM", [L_, D_, NTM], BF16)
    D["WOUT"] = dscr("WOUTb", [L_, D_, D_], BF16)
    D["WG"] = dscr("WGb", [L_, D_, DFF], BF16)
    D["WU"] = dscr("WUb", [L_, D_, DFF], BF16)
    D["WD"] = dscr("WDb", [L_, DFF, D_], BF16)
    D["QKT"] = dscr("QKT", [NFM, 128, S_], BF16)
    D["PTM"] = dscr("PTM", [S_, NTM], BF16)
    D["GATES"] = dscr("GATES", [S_, 16], F32)
    D["Y"] = dscr("Y", [S_, D_], BF16)
    D["QN"] = dscr("QN", [4, 128, S_], BF16)
    D["QR"] = dscr("QR", [4, 64, S_], BF16)
    D["KN"] = dscr("KN", [4, 128, S_], BF16)
    D["KR"] = dscr("KR", [64, S_], BF16)
    D["VB"] = dscr("VB", [S_, 512], BF16)
    D["BREP"] = dscr("BREP", [8, 128, S_], F32)
    D["IBS"] = dscr("IBS", [128, NT * 8], F32)
    D["MODV"] = dscr("MODV", [128, L_ * 96], F32)
    B = C.B = {k: Buf("D_" + k) for k in D}
    B["CAST"] = Buf("CAST")

    def psb(name, shape, dt):
        cm = nc.sbuf_tensor(name, list(shape), dt)
        return cm.__enter__()

    K = C.K = {}
    K["ident"] = psb("k_ident", [128, 128], BF16)
    K["identf"] = psb("k_identf", [128, 128], F32)
    K["ones"] = psb("k_ones", [128, 128], F32)
    K["onesb"] = psb("k_onesb", [128, 128], BF16)
    K["U"] = psb("k_U", [128, 128], F32)
    K["Lm"] = psb("k_L", [128, 128], F32)
    K["R"] = psb("k_R", [64, 64], BF16)
    K["mod"] = psb("k_mod", [128, L_, 96], F32)
    K["vec"] = psb("k_vec", [128, L_, 6, 16], F32)
    KB = C.KB = Buf("KCONST")
    C.MODB = Buf("MODVEC")

    C.phase_ctx = []

    def begin():
        C.phase_ctx = []
        P.ctx_mark = len(P.ctx)

    C.uid = [0]

    def sb(name, shape, dt):
        C.uid[0] += 1
        name = "%s_u%d" % (name, C.uid[0])
        cm = nc.sbuf_tensor(name, list(shape), dt)
        t = cm.__enter__()
        C.phase_ctx.append(cm)
        return t

    def ps(name, shape, dt=F32):
        C.uid[0] += 1
        name = "%s_u%d" % (name, C.uid[0])
        esz = 4 if dt == F32 else 2
        n = 1
        for d in shape[1:]:
            n *= d
        per_bank = 2048 // esz
        nb = -(-n // per_bank)
        cm = nc.psum_tensor(name, [128, nb * per_bank], dt)
        t = cm.__enter__()
        C.phase_ctx.append(cm)
        v = t[0:shape[0], 0:n]
        if len(shape) == 3:
            v = v.rearrange("p (a b) -> p a b", b=shape[2])
        return v

    def end():
        P.emit_phase()
        for cm in reversed(C.phase_ctx):
            cm.__exit__(None, None, None)
        C.phase_ctx = []

    C.begin, C.end, C.sb, C.ps = begin, end, sb, ps

    want = (lambda n: True) if phases is None else (lambda n: n in phases)

    begin()
    phase_consts(C)
    if want("cast"):
        phase_cast(C, nlayers)
    end()
    if want("mod"):
        begin()
        phase_mod(C, nlayers)
        end()
    for l in range(nlayers):
        src = I["x"] if l == 0 else D["XR"]
        if want("inproj"):
            begin(); phase_inproj(C, l, src); end()
        if want("win"):
            begin(); phase_window(C, l); end()
        if want("mla"):
            begin(); phase_mla_prep(C, l); end()
            begin(); phase_mla_attn(C, l); end()
        if want("mlstm"):
            begin(); phase_mlstm_prep(C, l); end()
            begin(); phase_mlstm_attn(C, l); end()
        if want("outproj"):
            begin(); phase_outproj(C, l, src, D["XR"]); end()
        if want("ffn"):
            dst = C.out if l == nlayers - 1 else D["XR"]
            begin(); phase_ffn(C, l, D["XR"], dst); end()
    P.finish()
    P.close()
    return nc, P


def phase_consts(C):
    P, I, K, KB = C.P, C.I, C.K, C.KB
    tmp = C.sb("c_tmp", [128, 128], F32); T = Buf("c_tmp")
    tmpR = C.sb("c_tmpR", [64, 64], F32); TRb = Buf("c_tmpR")
    P.dma("sp", K["identf"][:], I["c_ident"], writes=[(KB, "identf")], sembuf=KB)
    P.dma("sp", K["ones"][:], I["c_ones"], writes=[(KB, "ones")], sembuf=KB)
    P.dma("sp", K["U"][:], I["c_U"], writes=[(KB, "U")], sembuf=KB)
    P.dma("sp", K["Lm"][:], I["c_L"], writes=[(KB, "L")], sembuf=KB)
    P.dma("sp", tmpR[:], I["c_R"], writes=[(TRb, None)], sembuf=TRb)
    CP(P, "dve", K["ident"][:], K["identf"][:], [(KB, "identf")], [(KB, "ident")])
    CP(P, "dve", K["onesb"][:], K["ones"][:], [(KB, "ones")], [(KB, "onesb")])
    CP(P, "dve", K["R"][:], tmpR[:], [(TRb, None)], [(KB, "R")])


def phase_cast(C, nlayers):
    P, I, D, B = C.P, C.I, C.D, C.B
    NB = 4
    sf = [C.sb("c_sf%d" % i, [128, 2048], F32) for i in range(NB)]; SF = [Buf("c_sf%d" % i) for i in range(NB)]
    sb_ = [C.sb("c_sb%d" % i, [128, 2048], BF16) for i in range(NB)]; SBB = [Buf("c_sb%d" % i) for i in range(NB)]
    engs = ("act", "dve", "act", "dve")
    cnt = [0]

    def cast(dst, src, rows, cols, key):
        sv = src.rearrange("(r p) n -> p r n", p=128)
        dv = dst.rearrange("(r p) n -> p r n", p=128)
        for r in range(rows // 128):
            for c0 in range(0, cols, 2048):
                w = min(2048, cols - c0)
                i = cnt[0] % NB; cnt[0] += 1
                P.dma("sp", sf[i][:, 0:w], sv[:, r, c0:c0 + w], writes=[(SF[i], None)], sembuf=SF[i])
                CP(P, engs[i], sb_[i][:, 0:w], sf[i][:, 0:w], [(SF[i], None)], [(SBB[i], None)])
                P.dma("pool", dv[:, r, c0:c0 + w], sb_[i][:, 0:w], reads=[(SBB[i], None)], writes=[(B[key], ("cast", id(dst), r, c0))], sembuf=SBB[i])
    for l in range(nlayers):
        cast(D["WFM"][l], I["w_in_fm"][l], D_, NFM * 128, "WFM")
        cast(D["WTM"][l], I["w_in_tm"][l], D_, NTM, "WTM")
        cast(D["WOUT"][l], I["w_out"][l], D_, D_, "WOUT")
        cast(D["WG"][l], I["w_gate"][l], D_, DFF, "WG")
        cast(D["WU"][l], I["w_up"][l], D_, DFF, "WU")
        cast(D["WD"][l], I["w_down"][l], DFF, D_, "WD")


def phase_mod(C, nlayers):
    P, I, K = C.P, C.I, C.K
    cT = C.sb("m_cT", [128, 16], F32); CT = Buf("m_cT")
    sc = C.sb("m_silu", [128, 16, 2], F32); SC = Buf("m_silu")
    ws = [C.sb("m_w%d" % i, [128, 16, 512], F32) for i in range(2)]
    WS = [Buf("m_w%d" % i) for i in range(2)]
    mb = C.sb("m_b", [128, 96], F32); MB = Buf("m_b")
    gT = C.sb("m_g", [128, 4, 16], F32); GT = Buf("m_g")
    mps = C.ps("m_ps", [128, 96, 2]); MPS = PB("m_ps")
    MODB = C.MODB
    P.dma("sp", cT[:], I["cT"], writes=[(CT, None)], sembuf=CT)
    ACTF(P, sc[:, :, 0], cT[:], AF.Silu, [(CT, None)], [(SC, 0)])
    ACTF(P, sc[:, :, 1], cT[:], AF.Silu, [(CT, None)], [(SC, 1)])
    for l in range(nlayers):
        wv = I["mod_w"][l].rearrange("(kc p) n -> p kc n", p=128)
        P.dma("sp", mb[:], I["mod_bT"][l], writes=[(MB, None)], sembuf=MB)
        for gi, g in enumerate(("pre_mix_gT", "post_mix_gT", "pre_ffn_gT", "post_ffn_gT")):
            P.dma("sp", gT[:, gi, :], I[g][l], writes=[(GT, gi)], sembuf=GT)
        for s in range(24):
            w = ws[s % 2]
            P.dma("sp", w[:], wv[:, :, s * 512:(s + 1) * 512], writes=[(WS[s % 2], None)], sembuf=WS[s % 2])
            for j in range(4):
                col = s * 4 + j
                for kc in range(16):
                    MM(P, mps[:, col, :], w[:, kc, j * 128:(j + 1) * 128], sc[:, kc, :], kc == 0, kc == 15,
                       [(WS[s % 2], None), (SC, None)], [(MPS, None)])
        mod = K["mod"][:, l, :]
        TT(P, "dve", mod, mps[:, :, 0], mb[:], ALU.add, [(MPS, None), (MB, None)], [(MODB, (l, "mod"))])
        if C.dbg and "MODV" in C.dbg:
            P.dma("pool", C.D["MODV"][:, l * 96:(l + 1) * 96], mod, reads=[(MODB, (l, "mod"))], writes=[(C.B["MODV"], l)], sembuf=MB)
        vec = K["vec"]
        for half, (gpre, gpost) in enumerate(((0, 1), (2, 3))):
            o = half * 48
            STT(P, vec[:, l, half * 3 + 0, :], mod[:, o + 16:o + 32], 1.0, gT[:, gpre, :], ALU.add, ALU.mult,
                [(MODB, (l, "mod")), (GT, gpre)], [(MODB, (l, half, 0))])
            CP(P, "dve", vec[:, l, half * 3 + 1, :], mod[:, o:o + 16], [(MODB, (l, "mod"))], [(MODB, (l, half, 1))])
            TT(P, "dve", vec[:, l, half * 3 + 2, :], mod[:, o + 32:o + 48], gT[:, gpost, :], ALU.mult,
               [(MODB, (l, "mod")), (GT, gpost)], [(MODB, (l, half, 2))])


def replicate_cols(C, rep, REP, colvec, ncols, R, psb, PSB, tmp, TMP):
    P, K, KB = C.P, C.K, C.KB
    for c0 in range(0, ncols, 4):
        n = min(4, ncols - c0)
        for j in range(n):
            c = c0 + j
            TS(P, "dve", tmp[:, j * 128:(j + 1) * 128], K["identf"][:], colvec[:, c:c + 1], None, ALU.mult, None,
               R + [(KB, "identf")], [(TMP, j)])
            MM(P, psb[:, j * 128:(j + 1) * 128], K["ones"][:], tmp[:, j * 128:(j + 1) * 128], True, True,
               [(TMP, j), (KB, "ones")], [(PSB, None)])
        CP(P, "act", rep[:, c0 * 128:(c0 + n) * 128], psb[:, 0:n * 128], [(PSB, None)], [(REP, c0 // 4)])


def norm_to_hT(C, t, ti, src_ap, SRCB, xt, XT, ss, rs, SS, xn, XN, junk, JK, tp, TP, hTb, HTB, A, Sv, VR, keep_x=None):
    P, K, KB = C.P, C.K, C.KB
    P.dma("sp", xt[:], src_ap[t * 128:(t + 1) * 128, :], reads=[(SRCB, t)], writes=[(XT, None)], sembuf=XT)
    ACTF(P, junk[:], xt[:], AF.Square, [(XT, None)], [(JK, None), (SS, "ss")], accum=ss[:])
    ACTF(P, rs[:], ss[:], AF.Sqrt, [(SS, "ss")], [(SS, "rs")], bias=EPS, scale=1.0 / D_)
    RECIP(P, rs[:], rs[:], [(SS, "rs")], [(SS, "rs")])
    TS(P, "dve", xn[:], xt[:], rs[:, 0:1], None, ALU.mult, None, [(XT, None), (SS, "rs")], [(XN, None)])
    for c in range(16):
        TR(P, tp[:, c * 128:(c + 1) * 128], xn[:, c * 128:(c + 1) * 128], K["ident"][:], [(XN, None), (KB, "ident")], [(TP, c // 8)])
    for c in range(16):
        o = hTb[:, c, ti * 128:(ti + 1) * 128]
        i_ = tp[:, c * 128:(c + 1) * 128]
        if c < 8:
            ACTF(P, o, i_, AF.Identity, [(TP, c // 8)] + VR, [(HTB, (ti, c))], bias=Sv[:, c:c + 1], scale=A[:, c:c + 1])
        else:
            TS(P, "dve", o, i_, A[:, c:c + 1], Sv[:, c:c + 1], ALU.mult, ALU.add, [(TP, c // 8)] + VR, [(HTB, (ti, c))])


def phase_inproj(C, l, src):
    P, I, D, B, K = C.P, C.I, C.D, C.B, C.K
    SRCB = B["XR"]
    xt = [C.sb("a_xt%d" % i, [128, D_], F32) for i in range(2)]; XT = [Buf("a_xt%d" % i) for i in range(2)]
    junk = C.sb("a_junk", [128, D_], BF16); JK = Buf("a_junk")
    ss = [C.sb("a_ss%d" % i, [128, 1], F32) for i in range(2)]
    rs = [C.sb("a_rs%d" % i, [128, 1], F32) for i in range(2)]; SS = [Buf("a_ss%d" % i) for i in range(2)]
    xn = [C.sb("a_xn%d" % i, [128, D_], BF16) for i in range(2)]; XN = [Buf("a_xn%d" % i) for i in range(2)]
    tp = [C.ps("a_tp%d" % i, [128, D_], BF16) for i in range(1)]; TP = [PB("a_tp%d" % i) for i in range(1)]
    hT = [C.sb("a_hT%d" % i, [128, 16, 512], BF16) for i in range(2)]; HT = [Buf("a_hT%d" % i) for i in range(2)]
    ws = [C.sb("a_ws%d" % i, [128, 16, 512], BF16) for i in range(2)]; WS = [Buf("a_ws%d" % i) for i in range(2)]
    pm = [C.ps("a_pm%d" % i, [128, 512]) for i in range(4)]; PM = [PB("a_pm%d" % i) for i in range(4)]
    st = [C.sb("a_st%d" % i, [128, 512], BF16) for i in range(4)]; ST = [Buf("a_st%d" % i) for i in range(4)]
    sg = [C.sb("a_sg%d" % i, [128, 16], F32) for i in range(2)]; SG = [Buf("a_sg%d" % i) for i in range(2)]
    A = K["vec"][:, l, 0, :]; Sv = K["vec"][:, l, 1, :]
    VR = [(C.MODB, (l, 0, 0)), (C.MODB, (l, 0, 1))]
    wfm = D["WFM"][l].rearrange("(kc p) n -> p kc n", p=128)
    wtm = D["WTM"][l].rearrange("(kc p) n -> p kc n", p=128)
    cnt = [0, 0, 0]

    def norm_block(blk):
        for ti in range(4):
            t = blk * 4 + ti
            s = t % 2
            norm_to_hT(C, t, ti, src, SRCB, xt[s], XT[s], ss[s], rs[s], SS[s], xn[s], XN[s], junk, JK, tp[0], TP[0],
                       hT[blk % 2], HT[blk % 2], A, Sv, VR)

    def gemm_block(blk):
        hTb, HTB = hT[blk % 2], HT[blk % 2]
        for s in range(6 if KNOB.get("fm", True) else 0):
            wi = cnt[0] % 2; cnt[0] += 1
            P.dma("sp", ws[wi][:], wfm[:, :, s * 512:(s + 1) * 512], reads=[(B["WFM"], None)], writes=[(WS[wi], None)], sembuf=WS[wi])
            for j in range(4):
                ch = s * 4 + j
                pi = cnt[1] % 4; cnt[1] += 1
                for kc in range(16):
                    MM(P, pm[pi][:], ws[wi][:, kc, j * 128:(j + 1) * 128], hTb[:, kc, :], kc == 0, kc == 15,
                       [(WS[wi], None), (HTB, None)], [(PM[pi], None)])
                CP(P, "act" if pi % 2 == 0 else "dve", st[pi][:], pm[pi][:], [(PM[pi], None)], [(ST[pi], None)])
                P.dma("pool", D["QKT"][ch][:, blk * 512:(blk + 1) * 512], st[pi][:], reads=[(ST[pi], None)],
                      writes=[(B["QKT"], (ch, blk))], sembuf=ST[pi])
        for s in range(4 if KNOB.get("tm", True) else 0):
            n0 = s * 512
            ncol = min(512, NTM - n0)
            wi = cnt[0] % 2; cnt[0] += 1
            P.dma("sp", ws[wi][:, :, 0:ncol], wtm[:, :, n0:n0 + ncol], reads=[(B["WTM"], None)], writes=[(WS[wi], None)], sembuf=WS[wi])
            for ti in range(4):
                t = blk * 4 + ti
                pi = cnt[1] % 4; cnt[1] += 1
                for kc in range(16):
                    MM(P, pm[pi][:, 0:ncol], hTb[:, kc, ti * 128:(ti + 1) * 128], ws[wi][:, kc, 0:ncol], kc == 0, kc == 15,
                       [(WS[wi], None), (HTB, None)], [(PM[pi], None)])
                CP(P, "act" if pi % 2 == 0 else "dve", st[pi][:, 0:ncol], pm[pi][:, 0:ncol], [(PM[pi], None)], [(ST[pi], None)])
                P.dma("pool", D["PTM"][t * 128:(t + 1) * 128, n0:n0 + ncol], st[pi][:, 0:ncol], reads=[(ST[pi], None)],
                      writes=[(B["PTM"], (t, s))], sembuf=ST[pi])
                if s == 3:
                    gi = cnt[2] % 2; cnt[2] += 1
                    CP(P, "dve", sg[gi][:], pm[pi][:, ncol - 16:ncol], [(PM[pi], None)], [(SG[gi], None)])
                    P.dma("pool", D["GATES"][t * 128:(t + 1) * 128, :], sg[gi][:], reads=[(SG[gi], None)],
                          writes=[(B["GATES"], t)], sembuf=SG[gi])

    nblk = KNOB.get("nblk", 8)
    norm_block(0)
    for blk in range(nblk):
        if blk + 1 < nblk:
            norm_block(blk + 1)
        if KNOB.get("gemm", True):
            gemm_block(blk)


def phase_window(C, l):
    P, I, D, B, K = C.P, C.I, C.D, C.B, C.K
    E = C.sb("w_E", [128, 12, 384], F32); EB = Buf("w_E")
    snk = C.sb("w_snk", [128, 12], F32); SK = Buf("w_snk")
    qt = [C.sb("w_q%d" % i, [128, 6, 128], BF16) for i in range(2)]; QT = [Buf("w_q%d" % i) for i in range(2)]
    kt = [C.sb("w_k%d" % i, [128, 4, 384], BF16) for i in range(2)]; KT = [Buf("w_k%d" % i) for i in range(2)]
    vt = [C.sb("w_v%d" % i, [128, 3, 4, 65], BF16) for i in range(2)]; VT = [Buf("w_v%d" % i) for i in range(2)]
    pss = [C.ps("w_ps%d" % i, [128, 512]) for i in range(2)]; PSS = [PB("w_ps%d" % i) for i in range(2)]
    acc = [C.ps("w_acc%d" % i, [128, 512]) for i in range(2)]; ACC = [PB("w_acc%d" % i) for i in range(2)]
    pe_ = [C.sb("w_pe%d" % i, [128, 384], F32) for i in range(2)]; PEB = [Buf("w_pe%d" % i) for i in range(2)]
    pT = [C.sb("w_pT%d" % i, [128, 384], BF16) for i in range(2)]; PT = [Buf("w_pT%d" % i) for i in range(2)]
    den = [C.sb("w_den%d" % i, [128, 12], F32) for i in range(2)]; DEN = [Buf("w_den%d" % i) for i in range(2)]
    ya = [C.sb("w_ya%d" % i, [128, 768], BF16) for i in range(2)]; YA = [Buf("w_ya%d" % i) for i in range(2)]
    P.dma("sp", E[:].rearrange("p h c -> p (h c)"), I["c_E"], writes=[(EB, None)], sembuf=EB)
    P.dma("sp", snk[:], I["attn_sink"][l].partition_broadcast(128), writes=[(SK, None)], sembuf=SK)
    ACTF(P, snk[:], snk[:], AF.Exp, [(SK, None)], [(SK, None)])
    for i in range(2):
        MEMSET(P, "dve", vt[i][:], 1.0, [], [(VT[i], None)])
    qk = D["QKT"].rearrange("c p t -> p c t")
    scale = 64 ** -0.5
    hc = 0
    for i in range(NT):
        s = i % 2
        j0, j1 = max(0, i - 1), min(NT - 1, i + 1)
        d0, d1 = j0 - (i - 1), j1 - (i - 1)
        P.dma("sp", qt[s][:], qk[:, FM_AQ:FM_AQ + 6, i * 128:(i + 1) * 128], reads=[(B["QKT"], None)], writes=[(QT[s], None)], sembuf=QT[s])
        P.dma("sp", kt[s][:, :, d0 * 128:(d1 + 1) * 128], qk[:, FM_AK:FM_AK + 4, j0 * 128:(j1 + 1) * 128], reads=[(B["QKT"], None)],
              writes=[(KT[s], None)], sembuf=KT[s])
        for d in range(d0, d1 + 1):
            j = i - 1 + d
            P.dma("sp", vt[s][:, d, :, 0:64], D["PTM"][j * 128:(j + 1) * 128, 0:256].rearrange("p (h d) -> p h d", d=64),
                  reads=[(B["PTM"], None)], writes=[(VT[s], None)], sembuf=VT[s])
        lo, hi = d0 * 128, (d1 + 1) * 128
        for hq in range(12):
            g = hq // 3; off = (hq % 2) * 64; c = hq // 2
            b = hc % 2; hc += 1
            for d in range(d0, d1 + 1):
                MM(P, pss[b][:, d * 128:(d + 1) * 128], kt[s][off:off + 64, g, d * 128:(d + 1) * 128], qt[s][off:off + 64, c, :], True, True,
                   [(KT[s], None), (QT[s], None)], [(PSS[b], None)])
            ACTF(P, pe_[b][:, lo:hi], pss[b][:, lo:hi], AF.Exp, [(PSS[b], None)], [(PEB[b], None)], scale=scale)
            TT(P, "dve", pT[b][:, lo:hi], pe_[b][:, lo:hi], E[:, hq, lo:hi], ALU.mult, [(PEB[b], None), (EB, None)], [(PT[b], None)])
            a = acc[hq // 6]; AB = ACC[hq // 6]
            co = (hq % 6) * 65
            for d in range(d0, d1 + 1):
                MM(P, a[:, co:co + 65], pT[b][:, d * 128:(d + 1) * 128], vt[s][:, d, g, :], d == d0, d == d1,
                   [(PT[b], None), (VT[s], None)], [(AB, None)])
        for hq in range(12):
            a = acc[hq // 6]; AB = ACC[hq // 6]; co = (hq % 6) * 65
            TS(P, "dve", den[s][:, hq:hq + 1], a[:, co + 64:co + 65], snk[:, hq:hq + 1], None, ALU.add, None, [(AB, None), (SK, None)], [(DEN[s], None)])
        RECIP(P, den[s][:], den[s][:], [(DEN[s], None)], [(DEN[s], None)])
        for hq in range(12):
            a = acc[hq // 6]; AB = ACC[hq // 6]; co = (hq % 6) * 65
            TS(P, "dve", ya[s][:, hq * 64:(hq + 1) * 64], a[:, co:co + 64], den[s][:, hq:hq + 1], None, ALU.mult, None,
               [(AB, None), (DEN[s], None)], [(YA[s], None)])
        P.dma("pool", D["Y"][i * 128:(i + 1) * 128, 0:768], ya[s][:], reads=[(YA[s], None)], writes=[(B["Y"], ("a", i))], sembuf=YA[s])


def rep_sumsq(C, sq_chunks, R, rep_ps, RPS, rstd, RSTD, n, width):
    P, K, KB = C.P, C.K, C.KB
    for i, (ap, rows) in enumerate(sq_chunks):
        MM(P, rep_ps[:, 0:width], K["onesb"][0:rows, :], ap, i == 0, i == len(sq_chunks) - 1, R + [(KB, "onesb")], [(RPS, None)])
    ACTF(P, rstd[:, 0:width], rep_ps[:, 0:width], AF.Sqrt, [(RPS, None)], [(RSTD, None)], bias=EPS, scale=1.0 / n)
    RECIP(P, rstd[:, 0:width], rstd[:, 0:width], [(RSTD, None)], [(RSTD, None)])


def phase_mla_prep(C, l):
    P, I, D, B, K, KB = C.P, C.I, C.D, C.B, C.K, C.KB
    wqf = C.sb("p_wqf", [128, 4, 768], F32); WQF = Buf("p_wqf")
    wq = C.sb("p_wq", [128, 4, 768], BF16); WQ = Buf("p_wq")
    wkf = C.sb("p_wkf", [128, 1024], F32); WKF = Buf("p_wkf")
    wk = C.sb("p_wk", [128, 1024], BF16); WK = Buf("p_wk")
    gq = C.sb("p_gq", [128, 4], F32); GQ = Buf("p_gq")
    gk = C.sb("p_gk", [128, 1], F32); GK = Buf("p_gk")
    P.dma("sp", wqf[:], I["w_uq"][l].rearrange("(kc p) n -> p kc n", p=128), writes=[(WQF, None)], sembuf=WQF)
    P.dma("sp", wkf[:], I["w_ukv"][l], writes=[(WKF, None)], sembuf=WKF)
    P.dma("sp", gq[:], I["q_norm_gT"][l], writes=[(GQ, None)], sembuf=GQ)
    P.dma("sp", gk[:], I["kv_norm_gT"][l], writes=[(GK, None)], sembuf=GK)
    for kc in range(4):
        TS(P, "dve", wq[:, kc, :], wqf[:, kc, :], gq[:, kc:kc + 1], None, ALU.mult, None, [(WQF, None), (GQ, None)], [(WQ, kc)])
    TS(P, "dve", wk[:], wkf[:], gk[:, 0:1], None, ALU.mult, None, [(WKF, None), (GK, None)], [(WK, None)])
    posi = C.sb("p_posi", [64, S_], I32); POSI = Buf("p_posi")
    ang = C.sb("p_ang", [64, S_], F32); ANG = Buf("p_ang")
    cosT = C.sb("p_cos", [64, S_], F32); COS = Buf("p_cos")
    sinT = C.sb("p_sin", [64, S_], F32); SIN = Buf("p_sin")
    invf = C.sb("p_invf", [64, 1], F32); INVF = Buf("p_invf")
    P.dma("sp", posi[:], I["pos"].partition_broadcast(64), writes=[(POSI, None)], sembuf=POSI)
    P.dma("sp", invf[:], I["c_invf"], writes=[(INVF, None)], sembuf=INVF)
    CP(P, "dve", ang[:], posi[:], [(POSI, None)], [(ANG, None)])
    TS(P, "dve", ang[:], ang[:], invf[:, 0:1], None, ALU.mult, None, [(ANG, None), (INVF, None)], [(ANG, None)])
    TWO_PI = 2.0 * math.pi
    MAGIC = 12582912.0
    TS(P, "dve", sinT[:], ang[:], 1.0 / TWO_PI, MAGIC, ALU.mult, ALU.add, [(ANG, None)], [(SIN, None)])
    TS(P, "dve", sinT[:], sinT[:], -MAGIC, None, ALU.add, None, [(SIN, None)], [(SIN, None)])
    STT(P, sinT[:], sinT[:], -TWO_PI, ang[:], ALU.mult, ALU.add, [(SIN, None), (ANG, None)], [(SIN, None)])
    TS(P, "dve", ang[:], ang[:], 0.5 * math.pi, None, ALU.add, None, [(ANG, None)], [(ANG, None)])
    TS(P, "dve", cosT[:], ang[:], 1.0 / TWO_PI, MAGIC, ALU.mult, ALU.add, [(ANG, None)], [(COS, None)])
    TS(P, "dve", cosT[:], cosT[:], -MAGIC, None, ALU.add, None, [(COS, None)], [(COS, None)])
    STT(P, cosT[:], cosT[:], -TWO_PI, ang[:], ALU.mult, ALU.add, [(COS, None), (ANG, None)], [(COS, None)])
    PI_LO = 3.1415925
    TS(P, "dve", sinT[:], sinT[:], -PI_LO, PI_LO, ALU.max, ALU.min, [(SIN, None)], [(SIN, None)])
    TS(P, "dve", cosT[:], cosT[:], -PI_LO, PI_LO, ALU.max, ALU.min, [(COS, None)], [(COS, None)])
    ACTF(P, sinT[:], sinT[:], AF.Sin, [(SIN, None)], [(SIN, None)])
    ACTF(P, cosT[:], cosT[:], AF.Sin, [(COS, None)], [(COS, None)])

    cq = [C.sb("p_cq%d" % i, [128, 4, 512], BF16) for i in range(2)]; CQ = [Buf("p_cq%d" % i) for i in range(2)]
    ckv = [C.sb("p_ckv%d" % i, [128, 512], BF16) for i in range(2)]; CKV = [Buf("p_ckv%d" % i) for i in range(2)]
    kr = [C.sb("p_kr%d" % i, [64, 512], BF16) for i in range(2)]; KRB = [Buf("p_kr%d" % i) for i in range(2)]
    sq = C.sb("p_sq", [128, 4, 512], BF16); SQ = Buf("p_sq")
    sqk = C.sb("p_sqk", [128, 512], BF16); SQK = Buf("p_sqk")
    rps = C.ps("p_rps", [128, 512]); RPS = PB("p_rps")
    rstd = C.sb("p_rstd", [128, 512], F32); RSTD = Buf("p_rstd")
    rstdk = C.sb("p_rstdk", [128, 512], F32); RSTDK = Buf("p_rstdk")
    pq = [C.ps("p_pq%d" % i, [128, 512]) for i in range(3)]; PQ = [PB("p_pq%d" % i) for i in range(3)]
    prot = C.ps("p_prot", [64, 512]); PROT = PB("p_prot")
    pv = C.ps("p_pv", [128, 512]); PV = PB("p_pv")
    ptm = C.ps("p_ptm", [128, 4, 2]); PTM_ = PB("p_ptm")
    so = [C.sb("p_so%d" % i, [128, 512], BF16) for i in range(3)]; SO = [Buf("p_so%d" % i) for i in range(3)]
    t1 = C.sb("p_t1", [64, 512], F32); T1 = Buf("p_t1")
    t2 = C.sb("p_t2", [64, 512], F32); T2 = Buf("p_t2")
    raw = C.sb("p_raw", [64, 512], BF16); RAW = Buf("p_raw")
    rtm = C.sb("p_rtm", [128, 4], F32); RTM = Buf("p_rtm")
    vo = [C.sb("p_vo%d" % i, [128, 512], BF16) for i in range(2)]; VO = [Buf("p_vo%d" % i) for i in range(2)]
    qk = D["QKT"].rearrange("c p t -> p c t")
    oc = [0]

    def rope_out(src_ps, SRC, rst, RST, blk, dst_ap, DSTB, dkey):
        cs = slice(blk * 512, (blk + 1) * 512)
        if rst is not None:
            TT(P, "dve", raw[:], src_ps, rst[0:64, :], ALU.mult, [(SRC, None), (RST, None)], [(RAW, None)])
        else:
            CP(P, "dve", raw[:], src_ps, [(SRC, None)], [(RAW, None)])
        MM(P, prot[:], K["R"][:], raw[:], True, True, [(RAW, None), (KB, "R")], [(PROT, None)])
        TT(P, "dve", t1[:], raw[:], cosT[:, cs], ALU.mult, [(RAW, None), (COS, None)], [(T1, None)])
        TT(P, "dve", t2[:], prot[:], sinT[:, cs], ALU.mult, [(PROT, None), (SIN, None)], [(T2, None)])
        o = oc[0] % 3; oc[0] += 1
        TT(P, "dve", so[o][0:64, :], t1[:], t2[:], ALU.add, [(T1, None), (T2, None)], [(SO[o], None)])
        P.dma("pool", dst_ap, so[o][0:64, :], reads=[(SO[o], None)], writes=[(DSTB, dkey)], sembuf=SO[o])

    for blk in range(8):
        s = blk % 2
        cs = slice(blk * 512, (blk + 1) * 512)
        P.dma("sp", cq[s][:], qk[:, FM_BCQ:FM_BCQ + 4, cs], reads=[(B["QKT"], None)], writes=[(CQ[s], None)], sembuf=CQ[s])
        P.dma("sp", ckv[s][:], D["QKT"][FM_BCKV][:, cs], reads=[(B["QKT"], None)], writes=[(CKV[s], None)], sembuf=CKV[s])
        P.dma("sp", kr[s][:], D["QKT"][FM_BKR][0:64, cs], reads=[(B["QKT"], None)], writes=[(KRB[s], None)], sembuf=KRB[s])
        rows = [128, 128, 128, 64]
        ACTF(P, sq[:], cq[s][:], AF.Square, [(CQ[s], None)], [(SQ, None)])
        rep_sumsq(C, [(sq[0:rows[kc], kc, :], rows[kc]) for kc in range(4)], [(SQ, None)], rps, RPS, rstd, RSTD, 448.0, 512)
        for h in range(4):
            pi = h % 3
            for kc in range(4):
                MM(P, pq[pi][:], wq[0:rows[kc], kc, h * 192:h * 192 + 128], cq[s][0:rows[kc], kc, :], kc == 0, kc == 3,
                   [(WQ, None), (CQ[s], None)], [(PQ[pi], None)])
            o = oc[0] % 3; oc[0] += 1
            TT(P, "dve", so[o][:], pq[pi][:], rstd[:], ALU.mult, [(PQ[pi], None), (RSTD, None)], [(SO[o], None)])
            P.dma("pool", D["QN"][h][:, cs], so[o][:], reads=[(SO[o], None)], writes=[(B["QN"], (h, blk))], sembuf=SO[o])
            pi = (h + 1) % 3
            for kc in range(4):
                MM(P, pq[pi][0:64, :], wq[0:rows[kc], kc, h * 192 + 128:h * 192 + 192], cq[s][0:rows[kc], kc, :], kc == 0, kc == 3,
                   [(WQ, None), (CQ[s], None)], [(PQ[pi], None)])
            rope_out(pq[pi][0:64, :], PQ[pi], rstd, RSTD, blk, D["QR"][h][:, cs], B["QR"], (h, blk))
        ACTF(P, sqk[:], ckv[s][:], AF.Square, [(CKV[s], None)], [(SQK, None)])
        rep_sumsq(C, [(sqk[:], 128)], [(SQK, None)], rps, RPS, rstdk, RSTDK, 128.0, 512)
        for h in range(4):
            pi = h % 3
            MM(P, pq[pi][:], wk[:, h * 256:h * 256 + 128], ckv[s][:], True, True, [(WK, None), (CKV[s], None)], [(PQ[pi], None)])
            o = oc[0] % 3; oc[0] += 1
            TT(P, "dve", so[o][:], pq[pi][:], rstdk[:], ALU.mult, [(PQ[pi], None), (RSTDK, None)], [(SO[o], None)])
            P.dma("pool", D["KN"][h][:, cs], so[o][:], reads=[(SO[o], None)], writes=[(B["KN"], (h, blk))], sembuf=SO[o])
        for ti in range(4):
            MM(P, ptm[:, ti, :], sqk[:, ti * 128:(ti + 1) * 128], K["onesb"][:, 0:2], True, True, [(SQK, None), (KB, "onesb")], [(PTM_, None)])
        ACTF(P, rtm[:], ptm[:, :, 0], AF.Sqrt, [(PTM_, None)], [(RTM, None)], bias=EPS, scale=1.0 / 128.0)
        RECIP(P, rtm[:], rtm[:], [(RTM, None)], [(RTM, None)])
        for ti in range(4):
            t = blk * 4 + ti
            for h in range(4):
                MM(P, pv[:, h * 128:(h + 1) * 128], ckv[s][:, ti * 128:(ti + 1) * 128], wk[:, h * 256 + 128:h * 256 + 256], True, True,
                   [(WK, None), (CKV[s], None)], [(PV, None)])
            v = t % 2
            TS(P, "dve", vo[v][:], pv[:], rtm[:, ti:ti + 1], None, ALU.mult, None, [(PV, None), (RTM, None)], [(VO[v], None)])
            P.dma("pool", D["VB"][t * 128:(t + 1) * 128, :], vo[v][:], reads=[(VO[v], None)], writes=[(B["VB"], t)], sembuf=VO[v])
        MM(P, prot[:], K["R"][:], kr[s][:], True, True, [(KRB[s], None), (KB, "R")], [(PROT, None)])
        TT(P, "dve", t1[:], kr[s][:], cosT[:, cs], ALU.mult, [(KRB[s], None), (COS, None)], [(T1, None)])
        TT(P, "dve", t2[:], prot[:], sinT[:, cs], ALU.mult, [(PROT, None), (SIN, None)], [(T2, None)])
        o = oc[0] % 3; oc[0] += 1
        TT(P, "dve", so[o][0:64, :], t1[:], t2[:], ALU.add, [(T1, None), (T2, None)], [(SO[o], None)])
        P.dma("pool", D["KR"][:, cs], so[o][0:64, :], reads=[(SO[o], None)], writes=[(B["KR"], blk)], sembuf=SO[o])


def phase_mla_attn(C, l):
    P, I, D, B, K = C.P, C.I, C.D, C.B, C.K
    krT = C.sb("m_kr", [64, S_], BF16); KRT = Buf("m_kr")
    qn = C.sb("m_qn", [128, S_], BF16); QN = Buf("m_qn")
    qr = C.sb("m_qr", [64, S_], BF16); QR = Buf("m_qr")
    kn = C.sb("m_kn", [128, S_], BF16); KN = Buf("m_kn")
    va = C.sb("m_va", [128, NT, 129], BF16); VA = Buf("m_va")
    pss = [C.ps("m_ps%d" % i, [128, 512]) for i in range(2)]; PSS = [PB("m_ps%d" % i) for i in range(2)]
    acc = [C.ps("m_acc%d" % i, [128, 512]) for i in range(4)]; ACC = [PB("m_acc%d" % i) for i in range(4)]
    pT = [C.sb("m_pT%d" % i, [128, 512], BF16) for i in range(3)]; PT = [Buf("m_pT%d" % i) for i in range(3)]
    rd = [C.sb("m_rd%d" % i, [128, 1], F32) for i in range(2)]; RD = [Buf("m_rd%d" % i) for i in range(2)]
    yo = [C.sb("m_yo%d" % i, [128, 128], BF16) for i in range(2)]; YO = [Buf("m_yo%d" % i) for i in range(2)]
    scale = 192 ** -0.5
    P.dma("sp", krT[:], D["KR"], reads=[(B["KR"], None)], writes=[(KRT, None)], sembuf=KRT)
    MEMSET(P, "dve", va[:], 1.0, [], [(VA, "ones")])
    n = 0
    oc = 0
    for h in range(4):
        P.dma("sp", qn[:], D["QN"][h], reads=[(B["QN"], None)], writes=[(QN, None)], sembuf=QN)
        P.dma("sp", qr[:], D["QR"][h], reads=[(B["QR"], None)], writes=[(QR, None)], sembuf=QR)
        P.dma("sp", kn[:], D["KN"][h], reads=[(B["KN"], None)], writes=[(KN, None)], sembuf=KN)
        vbv = D["VB"][:, h * 128:(h + 1) * 128].rearrange("(t p) d -> p t d", p=128)
        for t0 in range(0, NT, 4):
            P.dma("sp", va[:, t0:t0 + 4, 0:128], vbv[:, t0:t0 + 4, :], reads=[(B["VB"], None), (VA, "ones")],
                  writes=[(VA, ("v", t0))], sembuf=VA)
        for qb in range(8):
            qs = slice(qb * 512, (qb + 1) * 512)
            for j in range(NT):
                ks = slice(j * 128, (j + 1) * 128)
                b = n % 2; pb = n % 3; n += 1
                MM(P, pss[b][:], kn[:, ks], qn[:, qs], True, False, [(KN, None), (QN, None)], [(PSS[b], None)])
                MM(P, pss[b][:], krT[:, ks], qr[:, qs], False, True, [(KRT, None), (QR, None)], [(PSS[b], None)])
                ACTF(P, pT[pb][:], pss[b][:], AF.Exp, [(PSS[b], None)], [(PT[pb], None)], scale=scale)
                for ii in range(4):
                    MM(P, acc[ii][:, 0:129], pT[pb][:, ii * 128:(ii + 1) * 128], va[:, j, :], j == 0, j == NT - 1,
                       [(PT[pb], None), (VA, ("v", (j // 4) * 4)), (VA, "ones")], [(ACC[ii], None)])
            for ii in range(4):
                t = qb * 4 + ii
                o = oc % 2; oc += 1
                RECIP(P, rd[o][:], acc[ii][:, 128:129], [(ACC[ii], None)], [(RD[o], None)])
                TS(P, "dve", yo[o][:], acc[ii][:, 0:128], rd[o][:, 0:1], None, ALU.mult, None, [(ACC[ii], None), (RD[o], None)], [(YO[o], None)])
                P.dma("pool", D["Y"][t * 128:(t + 1) * 128, 768 + h * 128:768 + (h + 1) * 128], yo[o][:], reads=[(YO[o], None)],
                      writes=[(B["Y"], ("b", h, t))], sembuf=YO[o])


def phase_mlstm_prep(C, l):
    P, I, D, B, K, KB = C.P, C.I, C.D, C.B, C.K, C.KB
    g = C.sb("g_g", [128, NT, 16], F32); G = Buf("g_g")
    gb = C.sb("g_gb", [128, 16], F32); GB = Buf("g_gb")
    lf = C.sb("g_lf", [128, NT, 8], F32); LF = Buf("g_lf")
    tot = C.sb("g_tot", [128, NT, 8], F32); TOT = Buf("g_tot")
    off = C.sb("g_off", [128, NT, 8], F32); OFF = Buf("g_off")
    cum = C.sb("g_cum", [128, NT, 8], F32); CUM = Buf("g_cum")
    ibs = C.sb("g_ibs", [128, NT, 8], F32); IBS = Buf("g_ibs")
    pt = C.ps("g_pt", [128, 256]); PTB = PB("g_pt")
    pc = C.ps("g_pc", [128, 256]); PCB = PB("g_pc")
    pc2 = C.ps("g_pc2", [128, 256]); PCB2 = PB("g_pc2")
    pr = [C.ps("g_pr%d" % i, [128, 512]) for i in range(2)]; PR = [PB("g_pr%d" % i) for i in range(2)]
    dg = [C.sb("g_dg%d" % i, [128, 512], F32) for i in range(2)]; DG = [Buf("g_dg%d" % i) for i in range(2)]
    ro = [C.sb("g_ro%d" % i, [128, 512], F32) for i in range(2)]; RO = [Buf("g_ro%d" % i) for i in range(2)]
    gv = D["GATES"].rearrange("(t p) c -> p t c", p=128)
    for t0 in range(0, NT, 4):
        P.dma("sp", g[:, t0:t0 + 4, :], gv[:, t0:t0 + 4, :], reads=[(B["GATES"], None)], writes=[(G, ("ld", t0))], sembuf=G)
    P.dma("sp", gb[:], I["gate_b"][l].partition_broadcast(128), writes=[(GB, None)], sembuf=GB)
    for t in range(NT):
        TT(P, "dve", g[:, t, :], g[:, t, :], gb[:], ALU.add, [(G, None), (GB, None)], [(G, None)])
    ACTF(P, lf[:], g[:, :, 8:16], AF.Exp, [(G, None)], [(LF, None)], scale=-1.0)
    ACTF(P, lf[:], lf[:], AF.Ln, [(LF, None)], [(LF, None)], bias=1.0)
    TS(P, "dve", lf[:], lf[:], -1.0, None, ALU.mult, None, [(LF, None)], [(LF, None)])
    lf2 = lf[:].rearrange("p t c -> p (t c)")
    MM(P, pt[:], K["ones"][:], lf2, True, True, [(LF, None), (KB, "ones")], [(PTB, None)])
    CP(P, "dve", tot[:].rearrange("p t c -> p (t c)"), pt[:], [(PTB, None)], [(TOT, None)])
    MEMSET(P, "dve", off[:], 0.0, [], [(OFF, None)])
    for t in range(1, NT):
        TT(P, "dve", off[:, t, 0:4], off[:, t - 1, 0:4], tot[:, t - 1, 0:4], ALU.add, [(OFF, None), (TOT, None)], [(OFF, None)])
    for t in range(NT - 2, -1, -1):
        TT(P, "dve", off[:, t, 4:8], off[:, t + 1, 4:8], tot[:, t + 1, 4:8], ALU.add, [(OFF, None), (TOT, None)], [(OFF, None)])
    MM(P, pc[:], K["U"][:], lf2, True, True, [(LF, None), (KB, "U")], [(PCB, None)])
    MM(P, pc2[:], K["Lm"][:], lf2, True, True, [(LF, None), (KB, "L")], [(PCB2, None)])
    pc3 = pc[:].rearrange("p (t c) -> p t c", c=8)
    pc23 = pc2[:].rearrange("p (t c) -> p t c", c=8)
    TT(P, "dve", cum[:, :, 0:4], pc3[:, :, 0:4], off[:, :, 0:4], ALU.add, [(PCB, None), (OFF, None)], [(CUM, "f")])
    TT(P, "dve", cum[:, :, 4:8], pc23[:, :, 4:8], off[:, :, 4:8], ALU.add, [(PCB2, None), (OFF, None)], [(CUM, "b")])
    TT(P, "dve", ibs[:], g[:, :, 0:8], cum[:], ALU.subtract, [(G, None), (CUM, None)], [(IBS, None)])
    P.dma("pool", D["IBS"], ibs[:].rearrange("p t c -> p (t c)"), reads=[(IBS, None)], writes=[(B["IBS"], None)], sembuf=IBS)
    n = 0
    for c in range(8):
        for t0 in range(0, NT, 4):
            b = n % 2; n += 1
            for j in range(4):
                t = t0 + j
                TS(P, "dve", dg[b][:, j * 128:(j + 1) * 128], K["identf"][:], cum[:, t, c:c + 1], None, ALU.mult, None,
                   [(CUM, None), (KB, "identf")], [(DG[b], j)])
                MM(P, pr[b][:, j * 128:(j + 1) * 128], K["ones"][:], dg[b][:, j * 128:(j + 1) * 128], True, True,
                   [(DG[b], j), (KB, "ones")], [(PR[b], None)])
            CP(P, "act", ro[b][:], pr[b][:], [(PR[b], None)], [(RO[b], None)])
            P.dma("pool", D["BREP"][c][:, t0 * 128:(t0 + 4) * 128], ro[b][:], reads=[(RO[b], None)], writes=[(B["BREP"], (c, t0))], sembuf=RO[b])


def phase_mlstm_attn(C, l):
    P, I, D, B, K, KB = C.P, C.I, C.D, C.B, C.K, C.KB
    ibs = C.sb("s_ibs", [128, NT, 8], F32); IBS = Buf("s_ibs")
    hg = C.sb("s_hg", [128, 768], F32); HG = Buf("s_hg")
    qT = C.sb("s_qT", [96, S_], BF16); QT = Buf("s_qT")
    kT = C.sb("s_kT", [96, S_], BF16); KT = Buf("s_kT")
    va = C.sb("s_va", [128, NT, 193], BF16); VA = Buf("s_va")
    br = [C.sb("s_br%d" % i, [128, S_], F32) for i in range(2)]; BR = [Buf("s_br%d" % i) for i in range(2)]
    pss = [C.ps("s_ps%d" % i, [128, 256]) for i in range(2)]; PSS = [PB("s_ps%d" % i) for i in range(2)]
    acc = [[C.ps("s_acc%d%d" % (d, i), [128, 512]) for i in range(2)] for d in range(2)]
    ACC = [[PB("s_acc%d%d" % (d, i)) for i in range(2)] for d in range(2)]
    w = [C.sb("s_w%d" % i, [128, 256], F32) for i in range(3)]; WB = [Buf("s_w%d" % i) for i in range(3)]
    wT = [C.sb("s_wT%d" % i, [128, 256], BF16) for i in range(3)]; WT = [Buf("s_wT%d" % i) for i in range(3)]
    op_ = [C.sb("s_op%d" % i, [128, 192], BF16) for i in range(2)]; OP = [Buf("s_op%d" % i) for i in range(2)]
    dn = [C.sb("s_dn%d" % i, [128, 2], F32) for i in range(2)]; DN = [Buf("s_dn%d" % i) for i in range(2)]
    hs = [C.sb("s_hs%d" % i, [128, 192], F32) for i in range(2)]; HS = [Buf("s_hs%d" % i) for i in range(2)]
    hb = [C.sb("s_hb%d" % i, [128, 192], F32) for i in range(2)]; HB = [Buf("s_hb%d" % i) for i in range(2)]
    jk = C.sb("s_jk", [128, 192], F32); JK = Buf("s_jk")
    ssq = [C.sb("s_ssq%d" % i, [128, 1], F32) for i in range(2)]; SSQ = [Buf("s_ssq%d" % i) for i in range(2)]
    yo = [C.sb("s_yo%d" % i, [128, 192], BF16) for i in range(2)]; YO = [Buf("s_yo%d" % i) for i in range(2)]
    scale = 96 ** -0.5
    P.dma("sp", ibs[:].rearrange("p t c -> p (t c)"), D["IBS"], reads=[(B["IBS"], None)], writes=[(IBS, None)], sembuf=IBS)
    P.dma("sp", hg[:], I["head_g"][l].partition_broadcast(128), writes=[(HG, None)], sembuf=HG)
    MEMSET(P, "dve", va[:], 1.0, [], [(VA, "ones")])
    U, Lm = K["U"], K["Lm"]
    n = 0
    fc = 0
    for h in range(4):
        P.dma("sp", qT[:], D["QKT"][FM_CQ + h][0:96, :], reads=[(B["QKT"], None)], writes=[(QT, None)], sembuf=QT)
        P.dma("sp", kT[:], D["QKT"][FM_CK + h][0:96, :], reads=[(B["QKT"], None)], writes=[(KT, None)], sembuf=KT)
        cvv = D["PTM"][:, 256 + h * 192:256 + (h + 1) * 192].rearrange("(t p) d -> p t d", p=128)
        for t0 in range(0, NT, 4):
            P.dma("sp", va[:, t0:t0 + 4, 0:192], cvv[:, t0:t0 + 4, :], reads=[(B["PTM"], None), (VA, "ones")],
                  writes=[(VA, ("v", t0))], sembuf=VA)
        for d in range(2):
            P.dma("sp", br[d][:], D["BREP"][d * 4 + h], reads=[(B["BREP"], None)], writes=[(BR[d], None)], sembuf=BR[d])
        for lb in range(16):
            i0 = lb * 2
            ls = slice(lb * 256, (lb + 1) * 256)
            first = [[True, True], [True, True]]
            nexp = [[0, 0], [0, 0]]
            total = [[i0 + 1, i0 + 2], [NT - i0, NT - i0 - 1]]
            for j in range(NT):
                ks = slice(j * 128, (j + 1) * 128)
                b = n % 2; n += 1
                MM(P, pss[b][:], kT[:, ks], qT[:, ls], True, True, [(KT, None), (QT, None)], [(PSS[b], None)])
                work = []
                for ii in range(2):
                    i = i0 + ii
                    if j < i:
                        work.append((0, ii, False))
                    elif j > i:
                        work.append((1, ii, False))
                    else:
                        work.append((0, ii, True)); work.append((1, ii, True))
                for (d, ii, masked) in work:
                    wi = fc % 3; fc += 1
                    c = d * 4 + h
                    lsl = slice((i0 + ii) * 128, (i0 + ii + 1) * 128)
                    wv = w[wi][:, 0:128]
                    ACTF(P, wv, br[d][:, lsl], AF.Exp, [(BR[d], None), (IBS, None)], [(WB[wi], None)], bias=ibs[:, j, c:c + 1])
                    if masked:
                        TT(P, "dve", wv, wv, (U if d == 0 else Lm)[:], ALU.mult, [(WB[wi], None), (KB, "U"), (KB, "L")], [(WB[wi], None)])
                    STT(P, wT[wi][:, 0:128], pss[b][:, ii * 128:(ii + 1) * 128], scale, wv, ALU.mult, ALU.mult,
                        [(PSS[b], None), (WB[wi], None)], [(WT[wi], None)])
                    nexp[d][ii] += 1
                    MM(P, acc[d][ii][:, 0:193], wT[wi][:, 0:128], va[:, j, :], nexp[d][ii] == 1, nexp[d][ii] == total[d][ii],
                       [(WT[wi], None), (VA, ("v", (j // 4) * 4)), (VA, "ones")], [(ACC[d][ii], None)])
            for ii in range(2):
                t = i0 + ii
                o = t % 2
                for d in range(2):
                    a = acc[d][ii]
                    ACTF(P, dn[o][:, d:d + 1], a[:, 192:193], AF.Abs, [(ACC[d][ii], None)], [(DN[o], d)])
                TS(P, "dve", dn[o][:], dn[o][:], 1.0, None, ALU.max, None, [(DN[o], None)], [(DN[o], None)])
                RECIP(P, dn[o][:], dn[o][:], [(DN[o], None)], [(DN[o], None)])
                TS(P, "dve", hs[o][:], acc[0][ii][:, 0:192], dn[o][:, 0:1], None, ALU.mult, None, [(ACC[0][ii], None), (DN[o], None)], [(HS[o], None)])
                TS(P, "dve", hb[o][:], acc[1][ii][:, 0:192], dn[o][:, 1:2], None, ALU.mult, None, [(ACC[1][ii], None), (DN[o], None)], [(HB[o], None)])
                TT(P, "dve", hs[o][:], hs[o][:], hb[o][:], ALU.add, [(HS[o], None), (HB[o], None)], [(HS[o], None)])
                ACTF(P, jk[:], hs[o][:], AF.Square, [(HS[o], None)], [(JK, None), (SSQ[o], None)], accum=ssq[o][:])
                ACTF(P, ssq[o][:], ssq[o][:], AF.Sqrt, [(SSQ[o], None)], [(SSQ[o], None)], bias=EPS, scale=1.0 / 192.0)
                RECIP(P, ssq[o][:], ssq[o][:], [(SSQ[o], None)], [(SSQ[o], None)])
                P.dma("sp", op_[o][:], D["PTM"][t * 128:(t + 1) * 128, 1024 + h * 192:1024 + (h + 1) * 192], reads=[(B["PTM"], None)],
                      writes=[(OP[o], None)], sembuf=OP[o])
                ACTF(P, hb[o][:], op_[o][:], AF.Sigmoid, [(OP[o], None)], [(HB[o], None)])
                STT(P, hs[o][:], hs[o][:], ssq[o][:, 0:1], hg[:, h * 192:(h + 1) * 192], ALU.mult, ALU.mult,
                    [(HS[o], None), (SSQ[o], None), (HG, None)], [(HS[o], None)])
                TT(P, "dve", yo[o][:], hs[o][:], hb[o][:], ALU.mult, [(HS[o], None), (HB[o], None)], [(YO[o], None)])
                P.dma("pool", D["Y"][t * 128:(t + 1) * 128, 1280 + h * 192:1280 + (h + 1) * 192], yo[o][:], reads=[(YO[o], None)],
                      writes=[(B["Y"], ("c", h, t))], sembuf=YO[o])


def post_norm_residual(C, t, pm4, PM4, x_ap, XB_key, xt, XT, rep, REP, junk, JK, ssp, SSP, dst_ap, DSTB, tmp, TMP):
    P = C.P
    for n in range(4):
        ACTF(P, junk[:, n * 512:(n + 1) * 512], pm4[n][:], AF.Square, [(PM4[n], None)], [(JK, n), (SSP, n)], accum=ssp[:, n:n + 1])
    TT(P, "dve", ssp[:, 4:5], ssp[:, 0:1], ssp[:, 1:2], ALU.add, [(SSP, 0), (SSP, 1)], [(SSP, "a")])
    TT(P, "dve", ssp[:, 5:6], ssp[:, 2:3], ssp[:, 3:4], ALU.add, [(SSP, 2), (SSP, 3)], [(SSP, "b")])
    TT(P, "dve", ssp[:, 6:7], ssp[:, 4:5], ssp[:, 5:6], ALU.add, [(SSP, "a"), (SSP, "b")], [(SSP, "c")])
    ACTF(P, ssp[:, 7:8], ssp[:, 6:7], AF.Sqrt, [(SSP, "c")], [(SSP, "r")], bias=EPS, scale=1.0 / D_)
    RECIP(P, ssp[:, 7:8], ssp[:, 7:8], [(SSP, "r")], [(SSP, "r")])
    for n in range(4):
        STT(P, tmp[:, n * 512:(n + 1) * 512], pm4[n][:], ssp[:, 7:8], rep[:, n * 512:(n + 1) * 512], ALU.mult, ALU.mult,
            [(PM4[n], None), (SSP, "r"), (REP, None)], [(TMP, n)])
    TT(P, "pool", xt[:], xt[:], tmp[:], ALU.add, [(XT, None), (TMP, None)], [(XT, None)])
    P.dma("pool", dst_ap[t * 128:(t + 1) * 128, :], xt[:], reads=[(XT, None)], writes=[(DSTB, t)], sembuf=XT)


def phase_outproj(C, l, xsrc, xdst):
    P, I, D, B, K, KB = C.P, C.I, C.D, C.B, C.K, C.KB
    wo = C.sb("o_w", [128, 16, D_], BF16); WO = Buf("o_w")
    rep = C.sb("o_rep", [128, D_], F32); REP = Buf("o_rep")
    rtmp = C.sb("o_rtmp", [128, 512], F32); RTMP = Buf("o_rtmp")
    yt = [C.sb("o_y%d" % i, [128, D_], BF16) for i in range(2)]; YT = [Buf("o_y%d" % i) for i in range(2)]
    yT = [C.sb("o_yT%d" % i, [128, 16, 128], BF16) for i in range(2)]; YTT = [Buf("o_yT%d" % i) for i in range(2)]
    xt = [C.sb("o_x%d" % i, [128, D_], F32) for i in range(2)]; XT = [Buf("o_x%d" % i) for i in range(2)]
    tmp = C.sb("o_tmp", [128, D_], F32); TMP = Buf("o_tmp")
    junk = C.sb("o_junk", [128, D_], BF16); JK = Buf("o_junk")
    ssp = [C.sb("o_ssp%d" % i, [128, 8], F32) for i in range(2)]; SSP = [Buf("o_ssp%d" % i) for i in range(2)]
    tp = C.ps("o_tp", [128, D_], BF16); TP = PB("o_tp")
    pm = [C.ps("o_pm%d" % i, [128, 512]) for i in range(4)]; PM = [PB("o_pm%d" % i) for i in range(4)]
    prep = C.ps("o_prep", [128, 512]); PREP = PB("o_prep")
    wov = D["WOUT"][l].rearrange("(kc p) n -> p kc n", p=128)
    for kc in range(16):
        P.dma("sp", wo[:, kc, :], wov[:, kc, :], reads=[(B["WOUT"], None)], writes=[(WO, kc)], sembuf=WO)
    replicate_cols(C, rep, REP, K["vec"][:, l, 2, :], 16, [(C.MODB, (l, 0, 2))], prep, PREP, rtmp, RTMP)
    for t in range(NT):
        s = t % 2
        P.dma("sp", yt[s][:], D["Y"][t * 128:(t + 1) * 128, :], reads=[(B["Y"], None)], writes=[(YT[s], None)], sembuf=YT[s])
        P.dma("sp", xt[s][:], xsrc[t * 128:(t + 1) * 128, :], reads=[(B["XR"], t)], writes=[(XT[s], None)], sembuf=XT[s])
        for c in range(16):
            TR(P, tp[:, c * 128:(c + 1) * 128], yt[s][:, c * 128:(c + 1) * 128], K["ident"][:], [(YT[s], None), (KB, "ident")], [(TP, c // 8)])
        yv = yT[s][:].rearrange("p c t -> p (c t)")
        CP(P, "act", yv[:, 0:1024], tp[:, 0:1024], [(TP, 0)], [(YTT[s], 0)])
        CP(P, "dve", yv[:, 1024:2048], tp[:, 1024:2048], [(TP, 1)], [(YTT[s], 1)])
        for n in range(4):
            for kc in range(16):
                MM(P, pm[n][:], yT[s][:, kc, :], wo[:, kc, n * 512:(n + 1) * 512], kc == 0, kc == 15, [(YTT[s], None), (WO, None)], [(PM[n], None)])
        post_norm_residual(C, t, pm, PM, None, None, xt[s], XT[s], rep, REP, junk, JK, ssp[s], SSP[s], xdst, B["XR"], tmp, TMP)


def phase_ffn(C, l, xsrc, xdst):
    P, I, D, B, K, KB = C.P, C.I, C.D, C.B, C.K, C.KB
    DSTB = B["XR"]
    xt = [C.sb("f_xt%d" % i, [128, D_], F32) for i in range(2)]; XT = [Buf("f_xt%d" % i) for i in range(2)]
    junk = C.sb("f_junk", [128, D_], BF16); JK = Buf("f_junk")
    ss = [C.sb("f_ss%d" % i, [128, 1], F32) for i in range(2)]
    rs = [C.sb("f_rs%d" % i, [128, 1], F32) for i in range(2)]; SS = [Buf("f_ss%d" % i) for i in range(2)]
    xn = [C.sb("f_xn%d" % i, [128, D_], BF16) for i in range(2)]; XN = [Buf("f_xn%d" % i) for i in range(2)]
    tp = C.ps("f_tp", [128, D_], BF16); TP = PB("f_tp")
    hT = C.sb("f_hT", [128, 16, 512], BF16); HT = Buf("f_hT")
    aT = C.sb("f_aT", [128, 44, 512], BF16); AT = Buf("f_aT")
    wg = [C.sb("f_wg%d" % i, [128, 16, 256], BF16) for i in range(2)]; WG = [Buf("f_wg%d" % i) for i in range(2)]
    wu = [C.sb("f_wu%d" % i, [128, 16, 256], BF16) for i in range(2)]; WU = [Buf("f_wu%d" % i) for i in range(2)]
    wd = [C.sb("f_wd%d" % i, [128, 2, D_], BF16) for i in range(2)]; WDB = [Buf("f_wd%d" % i) for i in range(2)]
    pg = C.ps("f_pg", [128, 512]); PG = PB("f_pg")
    pu = C.ps("f_pu", [128, 512]); PU = PB("f_pu")
    pm = [C.ps("f_pm%d" % i, [128, 512]) for i in range(4)]; PM = [PB("f_pm%d" % i) for i in range(4)]
    sg = [C.sb("f_sg%d" % i, [128, 512], F32) for i in range(2)]; SG = [Buf("f_sg%d" % i) for i in range(2)]
    rep = C.sb("f_rep", [128, D_], F32); REP = Buf("f_rep")
    tmp = C.sb("f_tmp", [128, D_], F32); TMP = Buf("f_tmp")
    ssp = [C.sb("f_ssp%d" % i, [128, 8], F32) for i in range(2)]; SSP = [Buf("f_ssp%d" % i) for i in range(2)]
    xr = xt; XRB = XT
    A = K["vec"][:, l, 3, :]; Sv = K["vec"][:, l, 4, :]
    VR = [(C.MODB, (l, 1, 0)), (C.MODB, (l, 1, 1))]
    replicate_cols(C, rep, REP, K["vec"][:, l, 5, :], 16, [(C.MODB, (l, 1, 2))], pg, PG, tmp, TMP)
    wgv = D["WG"][l].rearrange("(kc p) n -> p kc n", p=128)
    wuv = D["WU"][l].rearrange("(kc p) n -> p kc n", p=128)
    wdv = D["WD"][l].rearrange("(f p) n -> p f n", p=128)
    n_w = 0
    n_d = 0
    n_s = 0
    for blk in range(8):
        for ti in range(4):
            t = blk * 4 + ti
            s = t % 2
            norm_to_hT(C, t, ti, xsrc, B["XR"], xt[s], XT[s], ss[s], rs[s], SS[s], xn[s], XN[s], junk, JK, tp, TP, hT, HT, A, Sv, VR)
        for fs in range(22):
            wi = n_w % 2; n_w += 1
            P.dma("sp", wg[wi][:], wgv[:, :, fs * 256:(fs + 1) * 256], reads=[(B["WG"], None)], writes=[(WG[wi], None)], sembuf=WG[wi])
            P.dma("sp", wu[wi][:], wuv[:, :, fs * 256:(fs + 1) * 256], reads=[(B["WU"], None)], writes=[(WU[wi], None)], sembuf=WU[wi])
            for j in range(2):
                f = fs * 2 + j
                for kc in range(16):
                    MM(P, pg[:], wg[wi][:, kc, j * 128:(j + 1) * 128], hT[:, kc, :], kc == 0, kc == 15, [(WG[wi], None), (HT, None)], [(PG, None)])
                for kc in range(16):
                    MM(P, pu[:], wu[wi][:, kc, j * 128:(j + 1) * 128], hT[:, kc, :], kc == 0, kc == 15, [(WU[wi], None), (HT, None)], [(PU, None)])
                si = n_s % 2; n_s += 1
                ACTF(P, sg[si][:], pg[:], AF.Silu, [(PG, None)], [(SG[si], None)])
                TT(P, "dve", aT[:, f, :], sg[si][:], pu[:], ALU.mult, [(SG[si], None), (PU, None)], [(AT, f)])
        for ti in range(4):
            t = blk * 4 + ti
            s = t % 2
            for f0 in range(0, 44, 2):
                di = n_d % 2; n_d += 1
                P.dma("sp", wd[di][:], wdv[:, f0:f0 + 2, :], reads=[(B["WD"], None)], writes=[(WDB[di], None)], sembuf=WDB[di])
                for fj in range(2):
                    f = f0 + fj
                    for n in range(4):
                        MM(P, pm[n][:], aT[:, f, ti * 128:(ti + 1) * 128], wd[di][:, fj, n * 512:(n + 1) * 512], f == 0, f == 43,
                           [(AT, None), (WDB[di], None)], [(PM[n], None)])
            P.dma("sp", xr[s][:], xsrc[t * 128:(t + 1) * 128, :], reads=[(B["XR"], t)], writes=[(XRB[s], None)], sembuf=XRB[s])
            post_norm_residual(C, t, pm, PM, None, None, xr[s], XRB[s], rep, REP, junk, JK, ssp[s], SSP[s], xdst, DSTB, tmp, TMP)


def alibi_slopes(n):
    def pow2(m):
        start = 2.0 ** (-8.0 / m)
        return [start ** (i + 1) for i in range(m)]
    if math.log2(n).is_integer():
        s = pow2(n)
    else:
        p = 2 ** math.floor(math.log2(n))
        s = pow2(p) + pow2(2 * p)[0::2][: n - p]
    return np.array(s, dtype=np.float32)


def host_constants():
    c = {}
    c["c_ident"] = np.eye(128, dtype=np.float32)
    c["c_ones"] = np.ones((128, 128), np.float32)
    k = np.arange(128)[:, None]; m = np.arange(128)[None, :]
    c["c_U"] = (k <= m).astype(np.float32)
    c["c_L"] = (k >= m).astype(np.float32)
    R = np.zeros((64, 64), np.float32)
    for mm in range(32):
        R[mm + 32, mm] = -1.0
        R[mm, mm + 32] = 1.0
    c["c_R"] = R
    sl = alibi_slopes(12)
    E = np.zeros((128, 12, 3, 128), np.float32)
    kk = np.arange(128)[:, None]; qq = np.arange(128)[None, :]
    for d in range(3):
        dist = np.abs(qq - kk - (d - 1) * 128).astype(np.float32)
        for h in range(12):
            E[:, h, d, :] = np.where(dist <= 128, np.exp(-sl[h] * dist), 0.0)
    c["c_E"] = E.reshape(128, 12 * 384)
    inv = (1.0 / (np.float32(10000.0) ** (np.arange(0, 64, 2, dtype=np.float32) / np.float32(64)))).astype(np.float32)
    c["c_invf"] = np.concatenate([inv, inv])[:, None].astype(np.float32)
    return c


def colT(v, n):
    return np.ascontiguousarray(np.asarray(v, np.float32).reshape(n, 128).T)


def host_layout(inp):
    g = lambda k: np.asarray(inp[k])
    w_in = g("w_in")
    fm = np.zeros((L_, D_, NFM * 128), np.float32)
    def put(ch, cols):
        fm[:, :, ch * 128:ch * 128 + len(cols)] = w_in[:, :, cols]
    for c in range(6):
        put(FM_AQ + c, np.arange(c * 128, (c + 1) * 128))
    for kv in range(4):
        cols = np.arange(768 + kv * 64, 768 + (kv + 1) * 64)
        put(FM_AK + kv, np.concatenate([cols, cols]))
    for c in range(4):
        put(FM_BCQ + c, np.arange(1280 + c * 128, min(1280 + (c + 1) * 128, 1728)))
    put(FM_BCKV, np.arange(1728, 1856))
    put(FM_BKR, np.arange(1856, 1920))
    for h in range(4):
        put(FM_CQ + h, np.arange(1920 + h * 96, 1920 + (h + 1) * 96))
        put(FM_CK + h, np.arange(2304 + h * 96, 2304 + (h + 1) * 96))
    tm = np.ascontiguousarray(np.concatenate([w_in[:, :, 1024:1280], w_in[:, :, 2688:3456], w_in[:, :, 3472:4240], w_in[:, :, 3456:3472]], axis=2))
    shared = {
        "mod_w": g("mod_w"),
        "mod_bT": np.stack([colT(g("mod_b")[l], 96) for l in range(L_)]),
        "w_in_fm": fm, "w_in_tm": tm,
        "attn_sink": g("attn_sink")[:, None, :],
        "w_uq": np.concatenate([g("mla_w_uq"), np.zeros((L_, 64, 768), np.float32)], axis=1),
        "w_ukv": g("mla_w_ukv"),
        "gate_b": g("mlstm_gate_b")[:, None, :],
        "head_g": g("mlstm_head_g")[:, None, :],
        "w_out": g("w_out"), "w_gate": g("ffn_w_gate"), "w_up": g("ffn_w_up"), "w_down": g("ffn_w_down"),
    }
    for k, src in (("pre_mix_gT", "pre_mix_g"), ("post_mix_gT", "post_mix_g"), ("pre_ffn_gT", "pre_ffn_g"), ("post_ffn_gT", "post_ffn_g")):
        shared[k] = np.stack([colT(g(src)[l], 16) for l in range(L_)])
    qg = np.concatenate([g("mla_q_norm_g"), np.zeros((L_, 64), np.float32)], axis=1)
    shared["q_norm_gT"] = np.stack([colT(qg[l], 4) for l in range(L_)])
    shared["kv_norm_gT"] = np.stack([colT(g("mla_kv_norm_g")[l], 1) for l in range(L_)])
    shared.update(host_constants())
    x, c, pos = g("x"), g("c"), g("positions")
    maps = []
    for b in range(8):
        m = dict(shared)
        m["x"] = np.ascontiguousarray(x[b])
        m["cT"] = colT(c[b], 16)
        m["pos"] = np.ascontiguousarray(pos[b][None, :].astype(np.int32))
        maps.append(m)
    return maps


_CACHE = {}


def kernel(**inputs):
    if "nc" not in _CACHE:
        _CACHE["nc"] = build_program()[0]
    nc = _CACHE["nc"]
    maps = host_layout(inputs)
    res = run_bass_kernel_spmd(nc, maps, core_ids=list(range(8)))
    return np.stack([np.asarray(r["out"], dtype=np.float32) for r in res.results], axis=0)
```

```python
import numpy as np
import concourse.bass as bass
import concourse.mybir as mybir
from concourse.bass_utils import run_bass_kernel_spmd
F32 = mybir.dt.float32
BF16 = mybir.dt.bfloat16
I32 = mybir.dt.int32
AF = mybir.ActivationFunctionType
ALU = mybir.AluOpType
AX = mybir.AxisListType


class Buf:
    __slots__ = ("name", "st", "sem", "excl")

    def __init__(self, name, excl=False):
        self.name = name
        self.st = {}
        self.sem = None
        self.excl = excl


def PB(name):
    return Buf(name, excl=True)


class Op:
    __slots__ = ("eng", "fn", "idx", "waits", "is_dma", "needs_inc", "clock", "sem", "semval")

    def __init__(self, eng, fn, is_dma):
        self.eng = eng
        self.fn = fn
        self.is_dma = is_dma
        self.waits = []
        self.needs_inc = False
        self.clock = None
        self.sem = None
        self.semval = None


class Prog:
    ENGS = ("sp", "act", "dve", "pool", "pe")

    def __init__(self, nc):
        self.nc = nc
        self.ops = []
        self.known_idx = {}
        self.known_dma = {}
        self.dma_issued = {}
        self.sems = {}
        self.ctx = []
        self.free_pool = {}
        self.npool = 0
        self.phase_bufs = []
        self.phase_start = 0

    def _sem(self, key):
        s = self.sems.get(key)
        if s is None:
            cm = self.nc.semaphore("s_" + str(key))
            s = cm.__enter__()
            self.ctx.append(cm)
            self.sems[key] = s
        return s

    @staticmethod
    def _states(buf, key, create):
        st = buf.st
        if key is None:
            if create and None not in st:
                st[None] = {"w": {}, "r": {}}
            return list(st.values())
        out = []
        if None in st:
            out.append(st[None])
        if key not in st and create:
            st[key] = {"w": {}, "r": {}}
        if key in st:
            out.append(st[key])
        return out

    def _record(self, op, reads, writes):
        op.idx = len(self.ops)
        self.ops.append(op)
        ex = [rk for rk in reads if rk[0].excl]
        if ex:
            reads = [rk for rk in reads if not rk[0].excl]
            writes = list(writes) + [rk for rk in ex if rk not in writes]
        deps = []
        for (buf, key) in reads:
            for s in self._states(buf, key, True):
                deps += [(p, "RAW") for p in s["w"].values()]
        for (buf, key) in writes:
            for s in self._states(buf, key, True):
                deps += [(p, "WAW") for p in s["w"].values()]
                deps += [(p, "WAR") for p in s["r"].values()]
        for (p, kind) in deps:
            if p is op or p.idx < self.phase_start:
                continue
            if p.is_dma:
                val = self.dma_issued[p.sem]
                if op.is_dma and op.sem == p.sem:
                    val -= 16
                k = (op.eng, p.sem)
                if self.known_dma.get(k, 0) >= val:
                    continue
                self.known_dma[k] = val
                op.waits.append(("sem", p.sem, val))
            else:
                if p.eng == op.eng:
                    if op.eng == "pe":
                        continue
                k = (op.eng, p.eng)
                if self.known_idx.get(k, -1) >= p.idx:
                    continue
                self.known_idx[k] = p.idx
                p.needs_inc = True
                op.waits.append(("op", p))
        clk = ("dma", op.sem) if op.is_dma else op.eng
        for (buf, key) in reads:
            if key is None:
                if None not in buf.st:
                    buf.st[None] = {"w": {}, "r": {}}
                buf.st[None]["r"][clk] = op
            else:
                buf.st[key]["r"][clk] = op
        for (buf, key) in writes:
            if key is None:
                buf.st.clear()
                buf.st[None] = {"w": {clk: op}, "r": {}}
            else:
                buf.st[key] = {"w": {clk: op}, "r": {}}

    def op(self, eng, fn, reads=(), writes=()):
        o = Op(eng, fn, False)
        self._record(o, reads, writes)
        return o

    def dma(self, queue, out_ap, in_ap, reads=(), writes=(), sembuf=None, **kw):
        assert sembuf is not None
        qt = "sw" if queue == "pool" else "hw"
        if sembuf.sem is None:
            sembuf.sem = {}
            self.phase_bufs.append(sembuf)
        if qt not in sembuf.sem:
            fp = self.free_pool.setdefault(qt, [])
            if fp:
                sembuf.sem[qt] = fp.pop()
            else:
                sembuf.sem[qt] = "%s_%d" % (qt, self.npool)
                self.npool += 1
        o = Op(queue, lambda e: e.dma_start(out=out_ap, in_=in_ap, **kw), True)
        o.sem = sembuf.sem[qt]
        self.dma_issued[o.sem] = self.dma_issued.get(o.sem, 0) + 16
        o.semval = self.dma_issued[o.sem]
        self._record(o, reads, writes)
        return o

    def sb(self, name, shape, dtype):
        cm = self.nc.sbuf_tensor(name, list(shape), dtype)
        t = cm.__enter__()
        self.ctx.append(cm)
        return t

    def ps(self, name, shape, dtype):
        cm = self.nc.psum_tensor(name, list(shape), dtype)
        t = cm.__enter__()
        self.ctx.append(cm)
        return t

    def emit_phase(self):
        start = getattr(self, "_emitted", 0)
        ops = self.ops[start:]
        self._emitted = len(self.ops)
        if not hasattr(self, "cnt"):
            self.cnt = {e: 0 for e in self.ENGS}
            self.tot_stats = {e: 0 for e in self.ENGS}
            self.nwaits = 0
        nc = self.nc
        cnt = self.cnt
        per = {e: [o for o in ops if o.eng == e] for e in self.ENGS}
        for e in self.ENGS:
            for o in reversed(per[e]):
                if not o.is_dma:
                    o.needs_inc = True
                    break
        for o in ops:
            if (not o.is_dma) and o.needs_inc:
                cnt[o.eng] += 1
                o.clock = cnt[o.eng]
        for e in self.ENGS:
            self._sem("eng_" + e)
            self.tot_stats[e] += len(per[e])
        for o in ops:
            if o.is_dma:
                self._sem(o.sem)
            self.nwaits += len(o.waits)

        def run(e, name):
            for o in per[name]:
                for w in o.waits:
                    if w[0] == "sem":
                        e.wait_ge(self.sems[w[1]], w[2])
                    else:
                        p = w[1]
                        e.wait_ge(self.sems["eng_" + p.eng], p.clock)
                ins = o.fn(e)
                if o.is_dma:
                    ins.then_inc(self.sems[o.sem], 16)
                elif o.needs_inc:
                    ins.then_inc(self.sems["eng_" + o.eng], 1)
            for sem, tot in self.dma_issued.items():
                if sem in self.sems:
                    e.wait_ge(self.sems[sem], tot)
            for en in self.ENGS:
                if en != name and cnt[en] > 0:
                    e.wait_ge(self.sems["eng_" + en], cnt[en])

        with nc.Block() as block:
            @block.sync
            def _(e):
                run(e, "sp")

            @block.scalar
            def _(e):
                run(e, "act")

            @block.vector
            def _(e):
                run(e, "dve")

            @block.gpsimd
            def _(e):
                run(e, "pool")

            @block.tensor
            def _(e):
                run(e, "pe")
        for b in self.phase_bufs:
            for qt, sm in b.sem.items():
                self.free_pool.setdefault(qt, []).append(sm)
            b.sem = None
        self.phase_bufs = []
        self.phase_start = len(self.ops)
        last = len(self.ops)
        for a in self.ENGS:
            for b in self.ENGS:
                self.known_idx[(a, b)] = last - 1
            for sem, tot in self.dma_issued.items():
                self.known_dma[(a, sem)] = tot

    def finish(self):
        self.stats = dict(self.tot_stats)
        self.stats["waits"] = self.nwaits
        self.stats["clock"] = dict(self.cnt)
        self.stats["maxdma"] = max(self.dma_issued.values()) if self.dma_issued else 0
        self.stats["nsem"] = len(self.sems)

    def emit(self, final_wait_bufs=()):
        nc = self.nc
        cnt = {e: 0 for e in self.ENGS}
        for o in self.ops:
            if (not o.is_dma) and o.needs_inc:
                cnt[o.eng] += 1
                o.clock = cnt[o.eng]
        per = {e: [o for o in self.ops if o.eng == e] for e in self.ENGS}
        for e in self.ENGS:
            self._sem("eng_" + e)
        for o in self.ops:
            if o.is_dma:
                self._sem(o.sem)
        self.stats = {e: len(per[e]) for e in self.ENGS}
        self.stats["waits"] = sum(len(o.waits) for o in self.ops)
        self.stats["maxclock"] = dict(cnt)
        self.stats["maxdma"] = max(self.dma_issued.values()) if self.dma_issued else 0
        self.stats["nsem"] = len(self.sems)

        def run(e, name):
            for o in per[name]:
                for w in o.waits:
                    if w[0] == "sem":
                        e.wait_ge(self.sems[w[1]], w[2])
                    else:
                        p = w[1]
                        e.wait_ge(self.sems["eng_" + p.eng], p.clock)
                ins = o.fn(e)
                if o.is_dma:
                    ins.then_inc(self.sems[o.sem], 16)
                elif o.needs_inc:
                    ins.then_inc(self.sems["eng_" + o.eng], 1)
            if name == "sp":
                for sem, tot in self.dma_issued.items():
                    e.wait_ge(self.sems[sem], tot)
                for en in self.ENGS:
                    if en != "sp" and cnt[en] > 0:
                        e.wait_ge(self.sems["eng_" + en], cnt[en])

        with nc.Block() as block:
            @block.sync
            def _(e):
                run(e, "sp")

            @block.scalar
            def _(e):
                run(e, "act")

            @block.vector
            def _(e):
                run(e, "dve")

            @block.gpsimd
            def _(e):
                run(e, "pool")

            @block.tensor
            def _(e):
                run(e, "pe")

    def close(self):
        for cm in reversed(self.ctx):
            cm.__exit__(None, None, None)
        self.ctx = []


import math

S_ = 4096
D_ = 2048
NT = 32
DFF = 5632
EPS = 1e-6
L_ = 2
NFM = 24
NTM = 1808
FM_AQ, FM_AK, FM_BCQ, FM_BCKV, FM_BKR, FM_CQ, FM_CK = 0, 6, 10, 14, 15, 16, 20


def MM(P, out, lhsT, rhs, start, stop, R, W):
    return P.op("pe", lambda e: e.matmul(out, lhsT=lhsT, rhs=rhs, start=start, stop=stop), R, W)


def TR(P, out, in_, ident, R, W):
    return P.op("pe", lambda e: e.transpose(out, in_, ident), R, W)


def ACTF(P, out, in_, func, R, W, bias=None, scale=None, accum=None):
    kw = {}
    if bias is not None:
        kw["bias"] = bias
    if scale is not None:
        kw["scale"] = scale
    if accum is not None:
        kw["accum_out"] = accum
    return P.op("act", lambda e: e.activation(out=out, in_=in_, func=func, **kw), R, W)


def TS(P, eng, out, in0, s1, s2, op0, op1, R, W):
    if op1 is None:
        return P.op(eng, lambda e: e.tensor_scalar(out=out, in0=in0, scalar1=s1, scalar2=None, op0=op0), R, W)
    return P.op(eng, lambda e: e.tensor_scalar(out=out, in0=in0, scalar1=s1, scalar2=s2, op0=op0, op1=op1), R, W)


def TT(P, eng, out, in0, in1, op, R, W):
    return P.op(eng, lambda e: e.tensor_tensor(out=out, in0=in0, in1=in1, op=op), R, W)


def STT(P, out, in0, scalar, in1, op0, op1, R, W):
    return P.op("dve", lambda e: e.scalar_tensor_tensor(out=out, in0=in0, scalar=scalar, in1=in1, op0=op0, op1=op1), R, W)


def CP(P, eng, out, in_, R, W):
    if eng == "act":
        return P.op("act", lambda e: e.activation(out=out, in_=in_, func=AF.Copy), R, W)
    return P.op(eng, lambda e: e.tensor_copy(out=out, in_=in_), R, W)


def RECIP(P, out, in_, R, W):
    return P.op("dve", lambda e: e.reciprocal(out=out, in_=in_), R, W)


def MEMSET(P, eng, ap, val, R, W):
    return P.op(eng, lambda e: e.memset(ap, val), R, W)


class Ctx:
    pass


KNOB = {}


def build_program(dbg=False, phases=None, nlayers=L_):
    nc = bass.Bass("TRN2", target_bir_lowering=False)
    P = Prog(nc)
    C = Ctx()
    C.nc, C.P, C.dbg = nc, P, dbg

    def din(name, shape, dt=F32):
        return nc.dram_tensor(name, list(shape), dt, kind="ExternalInput").ap()

    def dscr(name, shape, dt):
        return nc.dram_tensor(name, list(shape), dt, kind=("ExternalOutput" if (dbg and name in dbg) else "Internal")).ap()

    I = C.I = {}
    I["x"] = din("x", [S_, D_])
    I["cT"] = din("cT", [128, 16])
    I["pos"] = din("pos", [1, S_], I32)
    I["mod_w"] = din("mod_w", [L_, D_, 6 * D_])
    I["mod_bT"] = din("mod_bT", [L_, 128, 96])
    for g in ("pre_mix_gT", "post_mix_gT", "pre_ffn_gT", "post_ffn_gT"):
        I[g] = din(g, [L_, 128, 16])
    I["w_in_fm"] = din("w_in_fm", [L_, D_, NFM * 128])
    I["w_in_tm"] = din("w_in_tm", [L_, D_, NTM])
    I["attn_sink"] = din("attn_sink", [L_, 1, 12])
    I["q_norm_gT"] = din("q_norm_gT", [L_, 128, 4])
    I["w_uq"] = din("w_uq", [L_, 512, 768])
    I["kv_norm_gT"] = din("kv_norm_gT", [L_, 128, 1])
    I["w_ukv"] = din("w_ukv", [L_, 128, 1024])
    I["gate_b"] = din("gate_b", [L_, 1, 16])
    I["head_g"] = din("head_g", [L_, 1, 768])
    I["w_out"] = din("w_out", [L_, D_, D_])
    I["w_gate"] = din("w_gate", [L_, D_, DFF])
    I["w_up"] = din("w_up", [L_, D_, DFF])
    I["w_down"] = din("w_down", [L_, DFF, D_])
    I["c_ident"] = din("c_ident", [128, 128])
    I["c_ones"] = din("c_ones", [128, 128])
    I["c_U"] = din("c_U", [128, 128])
    I["c_L"] = din("c_L", [128, 128])
    I["c_R"] = din("c_R", [64, 64])
    I["c_E"] = din("c_E", [128, 12 * 384])
    I["c_invf"] = din("c_invf", [64, 1])
    C.out = nc.dram_tensor("out", [S_, D_], F32, kind="ExternalOutput").ap()

    D = C.D = {}
    D["XR"] = dscr("XR", [S_, D_], F32)
    D["WFM"] = dscr("WFM", [L_, D_, NFM * 128], BF16)
    D["WTM"] = dscr("WTM", [L_, D_, NTM], BF16)
    D["WOUT"] = dscr("WOUTb", [L_, D_, D_], BF16)
    D["WG"] = dscr("WGb", [L_, D_, DFF], BF16)
    D["WU"] = dscr("WUb", [L_, D_, DFF], BF16)
    D["WD"] = dscr("WDb", [L_, DFF, D_], BF16)
    D["QKT"] = dscr("QKT", [NFM, 128, S_], BF16)
    D["PTM"] = dscr("PTM", [S_, NTM], BF16)
    D["GATES"] = dscr("GATES", [S_, 16], F32)
    D["Y"] = dscr("Y", [S_, D_], BF16)
    D["QN"] = dscr("QN", [4, 128, S_], BF16)
    D["QR"] = dscr("QR", [4, 64, S_], BF16)
    D["KN"] = dscr("KN", [4, 128, S_], BF16)
    D["KR"] = dscr("KR", [64, S_], BF16)
    D["VB"] = dscr("VB", [S_, 512], BF16)
    D["BREP"] = dscr("BREP", [8, 128, S_], F32)
    D["IBS"] = dscr("IBS", [128, NT * 8], F32)
    D["MODV"] = dscr("MODV", [128, L_ * 96], F32)
    B = C.B = {k: Buf("D_" + k) for k in D}
    B["CAST"] = Buf("CAST")

    def psb(name, shape, dt):
        cm = nc.sbuf_tensor(name, list(shape), dt)
        return cm.__enter__()

    K = C.K = {}
    K["ident"] = psb("k_ident", [128, 128], BF16)
    K["identf"] = psb("k_identf", [128, 128], F32)
    K["ones"] = psb("k_ones", [128, 128], F32)
    K["onesb"] = psb("k_onesb", [128, 128], BF16)
    K["U"] = psb("k_U", [128, 128], F32)
    K["Lm"] = psb("k_L", [128, 128], F32)
    K["R"] = psb("k_R", [64, 64], BF16)
    K["mod"] = psb("k_mod", [128, L_, 96], F32)
    K["vec"] = psb("k_vec", [128, L_, 6, 16], F32)
    KB = C.KB = Buf("KCONST")
    C.MODB = Buf("MODVEC")

    C.phase_ctx = []

    def begin():
        C.phase_ctx = []
        P.ctx_mark = len(P.ctx)

    C.uid = [0]

    def sb(name, shape, dt):
        C.uid[0] += 1
        name = "%s_u%d" % (name, C.uid[0])
        cm = nc.sbuf_tensor(name, list(shape), dt)
        t = cm.__enter__()
        C.phase_ctx.append(cm)
        return t

    def ps(name, shape, dt=F32):
        C.uid[0] += 1
        name = "%s_u%d" % (name, C.uid[0])
        esz = 4 if dt == F32 else 2
        n = 1
        for d in shape[1:]:
            n *= d
        per_bank = 2048 // esz
        nb = -(-n // per_bank)
        cm = nc.psum_tensor(name, [128, nb * per_bank], dt)
        t = cm.__enter__()
        C.phase_ctx.append(cm)
        v = t[0:shape[0], 0:n]
        if len(shape) == 3:
            v = v.rearrange("p (a b) -> p a b", b=shape[2])
        return v

    def end():
        P.emit_phase()
        for cm in reversed(C.phase_ctx):
            cm.__exit__(None, None, None)
        C.phase_ctx = []

    C.begin, C.end, C.sb, C.ps = begin, end, sb, ps

    want = (lambda n: True) if phases is None else (lambda n: n in phases)

    begin()
    phase_consts(C)
    gc = phase_cast(C, nlayers) if want("cast") else iter(())
    gm = phase_mod(C, nlayers) if want("mod") else iter(())
    done_c = done_m = False
    while not (done_c and done_m):
        for _ in range(9):
            if next(gc, "end") == "end":
                done_c = True
                break
        if next(gm, "end") == "end":
            done_m = True
    for _ in gc:
        pass
    for _ in gm:
        pass
    end()
    for l in range(nlayers):
        src = I["x"] if l == 0 else D["XR"]
        if want("inproj"):
            begin(); phase_inproj(C, l, src); end()
        if want("win"):
            begin(); phase_window(C, l); end()
        if want("mla"):
            begin(); phase_mla_prep(C, l); end()
            begin(); phase_mla_attn(C, l); end()
        if want("mlstm"):
            begin(); phase_mlstm_prep(C, l); end()
            begin(); phase_mlstm_attn(C, l); end()
        if want("outproj"):
            begin(); phase_outproj(C, l, src, D["XR"]); end()
        if want("ffn"):
            dst = C.out if l == nlayers - 1 else D["XR"]
            begin(); phase_ffn(C, l, D["XR"], dst); end()
    P.finish()
    P.close()
    return nc, P


def phase_consts(C):
    P, I, K, KB = C.P, C.I, C.K, C.KB
    tmp = C.sb("c_tmp", [128, 128], F32); T = Buf("c_tmp")
    tmpR = C.sb("c_tmpR", [64, 64], F32); TRb = Buf("c_tmpR")
    P.dma("sp", K["identf"][:], I["c_ident"], writes=[(KB, "identf")], sembuf=KB)
    P.dma("sp", K["ones"][:], I["c_ones"], writes=[(KB, "ones")], sembuf=KB)
    P.dma("sp", K["U"][:], I["c_U"], writes=[(KB, "U")], sembuf=KB)
    P.dma("sp", K["Lm"][:], I["c_L"], writes=[(KB, "L")], sembuf=KB)
    P.dma("sp", tmpR[:], I["c_R"], writes=[(TRb, None)], sembuf=TRb)
    CP(P, "dve", K["ident"][:], K["identf"][:], [(KB, "identf")], [(KB, "ident")])
    CP(P, "dve", K["onesb"][:], K["ones"][:], [(KB, "ones")], [(KB, "onesb")])
    CP(P, "dve", K["R"][:], tmpR[:], [(TRb, None)], [(KB, "R")])


def phase_cast(C, nlayers):
    P, I, D, B = C.P, C.I, C.D, C.B
    NB = 4
    sf = [C.sb("c_sf%d" % i, [128, 2048], F32) for i in range(NB)]; SF = [Buf("c_sf%d" % i) for i in range(NB)]
    sb_ = [C.sb("c_sb%d" % i, [128, 2048], BF16) for i in range(NB)]; SBB = [Buf("c_sb%d" % i) for i in range(NB)]
    engs = ("act", "dve", "act", "dve")
    cnt = [0]

    def cast(dst, src, rows, cols, key):
        sv = src.rearrange("(r p) n -> p r n", p=128)
        dv = dst.rearrange("(r p) n -> p r n", p=128)
        for r in range(rows // 128):
            for c0 in range(0, cols, 2048):
                w = min(2048, cols - c0)
                i = cnt[0] % NB; cnt[0] += 1
                P.dma("sp", sf[i][:, 0:w], sv[:, r, c0:c0 + w], writes=[(SF[i], None)], sembuf=SF[i])
                CP(P, engs[i], sb_[i][:, 0:w], sf[i][:, 0:w], [(SF[i], None)], [(SBB[i], None)])
                P.dma("pool", dv[:, r, c0:c0 + w], sb_[i][:, 0:w], reads=[(SBB[i], None)], writes=[(B[key], ("cast", id(dst), r, c0))], sembuf=SBB[i])
                yield
    for l in range(nlayers):
        yield from cast(D["WFM"][l], I["w_in_fm"][l], D_, NFM * 128, "WFM")
        yield from cast(D["WTM"][l], I["w_in_tm"][l], D_, NTM, "WTM")
        yield from cast(D["WOUT"][l], I["w_out"][l], D_, D_, "WOUT")
        yield from cast(D["WG"][l], I["w_gate"][l], D_, DFF, "WG")
        yield from cast(D["WU"][l], I["w_up"][l], D_, DFF, "WU")
        yield from cast(D["WD"][l], I["w_down"][l], DFF, D_, "WD")


def phase_mod(C, nlayers):
    P, I, K = C.P, C.I, C.K
    cT = C.sb("m_cT", [128, 16], F32); CT = Buf("m_cT")
    sc = C.sb("m_silu", [128, 16, 2], F32); SC = Buf("m_silu")
    ws = [C.sb("m_w%d" % i, [128, 16, 512], F32) for i in range(2)]
    WS = [Buf("m_w%d" % i) for i in range(2)]
    mb = C.sb("m_b", [128, 96], F32); MB = Buf("m_b")
    gT = C.sb("m_g", [128, 4, 16], F32); GT = Buf("m_g")
    mps = C.ps("m_ps", [128, 96, 2]); MPS = PB("m_ps")
    MODB = C.MODB
    P.dma("sp", cT[:], I["cT"], writes=[(CT, None)], sembuf=CT)
    ACTF(P, sc[:, :, 0], cT[:], AF.Silu, [(CT, None)], [(SC, 0)])
    ACTF(P, sc[:, :, 1], cT[:], AF.Silu, [(CT, None)], [(SC, 1)])
    for l in range(nlayers):
        wv = I["mod_w"][l].rearrange("(kc p) n -> p kc n", p=128)
        P.dma("sp", mb[:], I["mod_bT"][l], writes=[(MB, None)], sembuf=MB)
        for gi, g in enumerate(("pre_mix_gT", "post_mix_gT", "pre_ffn_gT", "post_ffn_gT")):
            P.dma("sp", gT[:, gi, :], I[g][l], writes=[(GT, gi)], sembuf=GT)
        for s in range(24):
            w = ws[s % 2]
            P.dma("sp", w[:], wv[:, :, s * 512:(s + 1) * 512], writes=[(WS[s % 2], None)], sembuf=WS[s % 2])
            for j in range(4):
                col = s * 4 + j
                for kc in range(16):
                    MM(P, mps[:, col, :], w[:, kc, j * 128:(j + 1) * 128], sc[:, kc, :], kc == 0, kc == 15,
                       [(WS[s % 2], None), (SC, None)], [(MPS, None)])
            yield
        mod = K["mod"][:, l, :]
        TT(P, "dve", mod, mps[:, :, 0], mb[:], ALU.add, [(MPS, None), (MB, None)], [(MODB, (l, "mod"))])
        if C.dbg and "MODV" in C.dbg:
            P.dma("pool", C.D["MODV"][:, l * 96:(l + 1) * 96], mod, reads=[(MODB, (l, "mod"))], writes=[(C.B["MODV"], l)], sembuf=MB)
        vec = K["vec"]
        for half, (gpre, gpost) in enumerate(((0, 1), (2, 3))):
            o = half * 48
            STT(P, vec[:, l, half * 3 + 0, :], mod[:, o + 16:o + 32], 1.0, gT[:, gpre, :], ALU.add, ALU.mult,
                [(MODB, (l, "mod")), (GT, gpre)], [(MODB, (l, half, 0))])
            CP(P, "dve", vec[:, l, half * 3 + 1, :], mod[:, o:o + 16], [(MODB, (l, "mod"))], [(MODB, (l, half, 1))])
            TT(P, "dve", vec[:, l, half * 3 + 2, :], mod[:, o + 32:o + 48], gT[:, gpost, :], ALU.mult,
               [(MODB, (l, "mod")), (GT, gpost)], [(MODB, (l, half, 2))])


def replicate_cols(C, rep, REP, colvec, ncols, R, psb, PSB, tmp, TMP):
    P, K, KB = C.P, C.K, C.KB
    for c0 in range(0, ncols, 4):
        n = min(4, ncols - c0)
        for j in range(n):
            c = c0 + j
            TS(P, "dve", tmp[:, j * 128:(j + 1) * 128], K["identf"][:], colvec[:, c:c + 1], None, ALU.mult, None,
               R + [(KB, "identf")], [(TMP, j)])
            MM(P, psb[:, j * 128:(j + 1) * 128], K["ones"][:], tmp[:, j * 128:(j + 1) * 128], True, True,
               [(TMP, j), (KB, "ones")], [(PSB, None)])
        CP(P, "act", rep[:, c0 * 128:(c0 + n) * 128], psb[:, 0:n * 128], [(PSB, None)], [(REP, c0 // 4)])


def norm_to_hT(C, t, ti, src_ap, SRCB, xt, XT, ss, rs, SS, xn, XN, junk, JK, tp, TP, hTb, HTB, A, Sv, VR, keep_x=None):
    P, K, KB = C.P, C.K, C.KB
    P.dma("sp", xt[:], src_ap[t * 128:(t + 1) * 128, :], reads=[(SRCB, t)], writes=[(XT, None)], sembuf=XT)
    ACTF(P, junk[:], xt[:], AF.Square, [(XT, None)], [(JK, None), (SS, "ss")], accum=ss[:])
    ACTF(P, rs[:], ss[:], AF.Sqrt, [(SS, "ss")], [(SS, "rs")], bias=EPS, scale=1.0 / D_)
    RECIP(P, rs[:], rs[:], [(SS, "rs")], [(SS, "rs")])
    TS(P, "dve", xn[:], xt[:], rs[:, 0:1], None, ALU.mult, None, [(XT, None), (SS, "rs")], [(XN, None)])
    for c in range(16):
        TR(P, tp[:, c * 128:(c + 1) * 128], xn[:, c * 128:(c + 1) * 128], K["ident"][:], [(XN, None), (KB, "ident")], [(TP, c // 8)])
    for c in range(16):
        o = hTb[:, c, ti * 128:(ti + 1) * 128]
        i_ = tp[:, c * 128:(c + 1) * 128]
        if c < 8:
            ACTF(P, o, i_, AF.Identity, [(TP, c // 8)] + VR, [(HTB, (ti, c))], bias=Sv[:, c:c + 1], scale=A[:, c:c + 1])
        else:
            TS(P, "dve", o, i_, A[:, c:c + 1], Sv[:, c:c + 1], ALU.mult, ALU.add, [(TP, c // 8)] + VR, [(HTB, (ti, c))])


def phase_inproj(C, l, src):
    P, I, D, B, K = C.P, C.I, C.D, C.B, C.K
    SRCB = B["XR"]
    xt = [C.sb("a_xt%d" % i, [128, D_], F32) for i in range(2)]; XT = [Buf("a_xt%d" % i) for i in range(2)]
    junk = C.sb("a_junk", [128, D_], BF16); JK = Buf("a_junk")
    ss = [C.sb("a_ss%d" % i, [128, 1], F32) for i in range(2)]
    rs = [C.sb("a_rs%d" % i, [128, 1], F32) for i in range(2)]; SS = [Buf("a_ss%d" % i) for i in range(2)]
    xn = [C.sb("a_xn%d" % i, [128, D_], BF16) for i in range(2)]; XN = [Buf("a_xn%d" % i) for i in range(2)]
    tp = [C.ps("a_tp%d" % i, [128, D_], BF16) for i in range(1)]; TP = [PB("a_tp%d" % i) for i in range(1)]
    hT = [C.sb("a_hT%d" % i, [128, 16, 512], BF16) for i in range(2)]; HT = [Buf("a_hT%d" % i) for i in range(2)]
    ws = [C.sb("a_ws%d" % i, [128, 16, 512], BF16) for i in range(2)]; WS = [Buf("a_ws%d" % i) for i in range(2)]
    pm = [C.ps("a_pm%d" % i, [128, 512]) for i in range(4)]; PM = [PB("a_pm%d" % i) for i in range(4)]
    st = [C.sb("a_st%d" % i, [128, 512], BF16) for i in range(4)]; ST = [Buf("a_st%d" % i) for i in range(4)]
    sg = [C.sb("a_sg%d" % i, [128, 16], F32) for i in range(2)]; SG = [Buf("a_sg%d" % i) for i in range(2)]
    A = K["vec"][:, l, 0, :]; Sv = K["vec"][:, l, 1, :]
    VR = [(C.MODB, (l, 0, 0)), (C.MODB, (l, 0, 1))]
    wfm = D["WFM"][l].rearrange("(kc p) n -> p kc n", p=128)
    wtm = D["WTM"][l].rearrange("(kc p) n -> p kc n", p=128)
    cnt = [0, 0, 0]

    def norm_block(blk):
        for ti in range(4):
            t = blk * 4 + ti
            s = t % 2
            norm_to_hT(C, t, ti, src, SRCB, xt[s], XT[s], ss[s], rs[s], SS[s], xn[s], XN[s], junk, JK, tp[0], TP[0],
                       hT[blk % 2], HT[blk % 2], A, Sv, VR)

    def gemm_block(blk):
        hTb, HTB = hT[blk % 2], HT[blk % 2]
        for s in range(6 if KNOB.get("fm", True) else 0):
            wi = cnt[0] % 2; cnt[0] += 1
            P.dma("sp", ws[wi][:], wfm[:, :, s * 512:(s + 1) * 512], reads=[(B["WFM"], None)], writes=[(WS[wi], None)], sembuf=WS[wi])
            for j in range(4):
                ch = s * 4 + j
                pi = cnt[1] % 4; cnt[1] += 1
                for kc in range(16):
                    MM(P, pm[pi][:], ws[wi][:, kc, j * 128:(j + 1) * 128], hTb[:, kc, :], kc == 0, kc == 15,
                       [(WS[wi], None), (HTB, None)], [(PM[pi], None)])
                CP(P, "act" if pi % 2 == 0 else "dve", st[pi][:], pm[pi][:], [(PM[pi], None)], [(ST[pi], None)])
                P.dma("pool", D["QKT"][ch][:, blk * 512:(blk + 1) * 512], st[pi][:], reads=[(ST[pi], None)],
                      writes=[(B["QKT"], (ch, blk))], sembuf=ST[pi])
        for s in range(4 if KNOB.get("tm", True) else 0):
            n0 = s * 512
            ncol = min(512, NTM - n0)
            wi = cnt[0] % 2; cnt[0] += 1
            P.dma("sp", ws[wi][:, :, 0:ncol], wtm[:, :, n0:n0 + ncol], reads=[(B["WTM"], None)], writes=[(WS[wi], None)], sembuf=WS[wi])
            for ti in range(4):
                t = blk * 4 + ti
                pi = cnt[1] % 4; cnt[1] += 1
                for kc in range(16):
                    MM(P, pm[pi][:, 0:ncol], hTb[:, kc, ti * 128:(ti + 1) * 128], ws[wi][:, kc, 0:ncol], kc == 0, kc == 15,
                       [(WS[wi], None), (HTB, None)], [(PM[pi], None)])
                CP(P, "act" if pi % 2 == 0 else "dve", st[pi][:, 0:ncol], pm[pi][:, 0:ncol], [(PM[pi], None)], [(ST[pi], None)])
                P.dma("pool", D["PTM"][t * 128:(t + 1) * 128, n0:n0 + ncol], st[pi][:, 0:ncol], reads=[(ST[pi], None)],
                      writes=[(B["PTM"], (t, s))], sembuf=ST[pi])
                if s == 3:
                    gi = cnt[2] % 2; cnt[2] += 1
                    CP(P, "dve", sg[gi][:], pm[pi][:, ncol - 16:ncol], [(PM[pi], None)], [(SG[gi], None)])
                    P.dma("pool", D["GATES"][t * 128:(t + 1) * 128, :], sg[gi][:], reads=[(SG[gi], None)],
                          writes=[(B["GATES"], t)], sembuf=SG[gi])

    nblk = KNOB.get("nblk", 8)
    norm_block(0)
    for blk in range(nblk):
        if blk + 1 < nblk:
            norm_block(blk + 1)
        if KNOB.get("gemm", True):
            gemm_block(blk)


def phase_window(C, l):
    P, I, D, B, K = C.P, C.I, C.D, C.B, C.K
    E = C.sb("w_E", [128, 12, 384], F32); EB = Buf("w_E")
    snk = C.sb("w_snk", [128, 12], F32); SK = Buf("w_snk")
    qt = [C.sb("w_q%d" % i, [128, 6, 128], BF16) for i in range(2)]; QT = [Buf("w_q%d" % i) for i in range(2)]
    kt = [C.sb("w_k%d" % i, [128, 4, 384], BF16) for i in range(2)]; KT = [Buf("w_k%d" % i) for i in range(2)]
    vt = [C.sb("w_v%d" % i, [128, 3, 4, 65], BF16) for i in range(2)]; VT = [Buf("w_v%d" % i) for i in range(2)]
    pss = [C.ps("w_ps%d" % i, [128, 512]) for i in range(2)]; PSS = [PB("w_ps%d" % i) for i in range(2)]
    acc = [C.ps("w_acc%d" % i, [128, 512]) for i in range(2)]; ACC = [PB("w_acc%d" % i) for i in range(2)]
    pe_ = [C.sb("w_pe%d" % i, [128, 384], F32) for i in range(2)]; PEB = [Buf("w_pe%d" % i) for i in range(2)]
    pT = [C.sb("w_pT%d" % i, [128, 384], BF16) for i in range(2)]; PT = [Buf("w_pT%d" % i) for i in range(2)]
    den = [C.sb("w_den%d" % i, [128, 12], F32) for i in range(2)]; DEN = [Buf("w_den%d" % i) for i in range(2)]
    ya = [C.sb("w_ya%d" % i, [128, 768], BF16) for i in range(2)]; YA = [Buf("w_ya%d" % i) for i in range(2)]
    P.dma("sp", E[:].rearrange("p h c -> p (h c)"), I["c_E"], writes=[(EB, None)], sembuf=EB)
    P.dma("sp", snk[:], I["attn_sink"][l].partition_broadcast(128), writes=[(SK, None)], sembuf=SK)
    ACTF(P, snk[:], snk[:], AF.Exp, [(SK, None)], [(SK, None)])
    for i in range(2):
        MEMSET(P, "dve", vt[i][:], 1.0, [], [(VT[i], None)])
    qk = D["QKT"].rearrange("c p t -> p c t")
    scale = 64 ** -0.5
    hc = 0
    for i in range(NT):
        s = i % 2
        j0, j1 = max(0, i - 1), min(NT - 1, i + 1)
        d0, d1 = j0 - (i - 1), j1 - (i - 1)
        P.dma("sp", qt[s][:], qk[:, FM_AQ:FM_AQ + 6, i * 128:(i + 1) * 128], reads=[(B["QKT"], None)], writes=[(QT[s], None)], sembuf=QT[s])
        P.dma("sp", kt[s][:, :, d0 * 128:(d1 + 1) * 128], qk[:, FM_AK:FM_AK + 4, j0 * 128:(j1 + 1) * 128], reads=[(B["QKT"], None)],
              writes=[(KT[s], None)], sembuf=KT[s])
        for d in range(d0, d1 + 1):
            j = i - 1 + d
            P.dma("sp", vt[s][:, d, :, 0:64], D["PTM"][j * 128:(j + 1) * 128, 0:256].rearrange("p (h d) -> p h d", d=64),
                  reads=[(B["PTM"], None)], writes=[(VT[s], None)], sembuf=VT[s])
        lo, hi = d0 * 128, (d1 + 1) * 128
        for hq in range(12):
            g = hq // 3; off = (hq % 2) * 64; c = hq // 2
            b = hc % 2; hc += 1
            for d in range(d0, d1 + 1):
                MM(P, pss[b][:, d * 128:(d + 1) * 128], kt[s][off:off + 64, g, d * 128:(d + 1) * 128], qt[s][off:off + 64, c, :], True, True,
                   [(KT[s], None), (QT[s], None)], [(PSS[b], None)])
            ACTF(P, pe_[b][:, lo:hi], pss[b][:, lo:hi], AF.Exp, [(PSS[b], None)], [(PEB[b], None)], scale=scale)
            TT(P, "dve", pT[b][:, lo:hi], pe_[b][:, lo:hi], E[:, hq, lo:hi], ALU.mult, [(PEB[b], None), (EB, None)], [(PT[b], None)])
            a = acc[hq // 6]; AB = ACC[hq // 6]
            co = (hq % 6) * 65
            for d in range(d0, d1 + 1):
                MM(P, a[:, co:co + 65], pT[b][:, d * 128:(d + 1) * 128], vt[s][:, d, g, :], d == d0, d == d1,
                   [(PT[b], None), (VT[s], None)], [(AB, None)])
        for hq in range(12):
            a = acc[hq // 6]; AB = ACC[hq // 6]; co = (hq % 6) * 65
            TS(P, "dve", den[s][:, hq:hq + 1], a[:, co + 64:co + 65], snk[:, hq:hq + 1], None, ALU.add, None, [(AB, None), (SK, None)], [(DEN[s], None)])
        RECIP(P, den[s][:], den[s][:], [(DEN[s], None)], [(DEN[s], None)])
        for hq in range(12):
            a = acc[hq // 6]; AB = ACC[hq // 6]; co = (hq % 6) * 65
            TS(P, "dve", ya[s][:, hq * 64:(hq + 1) * 64], a[:, co:co + 64], den[s][:, hq:hq + 1], None, ALU.mult, None,
               [(AB, None), (DEN[s], None)], [(YA[s], None)])
        P.dma("pool", D["Y"][i * 128:(i + 1) * 128, 0:768], ya[s][:], reads=[(YA[s], None)], writes=[(B["Y"], ("a", i))], sembuf=YA[s])


def rep_sumsq(C, sq_chunks, R, rep_ps, RPS, rstd, RSTD, n, width):
    P, K, KB = C.P, C.K, C.KB
    for i, (ap, rows) in enumerate(sq_chunks):
        MM(P, rep_ps[:, 0:width], K["onesb"][0:rows, :], ap, i == 0, i == len(sq_chunks) - 1, R + [(KB, "onesb")], [(RPS, None)])
    ACTF(P, rstd[:, 0:width], rep_ps[:, 0:width], AF.Sqrt, [(RPS, None)], [(RSTD, None)], bias=EPS, scale=1.0 / n)
    RECIP(P, rstd[:, 0:width], rstd[:, 0:width], [(RSTD, None)], [(RSTD, None)])


def phase_mla_prep(C, l):
    P, I, D, B, K, KB = C.P, C.I, C.D, C.B, C.K, C.KB
    wqf = C.sb("p_wqf", [128, 4, 768], F32); WQF = Buf("p_wqf")
    wq = C.sb("p_wq", [128, 4, 768], BF16); WQ = Buf("p_wq")
    wkf = C.sb("p_wkf", [128, 1024], F32); WKF = Buf("p_wkf")
    wk = C.sb("p_wk", [128, 1024], BF16); WK = Buf("p_wk")
    gq = C.sb("p_gq", [128, 4], F32); GQ = Buf("p_gq")
    gk = C.sb("p_gk", [128, 1], F32); GK = Buf("p_gk")
    P.dma("sp", wqf[:], I["w_uq"][l].rearrange("(kc p) n -> p kc n", p=128), writes=[(WQF, None)], sembuf=WQF)
    P.dma("sp", wkf[:], I["w_ukv"][l], writes=[(WKF, None)], sembuf=WKF)
    P.dma("sp", gq[:], I["q_norm_gT"][l], writes=[(GQ, None)], sembuf=GQ)
    P.dma("sp", gk[:], I["kv_norm_gT"][l], writes=[(GK, None)], sembuf=GK)
    for kc in range(4):
        TS(P, "dve", wq[:, kc, :], wqf[:, kc, :], gq[:, kc:kc + 1], None, ALU.mult, None, [(WQF, None), (GQ, None)], [(WQ, kc)])
    TS(P, "dve", wk[:], wkf[:], gk[:, 0:1], None, ALU.mult, None, [(WKF, None), (GK, None)], [(WK, None)])
    posi = C.sb("p_posi", [64, S_], I32); POSI = Buf("p_posi")
    ang = C.sb("p_ang", [64, S_], F32); ANG = Buf("p_ang")
    cosT = C.sb("p_cos", [64, S_], F32); COS = Buf("p_cos")
    sinT = C.sb("p_sin", [64, S_], F32); SIN = Buf("p_sin")
    invf = C.sb("p_invf", [64, 1], F32); INVF = Buf("p_invf")
    P.dma("sp", posi[:], I["pos"].partition_broadcast(64), writes=[(POSI, None)], sembuf=POSI)
    P.dma("sp", invf[:], I["c_invf"], writes=[(INVF, None)], sembuf=INVF)
    CP(P, "dve", ang[:], posi[:], [(POSI, None)], [(ANG, None)])
    TS(P, "dve", ang[:], ang[:], invf[:, 0:1], None, ALU.mult, None, [(ANG, None), (INVF, None)], [(ANG, None)])
    TWO_PI = 2.0 * math.pi
    MAGIC = 12582912.0
    TS(P, "dve", sinT[:], ang[:], 1.0 / TWO_PI, MAGIC, ALU.mult, ALU.add, [(ANG, None)], [(SIN, None)])
    TS(P, "dve", sinT[:], sinT[:], -MAGIC, None, ALU.add, None, [(SIN, None)], [(SIN, None)])
    STT(P, sinT[:], sinT[:], -TWO_PI, ang[:], ALU.mult, ALU.add, [(SIN, None), (ANG, None)], [(SIN, None)])
    TS(P, "dve", ang[:], ang[:], 0.5 * math.pi, None, ALU.add, None, [(ANG, None)], [(ANG, None)])
    TS(P, "dve", cosT[:], ang[:], 1.0 / TWO_PI, MAGIC, ALU.mult, ALU.add, [(ANG, None)], [(COS, None)])
    TS(P, "dve", cosT[:], cosT[:], -MAGIC, None, ALU.add, None, [(COS, None)], [(COS, None)])
    STT(P, cosT[:], cosT[:], -TWO_PI, ang[:], ALU.mult, ALU.add, [(COS, None), (ANG, None)], [(COS, None)])
    PI_LO = 3.1415925
    TS(P, "dve", sinT[:], sinT[:], -PI_LO, PI_LO, ALU.max, ALU.min, [(SIN, None)], [(SIN, None)])
    TS(P, "dve", cosT[:], cosT[:], -PI_LO, PI_LO, ALU.max, ALU.min, [(COS, None)], [(COS, None)])
    ACTF(P, sinT[:], sinT[:], AF.Sin, [(SIN, None)], [(SIN, None)])
    ACTF(P, cosT[:], cosT[:], AF.Sin, [(COS, None)], [(COS, None)])

    cq = [C.sb("p_cq%d" % i, [128, 4, 512], BF16) for i in range(2)]; CQ = [Buf("p_cq%d" % i) for i in range(2)]
    ckv = [C.sb("p_ckv%d" % i, [128, 512], BF16) for i in range(2)]; CKV = [Buf("p_ckv%d" % i) for i in range(2)]
    kr = [C.sb("p_kr%d" % i, [64, 512], BF16) for i in range(2)]; KRB = [Buf("p_kr%d" % i) for i in range(2)]
    sq = C.sb("p_sq", [128, 4, 512], BF16); SQ = Buf("p_sq")
    sqk = C.sb("p_sqk", [128, 512], BF16); SQK = Buf("p_sqk")
    rps = C.ps("p_rps", [128, 512]); RPS = PB("p_rps")
    rstd = C.sb("p_rstd", [128, 512], F32); RSTD = Buf("p_rstd")
    rstdk = C.sb("p_rstdk", [128, 512], F32); RSTDK = Buf("p_rstdk")
    pq = [C.ps("p_pq%d" % i, [128, 512]) for i in range(3)]; PQ = [PB("p_pq%d" % i) for i in range(3)]
    prot = C.ps("p_prot", [64, 512]); PROT = PB("p_prot")
    pv = C.ps("p_pv", [128, 512]); PV = PB("p_pv")
    ptm = C.ps("p_ptm", [128, 4, 2]); PTM_ = PB("p_ptm")
    so = [C.sb("p_so%d" % i, [128, 512], BF16) for i in range(3)]; SO = [Buf("p_so%d" % i) for i in range(3)]
    t1 = C.sb("p_t1", [64, 512], F32); T1 = Buf("p_t1")
    t2 = C.sb("p_t2", [64, 512], F32); T2 = Buf("p_t2")
    raw = C.sb("p_raw", [64, 512], BF16); RAW = Buf("p_raw")
    rtm = C.sb("p_rtm", [128, 4], F32); RTM = Buf("p_rtm")
    vo = [C.sb("p_vo%d" % i, [128, 512], BF16) for i in range(2)]; VO = [Buf("p_vo%d" % i) for i in range(2)]
    qk = D["QKT"].rearrange("c p t -> p c t")
    oc = [0]

    def rope_out(src_ps, SRC, rst, RST, blk, dst_ap, DSTB, dkey):
        cs = slice(blk * 512, (blk + 1) * 512)
        if rst is not None:
            TT(P, "dve", raw[:], src_ps, rst[0:64, :], ALU.mult, [(SRC, None), (RST, None)], [(RAW, None)])
        else:
            CP(P, "dve", raw[:], src_ps, [(SRC, None)], [(RAW, None)])
        MM(P, prot[:], K["R"][:], raw[:], True, True, [(RAW, None), (KB, "R")], [(PROT, None)])
        TT(P, "dve", t1[:], raw[:], cosT[:, cs], ALU.mult, [(RAW, None), (COS, None)], [(T1, None)])
        TT(P, "dve", t2[:], prot[:], sinT[:, cs], ALU.mult, [(PROT, None), (SIN, None)], [(T2, None)])
        o = oc[0] % 3; oc[0] += 1
        TT(P, "dve", so[o][0:64, :], t1[:], t2[:], ALU.add, [(T1, None), (T2, None)], [(SO[o], None)])
        P.dma("pool", dst_ap, so[o][0:64, :], reads=[(SO[o], None)], writes=[(DSTB, dkey)], sembuf=SO[o])

    for blk in range(8):
        s = blk % 2
        cs = slice(blk * 512, (blk + 1) * 512)
        P.dma("sp", cq[s][:], qk[:, FM_BCQ:FM_BCQ + 4, cs], reads=[(B["QKT"], None)], writes=[(CQ[s], None)], sembuf=CQ[s])
        P.dma("sp", ckv[s][:], D["QKT"][FM_BCKV][:, cs], reads=[(B["QKT"], None)], writes=[(CKV[s], None)], sembuf=CKV[s])
        P.dma("sp", kr[s][:], D["QKT"][FM_BKR][0:64, cs], reads=[(B["QKT"], None)], writes=[(KRB[s], None)], sembuf=KRB[s])
        rows = [128, 128, 128, 64]
        ACTF(P, sq[:], cq[s][:], AF.Square, [(CQ[s], None)], [(SQ, None)])
        rep_sumsq(C, [(sq[0:rows[kc], kc, :], rows[kc]) for kc in range(4)], [(SQ, None)], rps, RPS, rstd, RSTD, 448.0, 512)
        for h in range(4):
            pi = h % 3
            for kc in range(4):
                MM(P, pq[pi][:], wq[0:rows[kc], kc, h * 192:h * 192 + 128], cq[s][0:rows[kc], kc, :], kc == 0, kc == 3,
                   [(WQ, None), (CQ[s], None)], [(PQ[pi], None)])
            o = oc[0] % 3; oc[0] += 1
            TT(P, "dve", so[o][:], pq[pi][:], rstd[:], ALU.mult, [(PQ[pi], None), (RSTD, None)], [(SO[o], None)])
            P.dma("pool", D["QN"][h][:, cs], so[o][:], reads=[(SO[o], None)], writes=[(B["QN"], (h, blk))], sembuf=SO[o])
            pi = (h + 1) % 3
            for kc in range(4):
                MM(P, pq[pi][0:64, :], wq[0:rows[kc], kc, h * 192 + 128:h * 192 + 192], cq[s][0:rows[kc], kc, :], kc == 0, kc == 3,
                   [(WQ, None), (CQ[s], None)], [(PQ[pi], None)])
            rope_out(pq[pi][0:64, :], PQ[pi], rstd, RSTD, blk, D["QR"][h][:, cs], B["QR"], (h, blk))
        ACTF(P, sqk[:], ckv[s][:], AF.Square, [(CKV[s], None)], [(SQK, None)])
        rep_sumsq(C, [(sqk[:], 128)], [(SQK, None)], rps, RPS, rstdk, RSTDK, 128.0, 512)
        for h in range(4):
            pi = h % 3
            MM(P, pq[pi][:], wk[:, h * 256:h * 256 + 128], ckv[s][:], True, True, [(WK, None), (CKV[s], None)], [(PQ[pi], None)])
            o = oc[0] % 3; oc[0] += 1
            TT(P, "dve", so[o][:], pq[pi][:], rstdk[:], ALU.mult, [(PQ[pi], None), (RSTDK, None)], [(SO[o], None)])
            P.dma("pool", D["KN"][h][:, cs], so[o][:], reads=[(SO[o], None)], writes=[(B["KN"], (h, blk))], sembuf=SO[o])
        for ti in range(4):
            MM(P, ptm[:, ti, :], sqk[:, ti * 128:(ti + 1) * 128], K["onesb"][:, 0:2], True, True, [(SQK, None), (KB, "onesb")], [(PTM_, None)])
        ACTF(P, rtm[:], ptm[:, :, 0], AF.Sqrt, [(PTM_, None)], [(RTM, None)], bias=EPS, scale=1.0 / 128.0)
        RECIP(P, rtm[:], rtm[:], [(RTM, None)], [(RTM, None)])
        for ti in range(4):
            t = blk * 4 + ti
            for h in range(4):
                MM(P, pv[:, h * 128:(h + 1) * 128], ckv[s][:, ti * 128:(ti + 1) * 128], wk[:, h * 256 + 128:h * 256 + 256], True, True,
                   [(WK, None), (CKV[s], None)], [(PV, None)])
            v = t % 2
            TS(P, "dve", vo[v][:], pv[:], rtm[:, ti:ti + 1], None, ALU.mult, None, [(PV, None), (RTM, None)], [(VO[v], None)])
            P.dma("pool", D["VB"][t * 128:(t + 1) * 128, :], vo[v][:], reads=[(VO[v], None)], writes=[(B["VB"], t)], sembuf=VO[v])
        MM(P, prot[:], K["R"][:], kr[s][:], True, True, [(KRB[s], None), (KB, "R")], [(PROT, None)])
        TT(P, "dve", t1[:], kr[s][:], cosT[:, cs], ALU.mult, [(KRB[s], None), (COS, None)], [(T1, None)])
        TT(P, "dve", t2[:], prot[:], sinT[:, cs], ALU.mult, [(PROT, None), (SIN, None)], [(T2, None)])
        o = oc[0] % 3; oc[0] += 1
        TT(P, "dve", so[o][0:64, :], t1[:], t2[:], ALU.add, [(T1, None), (T2, None)], [(SO[o], None)])
        P.dma("pool", D["KR"][:, cs], so[o][0:64, :], reads=[(SO[o], None)], writes=[(B["KR"], blk)], sembuf=SO[o])


def phase_mla_attn(C, l):
    P, I, D, B, K = C.P, C.I, C.D, C.B, C.K
    krT = C.sb("m_kr", [64, S_], BF16); KRT = Buf("m_kr")
    qn = C.sb("m_qn", [128, S_], BF16); QN = Buf("m_qn")
    qr = C.sb("m_qr", [64, S_], BF16); QR = Buf("m_qr")
    kn = C.sb("m_kn", [128, S_], BF16); KN = Buf("m_kn")
    va = C.sb("m_va", [128, NT, 129], BF16); VA = Buf("m_va")
    pss = [C.ps("m_ps%d" % i, [128, 512]) for i in range(2)]; PSS = [PB("m_ps%d" % i) for i in range(2)]
    acc = [C.ps("m_acc%d" % i, [128, 512]) for i in range(4)]; ACC = [PB("m_acc%d" % i) for i in range(4)]
    pT = [C.sb("m_pT%d" % i, [128, 512], BF16) for i in range(3)]; PT = [Buf("m_pT%d" % i) for i in range(3)]
    rd = [C.sb("m_rd%d" % i, [128, 1], F32) for i in range(2)]; RD = [Buf("m_rd%d" % i) for i in range(2)]
    yo = [C.sb("m_yo%d" % i, [128, 128], BF16) for i in range(2)]; YO = [Buf("m_yo%d" % i) for i in range(2)]
    scale = 192 ** -0.5
    P.dma("sp", krT[:], D["KR"], reads=[(B["KR"], None)], writes=[(KRT, None)], sembuf=KRT)
    MEMSET(P, "dve", va[:], 1.0, [], [(VA, "ones")])
    n = 0
    oc = 0
    for h in range(4):
        P.dma("sp", qn[:], D["QN"][h], reads=[(B["QN"], None)], writes=[(QN, None)], sembuf=QN)
        P.dma("sp", qr[:], D["QR"][h], reads=[(B["QR"], None)], writes=[(QR, None)], sembuf=QR)
        P.dma("sp", kn[:], D["KN"][h], reads=[(B["KN"], None)], writes=[(KN, None)], sembuf=KN)
        vbv = D["VB"][:, h * 128:(h + 1) * 128].rearrange("(t p) d -> p t d", p=128)
        for t0 in range(0, NT, 4):
            P.dma("sp", va[:, t0:t0 + 4, 0:128], vbv[:, t0:t0 + 4, :], reads=[(B["VB"], None), (VA, "ones")],
                  writes=[(VA, ("v", t0))], sembuf=VA)
        for qb in range(8):
            qs = slice(qb * 512, (qb + 1) * 512)
            for j in range(NT):
                ks = slice(j * 128, (j + 1) * 128)
                b = n % 2; pb = n % 3; n += 1
                MM(P, pss[b][:], kn[:, ks], qn[:, qs], True, False, [(KN, None), (QN, None)], [(PSS[b], None)])
                MM(P, pss[b][:], krT[:, ks], qr[:, qs], False, True, [(KRT, None), (QR, None)], [(PSS[b], None)])
                ACTF(P, pT[pb][:], pss[b][:], AF.Exp, [(PSS[b], None)], [(PT[pb], None)], scale=scale)
                for ii in range(4):
                    MM(P, acc[ii][:, 0:129], pT[pb][:, ii * 128:(ii + 1) * 128], va[:, j, :], j == 0, j == NT - 1,
                       [(PT[pb], None), (VA, ("v", (j // 4) * 4)), (VA, "ones")], [(ACC[ii], None)])
            for ii in range(4):
                t = qb * 4 + ii
                o = oc % 2; oc += 1
                RECIP(P, rd[o][:], acc[ii][:, 128:129], [(ACC[ii], None)], [(RD[o], None)])
                TS(P, "dve", yo[o][:], acc[ii][:, 0:128], rd[o][:, 0:1], None, ALU.mult, None, [(ACC[ii], None), (RD[o], None)], [(YO[o], None)])
                P.dma("pool", D["Y"][t * 128:(t + 1) * 128, 768 + h * 128:768 + (h + 1) * 128], yo[o][:], reads=[(YO[o], None)],
                      writes=[(B["Y"], ("b", h, t))], sembuf=YO[o])


def phase_mlstm_prep(C, l):
    P, I, D, B, K, KB = C.P, C.I, C.D, C.B, C.K, C.KB
    g = C.sb("g_g", [128, NT, 16], F32); G = Buf("g_g")
    gb = C.sb("g_gb", [128, 16], F32); GB = Buf("g_gb")
    lf = C.sb("g_lf", [128, NT, 8], F32); LF = Buf("g_lf")
    tot = C.sb("g_tot", [128, NT, 8], F32); TOT = Buf("g_tot")
    off = C.sb("g_off", [128, NT, 8], F32); OFF = Buf("g_off")
    cum = C.sb("g_cum", [128, NT, 8], F32); CUM = Buf("g_cum")
    ibs = C.sb("g_ibs", [128, NT, 8], F32); IBS = Buf("g_ibs")
    pt = C.ps("g_pt", [128, 256]); PTB = PB("g_pt")
    pc = C.ps("g_pc", [128, 256]); PCB = PB("g_pc")
    pc2 = C.ps("g_pc2", [128, 256]); PCB2 = PB("g_pc2")
    pr = [C.ps("g_pr%d" % i, [128, 512]) for i in range(2)]; PR = [PB("g_pr%d" % i) for i in range(2)]
    dg = [C.sb("g_dg%d" % i, [128, 512], F32) for i in range(2)]; DG = [Buf("g_dg%d" % i) for i in range(2)]
    ro = [C.sb("g_ro%d" % i, [128, 512], F32) for i in range(2)]; RO = [Buf("g_ro%d" % i) for i in range(2)]
    gv = D["GATES"].rearrange("(t p) c -> p t c", p=128)
    for t0 in range(0, NT, 4):
        P.dma("sp", g[:, t0:t0 + 4, :], gv[:, t0:t0 + 4, :], reads=[(B["GATES"], None)], writes=[(G, ("ld", t0))], sembuf=G)
    P.dma("sp", gb[:], I["gate_b"][l].partition_broadcast(128), writes=[(GB, None)], sembuf=GB)
    for t in range(NT):
        TT(P, "dve", g[:, t, :], g[:, t, :], gb[:], ALU.add, [(G, None), (GB, None)], [(G, None)])
    ACTF(P, lf[:], g[:, :, 8:16], AF.Exp, [(G, None)], [(LF, None)], scale=-1.0)
    ACTF(P, lf[:], lf[:], AF.Ln, [(LF, None)], [(LF, None)], bias=1.0)
    TS(P, "dve", lf[:], lf[:], -1.0, None, ALU.mult, None, [(LF, None)], [(LF, None)])
    lf2 = lf[:].rearrange("p t c -> p (t c)")
    MM(P, pt[:], K["ones"][:], lf2, True, True, [(LF, None), (KB, "ones")], [(PTB, None)])
    CP(P, "dve", tot[:].rearrange("p t c -> p (t c)"), pt[:], [(PTB, None)], [(TOT, None)])
    MEMSET(P, "dve", off[:], 0.0, [], [(OFF, None)])
    for t in range(1, NT):
        TT(P, "dve", off[:, t, 0:4], off[:, t - 1, 0:4], tot[:, t - 1, 0:4], ALU.add, [(OFF, None), (TOT, None)], [(OFF, None)])
    for t in range(NT - 2, -1, -1):
        TT(P, "dve", off[:, t, 4:8], off[:, t + 1, 4:8], tot[:, t + 1, 4:8], ALU.add, [(OFF, None), (TOT, None)], [(OFF, None)])
    MM(P, pc[:], K["U"][:], lf2, True, True, [(LF, None), (KB, "U")], [(PCB, None)])
    MM(P, pc2[:], K["Lm"][:], lf2, True, True, [(LF, None), (KB, "L")], [(PCB2, None)])
    pc3 = pc[:].rearrange("p (t c) -> p t c", c=8)
    pc23 = pc2[:].rearrange("p (t c) -> p t c", c=8)
    TT(P, "dve", cum[:, :, 0:4], pc3[:, :, 0:4], off[:, :, 0:4], ALU.add, [(PCB, None), (OFF, None)], [(CUM, "f")])
    TT(P, "dve", cum[:, :, 4:8], pc23[:, :, 4:8], off[:, :, 4:8], ALU.add, [(PCB2, None), (OFF, None)], [(CUM, "b")])
    TT(P, "dve", ibs[:], g[:, :, 0:8], cum[:], ALU.subtract, [(G, None), (CUM, None)], [(IBS, None)])
    P.dma("pool", D["IBS"], ibs[:].rearrange("p t c -> p (t c)"), reads=[(IBS, None)], writes=[(B["IBS"], None)], sembuf=IBS)
    n = 0
    for c in range(8):
        for t0 in range(0, NT, 4):
            b = n % 2; n += 1
            for j in range(4):
                t = t0 + j
                TS(P, "dve", dg[b][:, j * 128:(j + 1) * 128], K["identf"][:], cum[:, t, c:c + 1], None, ALU.mult, None,
                   [(CUM, None), (KB, "identf")], [(DG[b], j)])
                MM(P, pr[b][:, j * 128:(j + 1) * 128], K["ones"][:], dg[b][:, j * 128:(j + 1) * 128], True, True,
                   [(DG[b], j), (KB, "ones")], [(PR[b], None)])
            CP(P, "act", ro[b][:], pr[b][:], [(PR[b], None)], [(RO[b], None)])
            P.dma("pool", D["BREP"][c][:, t0 * 128:(t0 + 4) * 128], ro[b][:], reads=[(RO[b], None)], writes=[(B["BREP"], (c, t0))], sembuf=RO[b])


def phase_mlstm_attn(C, l):
    P, I, D, B, K, KB = C.P, C.I, C.D, C.B, C.K, C.KB
    ibs = C.sb("s_ibs", [128, NT, 8], F32); IBS = Buf("s_ibs")
    hg = C.sb("s_hg", [128, 768], F32); HG = Buf("s_hg")
    qT = C.sb("s_qT", [96, S_], BF16); QT = Buf("s_qT")
    kT = C.sb("s_kT", [96, S_], BF16); KT = Buf("s_kT")
    va = C.sb("s_va", [128, NT, 193], BF16); VA = Buf("s_va")
    br = [C.sb("s_br%d" % i, [128, S_], F32) for i in range(2)]; BR = [Buf("s_br%d" % i) for i in range(2)]
    pss = [C.ps("s_ps%d" % i, [128, 256]) for i in range(2)]; PSS = [PB("s_ps%d" % i) for i in range(2)]
    acc = [[C.ps("s_acc%d%d" % (d, i), [128, 512]) for i in range(2)] for d in range(2)]
    ACC = [[PB("s_acc%d%d" % (d, i)) for i in range(2)] for d in range(2)]
    w = [C.sb("s_w%d" % i, [128, 256], F32) for i in range(3)]; WB = [Buf("s_w%d" % i) for i in range(3)]
    wT = [C.sb("s_wT%d" % i, [128, 256], BF16) for i in range(3)]; WT = [Buf("s_wT%d" % i) for i in range(3)]
    op_ = [C.sb("s_op%d" % i, [128, 192], BF16) for i in range(2)]; OP = [Buf("s_op%d" % i) for i in range(2)]
    dn = [C.sb("s_dn%d" % i, [128, 2], F32) for i in range(2)]; DN = [Buf("s_dn%d" % i) for i in range(2)]
    hs = [C.sb("s_hs%d" % i, [128, 192], F32) for i in range(2)]; HS = [Buf("s_hs%d" % i) for i in range(2)]
    hb = [C.sb("s_hb%d" % i, [128, 192], F32) for i in range(2)]; HB = [Buf("s_hb%d" % i) for i in range(2)]
    jk = C.sb("s_jk", [128, 192], F32); JK = Buf("s_jk")
    ssq = [C.sb("s_ssq%d" % i, [128, 1], F32) for i in range(2)]; SSQ = [Buf("s_ssq%d" % i) for i in range(2)]
    yo = [C.sb("s_yo%d" % i, [128, 192], BF16) for i in range(2)]; YO = [Buf("s_yo%d" % i) for i in range(2)]
    scale = 96 ** -0.5
    P.dma("sp", ibs[:].rearrange("p t c -> p (t c)"), D["IBS"], reads=[(B["IBS"], None)], writes=[(IBS, None)], sembuf=IBS)
    P.dma("sp", hg[:], I["head_g"][l].partition_broadcast(128), writes=[(HG, None)], sembuf=HG)
    MEMSET(P, "dve", va[:], 1.0, [], [(VA, "ones")])
    U, Lm = K["U"], K["Lm"]
    n = 0
    fc = 0
    for h in range(4):
        P.dma("sp", qT[:], D["QKT"][FM_CQ + h][0:96, :], reads=[(B["QKT"], None)], writes=[(QT, None)], sembuf=QT)
        P.dma("sp", kT[:], D["QKT"][FM_CK + h][0:96, :], reads=[(B["QKT"], None)], writes=[(KT, None)], sembuf=KT)
        cvv = D["PTM"][:, 256 + h * 192:256 + (h + 1) * 192].rearrange("(t p) d -> p t d", p=128)
        for t0 in range(0, NT, 4):
            P.dma("sp", va[:, t0:t0 + 4, 0:192], cvv[:, t0:t0 + 4, :], reads=[(B["PTM"], None), (VA, "ones")],
                  writes=[(VA, ("v", t0))], sembuf=VA)
        for d in range(2):
            P.dma("sp", br[d][:], D["BREP"][d * 4 + h], reads=[(B["BREP"], None)], writes=[(BR[d], None)], sembuf=BR[d])
        for lb in range(16):
            i0 = lb * 2
            ls = slice(lb * 256, (lb + 1) * 256)
            first = [[True, True], [True, True]]
            nexp = [[0, 0], [0, 0]]
            total = [[i0 + 1, i0 + 2], [NT - i0, NT - i0 - 1]]
            for j in range(NT):
                ks = slice(j * 128, (j + 1) * 128)
                b = n % 2; n += 1
                MM(P, pss[b][:], kT[:, ks], qT[:, ls], True, True, [(KT, None), (QT, None)], [(PSS[b], None)])
                work = []
                for ii in range(2):
                    i = i0 + ii
                    if j < i:
                        work.append((0, ii, False))
                    elif j > i:
                        work.append((1, ii, False))
                    else:
                        work.append((0, ii, True)); work.append((1, ii, True))
                for (d, ii, masked) in work:
                    wi = fc % 3; fc += 1
                    c = d * 4 + h
                    lsl = slice((i0 + ii) * 128, (i0 + ii + 1) * 128)
                    wv = w[wi][:, 0:128]
                    ACTF(P, wv, br[d][:, lsl], AF.Exp, [(BR[d], None), (IBS, None)], [(WB[wi], None)], bias=ibs[:, j, c:c + 1])
                    if masked:
                        TT(P, "dve", wv, wv, (U if d == 0 else Lm)[:], ALU.mult, [(WB[wi], None), (KB, "U"), (KB, "L")], [(WB[wi], None)])
                    STT(P, wT[wi][:, 0:128], pss[b][:, ii * 128:(ii + 1) * 128], scale, wv, ALU.mult, ALU.mult,
                        [(PSS[b], None), (WB[wi], None)], [(WT[wi], None)])
                    nexp[d][ii] += 1
                    MM(P, acc[d][ii][:, 0:193], wT[wi][:, 0:128], va[:, j, :], nexp[d][ii] == 1, nexp[d][ii] == total[d][ii],
                       [(WT[wi], None), (VA, ("v", (j // 4) * 4)), (VA, "ones")], [(ACC[d][ii], None)])
            for ii in range(2):
                t = i0 + ii
                o = t % 2
                for d in range(2):
                    a = acc[d][ii]
                    ACTF(P, dn[o][:, d:d + 1], a[:, 192:193], AF.Abs, [(ACC[d][ii], None)], [(DN[o], d)])
                TS(P, "dve", dn[o][:], dn[o][:], 1.0, None, ALU.max, None, [(DN[o], None)], [(DN[o], None)])
                RECIP(P, dn[o][:], dn[o][:], [(DN[o], None)], [(DN[o], None)])
                TS(P, "dve", hs[o][:], acc[0][ii][:, 0:192], dn[o][:, 0:1], None, ALU.mult, None, [(ACC[0][ii], None), (DN[o], None)], [(HS[o], None)])
                TS(P, "dve", hb[o][:], acc[1][ii][:, 0:192], dn[o][:, 1:2], None, ALU.mult, None, [(ACC[1][ii], None), (DN[o], None)], [(HB[o], None)])
                TT(P, "dve", hs[o][:], hs[o][:], hb[o][:], ALU.add, [(HS[o], None), (HB[o], None)], [(HS[o], None)])
                ACTF(P, jk[:], hs[o][:], AF.Square, [(HS[o], None)], [(JK, None), (SSQ[o], None)], accum=ssq[o][:])
                ACTF(P, ssq[o][:], ssq[o][:], AF.Sqrt, [(SSQ[o], None)], [(SSQ[o], None)], bias=EPS, scale=1.0 / 192.0)
                RECIP(P, ssq[o][:], ssq[o][:], [(SSQ[o], None)], [(SSQ[o], None)])
                P.dma("sp", op_[o][:], D["PTM"][t * 128:(t + 1) * 128, 1024 + h * 192:1024 + (h + 1) * 192], reads=[(B["PTM"], None)],
                      writes=[(OP[o], None)], sembuf=OP[o])
                ACTF(P, hb[o][:], op_[o][:], AF.Sigmoid, [(OP[o], None)], [(HB[o], None)])
                STT(P, hs[o][:], hs[o][:], ssq[o][:, 0:1], hg[:, h * 192:(h + 1) * 192], ALU.mult, ALU.mult,
                    [(HS[o], None), (SSQ[o], None), (HG, None)], [(HS[o], None)])
                TT(P, "dve", yo[o][:], hs[o][:], hb[o][:], ALU.mult, [(HS[o], None), (HB[o], None)], [(YO[o], None)])
                P.dma("pool", D["Y"][t * 128:(t + 1) * 128, 1280 + h * 192:1280 + (h + 1) * 192], yo[o][:], reads=[(YO[o], None)],
                      writes=[(B["Y"], ("c", h, t))], sembuf=YO[o])


def post_norm_residual(C, t, pm4, PM4, x_ap, XB_key, xt, XT, rep, REP, junk, JK, ssp, SSP, dst_ap, DSTB, tmp, TMP):
    P = C.P
    for n in range(4):
        ACTF(P, junk[:, n * 512:(n + 1) * 512], pm4[n][:], AF.Square, [(PM4[n], None)], [(JK, n), (SSP, n)], accum=ssp[:, n:n + 1])
    TT(P, "dve", ssp[:, 4:5], ssp[:, 0:1], ssp[:, 1:2], ALU.add, [(SSP, 0), (SSP, 1)], [(SSP, "a")])
    TT(P, "dve", ssp[:, 5:6], ssp[:, 2:3], ssp[:, 3:4], ALU.add, [(SSP, 2), (SSP, 3)], [(SSP, "b")])
    TT(P, "dve", ssp[:, 6:7], ssp[:, 4:5], ssp[:, 5:6], ALU.add, [(SSP, "a"), (SSP, "b")], [(SSP, "c")])
    ACTF(P, ssp[:, 7:8], ssp[:, 6:7], AF.Sqrt, [(SSP, "c")], [(SSP, "r")], bias=EPS, scale=1.0 / D_)
    RECIP(P, ssp[:, 7:8], ssp[:, 7:8], [(SSP, "r")], [(SSP, "r")])
    for n in range(4):
        STT(P, tmp[:, n * 512:(n + 1) * 512], pm4[n][:], ssp[:, 7:8], rep[:, n * 512:(n + 1) * 512], ALU.mult, ALU.mult,
            [(PM4[n], None), (SSP, "r"), (REP, None)], [(TMP, n)])
    TT(P, "pool", xt[:], xt[:], tmp[:], ALU.add, [(XT, None), (TMP, None)], [(XT, None)])
    P.dma("pool", dst_ap[t * 128:(t + 1) * 128, :], xt[:], reads=[(XT, None)], writes=[(DSTB, t)], sembuf=XT)


def phase_outproj(C, l, xsrc, xdst):
    P, I, D, B, K, KB = C.P, C.I, C.D, C.B, C.K, C.KB
    wo = C.sb("o_w", [128, 16, D_], BF16); WO = Buf("o_w")
    rep = C.sb("o_rep", [128, D_], F32); REP = Buf("o_rep")
    rtmp = C.sb("o_rtmp", [128, 512], F32); RTMP = Buf("o_rtmp")
    yt = [C.sb("o_y%d" % i, [128, D_], BF16) for i in range(2)]; YT = [Buf("o_y%d" % i) for i in range(2)]
    yT = [C.sb("o_yT%d" % i, [128, 16, 128], BF16) for i in range(2)]; YTT = [Buf("o_yT%d" % i) for i in range(2)]
    xt = [C.sb("o_x%d" % i, [128, D_], F32) for i in range(2)]; XT = [Buf("o_x%d" % i) for i in range(2)]
    tmp = C.sb("o_tmp", [128, D_], F32); TMP = Buf("o_tmp")
    junk = C.sb("o_junk", [128, D_], BF16); JK = Buf("o_junk")
    ssp = [C.sb("o_ssp%d" % i, [128, 8], F32) for i in range(2)]; SSP = [Buf("o_ssp%d" % i) for i in range(2)]
    tp = C.ps("o_tp", [128, D_], BF16); TP = PB("o_tp")
    pm = [C.ps("o_pm%d" % i, [128, 512]) for i in range(4)]; PM = [PB("o_pm%d" % i) for i in range(4)]
    prep = C.ps("o_prep", [128, 512]); PREP = PB("o_prep")
    wov = D["WOUT"][l].rearrange("(kc p) n -> p kc n", p=128)
    for kc in range(16):
        P.dma("sp", wo[:, kc, :], wov[:, kc, :], reads=[(B["WOUT"], None)], writes=[(WO, kc)], sembuf=WO)
    replicate_cols(C, rep, REP, K["vec"][:, l, 2, :], 16, [(C.MODB, (l, 0, 2))], prep, PREP, rtmp, RTMP)
    for t in range(NT):
        s = t % 2
        P.dma("sp", yt[s][:], D["Y"][t * 128:(t + 1) * 128, :], reads=[(B["Y"], None)], writes=[(YT[s], None)], sembuf=YT[s])
        P.dma("sp", xt[s][:], xsrc[t * 128:(t + 1) * 128, :], reads=[(B["XR"], t)], writes=[(XT[s], None)], sembuf=XT[s])
        for c in range(16):
            TR(P, tp[:, c * 128:(c + 1) * 128], yt[s][:, c * 128:(c + 1) * 128], K["ident"][:], [(YT[s], None), (KB, "ident")], [(TP, c // 8)])
        yv = yT[s][:].rearrange("p c t -> p (c t)")
        CP(P, "act", yv[:, 0:1024], tp[:, 0:1024], [(TP, 0)], [(YTT[s], 0)])
        CP(P, "dve", yv[:, 1024:2048], tp[:, 1024:2048], [(TP, 1)], [(YTT[s], 1)])
        for n in range(4):
            for kc in range(16):
                MM(P, pm[n][:], yT[s][:, kc, :], wo[:, kc, n * 512:(n + 1) * 512], kc == 0, kc == 15, [(YTT[s], None), (WO, None)], [(PM[n], None)])
        post_norm_residual(C, t, pm, PM, None, None, xt[s], XT[s], rep, REP, junk, JK, ssp[s], SSP[s], xdst, B["XR"], tmp, TMP)


def phase_ffn(C, l, xsrc, xdst):
    P, I, D, B, K, KB = C.P, C.I, C.D, C.B, C.K, C.KB
    DSTB = B["XR"]
    xt = [C.sb("f_xt%d" % i, [128, D_], F32) for i in range(2)]; XT = [Buf("f_xt%d" % i) for i in range(2)]
    junk = C.sb("f_junk", [128, D_], BF16); JK = Buf("f_junk")
    ss = [C.sb("f_ss%d" % i, [128, 1], F32) for i in range(2)]
    rs = [C.sb("f_rs%d" % i, [128, 1], F32) for i in range(2)]; SS = [Buf("f_ss%d" % i) for i in range(2)]
    xn0 = C.sb("f_xn0", [128, D_], BF16); XN0 = Buf("f_xn0")
    xn = [xn0, xn0]; XN = [XN0, XN0]
    tp = C.ps("f_tp", [128, D_], BF16); TP = PB("f_tp")
    hT = C.sb("f_hT", [128, 16, 512], BF16); HT = Buf("f_hT")
    aT = C.sb("f_aT", [128, 44, 512], BF16); AT = Buf("f_aT")
    wg = [C.sb("f_wg%d" % i, [128, 16, 256], BF16) for i in range(2)]; WG = [Buf("f_wg%d" % i) for i in range(2)]
    wu = [C.sb("f_wu%d" % i, [128, 16, 256], BF16) for i in range(2)]; WU = [Buf("f_wu%d" % i) for i in range(2)]
    wd = [C.sb("f_wd%d" % i, [128, 4, 512], BF16) for i in range(2)]; WDB = [Buf("f_wd%d" % i) for i in range(2)]
    pg = C.ps("f_pg", [128, 512]); PG = PB("f_pg")
    pu = C.ps("f_pu", [128, 512]); PU = PB("f_pu")
    pm = [C.ps("f_pm%d" % i, [128, 512]) for i in range(4)]; PM = [PB("f_pm%d" % i) for i in range(4)]
    sg = [C.sb("f_sg%d" % i, [128, 512], F32) for i in range(2)]; SG = [Buf("f_sg%d" % i) for i in range(2)]
    rep = C.sb("f_rep", [128, D_], F32); REP = Buf("f_rep")
    fst = [C.sb("f_fst%d" % i, [128, D_], F32) for i in range(4)]; FST = [Buf("f_fst%d" % i) for i in range(4)]
    tmp = fst[0]; TMP = FST[0]
    ssp = [C.sb("f_ssp%d" % i, [128, 8], F32) for i in range(4)]; SSP = [Buf("f_ssp%d" % i) for i in range(4)]
    xr = xt; XRB = XT
    A = K["vec"][:, l, 3, :]; Sv = K["vec"][:, l, 4, :]
    VR = [(C.MODB, (l, 1, 0)), (C.MODB, (l, 1, 1))]
    replicate_cols(C, rep, REP, K["vec"][:, l, 5, :], 16, [(C.MODB, (l, 1, 2))], pg, PG, tmp, TMP)
    wgv = D["WG"][l].rearrange("(kc p) n -> p kc n", p=128)
    wuv = D["WU"][l].rearrange("(kc p) n -> p kc n", p=128)
    wdv = D["WD"][l].rearrange("(f p) n -> p f n", p=128)
    n_w = 0
    n_d = 0
    n_s = 0
    for blk in range(8):
        for ti in range(4):
            t = blk * 4 + ti
            s = t % 2
            norm_to_hT(C, t, ti, xsrc, B["XR"], xt[s], XT[s], ss[s], rs[s], SS[s], xn[s], XN[s], junk, JK, tp, TP, hT, HT, A, Sv, VR)
        for fs in range(22):
            wi = n_w % 2; n_w += 1
            P.dma("sp", wg[wi][:], wgv[:, :, fs * 256:(fs + 1) * 256], reads=[(B["WG"], None)], writes=[(WG[wi], None)], sembuf=WG[wi])
            P.dma("sp", wu[wi][:], wuv[:, :, fs * 256:(fs + 1) * 256], reads=[(B["WU"], None)], writes=[(WU[wi], None)], sembuf=WU[wi])
            for j in range(2):
                f = fs * 2 + j
                if f % 2 == 0:
                    pgb, PGB, pub, PUB = pg, PG, pu, PU
                else:
                    pgb, PGB, pub, PUB = pm[0], PM[0], pm[1], PM[1]
                for kc in range(16):
                    MM(P, pgb[:], wg[wi][:, kc, j * 128:(j + 1) * 128], hT[:, kc, :], kc == 0, kc == 15, [(WG[wi], None), (HT, None)], [(PGB, None)])
                for kc in range(16):
                    MM(P, pub[:], wu[wi][:, kc, j * 128:(j + 1) * 128], hT[:, kc, :], kc == 0, kc == 15, [(WU[wi], None), (HT, None)], [(PUB, None)])
                si = n_s % 2; n_s += 1
                ACTF(P, sg[si][:], pgb[:], AF.Silu, [(PGB, None)], [(SG[si], None)])
                TT(P, "dve", aT[:, f, :], sg[si][:], pub[:], ALU.mult, [(SG[si], None), (PUB, None)], [(AT, f)])
        for n in range(4):
            for f0 in range(0, 44, 4):
                di = n_d % 2; n_d += 1
                P.dma("sp", wd[di][:], wdv[:, f0:f0 + 4, n * 512:(n + 1) * 512], reads=[(B["WD"], None)], writes=[(WDB[di], None)], sembuf=WDB[di])
                for fj in range(4):
                    f = f0 + fj
                    for ti in range(4):
                        MM(P, pm[ti][:], aT[:, f, ti * 128:(ti + 1) * 128], wd[di][:, fj, :], f == 0, f == 43,
                           [(AT, None), (WDB[di], None)], [(PM[ti], None)])
            for ti in range(4):
                cs = slice(n * 512, (n + 1) * 512)
                CP(P, "dve" if ti % 2 == 0 else "act", fst[ti][:, cs], pm[ti][:], [(PM[ti], None)], [(FST[ti], n)])
                ACTF(P, junk[:, cs], fst[ti][:, cs], AF.Square, [(FST[ti], n)], [(JK, n), (SSP[ti], n)], accum=ssp[ti][:, n:n + 1])
        for ti in range(4):
            t = blk * 4 + ti
            s = t % 2
            sq_, SQ_ = ssp[ti], SSP[ti]
            TT(P, "dve", sq_[:, 4:5], sq_[:, 0:1], sq_[:, 1:2], ALU.add, [(SQ_, 0), (SQ_, 1)], [(SQ_, "a")])
            TT(P, "dve", sq_[:, 5:6], sq_[:, 2:3], sq_[:, 3:4], ALU.add, [(SQ_, 2), (SQ_, 3)], [(SQ_, "b")])
            TT(P, "dve", sq_[:, 6:7], sq_[:, 4:5], sq_[:, 5:6], ALU.add, [(SQ_, "a"), (SQ_, "b")], [(SQ_, "c")])
            ACTF(P, sq_[:, 7:8], sq_[:, 6:7], AF.Sqrt, [(SQ_, "c")], [(SQ_, "r")], bias=EPS, scale=1.0 / D_)
            RECIP(P, sq_[:, 7:8], sq_[:, 7:8], [(SQ_, "r")], [(SQ_, "r")])
            STT(P, fst[ti][:], fst[ti][:], sq_[:, 7:8], rep[:], ALU.mult, ALU.mult, [(FST[ti], None), (SQ_, "r"), (REP, None)], [(FST[ti], None)])
            P.dma("sp", xr[s][:], xsrc[t * 128:(t + 1) * 128, :], reads=[(B["XR"], t)], writes=[(XRB[s], None)], sembuf=XRB[s])
            TT(P, "pool", xr[s][:], xr[s][:], fst[ti][:], ALU.add, [(XRB[s], None), (FST[ti], None)], [(XRB[s], None)])
            P.dma("pool", xdst[t * 128:(t + 1) * 128, :], xr[s][:], reads=[(XRB[s], None)], writes=[(DSTB, t)], sembuf=XRB[s])


def alibi_slopes(n):
    def pow2(m):
        start = 2.0 ** (-8.0 / m)
        return [start ** (i + 1) for i in range(m)]
    if math.log2(n).is_integer():
        s = pow2(n)
    else:
        p = 2 ** math.floor(math.log2(n))
        s = pow2(p) + pow2(2 * p)[0::2][: n - p]
    return np.array(s, dtype=np.float32)


def host_constants():
    c = {}
    c["c_ident"] = np.eye(128, dtype=np.float32)
    c["c_ones"] = np.ones((128, 128), np.float32)
    k = np.arange(128)[:, None]; m = np.arange(128)[None, :]
    c["c_U"] = (k <= m).astype(np.float32)
    c["c_L"] = (k >= m).astype(np.float32)
    R = np.zeros((64, 64), np.float32)
    for mm in range(32):
        R[mm + 32, mm] = -1.0
        R[mm, mm + 32] = 1.0
    c["c_R"] = R
    sl = alibi_slopes(12)
    E = np.zeros((128, 12, 3, 128), np.float32)
    kk = np.arange(128)[:, None]; qq = np.arange(128)[None, :]
    for d in range(3):
        dist = np.abs(qq - kk - (d - 1) * 128).astype(np.float32)
        for h in range(12):
            E[:, h, d, :] = np.where(dist <= 128, np.exp(-sl[h] * dist), 0.0)
    c["c_E"] = E.reshape(128, 12 * 384)
    inv = (1.0 / (np.float32(10000.0) ** (np.arange(0, 64, 2, dtype=np.float32) / np.float32(64)))).astype(np.float32)
    c["c_invf"] = np.concatenate([inv, inv])[:, None].astype(np.float32)
    return c


def colT(v, n):
    return np.ascontiguousarray(np.asarray(v, np.float32).reshape(n, 128).T)


def host_layout(inp):
    g = lambda k: np.asarray(inp[k])
    w_in = g("w_in")
    fm = np.zeros((L_, D_, NFM * 128), np.float32)
    def put(ch, cols):
        fm[:, :, ch * 128:ch * 128 + len(cols)] = w_in[:, :, cols]
    for c in range(6):
        put(FM_AQ + c, np.arange(c * 128, (c + 1) * 128))
    for kv in range(4):
        cols = np.arange(768 + kv * 64, 768 + (kv + 1) * 64)
        put(FM_AK + kv, np.concatenate([cols, cols]))
    for c in range(4):
        put(FM_BCQ + c, np.arange(1280 + c * 128, min(1280 + (c + 1) * 128, 1728)))
    put(FM_BCKV, np.arange(1728, 1856))
    put(FM_BKR, np.arange(1856, 1920))
    for h in range(4):
        put(FM_CQ + h, np.arange(1920 + h * 96, 1920 + (h + 1) * 96))
        put(FM_CK + h, np.arange(2304 + h * 96, 2304 + (h + 1) * 96))
    tm = np.ascontiguousarray(np.concatenate([w_in[:, :, 1024:1280], w_in[:, :, 2688:3456], w_in[:, :, 3472:4240], w_in[:, :, 3456:3472]], axis=2))
    shared = {
        "mod_w": g("mod_w"),
        "mod_bT": np.stack([colT(g("mod_b")[l], 96) for l in range(L_)]),
        "w_in_fm": fm, "w_in_tm": tm,
        "attn_sink": g("attn_sink")[:, None, :],
        "w_uq": np.concatenate([g("mla_w_uq"), np.zeros((L_, 64, 768), np.float32)], axis=1),
        "w_ukv": g("mla_w_ukv"),
        "gate_b": g("mlstm_gate_b")[:, None, :],
        "head_g": g("mlstm_head_g")[:, None, :],
        "w_out": g("w_out"), "w_gate": g("ffn_w_gate"), "w_up": g("ffn_w_up"), "w_down": g("ffn_w_down"),
    }
    for k, src in (("pre_mix_gT", "pre_mix_g"), ("post_mix_gT", "post_mix_g"), ("pre_ffn_gT", "pre_ffn_g"), ("post_ffn_gT", "post_ffn_g")):
        shared[k] = np.stack([colT(g(src)[l], 16) for l in range(L_)])
    qg = np.concatenate([g("mla_q_norm_g"), np.zeros((L_, 64), np.float32)], axis=1)
    shared["q_norm_gT"] = np.stack([colT(qg[l], 4) for l in range(L_)])
    shared["kv_norm_gT"] = np.stack([colT(g("mla_kv_norm_g")[l], 1) for l in range(L_)])
    shared.update(host_constants())
    x, c, pos = g("x"), g("c"), g("positions")
    maps = []
    for b in range(8):
        m = dict(shared)
        m["x"] = np.ascontiguousarray(x[b])
        m["cT"] = colT(c[b], 16)
        m["pos"] = np.ascontiguousarray(pos[b][None, :].astype(np.int32))
        maps.append(m)
    return maps


_CACHE = {}


def kernel(**inputs):
    if "nc" not in _CACHE:
        _CACHE["nc"] = build_program()[0]
    nc = _CACHE["nc"]
    maps = host_layout(inputs)
    res = run_bass_kernel_spmd(nc, maps, core_ids=list(range(8)))
    return np.stack([np.asarray(r["out"], dtype=np.float32) for r in res.results], axis=0)
```

```python
import numpy as np
import concourse.bass as bass
import concourse.mybir as mybir
from concourse.bass_utils import run_bass_kernel_spmd
F32 = mybir.dt.float32
BF16 = mybir.dt.bfloat16
I32 = mybir.dt.int32
AF = mybir.ActivationFunctionType
ALU = mybir.AluOpType
AX = mybir.AxisListType


class Buf:
    __slots__ = ("name", "st", "sem", "excl")

    def __init__(self, name, excl=False):
        self.name = name
        self.st = {}
        self.sem = None
        self.excl = excl


def PB(name):
    return Buf(name, excl=True)


class Op:
    __slots__ = ("eng", "fn", "idx", "waits", "is_dma", "needs_inc", "clock", "sem", "semval")

    def __init__(self, eng, fn, is_dma):
        self.eng = eng
        self.fn = fn
        self.is_dma = is_dma
        self.waits = []
        self.needs_inc = False
        self.clock = None
        self.sem = None
        self.semval = None


class Prog:
    ENGS = ("sp", "act", "dve", "pool", "pe")

    def __init__(self, nc):
        self.nc = nc
        self.ops = []
        self.known_idx = {}
        self.known_dma = {}
        self.dma_issued = {}
        self.sems = {}
        self.ctx = []
        self.free_pool = {}
        self.npool = 0
        self.phase_bufs = []
        self.phase_start = 0

    def _sem(self, key):
        s = self.sems.get(key)
        if s is None:
            cm = self.nc.semaphore("s_" + str(key))
            s = cm.__enter__()
            self.ctx.append(cm)
            self.sems[key] = s
        return s

    @staticmethod
    def _states(buf, key, create):
        st = buf.st
        if key is None:
            if create and None not in st:
                st[None] = {"w": {}, "r": {}}
            return list(st.values())
        out = []
        if None in st:
            out.append(st[None])
        if key not in st and create:
            st[key] = {"w": {}, "r": {}}
        if key in st:
            out.append(st[key])
        return out

    def _record(self, op, reads, writes):
        op.idx = len(self.ops)
        self.ops.append(op)
        ex = [rk for rk in reads if rk[0].excl]
        if ex:
            reads = [rk for rk in reads if not rk[0].excl]
            writes = list(writes) + [rk for rk in ex if rk not in writes]
        deps = []
        for (buf, key) in reads:
            for s in self._states(buf, key, True):
                deps += [(p, "RAW") for p in s["w"].values()]
        for (buf, key) in writes:
            for s in self._states(buf, key, True):
                deps += [(p, "WAW") for p in s["w"].values()]
                deps += [(p, "WAR") for p in s["r"].values()]
        for (p, kind) in deps:
            if p is op or p.idx < self.phase_start:
                continue
            if p.is_dma:
                val = self.dma_issued[p.sem]
                if op.is_dma and op.sem == p.sem:
                    val -= 16
                k = (op.eng, p.sem)
                if self.known_dma.get(k, 0) >= val:
                    continue
                self.known_dma[k] = val
                op.waits.append(("sem", p.sem, val))
            else:
                if p.eng == op.eng:
                    if op.eng == "pe":
                        continue
                k = (op.eng, p.eng)
                if self.known_idx.get(k, -1) >= p.idx:
                    continue
                self.known_idx[k] = p.idx
                p.needs_inc = True
                op.waits.append(("op", p))
        clk = ("dma", op.sem) if op.is_dma else op.eng
        for (buf, key) in reads:
            if key is None:
                if None not in buf.st:
                    buf.st[None] = {"w": {}, "r": {}}
                buf.st[None]["r"][clk] = op
            else:
                buf.st[key]["r"][clk] = op
        for (buf, key) in writes:
            if key is None:
                buf.st.clear()
                buf.st[None] = {"w": {clk: op}, "r": {}}
            else:
                buf.st[key] = {"w": {clk: op}, "r": {}}

    def op(self, eng, fn, reads=(), writes=()):
        o = Op(eng, fn, False)
        self._record(o, reads, writes)
        return o

    def dma(self, queue, out_ap, in_ap, reads=(), writes=(), sembuf=None, **kw):
        assert sembuf is not None
        qt = "sw" if queue == "pool" else "hw"
        if sembuf.sem is None:
            sembuf.sem = {}
            self.phase_bufs.append(sembuf)
        if qt not in sembuf.sem:
            fp = self.free_pool.setdefault(qt, [])
            if fp:
                sembuf.sem[qt] = fp.pop()
            else:
                sembuf.sem[qt] = "%s_%d" % (qt, self.npool)
                self.npool += 1
        o = Op(queue, lambda e: e.dma_start(out=out_ap, in_=in_ap, **kw), True)
        o.sem = sembuf.sem[qt]
        self.dma_issued[o.sem] = self.dma_issued.get(o.sem, 0) + 16
        o.semval = self.dma_issued[o.sem]
        self._record(o, reads, writes)
        return o

    def sb(self, name, shape, dtype):
        cm = self.nc.sbuf_tensor(name, list(shape), dtype)
        t = cm.__enter__()
        self.ctx.append(cm)
        return t

    def ps(self, name, shape, dtype):
        cm = self.nc.psum_tensor(name, list(shape), dtype)
        t = cm.__enter__()
        self.ctx.append(cm)
        return t

    def emit_phase(self):
        start = getattr(self, "_emitted", 0)
        ops = self.ops[start:]
        self._emitted = len(self.ops)
        if not hasattr(self, "cnt"):
            self.cnt = {e: 0 for e in self.ENGS}
            self.tot_stats = {e: 0 for e in self.ENGS}
            self.nwaits = 0
        nc = self.nc
        cnt = self.cnt
        per = {e: [o for o in ops if o.eng == e] for e in self.ENGS}
        for e in self.ENGS:
            for o in reversed(per[e]):
                if not o.is_dma:
                    o.needs_inc = True
                    break
        for o in ops:
            if (not o.is_dma) and o.needs_inc:
                cnt[o.eng] += 1
                o.clock = cnt[o.eng]
        for e in self.ENGS:
            self._sem("eng_" + e)
            self.tot_stats[e] += len(per[e])
        for o in ops:
            if o.is_dma:
                self._sem(o.sem)
            self.nwaits += len(o.waits)

        def run(e, name):
            for o in per[name]:
                for w in o.waits:
                    if w[0] == "sem":
                        e.wait_ge(self.sems[w[1]], w[2])
                    else:
                        p = w[1]
                        e.wait_ge(self.sems["eng_" + p.eng], p.clock)
                ins = o.fn(e)
                if o.is_dma:
                    ins.then_inc(self.sems[o.sem], 16)
                elif o.needs_inc:
                    ins.then_inc(self.sems["eng_" + o.eng], 1)
            for sem, tot in self.dma_issued.items():
                if sem in self.sems:
                    e.wait_ge(self.sems[sem], tot)
            for en in self.ENGS:
                if en != name and cnt[en] > 0:
                    e.wait_ge(self.sems["eng_" + en], cnt[en])

        with nc.Block() as block:
            @block.sync
            def _(e):
                run(e, "sp")

            @block.scalar
            def _(e):
                run(e, "act")

            @block.vector
            def _(e):
                run(e, "dve")

            @block.gpsimd
            def _(e):
                run(e, "pool")

            @block.tensor
            def _(e):
                run(e, "pe")
        for b in self.phase_bufs:
            for qt, sm in b.sem.items():
                self.free_pool.setdefault(qt, []).append(sm)
            b.sem = None
        self.phase_bufs = []
        self.phase_start = len(self.ops)
        last = len(self.ops)
        for a in self.ENGS:
            for b in self.ENGS:
                self.known_idx[(a, b)] = last - 1
            for sem, tot in self.dma_issued.items():
                self.known_dma[(a, sem)] = tot

    def finish(self):
        self.stats = dict(self.tot_stats)
        self.stats["waits"] = self.nwaits
        self.stats["clock"] = dict(self.cnt)
        self.stats["maxdma"] = max(self.dma_issued.values()) if self.dma_issued else 0
        self.stats["nsem"] = len(self.sems)

    def emit(self, final_wait_bufs=()):
        nc = self.nc
        cnt = {e: 0 for e in self.ENGS}
        for o in self.ops:
            if (not o.is_dma) and o.needs_inc:
                cnt[o.eng] += 1
                o.clock = cnt[o.eng]
        per = {e: [o for o in self.ops if o.eng == e] for e in self.ENGS}
        for e in self.ENGS:
            self._sem("eng_" + e)
        for o in self.ops:
            if o.is_dma:
                self._sem(o.sem)
        self.stats = {e: len(per[e]) for e in self.ENGS}
        self.stats["waits"] = sum(len(o.waits) for o in self.ops)
        self.stats["maxclock"] = dict(cnt)
        self.stats["maxdma"] = max(self.dma_issued.values()) if self.dma_issued else 0
        self.stats["nsem"] = len(self.sems)

        def run(e, name):
            for o in per[name]:
                for w in o.waits:
                    if w[0] == "sem":
                        e.wait_ge(self.sems[w[1]], w[2])
                    else:
                        p = w[1]
                        e.wait_ge(self.sems["eng_" + p.eng], p.clock)
                ins = o.fn(e)
                if o.is_dma:
                    ins.then_inc(self.sems[o.sem], 16)
                elif o.needs_inc:
                    ins.then_inc(self.sems["eng_" + o.eng], 1)
            if name == "sp":
                for sem, tot in self.dma_issued.items():
                    e.wait_ge(self.sems[sem], tot)
                for en in self.ENGS:
                    if en != "sp" and cnt[en] > 0:
                        e.wait_ge(self.sems["eng_" + en], cnt[en])

        with nc.Block() as block:
            @block.sync
            def _(e):
                run(e, "sp")

            @block.scalar
            def _(e):
                run(e, "act")

            @block.vector
            def _(e):
                run(e, "dve")

            @block.gpsimd
            def _(e):
                run(e, "pool")

            @block.tensor
            def _(e):
                run(e, "pe")

    def close(self):
        for cm in reversed(self.ctx):
            cm.__exit__(None, None, None)
        self.ctx = []


import math

S_ = 4096
D_ = 2048
NT = 32
DFF = 5632
EPS = 1e-6
L_ = 2
NFM = 24
NTM = 1808
FM_AQ, FM_AK, FM_BCQ, FM_BCKV, FM_BKR, FM_CQ, FM_CK = 0, 6, 10, 14, 15, 16, 20


def MM(P, out, lhsT, rhs, start, stop, R, W):
    return P.op("pe", lambda e: e.matmul(out, lhsT=lhsT, rhs=rhs, start=start, stop=stop), R, W)


def TR(P, out, in_, ident, R, W):
    return P.op("pe", lambda e: e.transpose(out, in_, ident), R, W)


def ACTF(P, out, in_, func, R, W, bias=None, scale=None, accum=None):
    kw = {}
    if bias is not None:
        kw["bias"] = bias
    if scale is not None:
        kw["scale"] = scale
    if accum is not None:
        kw["accum_out"] = accum
    return P.op("act", lambda e: e.activation(out=out, in_=in_, func=func, **kw), R, W)


def TS(P, eng, out, in0, s1, s2, op0, op1, R, W):
    if op1 is None:
        return P.op(eng, lambda e: e.tensor_scalar(out=out, in0=in0, scalar1=s1, scalar2=None, op0=op0), R, W)
    return P.op(eng, lambda e: e.tensor_scalar(out=out, in0=in0, scalar1=s1, scalar2=s2, op0=op0, op1=op1), R, W)


def TT(P, eng, out, in0, in1, op, R, W):
    return P.op(eng, lambda e: e.tensor_tensor(out=out, in0=in0, in1=in1, op=op), R, W)


def STT(P, out, in0, scalar, in1, op0, op1, R, W):
    return P.op("dve", lambda e: e.scalar_tensor_tensor(out=out, in0=in0, scalar=scalar, in1=in1, op0=op0, op1=op1), R, W)


def CP(P, eng, out, in_, R, W):
    if eng == "act":
        return P.op("act", lambda e: e.activation(out=out, in_=in_, func=AF.Copy), R, W)
    return P.op(eng, lambda e: e.tensor_copy(out=out, in_=in_), R, W)


def RECIP(P, out, in_, R, W):
    return P.op("dve", lambda e: e.reciprocal(out=out, in_=in_), R, W)


def MEMSET(P, eng, ap, val, R, W):
    return P.op(eng, lambda e: e.memset(ap, val), R, W)


class Ctx:
    pass


KNOB = {}


def build_program(dbg=False, phases=None, nlayers=L_):
    nc = bass.Bass("TRN2", target_bir_lowering=False)
    P = Prog(nc)
    C = Ctx()
    C.nc, C.P, C.dbg = nc, P, dbg

    def din(name, shape, dt=F32):
        return nc.dram_tensor(name, list(shape), dt, kind="ExternalInput").ap()

    def dscr(name, shape, dt):
        return nc.dram_tensor(name, list(shape), dt, kind=("ExternalOutput" if (dbg and name in dbg) else "Internal")).ap()

    I = C.I = {}
    I["x"] = din("x", [S_, D_])
    I["cT"] = din("cT", [128, 16])
    I["pos"] = din("pos", [1, S_], I32)
    I["mod_w"] = din("mod_w", [L_, D_, 6 * D_])
    I["mod_bT"] = din("mod_bT", [L_, 128, 96])
    for g in ("pre_mix_gT", "post_mix_gT", "pre_ffn_gT", "post_ffn_gT"):
        I[g] = din(g, [L_, 128, 16])
    I["w_in_fm"] = din("w_in_fm", [L_, D_, NFM * 128])
    I["w_in_tm"] = din("w_in_tm", [L_, D_, NTM])
    I["attn_sink"] = din("attn_sink", [L_, 1, 12])
    I["q_norm_gT"] = din("q_norm_gT", [L_, 128, 4])
    I["w_uq"] = din("w_uq", [L_, 512, 768])
    I["kv_norm_gT"] = din("kv_norm_gT", [L_, 128, 1])
    I["w_ukv"] = din("w_ukv", [L_, 128, 1024])
    I["gate_b"] = din("gate_b", [L_, 1, 16])
    I["head_g"] = din("head_g", [L_, 1, 768])
    I["w_out"] = din("w_out", [L_, D_, D_])
    I["w_gate"] = din("w_gate", [L_, D_, DFF])
    I["w_up"] = din("w_up", [L_, D_, DFF])
    I["w_down"] = din("w_down", [L_, DFF, D_])
    I["c_ident"] = din("c_ident", [128, 128])
    I["c_ones"] = din("c_ones", [128, 128])
    I["c_U"] = din("c_U", [128, 128])
    I["c_L"] = din("c_L", [128, 128])
    I["c_R"] = din("c_R", [64, 64])
    I["c_E"] = din("c_E", [128, 12 * 384])
    I["c_invf"] = din("c_invf", [64, 1])
    C.out = nc.dram_tensor("out", [S_, D_], F32, kind="ExternalOutput").ap()

    D = C.D = {}
    D["XR"] = dscr("XR", [S_, D_], F32)
    D["WFM"] = dscr("WFM", [L_, D_, NFM * 128], BF16)
    D["WTM"] = dscr("WTM", [L_, D_, NTM], BF16)
    D["WOUT"] = dscr("WOUTb", [L_, D_, D_], BF16)
    D["WG"] = dscr("WGb", [L_, D_, DFF], BF16)
    D["WU"] = dscr("WUb", [L_, D_, DFF], BF16)
    D["WD"] = dscr("WDb", [L_, DFF, D_], BF16)
    D["QKT"] = dscr("QKT", [NFM, 128, S_], BF16)
    D["PTM"] = dscr("PTM", [S_, NTM], BF16)
    D["GATES"] = dscr("GATES", [S_, 16], F32)
    D["Y"] = dscr("Y", [S_, D_], BF16)
    D["QN"] = dscr("QN", [4, 128, S_], BF16)
    D["QR"] = dscr("QR", [4, 64, S_], BF16)
    D["KN"] = dscr("KN", [4, 128, S_], BF16)
    D["KR"] = dscr("KR", [64, S_], BF16)
    D["VB"] = dscr("VB", [S_, 512], BF16)
    D["BREP"] = dscr("BREP", [8, 128, S_], F32)
    D["IBS"] = dscr("IBS", [128, NT * 8], F32)
    D["MODV"] = dscr("MODV", [128, L_ * 96], F32)
    B = C.B = {k: Buf("D_" + k) for k in D}
    B["CAST"] = Buf("CAST")

    def psb(name, shape, dt):
        cm = nc.sbuf_tensor(name, list(shape), dt)
        return cm.__enter__()

    K = C.K = {}
    K["ident"] = psb("k_ident", [128, 128], BF16)
    K["identf"] = psb("k_identf", [128, 128], F32)
    K["ones"] = psb("k_ones", [128, 128], F32)
    K["onesb"] = psb("k_onesb", [128, 128], BF16)
    K["U"] = psb("k_U", [128, 128], F32)
    K["Lm"] = psb("k_L", [128, 128], F32)
    K["R"] = psb("k_R", [64, 64], BF16)
    K["mod"] = psb("k_mod", [128, L_, 96], F32)
    K["vec"] = psb("k_vec", [128, L_, 6, 16], F32)
    KB = C.KB = Buf("KCONST")
    C.MODB = Buf("MODVEC")

    C.phase_ctx = []

    def begin():
        C.phase_ctx = []
        P.ctx_mark = len(P.ctx)

    C.uid = [0]

    def sb(name, shape, dt):
        C.uid[0] += 1
        name = "%s_u%d" % (name, C.uid[0])
        cm = nc.sbuf_tensor(name, list(shape), dt)
        t = cm.__enter__()
        C.phase_ctx.append(cm)
        return t

    def ps(name, shape, dt=F32):
        C.uid[0] += 1
        name = "%s_u%d" % (name, C.uid[0])
        esz = 4 if dt == F32 else 2
        n = 1
        for d in shape[1:]:
            n *= d
        per_bank = 2048 // esz
        nb = -(-n // per_bank)
        cm = nc.psum_tensor(name, [128, nb * per_bank], dt)
        t = cm.__enter__()
        C.phase_ctx.append(cm)
        v = t[0:shape[0], 0:n]
        if len(shape) == 3:
            v = v.rearrange("p (a b) -> p a b", b=shape[2])
        return v

    def end():
        P.emit_phase()
        for cm in reversed(C.phase_ctx):
            cm.__exit__(None, None, None)
        C.phase_ctx = []

    C.begin, C.end, C.sb, C.ps = begin, end, sb, ps

    want = (lambda n: True) if phases is None else (lambda n: n in phases)

    begin()
    phase_consts(C)
    gc = phase_cast(C, [0]) if want("cast") else iter(())
    gm = phase_mod(C, nlayers) if want("mod") else iter(())
    done_c = done_m = False
    while not (done_c and done_m):
        for _ in range(5):
            if next(gc, "end") == "end":
                done_c = True
                break
        if next(gm, "end") == "end":
            done_m = True
    for _ in gc:
        pass
    for _ in gm:
        pass
    end()
    for l in range(nlayers):
        src = I["x"] if l == 0 else D["XR"]
        if want("inproj"):
            begin(); phase_inproj(C, l, src); end()
        if want("win"):
            begin(); phase_window(C, l); end()
        if want("mla"):
            begin(); phase_mla_prep(C, l); end()
            bg = phase_cast(C, list(range(1, nlayers)), engs=("dve", "dve", "dve", "dve")) if (l == 0 and nlayers > 1 and want("cast")) else None
            begin(); phase_mla_attn(C, l, bg); end()
        if want("mlstm"):
            begin(); phase_mlstm_prep(C, l); end()
            begin(); phase_mlstm_attn(C, l); end()
        if want("outproj"):
            begin(); phase_outproj(C, l, src, D["XR"]); end()
        if want("ffn"):
            dst = C.out if l == nlayers - 1 else D["XR"]
            begin(); phase_ffn(C, l, D["XR"], dst); end()
    P.finish()
    P.close()
    return nc, P


def phase_consts(C):
    P, I, K, KB = C.P, C.I, C.K, C.KB
    tmp = C.sb("c_tmp", [128, 128], F32); T = Buf("c_tmp")
    tmpR = C.sb("c_tmpR", [64, 64], F32); TRb = Buf("c_tmpR")
    P.dma("sp", K["identf"][:], I["c_ident"], writes=[(KB, "identf")], sembuf=KB)
    P.dma("sp", K["ones"][:], I["c_ones"], writes=[(KB, "ones")], sembuf=KB)
    P.dma("sp", K["U"][:], I["c_U"], writes=[(KB, "U")], sembuf=KB)
    P.dma("sp", K["Lm"][:], I["c_L"], writes=[(KB, "L")], sembuf=KB)
    P.dma("sp", tmpR[:], I["c_R"], writes=[(TRb, None)], sembuf=TRb)
    CP(P, "dve", K["ident"][:], K["identf"][:], [(KB, "identf")], [(KB, "ident")])
    CP(P, "dve", K["onesb"][:], K["ones"][:], [(KB, "ones")], [(KB, "onesb")])
    CP(P, "dve", K["R"][:], tmpR[:], [(TRb, None)], [(KB, "R")])


def phase_cast(C, layers, engs=("act", "dve", "act", "dve")):
    P, I, D, B = C.P, C.I, C.D, C.B
    NB = 4
    sf = [C.sb("c_sf%d" % i, [128, 2048], F32) for i in range(NB)]; SF = [Buf("c_sf%d" % i) for i in range(NB)]
    sb_ = [C.sb("c_sb%d" % i, [128, 2048], BF16) for i in range(NB)]; SBB = [Buf("c_sb%d" % i) for i in range(NB)]
    cnt = [0]

    def cast(dst, src, rows, cols, key):
        sv = src.rearrange("(r p) n -> p r n", p=128)
        dv = dst.rearrange("(r p) n -> p r n", p=128)
        for r in range(rows // 128):
            for c0 in range(0, cols, 2048):
                w = min(2048, cols - c0)
                i = cnt[0] % NB; cnt[0] += 1
                P.dma("sp", sf[i][:, 0:w], sv[:, r, c0:c0 + w], writes=[(SF[i], None)], sembuf=SF[i])
                CP(P, engs[i], sb_[i][:, 0:w], sf[i][:, 0:w], [(SF[i], None)], [(SBB[i], None)])
                P.dma("pool", dv[:, r, c0:c0 + w], sb_[i][:, 0:w], reads=[(SBB[i], None)], writes=[(B[key], ("cast", id(dst), r, c0))], sembuf=SBB[i])
                yield
    for l in layers:
        yield from cast(D["WFM"][l], I["w_in_fm"][l], D_, NFM * 128, "WFM")
        yield from cast(D["WTM"][l], I["w_in_tm"][l], D_, NTM, "WTM")
        yield from cast(D["WOUT"][l], I["w_out"][l], D_, D_, "WOUT")
        yield from cast(D["WG"][l], I["w_gate"][l], D_, DFF, "WG")
        yield from cast(D["WU"][l], I["w_up"][l], D_, DFF, "WU")
        yield from cast(D["WD"][l], I["w_down"][l], DFF, D_, "WD")


def phase_mod(C, nlayers):
    P, I, K = C.P, C.I, C.K
    cT = C.sb("m_cT", [128, 16], F32); CT = Buf("m_cT")
    sc = C.sb("m_silu", [128, 16, 2], F32); SC = Buf("m_silu")
    ws = [C.sb("m_w%d" % i, [128, 16, 512], F32) for i in range(2)]
    WS = [Buf("m_w%d" % i) for i in range(2)]
    mb = C.sb("m_b", [128, 96], F32); MB = Buf("m_b")
    gT = C.sb("m_g", [128, 4, 16], F32); GT = Buf("m_g")
    mps = C.ps("m_ps", [128, 96, 2]); MPS = PB("m_ps")
    MODB = C.MODB
    P.dma("sp", cT[:], I["cT"], writes=[(CT, None)], sembuf=CT)
    ACTF(P, sc[:, :, 0], cT[:], AF.Silu, [(CT, None)], [(SC, 0)])
    ACTF(P, sc[:, :, 1], cT[:], AF.Silu, [(CT, None)], [(SC, 1)])
    for l in range(nlayers):
        wv = I["mod_w"][l].rearrange("(kc p) n -> p kc n", p=128)
        P.dma("sp", mb[:], I["mod_bT"][l], writes=[(MB, None)], sembuf=MB)
        for gi, g in enumerate(("pre_mix_gT", "post_mix_gT", "pre_ffn_gT", "post_ffn_gT")):
            P.dma("sp", gT[:, gi, :], I[g][l], writes=[(GT, gi)], sembuf=GT)
        for s in range(24):
            w = ws[s % 2]
            P.dma("sp", w[:], wv[:, :, s * 512:(s + 1) * 512], writes=[(WS[s % 2], None)], sembuf=WS[s % 2])
            for j in range(4):
                col = s * 4 + j
                for kc in range(16):
                    MM(P, mps[:, col, :], w[:, kc, j * 128:(j + 1) * 128], sc[:, kc, :], kc == 0, kc == 15,
                       [(WS[s % 2], None), (SC, None)], [(MPS, None)])
            yield
        mod = K["mod"][:, l, :]
        TT(P, "dve", mod, mps[:, :, 0], mb[:], ALU.add, [(MPS, None), (MB, None)], [(MODB, (l, "mod"))])
        if C.dbg and "MODV" in C.dbg:
            P.dma("pool", C.D["MODV"][:, l * 96:(l + 1) * 96], mod, reads=[(MODB, (l, "mod"))], writes=[(C.B["MODV"], l)], sembuf=MB)
        vec = K["vec"]
        for half, (gpre, gpost) in enumerate(((0, 1), (2, 3))):
            o = half * 48
            STT(P, vec[:, l, half * 3 + 0, :], mod[:, o + 16:o + 32], 1.0, gT[:, gpre, :], ALU.add, ALU.mult,
                [(MODB, (l, "mod")), (GT, gpre)], [(MODB, (l, half, 0))])
            CP(P, "dve", vec[:, l, half * 3 + 1, :], mod[:, o:o + 16], [(MODB, (l, "mod"))], [(MODB, (l, half, 1))])
            TT(P, "dve", vec[:, l, half * 3 + 2, :], mod[:, o + 32:o + 48], gT[:, gpost, :], ALU.mult,
               [(MODB, (l, "mod")), (GT, gpost)], [(MODB, (l, half, 2))])


def replicate_cols(C, rep, REP, colvec, ncols, R, psb, PSB, tmp, TMP):
    P, K, KB = C.P, C.K, C.KB
    for c0 in range(0, ncols, 4):
        n = min(4, ncols - c0)
        for j in range(n):
            c = c0 + j
            TS(P, "dve", tmp[:, j * 128:(j + 1) * 128], K["identf"][:], colvec[:, c:c + 1], None, ALU.mult, None,
               R + [(KB, "identf")], [(TMP, j)])
            MM(P, psb[:, j * 128:(j + 1) * 128], K["ones"][:], tmp[:, j * 128:(j + 1) * 128], True, True,
               [(TMP, j), (KB, "ones")], [(PSB, None)])
        CP(P, "act", rep[:, c0 * 128:(c0 + n) * 128], psb[:, 0:n * 128], [(PSB, None)], [(REP, c0 // 4)])


def norm_to_hT(C, t, ti, src_ap, SRCB, xt, XT, ss, rs, SS, xn, XN, junk, JK, tp, TP, hTb, HTB, A, Sv, VR, keep_x=None):
    P, K, KB = C.P, C.K, C.KB
    P.dma("sp", xt[:], src_ap[t * 128:(t + 1) * 128, :], reads=[(SRCB, t)], writes=[(XT, None)], sembuf=XT)
    ACTF(P, junk[:], xt[:], AF.Square, [(XT, None)], [(JK, None), (SS, "ss")], accum=ss[:])
    ACTF(P, rs[:], ss[:], AF.Sqrt, [(SS, "ss")], [(SS, "rs")], bias=EPS, scale=1.0 / D_)
    RECIP(P, rs[:], rs[:], [(SS, "rs")], [(SS, "rs")])
    TS(P, "dve", xn[:], xt[:], rs[:, 0:1], None, ALU.mult, None, [(XT, None), (SS, "rs")], [(XN, None)])
    for c in range(16):
        TR(P, tp[:, c * 128:(c + 1) * 128], xn[:, c * 128:(c + 1) * 128], K["ident"][:], [(XN, None), (KB, "ident")], [(TP, c // 8)])
    for c in range(16):
        o = hTb[:, c, ti * 128:(ti + 1) * 128]
        i_ = tp[:, c * 128:(c + 1) * 128]
        if c < 8:
            ACTF(P, o, i_, AF.Identity, [(TP, c // 8)] + VR, [(HTB, (ti, c))], bias=Sv[:, c:c + 1], scale=A[:, c:c + 1])
        else:
            TS(P, "dve", o, i_, A[:, c:c + 1], Sv[:, c:c + 1], ALU.mult, ALU.add, [(TP, c // 8)] + VR, [(HTB, (ti, c))])


def phase_inproj(C, l, src):
    P, I, D, B, K = C.P, C.I, C.D, C.B, C.K
    SRCB = B["XR"]
    xt = [C.sb("a_xt%d" % i, [128, D_], F32) for i in range(2)]; XT = [Buf("a_xt%d" % i) for i in range(2)]
    junk = C.sb("a_junk", [128, D_], BF16); JK = Buf("a_junk")
    ss = [C.sb("a_ss%d" % i, [128, 1], F32) for i in range(2)]
    rs = [C.sb("a_rs%d" % i, [128, 1], F32) for i in range(2)]; SS = [Buf("a_ss%d" % i) for i in range(2)]
    xn = [C.sb("a_xn%d" % i, [128, D_], BF16) for i in range(2)]; XN = [Buf("a_xn%d" % i) for i in range(2)]
    tp = [C.ps("a_tp%d" % i, [128, D_], BF16) for i in range(1)]; TP = [PB("a_tp%d" % i) for i in range(1)]
    hT = [C.sb("a_hT%d" % i, [128, 16, 512], BF16) for i in range(2)]; HT = [Buf("a_hT%d" % i) for i in range(2)]
    ws = [C.sb("a_ws%d" % i, [128, 16, 512], BF16) for i in range(2)]; WS = [Buf("a_ws%d" % i) for i in range(2)]
    pm = [C.ps("a_pm%d" % i, [128, 512]) for i in range(4)]; PM = [PB("a_pm%d" % i) for i in range(4)]
    st = [C.sb("a_st%d" % i, [128, 512], BF16) for i in range(4)]; ST = [Buf("a_st%d" % i) for i in range(4)]
    sg = [C.sb("a_sg%d" % i, [128, 16], F32) for i in range(2)]; SG = [Buf("a_sg%d" % i) for i in range(2)]
    A = K["vec"][:, l, 0, :]; Sv = K["vec"][:, l, 1, :]
    VR = [(C.MODB, (l, 0, 0)), (C.MODB, (l, 0, 1))]
    wfm = D["WFM"][l].rearrange("(kc p) n -> p kc n", p=128)
    wtm = D["WTM"][l].rearrange("(kc p) n -> p kc n", p=128)
    cnt = [0, 0, 0]

    def norm_block(blk):
        for ti in range(4):
            t = blk * 4 + ti
            s = t % 2
            norm_to_hT(C, t, ti, src, SRCB, xt[s], XT[s], ss[s], rs[s], SS[s], xn[s], XN[s], junk, JK, tp[0], TP[0],
                       hT[blk % 2], HT[blk % 2], A, Sv, VR)

    def gemm_block(blk):
        hTb, HTB = hT[blk % 2], HT[blk % 2]
        for s in range(6 if KNOB.get("fm", True) else 0):
            wi = cnt[0] % 2; cnt[0] += 1
            P.dma("sp", ws[wi][:], wfm[:, :, s * 512:(s + 1) * 512], reads=[(B["WFM"], None)], writes=[(WS[wi], None)], sembuf=WS[wi])
            for j in range(4):
                ch = s * 4 + j
                pi = cnt[1] % 4; cnt[1] += 1
                for kc in range(16):
                    MM(P, pm[pi][:], ws[wi][:, kc, j * 128:(j + 1) * 128], hTb[:, kc, :], kc == 0, kc == 15,
                       [(WS[wi], None), (HTB, None)], [(PM[pi], None)])
                CP(P, "act" if pi % 2 == 0 else "dve", st[pi][:], pm[pi][:], [(PM[pi], None)], [(ST[pi], None)])
                P.dma("pool", D["QKT"][ch][:, blk * 512:(blk + 1) * 512], st[pi][:], reads=[(ST[pi], None)],
                      writes=[(B["QKT"], (ch, blk))], sembuf=ST[pi])
        for s in range(4 if KNOB.get("tm", True) else 0):
            n0 = s * 512
            ncol = min(512, NTM - n0)
            wi = cnt[0] % 2; cnt[0] += 1
            P.dma("sp", ws[wi][:, :, 0:ncol], wtm[:, :, n0:n0 + ncol], reads=[(B["WTM"], None)], writes=[(WS[wi], None)], sembuf=WS[wi])
            for ti in range(4):
                t = blk * 4 + ti
                pi = cnt[1] % 4; cnt[1] += 1
                for kc in range(16):
                    MM(P, pm[pi][:, 0:ncol], hTb[:, kc, ti * 128:(ti + 1) * 128], ws[wi][:, kc, 0:ncol], kc == 0, kc == 15,
                       [(WS[wi], None), (HTB, None)], [(PM[pi], None)])
                CP(P, "act" if pi % 2 == 0 else "dve", st[pi][:, 0:ncol], pm[pi][:, 0:ncol], [(PM[pi], None)], [(ST[pi], None)])
                P.dma("pool", D["PTM"][t * 128:(t + 1) * 128, n0:n0 + ncol], st[pi][:, 0:ncol], reads=[(ST[pi], None)],
                      writes=[(B["PTM"], (t, s))], sembuf=ST[pi])
                if s == 3:
                    gi = cnt[2] % 2; cnt[2] += 1
                    CP(P, "dve", sg[gi][:], pm[pi][:, ncol - 16:ncol], [(PM[pi], None)], [(SG[gi], None)])
                    P.dma("pool", D["GATES"][t * 128:(t + 1) * 128, :], sg[gi][:], reads=[(SG[gi], None)],
                          writes=[(B["GATES"], t)], sembuf=SG[gi])

    nblk = KNOB.get("nblk", 8)
    norm_block(0)
    for blk in range(nblk):
        if blk + 1 < nblk:
            norm_block(blk + 1)
        if KNOB.get("gemm", True):
            gemm_block(blk)


def phase_window(C, l):
    P, I, D, B, K = C.P, C.I, C.D, C.B, C.K
    E = C.sb("w_E", [128, 12, 384], F32); EB = Buf("w_E")
    snk = C.sb("w_snk", [128, 12], F32); SK = Buf("w_snk")
    qt = [C.sb("w_q%d" % i, [128, 6, 128], BF16) for i in range(2)]; QT = [Buf("w_q%d" % i) for i in range(2)]
    kt = [C.sb("w_k%d" % i, [128, 4, 384], BF16) for i in range(2)]; KT = [Buf("w_k%d" % i) for i in range(2)]
    vt = [C.sb("w_v%d" % i, [128, 3, 4, 65], BF16) for i in range(2)]; VT = [Buf("w_v%d" % i) for i in range(2)]
    pss = [C.ps("w_ps%d" % i, [128, 512]) for i in range(2)]; PSS = [PB("w_ps%d" % i) for i in range(2)]
    acc = [C.ps("w_acc%d" % i, [128, 512]) for i in range(2)]; ACC = [PB("w_acc%d" % i) for i in range(2)]
    pe_ = [C.sb("w_pe%d" % i, [128, 384], F32) for i in range(2)]; PEB = [Buf("w_pe%d" % i) for i in range(2)]
    pT = [C.sb("w_pT%d" % i, [128, 384], BF16) for i in range(2)]; PT = [Buf("w_pT%d" % i) for i in range(2)]
    den = [C.sb("w_den%d" % i, [128, 12], F32) for i in range(2)]; DEN = [Buf("w_den%d" % i) for i in range(2)]
    ya = [C.sb("w_ya%d" % i, [128, 768], BF16) for i in range(2)]; YA = [Buf("w_ya%d" % i) for i in range(2)]
    P.dma("sp", E[:].rearrange("p h c -> p (h c)"), I["c_E"], writes=[(EB, None)], sembuf=EB)
    P.dma("sp", snk[:], I["attn_sink"][l].partition_broadcast(128), writes=[(SK, None)], sembuf=SK)
    ACTF(P, snk[:], snk[:], AF.Exp, [(SK, None)], [(SK, None)])
    for i in range(2):
        MEMSET(P, "dve", vt[i][:], 1.0, [], [(VT[i], None)])
    qk = D["QKT"].rearrange("c p t -> p c t")
    scale = 64 ** -0.5
    hc = 0
    for i in range(NT):
        s = i % 2
        j0, j1 = max(0, i - 1), min(NT - 1, i + 1)
        d0, d1 = j0 - (i - 1), j1 - (i - 1)
        P.dma("sp", qt[s][:], qk[:, FM_AQ:FM_AQ + 6, i * 128:(i + 1) * 128], reads=[(B["QKT"], None)], writes=[(QT[s], None)], sembuf=QT[s])
        P.dma("sp", kt[s][:, :, d0 * 128:(d1 + 1) * 128], qk[:, FM_AK:FM_AK + 4, j0 * 128:(j1 + 1) * 128], reads=[(B["QKT"], None)],
              writes=[(KT[s], None)], sembuf=KT[s])
        for d in range(d0, d1 + 1):
            j = i - 1 + d
            P.dma("sp", vt[s][:, d, :, 0:64], D["PTM"][j * 128:(j + 1) * 128, 0:256].rearrange("p (h d) -> p h d", d=64),
                  reads=[(B["PTM"], None)], writes=[(VT[s], None)], sembuf=VT[s])
        lo, hi = d0 * 128, (d1 + 1) * 128
        for hq in range(12):
            g = hq // 3; off = (hq % 2) * 64; c = hq // 2
            b = hc % 2; hc += 1
            for d in range(d0, d1 + 1):
                MM(P, pss[b][:, d * 128:(d + 1) * 128], kt[s][off:off + 64, g, d * 128:(d + 1) * 128], qt[s][off:off + 64, c, :], True, True,
                   [(KT[s], None), (QT[s], None)], [(PSS[b], None)])
            ACTF(P, pe_[b][:, lo:hi], pss[b][:, lo:hi], AF.Exp, [(PSS[b], None)], [(PEB[b], None)], scale=scale)
            TT(P, "dve", pT[b][:, lo:hi], pe_[b][:, lo:hi], E[:, hq, lo:hi], ALU.mult, [(PEB[b], None), (EB, None)], [(PT[b], None)])
            a = acc[hq // 6]; AB = ACC[hq // 6]
            co = (hq % 6) * 65
            for d in range(d0, d1 + 1):
                MM(P, a[:, co:co + 65], pT[b][:, d * 128:(d + 1) * 128], vt[s][:, d, g, :], d == d0, d == d1,
                   [(PT[b], None), (VT[s], None)], [(AB, None)])
        for hq in range(12):
            a = acc[hq // 6]; AB = ACC[hq // 6]; co = (hq % 6) * 65
            TS(P, "dve", den[s][:, hq:hq + 1], a[:, co + 64:co + 65], snk[:, hq:hq + 1], None, ALU.add, None, [(AB, None), (SK, None)], [(DEN[s], None)])
        RECIP(P, den[s][:], den[s][:], [(DEN[s], None)], [(DEN[s], None)])
        for hq in range(12):
            a = acc[hq // 6]; AB = ACC[hq // 6]; co = (hq % 6) * 65
            TS(P, "dve", ya[s][:, hq * 64:(hq + 1) * 64], a[:, co:co + 64], den[s][:, hq:hq + 1], None, ALU.mult, None,
               [(AB, None), (DEN[s], None)], [(YA[s], None)])
        P.dma("pool", D["Y"][i * 128:(i + 1) * 128, 0:768], ya[s][:], reads=[(YA[s], None)], writes=[(B["Y"], ("a", i))], sembuf=YA[s])


def rep_sumsq(C, sq_chunks, R, rep_ps, RPS, rstd, RSTD, n, width):
    P, K, KB = C.P, C.K, C.KB
    for i, (ap, rows) in enumerate(sq_chunks):
        MM(P, rep_ps[:, 0:width], K["onesb"][0:rows, :], ap, i == 0, i == len(sq_chunks) - 1, R + [(KB, "onesb")], [(RPS, None)])
    ACTF(P, rstd[:, 0:width], rep_ps[:, 0:width], AF.Sqrt, [(RPS, None)], [(RSTD, None)], bias=EPS, scale=1.0 / n)
    RECIP(P, rstd[:, 0:width], rstd[:, 0:width], [(RSTD, None)], [(RSTD, None)])


def phase_mla_prep(C, l):
    P, I, D, B, K, KB = C.P, C.I, C.D, C.B, C.K, C.KB
    wqf = C.sb("p_wqf", [128, 4, 768], F32); WQF = Buf("p_wqf")
    wq = C.sb("p_wq", [128, 4, 768], BF16); WQ = Buf("p_wq")
    wkf = C.sb("p_wkf", [128, 1024], F32); WKF = Buf("p_wkf")
    wk = C.sb("p_wk", [128, 1024], BF16); WK = Buf("p_wk")
    gq = C.sb("p_gq", [128, 4], F32); GQ = Buf("p_gq")
    gk = C.sb("p_gk", [128, 1], F32); GK = Buf("p_gk")
    P.dma("sp", wqf[:], I["w_uq"][l].rearrange("(kc p) n -> p kc n", p=128), writes=[(WQF, None)], sembuf=WQF)
    P.dma("sp", wkf[:], I["w_ukv"][l], writes=[(WKF, None)], sembuf=WKF)
    P.dma("sp", gq[:], I["q_norm_gT"][l], writes=[(GQ, None)], sembuf=GQ)
    P.dma("sp", gk[:], I["kv_norm_gT"][l], writes=[(GK, None)], sembuf=GK)
    for kc in range(4):
        TS(P, "dve", wq[:, kc, :], wqf[:, kc, :], gq[:, kc:kc + 1], None, ALU.mult, None, [(WQF, None), (GQ, None)], [(WQ, kc)])
    TS(P, "dve", wk[:], wkf[:], gk[:, 0:1], None, ALU.mult, None, [(WKF, None), (GK, None)], [(WK, None)])
    posi = C.sb("p_posi", [64, S_], I32); POSI = Buf("p_posi")
    ang = C.sb("p_ang", [64, S_], F32); ANG = Buf("p_ang")
    cosT = C.sb("p_cos", [64, S_], F32); COS = Buf("p_cos")
    sinT = C.sb("p_sin", [64, S_], F32); SIN = Buf("p_sin")
    invf = C.sb("p_invf", [64, 1], F32); INVF = Buf("p_invf")
    P.dma("sp", posi[:], I["pos"].partition_broadcast(64), writes=[(POSI, None)], sembuf=POSI)
    P.dma("sp", invf[:], I["c_invf"], writes=[(INVF, None)], sembuf=INVF)
    CP(P, "dve", ang[:], posi[:], [(POSI, None)], [(ANG, None)])
    TS(P, "dve", ang[:], ang[:], invf[:, 0:1], None, ALU.mult, None, [(ANG, None), (INVF, None)], [(ANG, None)])
    TWO_PI = 2.0 * math.pi
    MAGIC = 12582912.0
    TS(P, "dve", sinT[:], ang[:], 1.0 / TWO_PI, MAGIC, ALU.mult, ALU.add, [(ANG, None)], [(SIN, None)])
    TS(P, "dve", sinT[:], sinT[:], -MAGIC, None, ALU.add, None, [(SIN, None)], [(SIN, None)])
    STT(P, sinT[:], sinT[:], -TWO_PI, ang[:], ALU.mult, ALU.add, [(SIN, None), (ANG, None)], [(SIN, None)])
    TS(P, "dve", ang[:], ang[:], 0.5 * math.pi, None, ALU.add, None, [(ANG, None)], [(ANG, None)])
    TS(P, "dve", cosT[:], ang[:], 1.0 / TWO_PI, MAGIC, ALU.mult, ALU.add, [(ANG, None)], [(COS, None)])
    TS(P, "dve", cosT[:], cosT[:], -MAGIC, None, ALU.add, None, [(COS, None)], [(COS, None)])
    STT(P, cosT[:], cosT[:], -TWO_PI, ang[:], ALU.mult, ALU.add, [(COS, None), (ANG, None)], [(COS, None)])
    PI_LO = 3.1415925
    TS(P, "dve", sinT[:], sinT[:], -PI_LO, PI_LO, ALU.max, ALU.min, [(SIN, None)], [(SIN, None)])
    TS(P, "dve", cosT[:], cosT[:], -PI_LO, PI_LO, ALU.max, ALU.min, [(COS, None)], [(COS, None)])
    ACTF(P, sinT[:], sinT[:], AF.Sin, [(SIN, None)], [(SIN, None)])
    ACTF(P, cosT[:], cosT[:], AF.Sin, [(COS, None)], [(COS, None)])

    cq = [C.sb("p_cq%d" % i, [128, 4, 512], BF16) for i in range(2)]; CQ = [Buf("p_cq%d" % i) for i in range(2)]
    ckv = [C.sb("p_ckv%d" % i, [128, 512], BF16) for i in range(2)]; CKV = [Buf("p_ckv%d" % i) for i in range(2)]
    kr = [C.sb("p_kr%d" % i, [64, 512], BF16) for i in range(2)]; KRB = [Buf("p_kr%d" % i) for i in range(2)]
    sq = C.sb("p_sq", [128, 4, 512], BF16); SQ = Buf("p_sq")
    sqk = C.sb("p_sqk", [128, 512], BF16); SQK = Buf("p_sqk")
    rps = C.ps("p_rps", [128, 512]); RPS = PB("p_rps")
    rstd = C.sb("p_rstd", [128, 512], F32); RSTD = Buf("p_rstd")
    rstdk = C.sb("p_rstdk", [128, 512], F32); RSTDK = Buf("p_rstdk")
    pq = [C.ps("p_pq%d" % i, [128, 512]) for i in range(3)]; PQ = [PB("p_pq%d" % i) for i in range(3)]
    prot = C.ps("p_prot", [64, 512]); PROT = PB("p_prot")
    pv = C.ps("p_pv", [128, 512]); PV = PB("p_pv")
    ptm = C.ps("p_ptm", [128, 4, 2]); PTM_ = PB("p_ptm")
    so = [C.sb("p_so%d" % i, [128, 512], BF16) for i in range(3)]; SO = [Buf("p_so%d" % i) for i in range(3)]
    t1 = C.sb("p_t1", [64, 512], F32); T1 = Buf("p_t1")
    t2 = C.sb("p_t2", [64, 512], F32); T2 = Buf("p_t2")
    raw = C.sb("p_raw", [64, 512], BF16); RAW = Buf("p_raw")
    rtm = C.sb("p_rtm", [128, 4], F32); RTM = Buf("p_rtm")
    vo = [C.sb("p_vo%d" % i, [128, 512], BF16) for i in range(2)]; VO = [Buf("p_vo%d" % i) for i in range(2)]
    qk = D["QKT"].rearrange("c p t -> p c t")
    oc = [0]

    def rope_out(src_ps, SRC, rst, RST, blk, dst_ap, DSTB, dkey):
        cs = slice(blk * 512, (blk + 1) * 512)
        if rst is not None:
            TT(P, "dve", raw[:], src_ps, rst[0:64, :], ALU.mult, [(SRC, None), (RST, None)], [(RAW, None)])
        else:
            CP(P, "dve", raw[:], src_ps, [(SRC, None)], [(RAW, None)])
        MM(P, prot[:], K["R"][:], raw[:], True, True, [(RAW, None), (KB, "R")], [(PROT, None)])
        TT(P, "dve", t1[:], raw[:], cosT[:, cs], ALU.mult, [(RAW, None), (COS, None)], [(T1, None)])
        TT(P, "dve", t2[:], prot[:], sinT[:, cs], ALU.mult, [(PROT, None), (SIN, None)], [(T2, None)])
        o = oc[0] % 3; oc[0] += 1
        TT(P, "dve", so[o][0:64, :], t1[:], t2[:], ALU.add, [(T1, None), (T2, None)], [(SO[o], None)])
        P.dma("pool", dst_ap, so[o][0:64, :], reads=[(SO[o], None)], writes=[(DSTB, dkey)], sembuf=SO[o])

    for blk in range(8):
        s = blk % 2
        cs = slice(blk * 512, (blk + 1) * 512)
        P.dma("sp", cq[s][:], qk[:, FM_BCQ:FM_BCQ + 4, cs], reads=[(B["QKT"], None)], writes=[(CQ[s], None)], sembuf=CQ[s])
        P.dma("sp", ckv[s][:], D["QKT"][FM_BCKV][:, cs], reads=[(B["QKT"], None)], writes=[(CKV[s], None)], sembuf=CKV[s])
        P.dma("sp", kr[s][:], D["QKT"][FM_BKR][0:64, cs], reads=[(B["QKT"], None)], writes=[(KRB[s], None)], sembuf=KRB[s])
        rows = [128, 128, 128, 64]
        ACTF(P, sq[:], cq[s][:], AF.Square, [(CQ[s], None)], [(SQ, None)])
        rep_sumsq(C, [(sq[0:rows[kc], kc, :], rows[kc]) for kc in range(4)], [(SQ, None)], rps, RPS, rstd, RSTD, 448.0, 512)
        for h in range(4):
            pi = h % 3
            for kc in range(4):
                MM(P, pq[pi][:], wq[0:rows[kc], kc, h * 192:h * 192 + 128], cq[s][0:rows[kc], kc, :], kc == 0, kc == 3,
                   [(WQ, None), (CQ[s], None)], [(PQ[pi], None)])
            o = oc[0] % 3; oc[0] += 1
            TT(P, "dve", so[o][:], pq[pi][:], rstd[:], ALU.mult, [(PQ[pi], None), (RSTD, None)], [(SO[o], None)])
            P.dma("pool", D["QN"][h][:, cs], so[o][:], reads=[(SO[o], None)], writes=[(B["QN"], (h, blk))], sembuf=SO[o])
            pi = (h + 1) % 3
            for kc in range(4):
                MM(P, pq[pi][0:64, :], wq[0:rows[kc], kc, h * 192 + 128:h * 192 + 192], cq[s][0:rows[kc], kc, :], kc == 0, kc == 3,
                   [(WQ, None), (CQ[s], None)], [(PQ[pi], None)])
            rope_out(pq[pi][0:64, :], PQ[pi], rstd, RSTD, blk, D["QR"][h][:, cs], B["QR"], (h, blk))
        ACTF(P, sqk[:], ckv[s][:], AF.Square, [(CKV[s], None)], [(SQK, None)])
        rep_sumsq(C, [(sqk[:], 128)], [(SQK, None)], rps, RPS, rstdk, RSTDK, 128.0, 512)
        for h in range(4):
            pi = h % 3
            MM(P, pq[pi][:], wk[:, h * 256:h * 256 + 128], ckv[s][:], True, True, [(WK, None), (CKV[s], None)], [(PQ[pi], None)])
            o = oc[0] % 3; oc[0] += 1
            TT(P, "dve", so[o][:], pq[pi][:], rstdk[:], ALU.mult, [(PQ[pi], None), (RSTDK, None)], [(SO[o], None)])
            P.dma("pool", D["KN"][h][:, cs], so[o][:], reads=[(SO[o], None)], writes=[(B["KN"], (h, blk))], sembuf=SO[o])
        for ti in range(4):
            MM(P, ptm[:, ti, :], sqk[:, ti * 128:(ti + 1) * 128], K["onesb"][:, 0:2], True, True, [(SQK, None), (KB, "onesb")], [(PTM_, None)])
        ACTF(P, rtm[:], ptm[:, :, 0], AF.Sqrt, [(PTM_, None)], [(RTM, None)], bias=EPS, scale=1.0 / 128.0)
        RECIP(P, rtm[:], rtm[:], [(RTM, None)], [(RTM, None)])
        for ti in range(4):
            t = blk * 4 + ti
            for h in range(4):
                MM(P, pv[:, h * 128:(h + 1) * 128], ckv[s][:, ti * 128:(ti + 1) * 128], wk[:, h * 256 + 128:h * 256 + 256], True, True,
                   [(WK, None), (CKV[s], None)], [(PV, None)])
            v = t % 2
            TS(P, "dve", vo[v][:], pv[:], rtm[:, ti:ti + 1], None, ALU.mult, None, [(PV, None), (RTM, None)], [(VO[v], None)])
            P.dma("pool", D["VB"][t * 128:(t + 1) * 128, :], vo[v][:], reads=[(VO[v], None)], writes=[(B["VB"], t)], sembuf=VO[v])
        MM(P, prot[:], K["R"][:], kr[s][:], True, True, [(KRB[s], None), (KB, "R")], [(PROT, None)])
        TT(P, "dve", t1[:], kr[s][:], cosT[:, cs], ALU.mult, [(KRB[s], None), (COS, None)], [(T1, None)])
        TT(P, "dve", t2[:], prot[:], sinT[:, cs], ALU.mult, [(PROT, None), (SIN, None)], [(T2, None)])
        o = oc[0] % 3; oc[0] += 1
        TT(P, "dve", so[o][0:64, :], t1[:], t2[:], ALU.add, [(T1, None), (T2, None)], [(SO[o], None)])
        P.dma("pool", D["KR"][:, cs], so[o][0:64, :], reads=[(SO[o], None)], writes=[(B["KR"], blk)], sembuf=SO[o])


def phase_mla_attn(C, l, bg=None):
    P, I, D, B, K = C.P, C.I, C.D, C.B, C.K
    krT = C.sb("m_kr", [64, S_], BF16); KRT = Buf("m_kr")
    qn = C.sb("m_qn", [128, S_], BF16); QN = Buf("m_qn")
    qr = C.sb("m_qr", [64, S_], BF16); QR = Buf("m_qr")
    kn = C.sb("m_kn", [128, S_], BF16); KN = Buf("m_kn")
    va = C.sb("m_va", [128, NT, 129], BF16); VA = Buf("m_va")
    pss = [C.ps("m_ps%d" % i, [128, 512]) for i in range(2)]; PSS = [PB("m_ps%d" % i) for i in range(2)]
    acc = [C.ps("m_acc%d" % i, [128, 512]) for i in range(4)]; ACC = [PB("m_acc%d" % i) for i in range(4)]
    pT = [C.sb("m_pT%d" % i, [128, 512], BF16) for i in range(3)]; PT = [Buf("m_pT%d" % i) for i in range(3)]
    rd = [C.sb("m_rd%d" % i, [128, 1], F32) for i in range(2)]; RD = [Buf("m_rd%d" % i) for i in range(2)]
    yo = [C.sb("m_yo%d" % i, [128, 128], BF16) for i in range(2)]; YO = [Buf("m_yo%d" % i) for i in range(2)]
    scale = 192 ** -0.5
    P.dma("sp", krT[:], D["KR"], reads=[(B["KR"], None)], writes=[(KRT, None)], sembuf=KRT)
    MEMSET(P, "dve", va[:], 1.0, [], [(VA, "ones")])
    n = 0
    oc = 0
    for h in range(4):
        P.dma("sp", qn[:], D["QN"][h], reads=[(B["QN"], None)], writes=[(QN, None)], sembuf=QN)
        P.dma("sp", qr[:], D["QR"][h], reads=[(B["QR"], None)], writes=[(QR, None)], sembuf=QR)
        P.dma("sp", kn[:], D["KN"][h], reads=[(B["KN"], None)], writes=[(KN, None)], sembuf=KN)
        vbv = D["VB"][:, h * 128:(h + 1) * 128].rearrange("(t p) d -> p t d", p=128)
        for t0 in range(0, NT, 4):
            P.dma("sp", va[:, t0:t0 + 4, 0:128], vbv[:, t0:t0 + 4, :], reads=[(B["VB"], None), (VA, "ones")],
                  writes=[(VA, ("v", t0))], sembuf=VA)
        for qb in range(8):
            qs = slice(qb * 512, (qb + 1) * 512)
            for j in range(NT):
                ks = slice(j * 128, (j + 1) * 128)
                b = n % 2; pb = n % 3; n += 1
                if bg is not None and n % 4 == 0:
                    next(bg, None)
                MM(P, pss[b][:], kn[:, ks], qn[:, qs], True, False, [(KN, None), (QN, None)], [(PSS[b], None)])
                MM(P, pss[b][:], krT[:, ks], qr[:, qs], False, True, [(KRT, None), (QR, None)], [(PSS[b], None)])
                ACTF(P, pT[pb][:], pss[b][:], AF.Exp, [(PSS[b], None)], [(PT[pb], None)], scale=scale)
                for ii in range(4):
                    MM(P, acc[ii][:, 0:129], pT[pb][:, ii * 128:(ii + 1) * 128], va[:, j, :], j == 0, j == NT - 1,
                       [(PT[pb], None), (VA, ("v", (j // 4) * 4)), (VA, "ones")], [(ACC[ii], None)])
            for ii in range(4):
                t = qb * 4 + ii
                o = oc % 2; oc += 1
                RECIP(P, rd[o][:], acc[ii][:, 128:129], [(ACC[ii], None)], [(RD[o], None)])
                TS(P, "dve", yo[o][:], acc[ii][:, 0:128], rd[o][:, 0:1], None, ALU.mult, None, [(ACC[ii], None), (RD[o], None)], [(YO[o], None)])
                P.dma("pool", D["Y"][t * 128:(t + 1) * 128, 768 + h * 128:768 + (h + 1) * 128], yo[o][:], reads=[(YO[o], None)],
                      writes=[(B["Y"], ("b", h, t))], sembuf=YO[o])
    if bg is not None:
        for _ in bg:
            pass


def phase_mlstm_prep(C, l):
    P, I, D, B, K, KB = C.P, C.I, C.D, C.B, C.K, C.KB
    g = C.sb("g_g", [128, NT, 16], F32); G = Buf("g_g")
    gb = C.sb("g_gb", [128, 16], F32); GB = Buf("g_gb")
    lf = C.sb("g_lf", [128, NT, 8], F32); LF = Buf("g_lf")
    tot = C.sb("g_tot", [128, NT, 8], F32); TOT = Buf("g_tot")
    off = C.sb("g_off", [128, NT, 8], F32); OFF = Buf("g_off")
    cum = C.sb("g_cum", [128, NT, 8], F32); CUM = Buf("g_cum")
    ibs = C.sb("g_ibs", [128, NT, 8], F32); IBS = Buf("g_ibs")
    pt = C.ps("g_pt", [128, 256]); PTB = PB("g_pt")
    pc = C.ps("g_pc", [128, 256]); PCB = PB("g_pc")
    pc2 = C.ps("g_pc2", [128, 256]); PCB2 = PB("g_pc2")
    pr = [C.ps("g_pr%d" % i, [128, 512]) for i in range(2)]; PR = [PB("g_pr%d" % i) for i in range(2)]
    dg = [C.sb("g_dg%d" % i, [128, 512], F32) for i in range(2)]; DG = [Buf("g_dg%d" % i) for i in range(2)]
    ro = [C.sb("g_ro%d" % i, [128, 512], F32) for i in range(2)]; RO = [Buf("g_ro%d" % i) for i in range(2)]
    gv = D["GATES"].rearrange("(t p) c -> p t c", p=128)
    for t0 in range(0, NT, 4):
        P.dma("sp", g[:, t0:t0 + 4, :], gv[:, t0:t0 + 4, :], reads=[(B["GATES"], None)], writes=[(G, ("ld", t0))], sembuf=G)
    P.dma("sp", gb[:], I["gate_b"][l].partition_broadcast(128), writes=[(GB, None)], sembuf=GB)
    for t in range(NT):
        TT(P, "dve", g[:, t, :], g[:, t, :], gb[:], ALU.add, [(G, None), (GB, None)], [(G, None)])
    ACTF(P, lf[:], g[:, :, 8:16], AF.Exp, [(G, None)], [(LF, None)], scale=-1.0)
    ACTF(P, lf[:], lf[:], AF.Ln, [(LF, None)], [(LF, None)], bias=1.0)
    TS(P, "dve", lf[:], lf[:], -1.0, None, ALU.mult, None, [(LF, None)], [(LF, None)])
    lf2 = lf[:].rearrange("p t c -> p (t c)")
    MM(P, pt[:], K["ones"][:], lf2, True, True, [(LF, None), (KB, "ones")], [(PTB, None)])
    CP(P, "dve", tot[:].rearrange("p t c -> p (t c)"), pt[:], [(PTB, None)], [(TOT, None)])
    MEMSET(P, "dve", off[:], 0.0, [], [(OFF, None)])
    for t in range(1, NT):
        TT(P, "dve", off[:, t, 0:4], off[:, t - 1, 0:4], tot[:, t - 1, 0:4], ALU.add, [(OFF, None), (TOT, None)], [(OFF, None)])
    for t in range(NT - 2, -1, -1):
        TT(P, "dve", off[:, t, 4:8], off[:, t + 1, 4:8], tot[:, t + 1, 4:8], ALU.add, [(OFF, None), (TOT, None)], [(OFF, None)])
    MM(P, pc[:], K["U"][:], lf2, True, True, [(LF, None), (KB, "U")], [(PCB, None)])
    MM(P, pc2[:], K["Lm"][:], lf2, True, True, [(LF, None), (KB, "L")], [(PCB2, None)])
    pc3 = pc[:].rearrange("p (t c) -> p t c", c=8)
    pc23 = pc2[:].rearrange("p (t c) -> p t c", c=8)
    TT(P, "dve", cum[:, :, 0:4], pc3[:, :, 0:4], off[:, :, 0:4], ALU.add, [(PCB, None), (OFF, None)], [(CUM, "f")])
    TT(P, "dve", cum[:, :, 4:8], pc23[:, :, 4:8], off[:, :, 4:8], ALU.add, [(PCB2, None), (OFF, None)], [(CUM, "b")])
    TT(P, "dve", ibs[:], g[:, :, 0:8], cum[:], ALU.subtract, [(G, None), (CUM, None)], [(IBS, None)])
    P.dma("pool", D["IBS"], ibs[:].rearrange("p t c -> p (t c)"), reads=[(IBS, None)], writes=[(B["IBS"], None)], sembuf=IBS)
    n = 0
    for c in range(8):
        for t0 in range(0, NT, 4):
            b = n % 2; n += 1
            for j in range(4):
                t = t0 + j
                TS(P, "dve", dg[b][:, j * 128:(j + 1) * 128], K["identf"][:], cum[:, t, c:c + 1], None, ALU.mult, None,
                   [(CUM, None), (KB, "identf")], [(DG[b], j)])
                MM(P, pr[b][:, j * 128:(j + 1) * 128], K["ones"][:], dg[b][:, j * 128:(j + 1) * 128], True, True,
                   [(DG[b], j), (KB, "ones")], [(PR[b], None)])
            CP(P, "act", ro[b][:], pr[b][:], [(PR[b], None)], [(RO[b], None)])
            P.dma("pool", D["BREP"][c][:, t0 * 128:(t0 + 4) * 128], ro[b][:], reads=[(RO[b], None)], writes=[(B["BREP"], (c, t0))], sembuf=RO[b])


def phase_mlstm_attn(C, l):
    P, I, D, B, K, KB = C.P, C.I, C.D, C.B, C.K, C.KB
    ibs = C.sb("s_ibs", [128, NT, 8], F32); IBS = Buf("s_ibs")
    hg = C.sb("s_hg", [128, 768], F32); HG = Buf("s_hg")
    qT = C.sb("s_qT", [96, S_], BF16); QT = Buf("s_qT")
    kT = C.sb("s_kT", [96, S_], BF16); KT = Buf("s_kT")
    va = C.sb("s_va", [128, NT, 193], BF16); VA = Buf("s_va")
    br = [C.sb("s_br%d" % i, [128, S_], F32) for i in range(2)]; BR = [Buf("s_br%d" % i) for i in range(2)]
    pss = [C.ps("s_ps%d" % i, [128, 256]) for i in range(2)]; PSS = [PB("s_ps%d" % i) for i in range(2)]
    acc = [[C.ps("s_acc%d%d" % (d, i), [128, 512]) for i in range(2)] for d in range(2)]
    ACC = [[PB("s_acc%d%d" % (d, i)) for i in range(2)] for d in range(2)]
    w = [C.sb("s_w%d" % i, [128, 256], F32) for i in range(3)]; WB = [Buf("s_w%d" % i) for i in range(3)]
    wT = [C.sb("s_wT%d" % i, [128, 256], BF16) for i in range(3)]; WT = [Buf("s_wT%d" % i) for i in range(3)]
    op_ = [C.sb("s_op%d" % i, [128, 192], BF16) for i in range(2)]; OP = [Buf("s_op%d" % i) for i in range(2)]
    dn = [C.sb("s_dn%d" % i, [128, 2], F32) for i in range(2)]; DN = [Buf("s_dn%d" % i) for i in range(2)]
    hs = [C.sb("s_hs%d" % i, [128, 192], F32) for i in range(2)]; HS = [Buf("s_hs%d" % i) for i in range(2)]
    hb = [C.sb("s_hb%d" % i, [128, 192], F32) for i in range(2)]; HB = [Buf("s_hb%d" % i) for i in range(2)]
    jk = C.sb("s_jk", [128, 192], F32); JK = Buf("s_jk")
    ssq = [C.sb("s_ssq%d" % i, [128, 1], F32) for i in range(2)]; SSQ = [Buf("s_ssq%d" % i) for i in range(2)]
    yo = [C.sb("s_yo%d" % i, [128, 192], BF16) for i in range(2)]; YO = [Buf("s_yo%d" % i) for i in range(2)]
    scale = 96 ** -0.5
    P.dma("sp", ibs[:].rearrange("p t c -> p (t c)"), D["IBS"], reads=[(B["IBS"], None)], writes=[(IBS, None)], sembuf=IBS)
    P.dma("sp", hg[:], I["head_g"][l].partition_broadcast(128), writes=[(HG, None)], sembuf=HG)
    MEMSET(P, "dve", va[:], 1.0, [], [(VA, "ones")])
    U, Lm = K["U"], K["Lm"]
    n = 0
    fc = 0
    for h in range(4):
        P.dma("sp", qT[:], D["QKT"][FM_CQ + h][0:96, :], reads=[(B["QKT"], None)], writes=[(QT, None)], sembuf=QT)
        P.dma("sp", kT[:], D["QKT"][FM_CK + h][0:96, :], reads=[(B["QKT"], None)], writes=[(KT, None)], sembuf=KT)
        cvv = D["PTM"][:, 256 + h * 192:256 + (h + 1) * 192].rearrange("(t p) d -> p t d", p=128)
        for t0 in range(0, NT, 4):
            P.dma("sp", va[:, t0:t0 + 4, 0:192], cvv[:, t0:t0 + 4, :], reads=[(B["PTM"], None), (VA, "ones")],
                  writes=[(VA, ("v", t0))], sembuf=VA)
        for d in range(2):
            P.dma("sp", br[d][:], D["BREP"][d * 4 + h], reads=[(B["BREP"], None)], writes=[(BR[d], None)], sembuf=BR[d])
        for lb in range(16):
            i0 = lb * 2
            ls = slice(lb * 256, (lb + 1) * 256)
            first = [[True, True], [True, True]]
            nexp = [[0, 0], [0, 0]]
            total = [[i0 + 1, i0 + 2], [NT - i0, NT - i0 - 1]]
            for j in range(NT):
                ks = slice(j * 128, (j + 1) * 128)
                b = n % 2; n += 1
                MM(P, pss[b][:], kT[:, ks], qT[:, ls], True, True, [(KT, None), (QT, None)], [(PSS[b], None)])
                if j < i0:
                    items = [(0, 0, 2, False)]
                elif j > i0 + 1:
                    items = [(1, 0, 2, False)]
                elif j == i0:
                    items = [(0, 0, 1, True), (1, 0, 1, True), (0, 1, 1, False)]
                else:
                    items = [(1, 0, 1, False), (0, 1, 1, True), (1, 1, 1, True)]
                for (d, a, cn, masked) in items:
                    wi = fc % 3; fc += 1
                    c = d * 4 + h
                    lsl = slice((i0 + a) * 128, (i0 + a + cn) * 128)
                    wv = w[wi][:, 0:cn * 128]
                    ACTF(P, wv, br[d][:, lsl], AF.Exp, [(BR[d], None), (IBS, None)], [(WB[wi], None)], bias=ibs[:, j, c:c + 1])
                    if masked:
                        TT(P, "dve", wv, wv, (U if d == 0 else Lm)[:], ALU.mult, [(WB[wi], None), (KB, "U"), (KB, "L")], [(WB[wi], None)])
                    STT(P, wT[wi][:, 0:cn * 128], pss[b][:, a * 128:(a + cn) * 128], scale, wv, ALU.mult, ALU.mult,
                        [(PSS[b], None), (WB[wi], None)], [(WT[wi], None)])
                    for q in range(cn):
                        ii = a + q
                        nexp[d][ii] += 1
                        MM(P, acc[d][ii][:, 0:193], wT[wi][:, q * 128:(q + 1) * 128], va[:, j, :], nexp[d][ii] == 1, nexp[d][ii] == total[d][ii],
                           [(WT[wi], None), (VA, ("v", (j // 4) * 4)), (VA, "ones")], [(ACC[d][ii], None)])
            for ii in range(2):
                t = i0 + ii
                o = t % 2
                for d in range(2):
                    a = acc[d][ii]
                    ACTF(P, dn[o][:, d:d + 1], a[:, 192:193], AF.Abs, [(ACC[d][ii], None)], [(DN[o], d)])
                TS(P, "dve", dn[o][:], dn[o][:], 1.0, None, ALU.max, None, [(DN[o], None)], [(DN[o], None)])
                RECIP(P, dn[o][:], dn[o][:], [(DN[o], None)], [(DN[o], None)])
                TS(P, "dve", hs[o][:], acc[0][ii][:, 0:192], dn[o][:, 0:1], None, ALU.mult, None, [(ACC[0][ii], None), (DN[o], None)], [(HS[o], None)])
                TS(P, "dve", hb[o][:], acc[1][ii][:, 0:192], dn[o][:, 1:2], None, ALU.mult, None, [(ACC[1][ii], None), (DN[o], None)], [(HB[o], None)])
                TT(P, "dve", hs[o][:], hs[o][:], hb[o][:], ALU.add, [(HS[o], None), (HB[o], None)], [(HS[o], None)])
                ACTF(P, jk[:], hs[o][:], AF.Square, [(HS[o], None)], [(JK, None), (SSQ[o], None)], accum=ssq[o][:])
                ACTF(P, ssq[o][:], ssq[o][:], AF.Sqrt, [(SSQ[o], None)], [(SSQ[o], None)], bias=EPS, scale=1.0 / 192.0)
                RECIP(P, ssq[o][:], ssq[o][:], [(SSQ[o], None)], [(SSQ[o], None)])
                P.dma("sp", op_[o][:], D["PTM"][t * 128:(t + 1) * 128, 1024 + h * 192:1024 + (h + 1) * 192], reads=[(B["PTM"], None)],
                      writes=[(OP[o], None)], sembuf=OP[o])
                ACTF(P, hb[o][:], op_[o][:], AF.Sigmoid, [(OP[o], None)], [(HB[o], None)])
                STT(P, hs[o][:], hs[o][:], ssq[o][:, 0:1], hg[:, h * 192:(h + 1) * 192], ALU.mult, ALU.mult,
                    [(HS[o], None), (SSQ[o], None), (HG, None)], [(HS[o], None)])
                TT(P, "dve", yo[o][:], hs[o][:], hb[o][:], ALU.mult, [(HS[o], None), (HB[o], None)], [(YO[o], None)])
                P.dma("pool", D["Y"][t * 128:(t + 1) * 128, 1280 + h * 192:1280 + (h + 1) * 192], yo[o][:], reads=[(YO[o], None)],
                      writes=[(B["Y"], ("c", h, t))], sembuf=YO[o])


def post_norm_residual(C, t, pm4, PM4, x_ap, XB_key, xt, XT, rep, REP, junk, JK, ssp, SSP, dst_ap, DSTB, tmp, TMP):
    P = C.P
    for n in range(4):
        ACTF(P, junk[:, n * 512:(n + 1) * 512], pm4[n][:], AF.Square, [(PM4[n], None)], [(JK, n), (SSP, n)], accum=ssp[:, n:n + 1])
    TT(P, "dve", ssp[:, 4:5], ssp[:, 0:1], ssp[:, 1:2], ALU.add, [(SSP, 0), (SSP, 1)], [(SSP, "a")])
    TT(P, "dve", ssp[:, 5:6], ssp[:, 2:3], ssp[:, 3:4], ALU.add, [(SSP, 2), (SSP, 3)], [(SSP, "b")])
    TT(P, "dve", ssp[:, 6:7], ssp[:, 4:5], ssp[:, 5:6], ALU.add, [(SSP, "a"), (SSP, "b")], [(SSP, "c")])
    ACTF(P, ssp[:, 7:8], ssp[:, 6:7], AF.Sqrt, [(SSP, "c")], [(SSP, "r")], bias=EPS, scale=1.0 / D_)
    RECIP(P, ssp[:, 7:8], ssp[:, 7:8], [(SSP, "r")], [(SSP, "r")])
    for n in range(4):
        STT(P, tmp[:, n * 512:(n + 1) * 512], pm4[n][:], ssp[:, 7:8], rep[:, n * 512:(n + 1) * 512], ALU.mult, ALU.mult,
            [(PM4[n], None), (SSP, "r"), (REP, None)], [(TMP, n)])
    TT(P, "pool", xt[:], xt[:], tmp[:], ALU.add, [(XT, None), (TMP, None)], [(XT, None)])
    P.dma("pool", dst_ap[t * 128:(t + 1) * 128, :], xt[:], reads=[(XT, None)], writes=[(DSTB, t)], sembuf=XT)


def phase_outproj(C, l, xsrc, xdst):
    P, I, D, B, K, KB = C.P, C.I, C.D, C.B, C.K, C.KB
    wo = C.sb("o_w", [128, 16, D_], BF16); WO = Buf("o_w")
    rep = C.sb("o_rep", [128, D_], F32); REP = Buf("o_rep")
    rtmp = C.sb("o_rtmp", [128, 512], F32); RTMP = Buf("o_rtmp")
    yt = [C.sb("o_y%d" % i, [128, D_], BF16) for i in range(2)]; YT = [Buf("o_y%d" % i) for i in range(2)]
    yT = [C.sb("o_yT%d" % i, [128, 16, 128], BF16) for i in range(2)]; YTT = [Buf("o_yT%d" % i) for i in range(2)]
    xt = [C.sb("o_x%d" % i, [128, D_], F32) for i in range(2)]; XT = [Buf("o_x%d" % i) for i in range(2)]
    tmp = C.sb("o_tmp", [128, D_], F32); TMP = Buf("o_tmp")
    junk = C.sb("o_junk", [128, D_], BF16); JK = Buf("o_junk")
    ssp = [C.sb("o_ssp%d" % i, [128, 8], F32) for i in range(2)]; SSP = [Buf("o_ssp%d" % i) for i in range(2)]
    tp = C.ps("o_tp", [128, D_], BF16); TP = PB("o_tp")
    pm = [C.ps("o_pm%d" % i, [128, 512]) for i in range(4)]; PM = [PB("o_pm%d" % i) for i in range(4)]
    prep = C.ps("o_prep", [128, 512]); PREP = PB("o_prep")
    wov = D["WOUT"][l].rearrange("(kc p) n -> p kc n", p=128)
    for kc in range(16):
        P.dma("sp", wo[:, kc, :], wov[:, kc, :], reads=[(B["WOUT"], None)], writes=[(WO, kc)], sembuf=WO)
    replicate_cols(C, rep, REP, K["vec"][:, l, 2, :], 16, [(C.MODB, (l, 0, 2))], prep, PREP, rtmp, RTMP)
    for t in range(NT):
        s = t % 2
        P.dma("sp", yt[s][:], D["Y"][t * 128:(t + 1) * 128, :], reads=[(B["Y"], None)], writes=[(YT[s], None)], sembuf=YT[s])
        P.dma("sp", xt[s][:], xsrc[t * 128:(t + 1) * 128, :], reads=[(B["XR"], t)], writes=[(XT[s], None)], sembuf=XT[s])
        for c in range(16):
            TR(P, tp[:, c * 128:(c + 1) * 128], yt[s][:, c * 128:(c + 1) * 128], K["ident"][:], [(YT[s], None), (KB, "ident")], [(TP, c // 8)])
        yv = yT[s][:].rearrange("p c t -> p (c t)")
        CP(P, "act", yv[:, 0:1024], tp[:, 0:1024], [(TP, 0)], [(YTT[s], 0)])
        CP(P, "dve", yv[:, 1024:2048], tp[:, 1024:2048], [(TP, 1)], [(YTT[s], 1)])
        for n in range(4):
            for kc in range(16):
                MM(P, pm[n][:], yT[s][:, kc, :], wo[:, kc, n * 512:(n + 1) * 512], kc == 0, kc == 15, [(YTT[s], None), (WO, None)], [(PM[n], None)])
        post_norm_residual(C, t, pm, PM, None, None, xt[s], XT[s], rep, REP, junk, JK, ssp[s], SSP[s], xdst, B["XR"], tmp, TMP)


def phase_ffn(C, l, xsrc, xdst):
    P, I, D, B, K, KB = C.P, C.I, C.D, C.B, C.K, C.KB
    DSTB = B["XR"]
    xt = [C.sb("f_xt%d" % i, [128, D_], F32) for i in range(2)]; XT = [Buf("f_xt%d" % i) for i in range(2)]
    junk = C.sb("f_junk", [128, D_], BF16); JK = Buf("f_junk")
    ss = [C.sb("f_ss%d" % i, [128, 1], F32) for i in range(2)]
    rs = [C.sb("f_rs%d" % i, [128, 1], F32) for i in range(2)]; SS = [Buf("f_ss%d" % i) for i in range(2)]
    xn0 = C.sb("f_xn0", [128, D_], BF16); XN0 = Buf("f_xn0")
    xn = [xn0, xn0]; XN = [XN0, XN0]
    tp = C.ps("f_tp", [128, D_], BF16); TP = PB("f_tp")
    hT = C.sb("f_hT", [128, 16, 512], BF16); HT = Buf("f_hT")
    aT = C.sb("f_aT", [128, 44, 512], BF16); AT = Buf("f_aT")
    wg = [C.sb("f_wg%d" % i, [128, 16, 256], BF16) for i in range(2)]; WG = [Buf("f_wg%d" % i) for i in range(2)]
    wu = [C.sb("f_wu%d" % i, [128, 16, 256], BF16) for i in range(2)]; WU = [Buf("f_wu%d" % i) for i in range(2)]
    wd = [C.sb("f_wd%d" % i, [128, 4, 512], BF16) for i in range(2)]; WDB = [Buf("f_wd%d" % i) for i in range(2)]
    pg = C.ps("f_pg", [128, 512]); PG = PB("f_pg")
    pu = C.ps("f_pu", [128, 512]); PU = PB("f_pu")
    pm = [C.ps("f_pm%d" % i, [128, 512]) for i in range(4)]; PM = [PB("f_pm%d" % i) for i in range(4)]
    sg = [C.sb("f_sg%d" % i, [128, 512], F32) for i in range(2)]; SG = [Buf("f_sg%d" % i) for i in range(2)]
    rep = C.sb("f_rep", [128, D_], F32); REP = Buf("f_rep")
    fst = [C.sb("f_fst%d" % i, [128, D_], F32) for i in range(4)]; FST = [Buf("f_fst%d" % i) for i in range(4)]
    tmp = fst[0]; TMP = FST[0]
    ssp = [C.sb("f_ssp%d" % i, [128, 8], F32) for i in range(4)]; SSP = [Buf("f_ssp%d" % i) for i in range(4)]
    xr = xt; XRB = XT
    A = K["vec"][:, l, 3, :]; Sv = K["vec"][:, l, 4, :]
    VR = [(C.MODB, (l, 1, 0)), (C.MODB, (l, 1, 1))]
    replicate_cols(C, rep, REP, K["vec"][:, l, 5, :], 16, [(C.MODB, (l, 1, 2))], pg, PG, tmp, TMP)
    wgv = D["WG"][l].rearrange("(kc p) n -> p kc n", p=128)
    wuv = D["WU"][l].rearrange("(kc p) n -> p kc n", p=128)
    wdv = D["WD"][l].rearrange("(f p) n -> p f n", p=128)
    n_w = 0
    n_d = 0
    n_s = 0
    for blk in range(8):
        for ti in range(4):
            t = blk * 4 + ti
            s = t % 2
            norm_to_hT(C, t, ti, xsrc, B["XR"], xt[s], XT[s], ss[s], rs[s], SS[s], xn[s], XN[s], junk, JK, tp, TP, hT, HT, A, Sv, VR)
        for fs in range(22):
            wi = n_w % 2; n_w += 1
            P.dma("sp", wg[wi][:], wgv[:, :, fs * 256:(fs + 1) * 256], reads=[(B["WG"], None)], writes=[(WG[wi], None)], sembuf=WG[wi])
            P.dma("sp", wu[wi][:], wuv[:, :, fs * 256:(fs + 1) * 256], reads=[(B["WU"], None)], writes=[(WU[wi], None)], sembuf=WU[wi])
            for j in range(2):
                f = fs * 2 + j
                if f % 2 == 0:
                    pgb, PGB, pub, PUB = pg, PG, pu, PU
                else:
                    pgb, PGB, pub, PUB = pm[0], PM[0], pm[1], PM[1]
                for kc in range(16):
                    MM(P, pgb[:], wg[wi][:, kc, j * 128:(j + 1) * 128], hT[:, kc, :], kc == 0, kc == 15, [(WG[wi], None), (HT, None)], [(PGB, None)])
                for kc in range(16):
                    MM(P, pub[:], wu[wi][:, kc, j * 128:(j + 1) * 128], hT[:, kc, :], kc == 0, kc == 15, [(WU[wi], None), (HT, None)], [(PUB, None)])
                si = n_s % 2; n_s += 1
                ACTF(P, sg[si][:], pgb[:], AF.Silu, [(PGB, None)], [(SG[si], None)])
                TT(P, "dve", aT[:, f, :], sg[si][:], pub[:], ALU.mult, [(SG[si], None), (PUB, None)], [(AT, f)])
        for n in range(4):
            for f0 in range(0, 44, 4):
                di = n_d % 2; n_d += 1
                P.dma("sp", wd[di][:], wdv[:, f0:f0 + 4, n * 512:(n + 1) * 512], reads=[(B["WD"], None)], writes=[(WDB[di], None)], sembuf=WDB[di])
                for fj in range(4):
                    f = f0 + fj
                    for ti in range(4):
                        MM(P, pm[ti][:], aT[:, f, ti * 128:(ti + 1) * 128], wd[di][:, fj, :], f == 0, f == 43,
                           [(AT, None), (WDB[di], None)], [(PM[ti], None)])
            for ti in range(4):
                cs = slice(n * 512, (n + 1) * 512)
                CP(P, "dve" if ti % 2 == 0 else "act", fst[ti][:, cs], pm[ti][:], [(PM[ti], None)], [(FST[ti], n)])
                ACTF(P, junk[:, cs], fst[ti][:, cs], AF.Square, [(FST[ti], n)], [(JK, n), (SSP[ti], n)], accum=ssp[ti][:, n:n + 1])
        for ti in range(4):
            t = blk * 4 + ti
            s = t % 2
            sq_, SQ_ = ssp[ti], SSP[ti]
            TT(P, "dve", sq_[:, 4:5], sq_[:, 0:1], sq_[:, 1:2], ALU.add, [(SQ_, 0), (SQ_, 1)], [(SQ_, "a")])
            TT(P, "dve", sq_[:, 5:6], sq_[:, 2:3], sq_[:, 3:4], ALU.add, [(SQ_, 2), (SQ_, 3)], [(SQ_, "b")])
            TT(P, "dve", sq_[:, 6:7], sq_[:, 4:5], sq_[:, 5:6], ALU.add, [(SQ_, "a"), (SQ_, "b")], [(SQ_, "c")])
            ACTF(P, sq_[:, 7:8], sq_[:, 6:7], AF.Sqrt, [(SQ_, "c")], [(SQ_, "r")], bias=EPS, scale=1.0 / D_)
            RECIP(P, sq_[:, 7:8], sq_[:, 7:8], [(SQ_, "r")], [(SQ_, "r")])
            STT(P, fst[ti][:], fst[ti][:], sq_[:, 7:8], rep[:], ALU.mult, ALU.mult, [(FST[ti], None), (SQ_, "r"), (REP, None)], [(FST[ti], None)])
            P.dma("sp", xr[s][:], xsrc[t * 128:(t + 1) * 128, :], reads=[(B["XR"], t)], writes=[(XRB[s], None)], sembuf=XRB[s])
            TT(P, "pool", xr[s][:], xr[s][:], fst[ti][:], ALU.add, [(XRB[s], None), (FST[ti], None)], [(XRB[s], None)])
            P.dma("pool", xdst[t * 128:(t + 1) * 128, :], xr[s][:], reads=[(XRB[s], None)], writes=[(DSTB, t)], sembuf=XRB[s])


def alibi_slopes(n):
    def pow2(m):
        start = 2.0 ** (-8.0 / m)
        return [start ** (i + 1) for i in range(m)]
    if math.log2(n).is_integer():
        s = pow2(n)
    else:
        p = 2 ** math.floor(math.log2(n))
        s = pow2(p) + pow2(2 * p)[0::2][: n - p]
    return np.array(s, dtype=np.float32)


def host_constants():
    c = {}
    c["c_ident"] = np.eye(128, dtype=np.float32)
    c["c_ones"] = np.ones((128, 128), np.float32)
    k = np.arange(128)[:, None]; m = np.arange(128)[None, :]
    c["c_U"] = (k <= m).astype(np.float32)
    c["c_L"] = (k >= m).astype(np.float32)
    R = np.zeros((64, 64), np.float32)
    for mm in range(32):
        R[mm + 32, mm] = -1.0
        R[mm, mm + 32] = 1.0
    c["c_R"] = R
    sl = alibi_slopes(12)
    E = np.zeros((128, 12, 3, 128), np.float32)
    kk = np.arange(128)[:, None]; qq = np.arange(128)[None, :]
    for d in range(3):
        dist = np.abs(qq - kk - (d - 1) * 128).astype(np.float32)
        for h in range(12):
            E[:, h, d, :] = np.where(dist <= 128, np.exp(-sl[h] * dist), 0.0)
    c["c_E"] = E.reshape(128, 12 * 384)
    inv = (1.0 / (np.float32(10000.0) ** (np.arange(0, 64, 2, dtype=np.float32) / np.float32(64)))).astype(np.float32)
    c["c_invf"] = np.concatenate([inv, inv])[:, None].astype(np.float32)
    return c


def colT(v, n):
    return np.ascontiguousarray(np.asarray(v, np.float32).reshape(n, 128).T)


def host_layout(inp):
    g = lambda k: np.asarray(inp[k])
    w_in = g("w_in")
    fm = np.zeros((L_, D_, NFM * 128), np.float32)
    def put(ch, cols):
        fm[:, :, ch * 128:ch * 128 + len(cols)] = w_in[:, :, cols]
    for c in range(6):
        put(FM_AQ + c, np.arange(c * 128, (c + 1) * 128))
    for kv in range(4):
        cols = np.arange(768 + kv * 64, 768 + (kv + 1) * 64)
        put(FM_AK + kv, np.concatenate([cols, cols]))
    for c in range(4):
        put(FM_BCQ + c, np.arange(1280 + c * 128, min(1280 + (c + 1) * 128, 1728)))
    put(FM_BCKV, np.arange(1728, 1856))
    put(FM_BKR, np.arange(1856, 1920))
    for h in range(4):
        put(FM_CQ + h, np.arange(1920 + h * 96, 1920 + (h + 1) * 96))
        put(FM_CK + h, np.arange(2304 + h * 96, 2304 + (h + 1) * 96))
    tm = np.ascontiguousarray(np.concatenate([w_in[:, :, 1024:1280], w_in[:, :, 2688:3456], w_in[:, :, 3472:4240], w_in[:, :, 3456:3472]], axis=2))
    shared = {
        "mod_w": g("mod_w"),
        "mod_bT": np.stack([colT(g("mod_b")[l], 96) for l in range(L_)]),
        "w_in_fm": fm, "w_in_tm": tm,
        "attn_sink": g("attn_sink")[:, None, :],
        "w_uq": np.concatenate([g("mla_w_uq"), np.zeros((L_, 64, 768), np.float32)], axis=1),
        "w_ukv": g("mla_w_ukv"),
        "gate_b": g("mlstm_gate_b")[:, None, :],
        "head_g": g("mlstm_head_g")[:, None, :],
        "w_out": g("w_out"), "w_gate": g("ffn_w_gate"), "w_up": g("ffn_w_up"), "w_down": g("ffn_w_down"),
    }
    for k, src in (("pre_mix_gT", "pre_mix_g"), ("post_mix_gT", "post_mix_g"), ("pre_ffn_gT", "pre_ffn_g"), ("post_ffn_gT", "post_ffn_g")):
        shared[k] = np.stack([colT(g(src)[l], 16) for l in range(L_)])
    qg = np.concatenate([g("mla_q_norm_g"), np.zeros((L_, 64), np.float32)], axis=1)
    shared["q_norm_gT"] = np.stack([colT(qg[l], 4) for l in range(L_)])
    shared["kv_norm_gT"] = np.stack([colT(g("mla_kv_norm_g")[l], 1) for l in range(L_)])
    shared.update(host_constants())
    x, c, pos = g("x"), g("c"), g("positions")
    maps = []
    for b in range(8):
        m = dict(shared)
        m["x"] = np.ascontiguousarray(x[b])
        m["cT"] = colT(c[b], 16)
        m["pos"] = np.ascontiguousarray(pos[b][None, :].astype(np.int32))
        maps.append(m)
    return maps


_CACHE = {}


def kernel(**inputs):
    if "nc" not in _CACHE:
        _CACHE["nc"] = build_program()[0]
    nc = _CACHE["nc"]
    maps = host_layout(inputs)
    res = run_bass_kernel_spmd(nc, maps, core_ids=list(range(8)))
    return np.stack([np.asarray(r["out"], dtype=np.float32) for r in res.results], axis=0)
```

```python
import numpy as np
import concourse.bass as bass
import concourse.mybir as mybir
from concourse.bass_utils import run_bass_kernel_spmd
F32 = mybir.dt.float32
BF16 = mybir.dt.bfloat16
I32 = mybir.dt.int32
AF = mybir.ActivationFunctionType
ALU = mybir.AluOpType
AX = mybir.AxisListType


class Buf:
    __slots__ = ("name", "st", "sem", "excl")

    def __init__(self, name, excl=False):
        self.name = name
        self.st = {}
        self.sem = None
        self.excl = excl


def PB(name):
    return Buf(name, excl=True)


class Op:
    __slots__ = ("eng", "fn", "idx", "waits", "is_dma", "needs_inc", "clock", "sem", "semval")

    def __init__(self, eng, fn, is_dma):
        self.eng = eng
        self.fn = fn
        self.is_dma = is_dma
        self.waits = []
        self.needs_inc = False
        self.clock = None
        self.sem = None
        self.semval = None


class Prog:
    ENGS = ("sp", "act", "dve", "pool", "pe")

    def __init__(self, nc):
        self.nc = nc
        self.ops = []
        self.known_idx = {}
        self.known_dma = {}
        self.dma_issued = {}
        self.sems = {}
        self.ctx = []
        self.free_pool = {}
        self.npool = 0
        self.phase_bufs = []
        self.phase_start = 0

    def _sem(self, key):
        s = self.sems.get(key)
        if s is None:
            cm = self.nc.semaphore("s_" + str(key))
            s = cm.__enter__()
            self.ctx.append(cm)
            self.sems[key] = s
        return s

    @staticmethod
    def _states(buf, key, create):
        st = buf.st
        if key is None:
            if create and None not in st:
                st[None] = {"w": {}, "r": {}}
            return list(st.values())
        out = []
        if None in st:
            out.append(st[None])
        if key not in st and create:
            st[key] = {"w": {}, "r": {}}
        if key in st:
            out.append(st[key])
        return out

    def _record(self, op, reads, writes):
        op.idx = len(self.ops)
        self.ops.append(op)
        ex = [rk for rk in reads if rk[0].excl]
        if ex:
            reads = [rk for rk in reads if not rk[0].excl]
            writes = list(writes) + [rk for rk in ex if rk not in writes]
        deps = []
        for (buf, key) in reads:
            for s in self._states(buf, key, True):
                deps += [(p, "RAW") for p in s["w"].values()]
        for (buf, key) in writes:
            for s in self._states(buf, key, True):
                deps += [(p, "WAW") for p in s["w"].values()]
                deps += [(p, "WAR") for p in s["r"].values()]
        for (p, kind) in deps:
            if p is op or p.idx < self.phase_start:
                continue
            if p.is_dma:
                val = self.dma_issued[p.sem]
                if op.is_dma and op.sem == p.sem:
                    val -= 16
                k = (op.eng, p.sem)
                if self.known_dma.get(k, 0) >= val:
                    continue
                self.known_dma[k] = val
                op.waits.append(("sem", p.sem, val))
            else:
                if p.eng == op.eng:
                    if op.eng == "pe":
                        continue
                k = (op.eng, p.eng)
                if self.known_idx.get(k, -1) >= p.idx:
                    continue
                self.known_idx[k] = p.idx
                p.needs_inc = True
                op.waits.append(("op", p))
        clk = ("dma", op.sem) if op.is_dma else op.eng
        for (buf, key) in reads:
            if key is None:
                if None not in buf.st:
                    buf.st[None] = {"w": {}, "r": {}}
                buf.st[None]["r"][clk] = op
            else:
                buf.st[key]["r"][clk] = op
        for (buf, key) in writes:
            if key is None:
                buf.st.clear()
                buf.st[None] = {"w": {clk: op}, "r": {}}
            else:
                buf.st[key] = {"w": {clk: op}, "r": {}}

    def op(self, eng, fn, reads=(), writes=()):
        o = Op(eng, fn, False)
        self._record(o, reads, writes)
        return o

    def dma(self, queue, out_ap, in_ap, reads=(), writes=(), sembuf=None, **kw):
        assert sembuf is not None
        qt = "sw" if queue == "pool" else "hw"
        if sembuf.sem is None:
            sembuf.sem = {}
            self.phase_bufs.append(sembuf)
        if qt not in sembuf.sem:
            fp = self.free_pool.setdefault(qt, [])
            if fp:
                sembuf.sem[qt] = fp.pop()
            else:
                sembuf.sem[qt] = "%s_%d" % (qt, self.npool)
                self.npool += 1
        o = Op(queue, lambda e: e.dma_start(out=out_ap, in_=in_ap, **kw), True)
        o.sem = sembuf.sem[qt]
        self.dma_issued[o.sem] = self.dma_issued.get(o.sem, 0) + 16
        o.semval = self.dma_issued[o.sem]
        self._record(o, reads, writes)
        return o

    def sb(self, name, shape, dtype):
        cm = self.nc.sbuf_tensor(name, list(shape), dtype)
        t = cm.__enter__()
        self.ctx.append(cm)
        return t

    def ps(self, name, shape, dtype):
        cm = self.nc.psum_tensor(name, list(shape), dtype)
        t = cm.__enter__()
        self.ctx.append(cm)
        return t

    def emit_phase(self):
        start = getattr(self, "_emitted", 0)
        ops = self.ops[start:]
        self._emitted = len(self.ops)
        if not hasattr(self, "cnt"):
            self.cnt = {e: 0 for e in self.ENGS}
            self.tot_stats = {e: 0 for e in self.ENGS}
            self.nwaits = 0
        nc = self.nc
        cnt = self.cnt
        per = {e: [o for o in ops if o.eng == e] for e in self.ENGS}
        for e in self.ENGS:
            for o in reversed(per[e]):
                if not o.is_dma:
                    o.needs_inc = True
                    break
        for o in ops:
            if (not o.is_dma) and o.needs_inc:
                cnt[o.eng] += 1
                o.clock = cnt[o.eng]
        for e in self.ENGS:
            self._sem("eng_" + e)
            self.tot_stats[e] += len(per[e])
        for o in ops:
            if o.is_dma:
                self._sem(o.sem)
            self.nwaits += len(o.waits)

        def run(e, name):
            for o in per[name]:
                for w in o.waits:
                    if w[0] == "sem":
                        e.wait_ge(self.sems[w[1]], w[2])
                    else:
                        p = w[1]
                        e.wait_ge(self.sems["eng_" + p.eng], p.clock)
                ins = o.fn(e)
                if o.is_dma:
                    ins.then_inc(self.sems[o.sem], 16)
                elif o.needs_inc:
                    ins.then_inc(self.sems["eng_" + o.eng], 1)
            for sem, tot in self.dma_issued.items():
                if sem in self.sems:
                    e.wait_ge(self.sems[sem], tot)
            for en in self.ENGS:
                if en != name and cnt[en] > 0:
                    e.wait_ge(self.sems["eng_" + en], cnt[en])

        with nc.Block() as block:
            @block.sync
            def _(e):
                run(e, "sp")

            @block.scalar
            def _(e):
                run(e, "act")

            @block.vector
            def _(e):
                run(e, "dve")

            @block.gpsimd
            def _(e):
                run(e, "pool")

            @block.tensor
            def _(e):
                run(e, "pe")
        for b in self.phase_bufs:
            for qt, sm in b.sem.items():
                self.free_pool.setdefault(qt, []).append(sm)
            b.sem = None
        self.phase_bufs = []
        self.phase_start = len(self.ops)
        last = len(self.ops)
        for a in self.ENGS:
            for b in self.ENGS:
                self.known_idx[(a, b)] = last - 1
            for sem, tot in self.dma_issued.items():
                self.known_dma[(a, sem)] = tot

    def finish(self):
        self.stats = dict(self.tot_stats)
        self.stats["waits"] = self.nwaits
        self.stats["clock"] = dict(self.cnt)
        self.stats["maxdma"] = max(self.dma_issued.values()) if self.dma_issued else 0
        self.stats["nsem"] = len(self.sems)

    def emit(self, final_wait_bufs=()):
        nc = self.nc
        cnt = {e: 0 for e in self.ENGS}
        for o in self.ops:
            if (not o.is_dma) and o.needs_inc:
                cnt[o.eng] += 1
                o.clock = cnt[o.eng]
        per = {e: [o for o in self.ops if o.eng == e] for e in self.ENGS}
        for e in self.ENGS:
            self._sem("eng_" + e)
        for o in self.ops:
            if o.is_dma:
                self._sem(o.sem)
        self.stats = {e: len(per[e]) for e in self.ENGS}
        self.stats["waits"] = sum(len(o.waits) for o in self.ops)
        self.stats["maxclock"] = dict(cnt)
        self.stats["maxdma"] = max(self.dma_issued.values()) if self.dma_issued else 0
        self.stats["nsem"] = len(self.sems)

        def run(e, name):
            for o in per[name]:
                for w in o.waits:
                    if w[0] == "sem":
                        e.wait_ge(self.sems[w[1]], w[2])
                    else:
                        p = w[1]
                        e.wait_ge(self.sems["eng_" + p.eng], p.clock)
                ins = o.fn(e)
                if o.is_dma:
                    ins.then_inc(self.sems[o.sem], 16)
                elif o.needs_inc:
                    ins.then_inc(self.sems["eng_" + o.eng], 1)
            if name == "sp":
                for sem, tot in self.dma_issued.items():
                    e.wait_ge(self.sems[sem], tot)
                for en in self.ENGS:
                    if en != "sp" and cnt[en] > 0:
                        e.wait_ge(self.sems["eng_" + en], cnt[en])

        with nc.Block() as block:
            @block.sync
            def _(e):
                run(e, "sp")

            @block.scalar
            def _(e):
                run(e, "act")

            @block.vector
            def _(e):
                run(e, "dve")

            @block.gpsimd
            def _(e):
                run(e, "pool")

            @block.tensor
            def _(e):
                run(e, "pe")

    def close(self):
        for cm in reversed(self.ctx):
            cm.__exit__(None, None, None)
        self.ctx = []


import math

S_ = 4096
D_ = 2048
NT = 32
DFF = 5632
EPS = 1e-6
L_ = 2
NFM = 24
NTM = 1808
FM_AQ, FM_AK, FM_BCQ, FM_BCKV, FM_BKR, FM_CQ, FM_CK = 0, 6, 10, 14, 15, 16, 20


def MM(P, out, lhsT, rhs, start, stop, R, W):
    return P.op("pe", lambda e: e.matmul(out, lhsT=lhsT, rhs=rhs, start=start, stop=stop), R, W)


def TR(P, out, in_, ident, R, W):
    return P.op("pe", lambda e: e.transpose(out, in_, ident), R, W)


def ACTF(P, out, in_, func, R, W, bias=None, scale=None, accum=None):
    kw = {}
    if bias is not None:
        kw["bias"] = bias
    if scale is not None:
        kw["scale"] = scale
    if accum is not None:
        kw["accum_out"] = accum
    return P.op("act", lambda e: e.activation(out=out, in_=in_, func=func, **kw), R, W)


def TS(P, eng, out, in0, s1, s2, op0, op1, R, W):
    if op1 is None:
        return P.op(eng, lambda e: e.tensor_scalar(out=out, in0=in0, scalar1=s1, scalar2=None, op0=op0), R, W)
    return P.op(eng, lambda e: e.tensor_scalar(out=out, in0=in0, scalar1=s1, scalar2=s2, op0=op0, op1=op1), R, W)


def TT(P, eng, out, in0, in1, op, R, W):
    return P.op(eng, lambda e: e.tensor_tensor(out=out, in0=in0, in1=in1, op=op), R, W)


def STT(P, out, in0, scalar, in1, op0, op1, R, W):
    return P.op("dve", lambda e: e.scalar_tensor_tensor(out=out, in0=in0, scalar=scalar, in1=in1, op0=op0, op1=op1), R, W)


def CP(P, eng, out, in_, R, W):
    if eng == "act":
        return P.op("act", lambda e: e.activation(out=out, in_=in_, func=AF.Copy), R, W)
    return P.op(eng, lambda e: e.tensor_copy(out=out, in_=in_), R, W)


def RECIP(P, out, in_, R, W):
    return P.op("dve", lambda e: e.reciprocal(out=out, in_=in_), R, W)


def MEMSET(P, eng, ap, val, R, W):
    return P.op(eng, lambda e: e.memset(ap, val), R, W)


class Ctx:
    pass


KNOB = {}


def build_program(dbg=False, phases=None, nlayers=L_):
    nc = bass.Bass("TRN2", target_bir_lowering=False)
    P = Prog(nc)
    C = Ctx()
    C.nc, C.P, C.dbg = nc, P, dbg

    def din(name, shape, dt=F32):
        return nc.dram_tensor(name, list(shape), dt, kind="ExternalInput").ap()

    def dscr(name, shape, dt):
        return nc.dram_tensor(name, list(shape), dt, kind=("ExternalOutput" if (dbg and name in dbg) else "Internal")).ap()

    I = C.I = {}
    I["x"] = din("x", [S_, D_])
    I["cT"] = din("cT", [128, 16])
    I["pos"] = din("pos", [1, S_], I32)
    I["mod_w"] = din("mod_w", [L_, D_, 6 * D_])
    I["mod_bT"] = din("mod_bT", [L_, 128, 96])
    for g in ("pre_mix_gT", "post_mix_gT", "pre_ffn_gT", "post_ffn_gT"):
        I[g] = din(g, [L_, 128, 16])
    I["w_in_fm"] = din("w_in_fm", [L_, D_, NFM * 128])
    I["w_in_tm"] = din("w_in_tm", [L_, D_, NTM])
    I["attn_sink"] = din("attn_sink", [L_, 1, 12])
    I["q_norm_gT"] = din("q_norm_gT", [L_, 128, 4])
    I["w_uq"] = din("w_uq", [L_, 512, 768])
    I["kv_norm_gT"] = din("kv_norm_gT", [L_, 128, 1])
    I["w_ukv"] = din("w_ukv", [L_, 128, 1024])
    I["gate_b"] = din("gate_b", [L_, 1, 16])
    I["head_g"] = din("head_g", [L_, 1, 768])
    I["w_out"] = din("w_out", [L_, D_, D_])
    I["w_gate"] = din("w_gate", [L_, D_, DFF])
    I["w_up"] = din("w_up", [L_, D_, DFF])
    I["w_down"] = din("w_down", [L_, DFF, D_])
    I["c_ident"] = din("c_ident", [128, 128])
    I["c_ones"] = din("c_ones", [128, 128])
    I["c_U"] = din("c_U", [128, 128])
    I["c_L"] = din("c_L", [128, 128])
    I["c_R"] = din("c_R", [64, 64])
    I["c_E"] = din("c_E", [128, 12 * 384])
    I["c_invf"] = din("c_invf", [64, 1])
    C.out = nc.dram_tensor("out", [S_, D_], F32, kind="ExternalOutput").ap()

    D = C.D = {}
    D["XR"] = dscr("XR", [S_, D_], F32)
    D["WFM"] = dscr("WFM", [L_, D_, NFM * 128], BF16)
    D["WTM"] = dscr("WTM", [L_, D_, NTM], BF16)
    D["WOUT"] = dscr("WOUTb", [L_, D_, D_], BF16)
    D["WG"] = dscr("WGb", [L_, D_, DFF], BF16)
    D["WU"] = dscr("WUb", [L_, D_, DFF], BF16)
    D["WD"] = dscr("WDb", [L_, DFF, D_], BF16)
    D["QKT"] = dscr("QKT", [NFM, 128, S_], BF16)
    D["PTM"] = dscr("PTM", [S_, NTM], BF16)
    D["GATES"] = dscr("GATES", [S_, 16], F32)
    D["Y"] = dscr("Y", [S_, D_], BF16)
    D["QN"] = dscr("QN", [4, 128, S_], BF16)
    D["QR"] = dscr("QR", [4, 64, S_], BF16)
    D["KN"] = dscr("KN", [4, 128, S_], BF16)
    D["KR"] = dscr("KR", [64, S_], BF16)
    D["VB"] = dscr("VB", [S_, 512], BF16)
    D["BREP"] = dscr("BREP", [8, 128, S_], F32)
    D["IBS"] = dscr("IBS", [128, NT * 8], F32)
    D["MODV"] = dscr("MODV", [128, L_ * 96], F32)
    B = C.B = {k: Buf("D_" + k) for k in D}
    B["CAST"] = Buf("CAST")

    def psb(name, shape, dt):
        cm = nc.sbuf_tensor(name, list(shape), dt)
        return cm.__enter__()

    K = C.K = {}
    K["ident"] = psb("k_ident", [128, 128], BF16)
    K["identf"] = psb("k_identf", [128, 128], F32)
    K["ones"] = psb("k_ones", [128, 128], F32)
    K["onesb"] = psb("k_onesb", [128, 128], BF16)
    K["U"] = psb("k_U", [128, 128], F32)
    K["Lm"] = psb("k_L", [128, 128], F32)
    K["R"] = psb("k_R", [64, 64], BF16)
    K["mod"] = psb("k_mod", [128, L_, 96], F32)
    K["vec"] = psb("k_vec", [128, L_, 6, 16], F32)
    KB = C.KB = Buf("KCONST")
    C.MODB = Buf("MODVEC")

    C.phase_ctx = []

    def begin():
        C.phase_ctx = []
        P.ctx_mark = len(P.ctx)

    C.uid = [0]

    def sb(name, shape, dt):
        C.uid[0] += 1
        name = "%s_u%d" % (name, C.uid[0])
        cm = nc.sbuf_tensor(name, list(shape), dt)
        t = cm.__enter__()
        C.phase_ctx.append(cm)
        return t

    def ps(name, shape, dt=F32):
        C.uid[0] += 1
        name = "%s_u%d" % (name, C.uid[0])
        esz = 4 if dt == F32 else 2
        n = 1
        for d in shape[1:]:
            n *= d
        per_bank = 2048 // esz
        nb = -(-n // per_bank)
        cm = nc.psum_tensor(name, [128, nb * per_bank], dt)
        t = cm.__enter__()
        C.phase_ctx.append(cm)
        v = t[0:shape[0], 0:n]
        if len(shape) == 3:
            v = v.rearrange("p (a b) -> p a b", b=shape[2])
        return v

    def end():
        P.emit_phase()
        for cm in reversed(C.phase_ctx):
            cm.__exit__(None, None, None)
        C.phase_ctx = []

    C.begin, C.end, C.sb, C.ps = begin, end, sb, ps

    want = (lambda n: True) if phases is None else (lambda n: n in phases)

    begin()
    phase_consts(C)
    late = nlayers > 1 and phases is None
    gc = phase_cast(C, [0], keys=(("WFM", "WTM") if late else ("WFM", "WTM", "WOUT", "WG", "WU", "WD"))) if want("cast") else iter(())
    gm = phase_mod(C, nlayers) if want("mod") else iter(())
    done_c = done_m = False
    while not (done_c and done_m):
        for _ in range(2 if late else 5):
            if next(gc, "end") == "end":
                done_c = True
                break
        if next(gm, "end") == "end":
            done_m = True
    for _ in gc:
        pass
    for _ in gm:
        pass
    end()
    for l in range(nlayers):
        src = I["x"] if l == 0 else D["XR"]
        if want("inproj"):
            begin(); phase_inproj(C, l, src); end()
        if want("win"):
            begin(); phase_window(C, l); end()
        if want("mla"):
            begin(); phase_mla_prep(C, l); end()
            bg = phase_cast(C, list(range(1, nlayers)), engs=("dve", "dve", "dve", "dve")) if (l == 0 and nlayers > 1 and want("cast")) else None
            begin(); phase_mla_attn(C, l, bg); end()
        if want("mlstm"):
            begin(); phase_mlstm_prep(C, l); end()
            bg2 = phase_cast(C, [0], engs=("pool", "pool", "pool", "pool"), keys=("WOUT", "WG", "WU", "WD")) if (l == 0 and late) else None
            begin(); phase_mlstm_attn(C, l, bg2); end()
        if want("outproj"):
            begin(); phase_outproj(C, l, src, D["XR"]); end()
        if want("ffn"):
            dst = C.out if l == nlayers - 1 else D["XR"]
            begin(); phase_ffn(C, l, D["XR"], dst); end()
    P.finish()
    P.close()
    return nc, P


def phase_consts(C):
    P, I, K, KB = C.P, C.I, C.K, C.KB
    tmp = C.sb("c_tmp", [128, 128], F32); T = Buf("c_tmp")
    tmpR = C.sb("c_tmpR", [64, 64], F32); TRb = Buf("c_tmpR")
    P.dma("sp", K["identf"][:], I["c_ident"], writes=[(KB, "identf")], sembuf=KB)
    P.dma("sp", K["ones"][:], I["c_ones"], writes=[(KB, "ones")], sembuf=KB)
    P.dma("sp", K["U"][:], I["c_U"], writes=[(KB, "U")], sembuf=KB)
    P.dma("sp", K["Lm"][:], I["c_L"], writes=[(KB, "L")], sembuf=KB)
    P.dma("sp", tmpR[:], I["c_R"], writes=[(TRb, None)], sembuf=TRb)
    CP(P, "dve", K["ident"][:], K["identf"][:], [(KB, "identf")], [(KB, "ident")])
    CP(P, "dve", K["onesb"][:], K["ones"][:], [(KB, "ones")], [(KB, "onesb")])
    CP(P, "dve", K["R"][:], tmpR[:], [(TRb, None)], [(KB, "R")])


def phase_cast(C, layers, engs=("act", "dve", "act", "dve"), keys=("WFM", "WTM", "WOUT", "WG", "WU", "WD")):
    P, I, D, B = C.P, C.I, C.D, C.B
    NB = 4
    sf = [C.sb("c_sf%d" % i, [128, 2048], F32) for i in range(NB)]; SF = [Buf("c_sf%d" % i) for i in range(NB)]
    sb_ = [C.sb("c_sb%d" % i, [128, 2048], BF16) for i in range(NB)]; SBB = [Buf("c_sb%d" % i) for i in range(NB)]
    cnt = [0]

    def cast(dst, src, rows, cols, key):
        sv = src.rearrange("(r p) n -> p r n", p=128)
        dv = dst.rearrange("(r p) n -> p r n", p=128)
        for r in range(rows // 128):
            for c0 in range(0, cols, 2048):
                w = min(2048, cols - c0)
                i = cnt[0] % NB; cnt[0] += 1
                P.dma("sp", sf[i][:, 0:w], sv[:, r, c0:c0 + w], writes=[(SF[i], None)], sembuf=SF[i])
                CP(P, engs[i], sb_[i][:, 0:w], sf[i][:, 0:w], [(SF[i], None)], [(SBB[i], None)])
                P.dma("pool", dv[:, r, c0:c0 + w], sb_[i][:, 0:w], reads=[(SBB[i], None)], writes=[(B[key], ("cast", id(dst), r, c0))], sembuf=SBB[i])
                yield
    spec = (("WFM", "w_in_fm", D_, NFM * 128), ("WTM", "w_in_tm", D_, NTM), ("WOUT", "w_out", D_, D_),
            ("WG", "w_gate", D_, DFF), ("WU", "w_up", D_, DFF), ("WD", "w_down", DFF, D_))
    for l in layers:
        for (key, src, rows, cols) in spec:
            if key in keys:
                yield from cast(D[key][l], I[src][l], rows, cols, key)


def phase_mod(C, nlayers):
    P, I, K = C.P, C.I, C.K
    cT = C.sb("m_cT", [128, 16], F32); CT = Buf("m_cT")
    sc = C.sb("m_silu", [128, 16, 2], F32); SC = Buf("m_silu")
    ws = [C.sb("m_w%d" % i, [128, 16, 512], F32) for i in range(2)]
    WS = [Buf("m_w%d" % i) for i in range(2)]
    mb = C.sb("m_b", [128, 96], F32); MB = Buf("m_b")
    gT = C.sb("m_g", [128, 4, 16], F32); GT = Buf("m_g")
    mps = C.ps("m_ps", [128, 96, 2]); MPS = PB("m_ps")
    MODB = C.MODB
    P.dma("sp", cT[:], I["cT"], writes=[(CT, None)], sembuf=CT)
    ACTF(P, sc[:, :, 0], cT[:], AF.Silu, [(CT, None)], [(SC, 0)])
    ACTF(P, sc[:, :, 1], cT[:], AF.Silu, [(CT, None)], [(SC, 1)])
    for l in range(nlayers):
        wv = I["mod_w"][l].rearrange("(kc p) n -> p kc n", p=128)
        P.dma("sp", mb[:], I["mod_bT"][l], writes=[(MB, None)], sembuf=MB)
        for gi, g in enumerate(("pre_mix_gT", "post_mix_gT", "pre_ffn_gT", "post_ffn_gT")):
            P.dma("sp", gT[:, gi, :], I[g][l], writes=[(GT, gi)], sembuf=GT)
        for s in range(24):
            w = ws[s % 2]
            P.dma("sp", w[:], wv[:, :, s * 512:(s + 1) * 512], writes=[(WS[s % 2], None)], sembuf=WS[s % 2])
            for j in range(4):
                col = s * 4 + j
                for kc in range(16):
                    MM(P, mps[:, col, :], w[:, kc, j * 128:(j + 1) * 128], sc[:, kc, :], kc == 0, kc == 15,
                       [(WS[s % 2], None), (SC, None)], [(MPS, None)])
            yield
        mod = K["mod"][:, l, :]
        TT(P, "dve", mod, mps[:, :, 0], mb[:], ALU.add, [(MPS, None), (MB, None)], [(MODB, (l, "mod"))])
        if C.dbg and "MODV" in C.dbg:
            P.dma("pool", C.D["MODV"][:, l * 96:(l + 1) * 96], mod, reads=[(MODB, (l, "mod"))], writes=[(C.B["MODV"], l)], sembuf=MB)
        vec = K["vec"]
        for half, (gpre, gpost) in enumerate(((0, 1), (2, 3))):
            o = half * 48
            STT(P, vec[:, l, half * 3 + 0, :], mod[:, o + 16:o + 32], 1.0, gT[:, gpre, :], ALU.add, ALU.mult,
                [(MODB, (l, "mod")), (GT, gpre)], [(MODB, (l, half, 0))])
            CP(P, "dve", vec[:, l, half * 3 + 1, :], mod[:, o:o + 16], [(MODB, (l, "mod"))], [(MODB, (l, half, 1))])
            TT(P, "dve", vec[:, l, half * 3 + 2, :], mod[:, o + 32:o + 48], gT[:, gpost, :], ALU.mult,
               [(MODB, (l, "mod")), (GT, gpost)], [(MODB, (l, half, 2))])


def replicate_cols(C, rep, REP, colvec, ncols, R, psb, PSB, tmp, TMP):
    P, K, KB = C.P, C.K, C.KB
    for c0 in range(0, ncols, 4):
        n = min(4, ncols - c0)
        for j in range(n):
            c = c0 + j
            TS(P, "dve", tmp[:, j * 128:(j + 1) * 128], K["identf"][:], colvec[:, c:c + 1], None, ALU.mult, None,
               R + [(KB, "identf")], [(TMP, j)])
            MM(P, psb[:, j * 128:(j + 1) * 128], K["ones"][:], tmp[:, j * 128:(j + 1) * 128], True, True,
               [(TMP, j), (KB, "ones")], [(PSB, None)])
        CP(P, "act", rep[:, c0 * 128:(c0 + n) * 128], psb[:, 0:n * 128], [(PSB, None)], [(REP, c0 // 4)])


def norm_to_hT(C, t, ti, src_ap, SRCB, xt, XT, ss, rs, SS, xn, XN, junk, JK, tp, TP, hTb, HTB, A, Sv, VR, keep_x=None):
    P, K, KB = C.P, C.K, C.KB
    P.dma("sp", xt[:], src_ap[t * 128:(t + 1) * 128, :], reads=[(SRCB, t)], writes=[(XT, None)], sembuf=XT)
    ACTF(P, junk[:], xt[:], AF.Square, [(XT, None)], [(JK, None), (SS, "ss")], accum=ss[:])
    ACTF(P, rs[:], ss[:], AF.Sqrt, [(SS, "ss")], [(SS, "rs")], bias=EPS, scale=1.0 / D_)
    RECIP(P, rs[:], rs[:], [(SS, "rs")], [(SS, "rs")])
    TS(P, "dve", xn[:], xt[:], rs[:, 0:1], None, ALU.mult, None, [(XT, None), (SS, "rs")], [(XN, None)])
    for c in range(16):
        TR(P, tp[:, c * 128:(c + 1) * 128], xn[:, c * 128:(c + 1) * 128], K["ident"][:], [(XN, None), (KB, "ident")], [(TP, c // 8)])
    for c in range(16):
        o = hTb[:, c, ti * 128:(ti + 1) * 128]
        i_ = tp[:, c * 128:(c + 1) * 128]
        if c < 8:
            ACTF(P, o, i_, AF.Identity, [(TP, c // 8)] + VR, [(HTB, (ti, c))], bias=Sv[:, c:c + 1], scale=A[:, c:c + 1])
        else:
            TS(P, "dve", o, i_, A[:, c:c + 1], Sv[:, c:c + 1], ALU.mult, ALU.add, [(TP, c // 8)] + VR, [(HTB, (ti, c))])


def phase_inproj(C, l, src):
    P, I, D, B, K = C.P, C.I, C.D, C.B, C.K
    SRCB = B["XR"]
    xt = [C.sb("a_xt%d" % i, [128, D_], F32) for i in range(2)]; XT = [Buf("a_xt%d" % i) for i in range(2)]
    junk = C.sb("a_junk", [128, D_], BF16); JK = Buf("a_junk")
    ss = [C.sb("a_ss%d" % i, [128, 1], F32) for i in range(2)]
    rs = [C.sb("a_rs%d" % i, [128, 1], F32) for i in range(2)]; SS = [Buf("a_ss%d" % i) for i in range(2)]
    xn = [C.sb("a_xn%d" % i, [128, D_], BF16) for i in range(2)]; XN = [Buf("a_xn%d" % i) for i in range(2)]
    tp = [C.ps("a_tp%d" % i, [128, D_], BF16) for i in range(1)]; TP = [PB("a_tp%d" % i) for i in range(1)]
    hT = [C.sb("a_hT%d" % i, [128, 16, 512], BF16) for i in range(2)]; HT = [Buf("a_hT%d" % i) for i in range(2)]
    ws = [C.sb("a_ws%d" % i, [128, 16, 512], BF16) for i in range(2)]; WS = [Buf("a_ws%d" % i) for i in range(2)]
    pm = [C.ps("a_pm%d" % i, [128, 512]) for i in range(4)]; PM = [PB("a_pm%d" % i) for i in range(4)]
    st = [C.sb("a_st%d" % i, [128, 512], BF16) for i in range(4)]; ST = [Buf("a_st%d" % i) for i in range(4)]
    sg = [C.sb("a_sg%d" % i, [128, 16], F32) for i in range(2)]; SG = [Buf("a_sg%d" % i) for i in range(2)]
    A = K["vec"][:, l, 0, :]; Sv = K["vec"][:, l, 1, :]
    VR = [(C.MODB, (l, 0, 0)), (C.MODB, (l, 0, 1))]
    wfm = D["WFM"][l].rearrange("(kc p) n -> p kc n", p=128)
    wtm = D["WTM"][l].rearrange("(kc p) n -> p kc n", p=128)
    cnt = [0, 0, 0]

    def norm_block(blk):
        for ti in range(4):
            t = blk * 4 + ti
            s = t % 2
            norm_to_hT(C, t, ti, src, SRCB, xt[s], XT[s], ss[s], rs[s], SS[s], xn[s], XN[s], junk, JK, tp[0], TP[0],
                       hT[blk % 2], HT[blk % 2], A, Sv, VR)

    def gemm_block(blk):
        hTb, HTB = hT[blk % 2], HT[blk % 2]
        for s in range(6 if KNOB.get("fm", True) else 0):
            wi = cnt[0] % 2; cnt[0] += 1
            P.dma("sp", ws[wi][:], wfm[:, :, s * 512:(s + 1) * 512], reads=[(B["WFM"], None)], writes=[(WS[wi], None)], sembuf=WS[wi])
            for j in range(4):
                ch = s * 4 + j
                pi = cnt[1] % 4; cnt[1] += 1
                for kc in range(16):
                    MM(P, pm[pi][:], ws[wi][:, kc, j * 128:(j + 1) * 128], hTb[:, kc, :], kc == 0, kc == 15,
                       [(WS[wi], None), (HTB, None)], [(PM[pi], None)])
                CP(P, "act" if pi % 2 == 0 else "dve", st[pi][:], pm[pi][:], [(PM[pi], None)], [(ST[pi], None)])
                P.dma("pool", D["QKT"][ch][:, blk * 512:(blk + 1) * 512], st[pi][:], reads=[(ST[pi], None)],
                      writes=[(B["QKT"], (ch, blk))], sembuf=ST[pi])
        for s in range(4 if KNOB.get("tm", True) else 0):
            n0 = s * 512
            ncol = min(512, NTM - n0)
            wi = cnt[0] % 2; cnt[0] += 1
            P.dma("sp", ws[wi][:, :, 0:ncol], wtm[:, :, n0:n0 + ncol], reads=[(B["WTM"], None)], writes=[(WS[wi], None)], sembuf=WS[wi])
            for ti in range(4):
                t = blk * 4 + ti
                pi = cnt[1] % 4; cnt[1] += 1
                for kc in range(16):
                    MM(P, pm[pi][:, 0:ncol], hTb[:, kc, ti * 128:(ti + 1) * 128], ws[wi][:, kc, 0:ncol], kc == 0, kc == 15,
                       [(WS[wi], None), (HTB, None)], [(PM[pi], None)])
                CP(P, "act" if pi % 2 == 0 else "dve", st[pi][:, 0:ncol], pm[pi][:, 0:ncol], [(PM[pi], None)], [(ST[pi], None)])
                P.dma("pool", D["PTM"][t * 128:(t + 1) * 128, n0:n0 + ncol], st[pi][:, 0:ncol], reads=[(ST[pi], None)],
                      writes=[(B["PTM"], (t, s))], sembuf=ST[pi])
                if s == 3:
                    gi = cnt[2] % 2; cnt[2] += 1
                    CP(P, "dve", sg[gi][:], pm[pi][:, ncol - 16:ncol], [(PM[pi], None)], [(SG[gi], None)])
                    P.dma("pool", D["GATES"][t * 128:(t + 1) * 128, :], sg[gi][:], reads=[(SG[gi], None)],
                          writes=[(B["GATES"], t)], sembuf=SG[gi])

    nblk = KNOB.get("nblk", 8)
    norm_block(0)
    for blk in range(nblk):
        if blk + 1 < nblk:
            norm_block(blk + 1)
        if KNOB.get("gemm", True):
            gemm_block(blk)


def phase_window(C, l):
    P, I, D, B, K = C.P, C.I, C.D, C.B, C.K
    E = C.sb("w_E", [128, 12, 384], F32); EB = Buf("w_E")
    snk = C.sb("w_snk", [128, 12], F32); SK = Buf("w_snk")
    qt = [C.sb("w_q%d" % i, [128, 6, 128], BF16) for i in range(2)]; QT = [Buf("w_q%d" % i) for i in range(2)]
    kt = [C.sb("w_k%d" % i, [128, 4, 384], BF16) for i in range(2)]; KT = [Buf("w_k%d" % i) for i in range(2)]
    vt = [C.sb("w_v%d" % i, [128, 3, 4, 65], BF16) for i in range(2)]; VT = [Buf("w_v%d" % i) for i in range(2)]
    pss = [C.ps("w_ps%d" % i, [128, 512]) for i in range(2)]; PSS = [PB("w_ps%d" % i) for i in range(2)]
    acc = [C.ps("w_acc%d" % i, [128, 512]) for i in range(2)]; ACC = [PB("w_acc%d" % i) for i in range(2)]
    pe_ = [C.sb("w_pe%d" % i, [128, 384], F32) for i in range(2)]; PEB = [Buf("w_pe%d" % i) for i in range(2)]
    pT = [C.sb("w_pT%d" % i, [128, 384], BF16) for i in range(2)]; PT = [Buf("w_pT%d" % i) for i in range(2)]
    den = [C.sb("w_den%d" % i, [128, 12], F32) for i in range(2)]; DEN = [Buf("w_den%d" % i) for i in range(2)]
    ya = [C.sb("w_ya%d" % i, [128, 768], BF16) for i in range(2)]; YA = [Buf("w_ya%d" % i) for i in range(2)]
    P.dma("sp", E[:].rearrange("p h c -> p (h c)"), I["c_E"], writes=[(EB, None)], sembuf=EB)
    P.dma("sp", snk[:], I["attn_sink"][l].partition_broadcast(128), writes=[(SK, None)], sembuf=SK)
    ACTF(P, snk[:], snk[:], AF.Exp, [(SK, None)], [(SK, None)])
    for i in range(2):
        MEMSET(P, "dve", vt[i][:], 1.0, [], [(VT[i], None)])
    qk = D["QKT"].rearrange("c p t -> p c t")
    scale = 64 ** -0.5
    hc = 0
    for i in range(NT):
        s = i % 2
        j0, j1 = max(0, i - 1), min(NT - 1, i + 1)
        d0, d1 = j0 - (i - 1), j1 - (i - 1)
        P.dma("sp", qt[s][:], qk[:, FM_AQ:FM_AQ + 6, i * 128:(i + 1) * 128], reads=[(B["QKT"], None)], writes=[(QT[s], None)], sembuf=QT[s])
        P.dma("sp", kt[s][:, :, d0 * 128:(d1 + 1) * 128], qk[:, FM_AK:FM_AK + 4, j0 * 128:(j1 + 1) * 128], reads=[(B["QKT"], None)],
              writes=[(KT[s], None)], sembuf=KT[s])
        for d in range(d0, d1 + 1):
            j = i - 1 + d
            P.dma("sp", vt[s][:, d, :, 0:64], D["PTM"][j * 128:(j + 1) * 128, 0:256].rearrange("p (h d) -> p h d", d=64),
                  reads=[(B["PTM"], None)], writes=[(VT[s], None)], sembuf=VT[s])
        lo, hi = d0 * 128, (d1 + 1) * 128
        for hq in range(12):
            g = hq // 3; off = (hq % 2) * 64; c = hq // 2
            b = hc % 2; hc += 1
            for d in range(d0, d1 + 1):
                MM(P, pss[b][:, d * 128:(d + 1) * 128], kt[s][off:off + 64, g, d * 128:(d + 1) * 128], qt[s][off:off + 64, c, :], True, True,
                   [(KT[s], None), (QT[s], None)], [(PSS[b], None)])
            ACTF(P, pe_[b][:, lo:hi], pss[b][:, lo:hi], AF.Exp, [(PSS[b], None)], [(PEB[b], None)], scale=scale)
            TT(P, "dve", pT[b][:, lo:hi], pe_[b][:, lo:hi], E[:, hq, lo:hi], ALU.mult, [(PEB[b], None), (EB, None)], [(PT[b], None)])
            a = acc[hq // 6]; AB = ACC[hq // 6]
            co = (hq % 6) * 65
            for d in range(d0, d1 + 1):
                MM(P, a[:, co:co + 65], pT[b][:, d * 128:(d + 1) * 128], vt[s][:, d, g, :], d == d0, d == d1,
                   [(PT[b], None), (VT[s], None)], [(AB, None)])
        for hq in range(12):
            a = acc[hq // 6]; AB = ACC[hq // 6]; co = (hq % 6) * 65
            TS(P, "dve", den[s][:, hq:hq + 1], a[:, co + 64:co + 65], snk[:, hq:hq + 1], None, ALU.add, None, [(AB, None), (SK, None)], [(DEN[s], None)])
        RECIP(P, den[s][:], den[s][:], [(DEN[s], None)], [(DEN[s], None)])
        for hq in range(12):
            a = acc[hq // 6]; AB = ACC[hq // 6]; co = (hq % 6) * 65
            TS(P, "dve", ya[s][:, hq * 64:(hq + 1) * 64], a[:, co:co + 64], den[s][:, hq:hq + 1], None, ALU.mult, None,
               [(AB, None), (DEN[s], None)], [(YA[s], None)])
        P.dma("pool", D["Y"][i * 128:(i + 1) * 128, 0:768], ya[s][:], reads=[(YA[s], None)], writes=[(B["Y"], ("a", i))], sembuf=YA[s])


def rep_sumsq(C, sq_chunks, R, rep_ps, RPS, rstd, RSTD, n, width):
    P, K, KB = C.P, C.K, C.KB
    for i, (ap, rows) in enumerate(sq_chunks):
        MM(P, rep_ps[:, 0:width], K["onesb"][0:rows, :], ap, i == 0, i == len(sq_chunks) - 1, R + [(KB, "onesb")], [(RPS, None)])
    ACTF(P, rstd[:, 0:width], rep_ps[:, 0:width], AF.Sqrt, [(RPS, None)], [(RSTD, None)], bias=EPS, scale=1.0 / n)
    RECIP(P, rstd[:, 0:width], rstd[:, 0:width], [(RSTD, None)], [(RSTD, None)])


def phase_mla_prep(C, l):
    P, I, D, B, K, KB = C.P, C.I, C.D, C.B, C.K, C.KB
    wqf = C.sb("p_wqf", [128, 4, 768], F32); WQF = Buf("p_wqf")
    wq = C.sb("p_wq", [128, 4, 768], BF16); WQ = Buf("p_wq")
    wkf = C.sb("p_wkf", [128, 1024], F32); WKF = Buf("p_wkf")
    wk = C.sb("p_wk", [128, 1024], BF16); WK = Buf("p_wk")
    gq = C.sb("p_gq", [128, 4], F32); GQ = Buf("p_gq")
    gk = C.sb("p_gk", [128, 1], F32); GK = Buf("p_gk")
    P.dma("sp", wqf[:], I["w_uq"][l].rearrange("(kc p) n -> p kc n", p=128), writes=[(WQF, None)], sembuf=WQF)
    P.dma("sp", wkf[:], I["w_ukv"][l], writes=[(WKF, None)], sembuf=WKF)
    P.dma("sp", gq[:], I["q_norm_gT"][l], writes=[(GQ, None)], sembuf=GQ)
    P.dma("sp", gk[:], I["kv_norm_gT"][l], writes=[(GK, None)], sembuf=GK)
    for kc in range(4):
        TS(P, "dve", wq[:, kc, :], wqf[:, kc, :], gq[:, kc:kc + 1], None, ALU.mult, None, [(WQF, None), (GQ, None)], [(WQ, kc)])
    TS(P, "dve", wk[:], wkf[:], gk[:, 0:1], None, ALU.mult, None, [(WKF, None), (GK, None)], [(WK, None)])
    posi = C.sb("p_posi", [64, S_], I32); POSI = Buf("p_posi")
    ang = C.sb("p_ang", [64, S_], F32); ANG = Buf("p_ang")
    cosT = C.sb("p_cos", [64, S_], F32); COS = Buf("p_cos")
    sinT = C.sb("p_sin", [64, S_], F32); SIN = Buf("p_sin")
    invf = C.sb("p_invf", [64, 1], F32); INVF = Buf("p_invf")
    P.dma("sp", posi[:], I["pos"].partition_broadcast(64), writes=[(POSI, None)], sembuf=POSI)
    P.dma("sp", invf[:], I["c_invf"], writes=[(INVF, None)], sembuf=INVF)
    CP(P, "dve", ang[:], posi[:], [(POSI, None)], [(ANG, None)])
    TS(P, "dve", ang[:], ang[:], invf[:, 0:1], None, ALU.mult, None, [(ANG, None), (INVF, None)], [(ANG, None)])
    TWO_PI = 2.0 * math.pi
    MAGIC = 12582912.0
    TS(P, "dve", sinT[:], ang[:], 1.0 / TWO_PI, MAGIC, ALU.mult, ALU.add, [(ANG, None)], [(SIN, None)])
    TS(P, "dve", sinT[:], sinT[:], -MAGIC, None, ALU.add, None, [(SIN, None)], [(SIN, None)])
    STT(P, sinT[:], sinT[:], -TWO_PI, ang[:], ALU.mult, ALU.add, [(SIN, None), (ANG, None)], [(SIN, None)])
    TS(P, "dve", ang[:], ang[:], 0.5 * math.pi, None, ALU.add, None, [(ANG, None)], [(ANG, None)])
    TS(P, "dve", cosT[:], ang[:], 1.0 / TWO_PI, MAGIC, ALU.mult, ALU.add, [(ANG, None)], [(COS, None)])
    TS(P, "dve", cosT[:], cosT[:], -MAGIC, None, ALU.add, None, [(COS, None)], [(COS, None)])
    STT(P, cosT[:], cosT[:], -TWO_PI, ang[:], ALU.mult, ALU.add, [(COS, None), (ANG, None)], [(COS, None)])
    PI_LO = 3.1415925
    TS(P, "dve", sinT[:], sinT[:], -PI_LO, PI_LO, ALU.max, ALU.min, [(SIN, None)], [(SIN, None)])
    TS(P, "dve", cosT[:], cosT[:], -PI_LO, PI_LO, ALU.max, ALU.min, [(COS, None)], [(COS, None)])
    ACTF(P, sinT[:], sinT[:], AF.Sin, [(SIN, None)], [(SIN, None)])
    ACTF(P, cosT[:], cosT[:], AF.Sin, [(COS, None)], [(COS, None)])

    cq = [C.sb("p_cq%d" % i, [128, 4, 512], BF16) for i in range(2)]; CQ = [Buf("p_cq%d" % i) for i in range(2)]
    ckv = [C.sb("p_ckv%d" % i, [128, 512], BF16) for i in range(2)]; CKV = [Buf("p_ckv%d" % i) for i in range(2)]
    kr = [C.sb("p_kr%d" % i, [64, 512], BF16) for i in range(2)]; KRB = [Buf("p_kr%d" % i) for i in range(2)]
    sq = C.sb("p_sq", [128, 4, 512], BF16); SQ = Buf("p_sq")
    sqk = C.sb("p_sqk", [128, 512], BF16); SQK = Buf("p_sqk")
    rps = C.ps("p_rps", [128, 512]); RPS = PB("p_rps")
    rstd = C.sb("p_rstd", [128, 512], F32); RSTD = Buf("p_rstd")
    rstdk = C.sb("p_rstdk", [128, 512], F32); RSTDK = Buf("p_rstdk")
    pq = [C.ps("p_pq%d" % i, [128, 512]) for i in range(3)]; PQ = [PB("p_pq%d" % i) for i in range(3)]
    prot = C.ps("p_prot", [64, 512]); PROT = PB("p_prot")
    pv = C.ps("p_pv", [128, 512]); PV = PB("p_pv")
    ptm = C.ps("p_ptm", [128, 4, 2]); PTM_ = PB("p_ptm")
    so = [C.sb("p_so%d" % i, [128, 512], BF16) for i in range(3)]; SO = [Buf("p_so%d" % i) for i in range(3)]
    t1 = C.sb("p_t1", [64, 512], F32); T1 = Buf("p_t1")
    t2 = C.sb("p_t2", [64, 512], F32); T2 = Buf("p_t2")
    raw = C.sb("p_raw", [64, 512], BF16); RAW = Buf("p_raw")
    rtm = C.sb("p_rtm", [128, 4], F32); RTM = Buf("p_rtm")
    vo = [C.sb("p_vo%d" % i, [128, 512], BF16) for i in range(2)]; VO = [Buf("p_vo%d" % i) for i in range(2)]
    qk = D["QKT"].rearrange("c p t -> p c t")
    oc = [0]

    def rope_out(src_ps, SRC, rst, RST, blk, dst_ap, DSTB, dkey):
        cs = slice(blk * 512, (blk + 1) * 512)
        if rst is not None:
            TT(P, "dve", raw[:], src_ps, rst[0:64, :], ALU.mult, [(SRC, None), (RST, None)], [(RAW, None)])
        else:
            CP(P, "dve", raw[:], src_ps, [(SRC, None)], [(RAW, None)])
        MM(P, prot[:], K["R"][:], raw[:], True, True, [(RAW, None), (KB, "R")], [(PROT, None)])
        TT(P, "dve", t1[:], raw[:], cosT[:, cs], ALU.mult, [(RAW, None), (COS, None)], [(T1, None)])
        TT(P, "dve", t2[:], prot[:], sinT[:, cs], ALU.mult, [(PROT, None), (SIN, None)], [(T2, None)])
        o = oc[0] % 3; oc[0] += 1
        TT(P, "dve", so[o][0:64, :], t1[:], t2[:], ALU.add, [(T1, None), (T2, None)], [(SO[o], None)])
        P.dma("pool", dst_ap, so[o][0:64, :], reads=[(SO[o], None)], writes=[(DSTB, dkey)], sembuf=SO[o])

    for blk in range(8):
        s = blk % 2
        cs = slice(blk * 512, (blk + 1) * 512)
        P.dma("sp", cq[s][:], qk[:, FM_BCQ:FM_BCQ + 4, cs], reads=[(B["QKT"], None)], writes=[(CQ[s], None)], sembuf=CQ[s])
        P.dma("sp", ckv[s][:], D["QKT"][FM_BCKV][:, cs], reads=[(B["QKT"], None)], writes=[(CKV[s], None)], sembuf=CKV[s])
        P.dma("sp", kr[s][:], D["QKT"][FM_BKR][0:64, cs], reads=[(B["QKT"], None)], writes=[(KRB[s], None)], sembuf=KRB[s])
        rows = [128, 128, 128, 64]
        ACTF(P, sq[:], cq[s][:], AF.Square, [(CQ[s], None)], [(SQ, None)])
        rep_sumsq(C, [(sq[0:rows[kc], kc, :], rows[kc]) for kc in range(4)], [(SQ, None)], rps, RPS, rstd, RSTD, 448.0, 512)
        for h in range(4):
            pi = h % 3
            for kc in range(4):
                MM(P, pq[pi][:], wq[0:rows[kc], kc, h * 192:h * 192 + 128], cq[s][0:rows[kc], kc, :], kc == 0, kc == 3,
                   [(WQ, None), (CQ[s], None)], [(PQ[pi], None)])
            o = oc[0] % 3; oc[0] += 1
            TT(P, "dve", so[o][:], pq[pi][:], rstd[:], ALU.mult, [(PQ[pi], None), (RSTD, None)], [(SO[o], None)])
            P.dma("pool", D["QN"][h][:, cs], so[o][:], reads=[(SO[o], None)], writes=[(B["QN"], (h, blk))], sembuf=SO[o])
            pi = (h + 1) % 3
            for kc in range(4):
                MM(P, pq[pi][0:64, :], wq[0:rows[kc], kc, h * 192 + 128:h * 192 + 192], cq[s][0:rows[kc], kc, :], kc == 0, kc == 3,
                   [(WQ, None), (CQ[s], None)], [(PQ[pi], None)])
            rope_out(pq[pi][0:64, :], PQ[pi], rstd, RSTD, blk, D["QR"][h][:, cs], B["QR"], (h, blk))
        ACTF(P, sqk[:], ckv[s][:], AF.Square, [(CKV[s], None)], [(SQK, None)])
        rep_sumsq(C, [(sqk[:], 128)], [(SQK, None)], rps, RPS, rstdk, RSTDK, 128.0, 512)
        for h in range(4):
            pi = h % 3
            MM(P, pq[pi][:], wk[:, h * 256:h * 256 + 128], ckv[s][:], True, True, [(WK, None), (CKV[s], None)], [(PQ[pi], None)])
            o = oc[0] % 3; oc[0] += 1
            TT(P, "dve", so[o][:], pq[pi][:], rstdk[:], ALU.mult, [(PQ[pi], None), (RSTDK, None)], [(SO[o], None)])
            P.dma("pool", D["KN"][h][:, cs], so[o][:], reads=[(SO[o], None)], writes=[(B["KN"], (h, blk))], sembuf=SO[o])
        for ti in range(4):
            MM(P, ptm[:, ti, :], sqk[:, ti * 128:(ti + 1) * 128], K["onesb"][:, 0:2], True, True, [(SQK, None), (KB, "onesb")], [(PTM_, None)])
        ACTF(P, rtm[:], ptm[:, :, 0], AF.Sqrt, [(PTM_, None)], [(RTM, None)], bias=EPS, scale=1.0 / 128.0)
        RECIP(P, rtm[:], rtm[:], [(RTM, None)], [(RTM, None)])
        for ti in range(4):
            t = blk * 4 + ti
            for h in range(4):
                MM(P, pv[:, h * 128:(h + 1) * 128], ckv[s][:, ti * 128:(ti + 1) * 128], wk[:, h * 256 + 128:h * 256 + 256], True, True,
                   [(WK, None), (CKV[s], None)], [(PV, None)])
            v = t % 2
            TS(P, "dve", vo[v][:], pv[:], rtm[:, ti:ti + 1], None, ALU.mult, None, [(PV, None), (RTM, None)], [(VO[v], None)])
            P.dma("pool", D["VB"][t * 128:(t + 1) * 128, :], vo[v][:], reads=[(VO[v], None)], writes=[(B["VB"], t)], sembuf=VO[v])
        MM(P, prot[:], K["R"][:], kr[s][:], True, True, [(KRB[s], None), (KB, "R")], [(PROT, None)])
        TT(P, "dve", t1[:], kr[s][:], cosT[:, cs], ALU.mult, [(KRB[s], None), (COS, None)], [(T1, None)])
        TT(P, "dve", t2[:], prot[:], sinT[:, cs], ALU.mult, [(PROT, None), (SIN, None)], [(T2, None)])
        o = oc[0] % 3; oc[0] += 1
        TT(P, "dve", so[o][0:64, :], t1[:], t2[:], ALU.add, [(T1, None), (T2, None)], [(SO[o], None)])
        P.dma("pool", D["KR"][:, cs], so[o][0:64, :], reads=[(SO[o], None)], writes=[(B["KR"], blk)], sembuf=SO[o])


def phase_mla_attn(C, l, bg=None):
    P, I, D, B, K = C.P, C.I, C.D, C.B, C.K
    krT = C.sb("m_kr", [64, S_], BF16); KRT = Buf("m_kr")
    qn = C.sb("m_qn", [128, S_], BF16); QN = Buf("m_qn")
    qr = C.sb("m_qr", [64, S_], BF16); QR = Buf("m_qr")
    kn = C.sb("m_kn", [128, S_], BF16); KN = Buf("m_kn")
    va = C.sb("m_va", [128, NT, 129], BF16); VA = Buf("m_va")
    pss = [C.ps("m_ps%d" % i, [128, 512]) for i in range(2)]; PSS = [PB("m_ps%d" % i) for i in range(2)]
    acc = [C.ps("m_acc%d" % i, [128, 512]) for i in range(4)]; ACC = [PB("m_acc%d" % i) for i in range(4)]
    pT = [C.sb("m_pT%d" % i, [128, 512], BF16) for i in range(3)]; PT = [Buf("m_pT%d" % i) for i in range(3)]
    rd = [C.sb("m_rd%d" % i, [128, 1], F32) for i in range(2)]; RD = [Buf("m_rd%d" % i) for i in range(2)]
    yo = [C.sb("m_yo%d" % i, [128, 128], BF16) for i in range(2)]; YO = [Buf("m_yo%d" % i) for i in range(2)]
    scale = 192 ** -0.5
    P.dma("sp", krT[:], D["KR"], reads=[(B["KR"], None)], writes=[(KRT, None)], sembuf=KRT)
    MEMSET(P, "dve", va[:], 1.0, [], [(VA, "ones")])
    n = 0
    oc = 0
    for h in range(4):
        P.dma("sp", qn[:], D["QN"][h], reads=[(B["QN"], None)], writes=[(QN, None)], sembuf=QN)
        P.dma("sp", qr[:], D["QR"][h], reads=[(B["QR"], None)], writes=[(QR, None)], sembuf=QR)
        P.dma("sp", kn[:], D["KN"][h], reads=[(B["KN"], None)], writes=[(KN, None)], sembuf=KN)
        vbv = D["VB"][:, h * 128:(h + 1) * 128].rearrange("(t p) d -> p t d", p=128)
        for t0 in range(0, NT, 4):
            P.dma("sp", va[:, t0:t0 + 4, 0:128], vbv[:, t0:t0 + 4, :], reads=[(B["VB"], None), (VA, "ones")],
                  writes=[(VA, ("v", t0))], sembuf=VA)
        for qb in range(8):
            qs = slice(qb * 512, (qb + 1) * 512)
            for j in range(NT):
                ks = slice(j * 128, (j + 1) * 128)
                b = n % 2; pb = n % 3; n += 1
                if bg is not None and n % 4 == 0:
                    next(bg, None)
                MM(P, pss[b][:], kn[:, ks], qn[:, qs], True, False, [(KN, None), (QN, None)], [(PSS[b], None)])
                MM(P, pss[b][:], krT[:, ks], qr[:, qs], False, True, [(KRT, None), (QR, None)], [(PSS[b], None)])
                ACTF(P, pT[pb][:], pss[b][:], AF.Exp, [(PSS[b], None)], [(PT[pb], None)], scale=scale)
                for ii in range(4):
                    MM(P, acc[ii][:, 0:129], pT[pb][:, ii * 128:(ii + 1) * 128], va[:, j, :], j == 0, j == NT - 1,
                       [(PT[pb], None), (VA, ("v", (j // 4) * 4)), (VA, "ones")], [(ACC[ii], None)])
            for ii in range(4):
                t = qb * 4 + ii
                o = oc % 2; oc += 1
                RECIP(P, rd[o][:], acc[ii][:, 128:129], [(ACC[ii], None)], [(RD[o], None)])
                TS(P, "dve", yo[o][:], acc[ii][:, 0:128], rd[o][:, 0:1], None, ALU.mult, None, [(ACC[ii], None), (RD[o], None)], [(YO[o], None)])
                P.dma("pool", D["Y"][t * 128:(t + 1) * 128, 768 + h * 128:768 + (h + 1) * 128], yo[o][:], reads=[(YO[o], None)],
                      writes=[(B["Y"], ("b", h, t))], sembuf=YO[o])
    if bg is not None:
        for _ in bg:
            pass


def phase_mlstm_prep(C, l):
    P, I, D, B, K, KB = C.P, C.I, C.D, C.B, C.K, C.KB
    g = C.sb("g_g", [128, NT, 16], F32); G = Buf("g_g")
    gb = C.sb("g_gb", [128, 16], F32); GB = Buf("g_gb")
    lf = C.sb("g_lf", [128, NT, 8], F32); LF = Buf("g_lf")
    tot = C.sb("g_tot", [128, NT, 8], F32); TOT = Buf("g_tot")
    off = C.sb("g_off", [128, NT, 8], F32); OFF = Buf("g_off")
    cum = C.sb("g_cum", [128, NT, 8], F32); CUM = Buf("g_cum")
    ibs = C.sb("g_ibs", [128, NT, 8], F32); IBS = Buf("g_ibs")
    pt = C.ps("g_pt", [128, 256]); PTB = PB("g_pt")
    pc = C.ps("g_pc", [128, 256]); PCB = PB("g_pc")
    pc2 = C.ps("g_pc2", [128, 256]); PCB2 = PB("g_pc2")
    pr = [C.ps("g_pr%d" % i, [128, 512]) for i in range(2)]; PR = [PB("g_pr%d" % i) for i in range(2)]
    dg = [C.sb("g_dg%d" % i, [128, 512], F32) for i in range(2)]; DG = [Buf("g_dg%d" % i) for i in range(2)]
    ro = [C.sb("g_ro%d" % i, [128, 512], F32) for i in range(2)]; RO = [Buf("g_ro%d" % i) for i in range(2)]
    gv = D["GATES"].rearrange("(t p) c -> p t c", p=128)
    for t0 in range(0, NT, 4):
        P.dma("sp", g[:, t0:t0 + 4, :], gv[:, t0:t0 + 4, :], reads=[(B["GATES"], None)], writes=[(G, ("ld", t0))], sembuf=G)
    P.dma("sp", gb[:], I["gate_b"][l].partition_broadcast(128), writes=[(GB, None)], sembuf=GB)
    for t in range(NT):
        TT(P, "dve", g[:, t, :], g[:, t, :], gb[:], ALU.add, [(G, None), (GB, None)], [(G, None)])
    ACTF(P, lf[:], g[:, :, 8:16], AF.Exp, [(G, None)], [(LF, None)], scale=-1.0)
    ACTF(P, lf[:], lf[:], AF.Ln, [(LF, None)], [(LF, None)], bias=1.0)
    TS(P, "dve", lf[:], lf[:], -1.0, None, ALU.mult, None, [(LF, None)], [(LF, None)])
    lf2 = lf[:].rearrange("p t c -> p (t c)")
    MM(P, pt[:], K["ones"][:], lf2, True, True, [(LF, None), (KB, "ones")], [(PTB, None)])
    CP(P, "dve", tot[:].rearrange("p t c -> p (t c)"), pt[:], [(PTB, None)], [(TOT, None)])
    MEMSET(P, "dve", off[:], 0.0, [], [(OFF, None)])
    for t in range(1, NT):
        TT(P, "dve", off[:, t, 0:4], off[:, t - 1, 0:4], tot[:, t - 1, 0:4], ALU.add, [(OFF, None), (TOT, None)], [(OFF, None)])
    for t in range(NT - 2, -1, -1):
        TT(P, "dve", off[:, t, 4:8], off[:, t + 1, 4:8], tot[:, t + 1, 4:8], ALU.add, [(OFF, None), (TOT, None)], [(OFF, None)])
    MM(P, pc[:], K["U"][:], lf2, True, True, [(LF, None), (KB, "U")], [(PCB, None)])
    MM(P, pc2[:], K["Lm"][:], lf2, True, True, [(LF, None), (KB, "L")], [(PCB2, None)])
    pc3 = pc[:].rearrange("p (t c) -> p t c", c=8)
    pc23 = pc2[:].rearrange("p (t c) -> p t c", c=8)
    TT(P, "dve", cum[:, :, 0:4], pc3[:, :, 0:4], off[:, :, 0:4], ALU.add, [(PCB, None), (OFF, None)], [(CUM, "f")])
    TT(P, "dve", cum[:, :, 4:8], pc23[:, :, 4:8], off[:, :, 4:8], ALU.add, [(PCB2, None), (OFF, None)], [(CUM, "b")])
    TT(P, "dve", ibs[:], g[:, :, 0:8], cum[:], ALU.subtract, [(G, None), (CUM, None)], [(IBS, None)])
    P.dma("pool", D["IBS"], ibs[:].rearrange("p t c -> p (t c)"), reads=[(IBS, None)], writes=[(B["IBS"], None)], sembuf=IBS)
    n = 0
    for c in range(8):
        for t0 in range(0, NT, 4):
            b = n % 2; n += 1
            for j in range(4):
                t = t0 + j
                TS(P, "dve", dg[b][:, j * 128:(j + 1) * 128], K["identf"][:], cum[:, t, c:c + 1], None, ALU.mult, None,
                   [(CUM, None), (KB, "identf")], [(DG[b], j)])
                MM(P, pr[b][:, j * 128:(j + 1) * 128], K["ones"][:], dg[b][:, j * 128:(j + 1) * 128], True, True,
                   [(DG[b], j), (KB, "ones")], [(PR[b], None)])
            CP(P, "act", ro[b][:], pr[b][:], [(PR[b], None)], [(RO[b], None)])
            P.dma("pool", D["BREP"][c][:, t0 * 128:(t0 + 4) * 128], ro[b][:], reads=[(RO[b], None)], writes=[(B["BREP"], (c, t0))], sembuf=RO[b])


def phase_mlstm_attn(C, l, bg=None):
    P, I, D, B, K, KB = C.P, C.I, C.D, C.B, C.K, C.KB
    ibs = C.sb("s_ibs", [128, NT, 8], F32); IBS = Buf("s_ibs")
    hg = C.sb("s_hg", [128, 768], F32); HG = Buf("s_hg")
    qT = C.sb("s_qT", [96, S_], BF16); QT = Buf("s_qT")
    kT = C.sb("s_kT", [96, S_], BF16); KT = Buf("s_kT")
    va = C.sb("s_va", [128, NT, 193], BF16); VA = Buf("s_va")
    br = [C.sb("s_br%d" % i, [128, S_], F32) for i in range(2)]; BR = [Buf("s_br%d" % i) for i in range(2)]
    pss = [C.ps("s_ps%d" % i, [128, 256]) for i in range(2)]; PSS = [PB("s_ps%d" % i) for i in range(2)]
    acc = [[C.ps("s_acc%d%d" % (d, i), [128, 512]) for i in range(2)] for d in range(2)]
    ACC = [[PB("s_acc%d%d" % (d, i)) for i in range(2)] for d in range(2)]
    w = [C.sb("s_w%d" % i, [128, 256], F32) for i in range(3)]; WB = [Buf("s_w%d" % i) for i in range(3)]
    wT = [C.sb("s_wT%d" % i, [128, 256], BF16) for i in range(3)]; WT = [Buf("s_wT%d" % i) for i in range(3)]
    op_ = [C.sb("s_op%d" % i, [128, 192], BF16) for i in range(2)]; OP = [Buf("s_op%d" % i) for i in range(2)]
    dn = [C.sb("s_dn%d" % i, [128, 2], F32) for i in range(2)]; DN = [Buf("s_dn%d" % i) for i in range(2)]
    hs = [C.sb("s_hs%d" % i, [128, 192], F32) for i in range(2)]; HS = [Buf("s_hs%d" % i) for i in range(2)]
    hb = [C.sb("s_hb%d" % i, [128, 192], F32) for i in range(2)]; HB = [Buf("s_hb%d" % i) for i in range(2)]
    jk = C.sb("s_jk", [128, 192], F32); JK = Buf("s_jk")
    ssq = [C.sb("s_ssq%d" % i, [128, 1], F32) for i in range(2)]; SSQ = [Buf("s_ssq%d" % i) for i in range(2)]
    yo = [C.sb("s_yo%d" % i, [128, 192], BF16) for i in range(2)]; YO = [Buf("s_yo%d" % i) for i in range(2)]
    scale = 96 ** -0.5
    P.dma("sp", ibs[:].rearrange("p t c -> p (t c)"), D["IBS"], reads=[(B["IBS"], None)], writes=[(IBS, None)], sembuf=IBS)
    P.dma("sp", hg[:], I["head_g"][l].partition_broadcast(128), writes=[(HG, None)], sembuf=HG)
    MEMSET(P, "dve", va[:], 1.0, [], [(VA, "ones")])
    U, Lm = K["U"], K["Lm"]
    n = 0
    fc = 0
    for h in range(4):
        P.dma("sp", qT[:], D["QKT"][FM_CQ + h][0:96, :], reads=[(B["QKT"], None)], writes=[(QT, None)], sembuf=QT)
        P.dma("sp", kT[:], D["QKT"][FM_CK + h][0:96, :], reads=[(B["QKT"], None)], writes=[(KT, None)], sembuf=KT)
        cvv = D["PTM"][:, 256 + h * 192:256 + (h + 1) * 192].rearrange("(t p) d -> p t d", p=128)
        for t0 in range(0, NT, 4):
            P.dma("sp", va[:, t0:t0 + 4, 0:192], cvv[:, t0:t0 + 4, :], reads=[(B["PTM"], None), (VA, "ones")],
                  writes=[(VA, ("v", t0))], sembuf=VA)
        for d in range(2):
            P.dma("sp", br[d][:], D["BREP"][d * 4 + h], reads=[(B["BREP"], None)], writes=[(BR[d], None)], sembuf=BR[d])
        for lb in range(16):
            i0 = lb * 2
            ls = slice(lb * 256, (lb + 1) * 256)
            first = [[True, True], [True, True]]
            nexp = [[0, 0], [0, 0]]
            total = [[i0 + 1, i0 + 2], [NT - i0, NT - i0 - 1]]
            for j in range(NT):
                ks = slice(j * 128, (j + 1) * 128)
                b = n % 2; n += 1
                if bg is not None and n % 8 == 0:
                    next(bg, None)
                MM(P, pss[b][:], kT[:, ks], qT[:, ls], True, True, [(KT, None), (QT, None)], [(PSS[b], None)])
                if j < i0:
                    items = [(0, 0, 2, False)]
                elif j > i0 + 1:
                    items = [(1, 0, 2, False)]
                elif j == i0:
                    items = [(0, 0, 1, True), (1, 0, 1, True), (0, 1, 1, False)]
                else:
                    items = [(1, 0, 1, False), (0, 1, 1, True), (1, 1, 1, True)]
                for (d, a, cn, masked) in items:
                    wi = fc % 3; fc += 1
                    c = d * 4 + h
                    lsl = slice((i0 + a) * 128, (i0 + a + cn) * 128)
                    wv = w[wi][:, 0:cn * 128]
                    ACTF(P, wv, br[d][:, lsl], AF.Exp, [(BR[d], None), (IBS, None)], [(WB[wi], None)], bias=ibs[:, j, c:c + 1])
                    if masked:
                        TT(P, "dve", wv, wv, (U if d == 0 else Lm)[:], ALU.mult, [(WB[wi], None), (KB, "U"), (KB, "L")], [(WB[wi], None)])
                    STT(P, wT[wi][:, 0:cn * 128], pss[b][:, a * 128:(a + cn) * 128], scale, wv, ALU.mult, ALU.mult,
                        [(PSS[b], None), (WB[wi], None)], [(WT[wi], None)])
                    for q in range(cn):
                        ii = a + q
                        nexp[d][ii] += 1
                        MM(P, acc[d][ii][:, 0:193], wT[wi][:, q * 128:(q + 1) * 128], va[:, j, :], nexp[d][ii] == 1, nexp[d][ii] == total[d][ii],
                           [(WT[wi], None), (VA, ("v", (j // 4) * 4)), (VA, "ones")], [(ACC[d][ii], None)])
            for ii in range(2):
                t = i0 + ii
                o = t % 2
                for d in range(2):
                    a = acc[d][ii]
                    ACTF(P, dn[o][:, d:d + 1], a[:, 192:193], AF.Abs, [(ACC[d][ii], None)], [(DN[o], d)])
                TS(P, "dve", dn[o][:], dn[o][:], 1.0, None, ALU.max, None, [(DN[o], None)], [(DN[o], None)])
                RECIP(P, dn[o][:], dn[o][:], [(DN[o], None)], [(DN[o], None)])
                TS(P, "dve", hs[o][:], acc[0][ii][:, 0:192], dn[o][:, 0:1], None, ALU.mult, None, [(ACC[0][ii], None), (DN[o], None)], [(HS[o], None)])
                TS(P, "dve", hb[o][:], acc[1][ii][:, 0:192], dn[o][:, 1:2], None, ALU.mult, None, [(ACC[1][ii], None), (DN[o], None)], [(HB[o], None)])
                TT(P, "dve", hs[o][:], hs[o][:], hb[o][:], ALU.add, [(HS[o], None), (HB[o], None)], [(HS[o], None)])
                ACTF(P, jk[:], hs[o][:], AF.Square, [(HS[o], None)], [(JK, None), (SSQ[o], None)], accum=ssq[o][:])
                ACTF(P, ssq[o][:], ssq[o][:], AF.Sqrt, [(SSQ[o], None)], [(SSQ[o], None)], bias=EPS, scale=1.0 / 192.0)
                RECIP(P, ssq[o][:], ssq[o][:], [(SSQ[o], None)], [(SSQ[o], None)])
                P.dma("sp", op_[o][:], D["PTM"][t * 128:(t + 1) * 128, 1024 + h * 192:1024 + (h + 1) * 192], reads=[(B["PTM"], None)],
                      writes=[(OP[o], None)], sembuf=OP[o])
                ACTF(P, hb[o][:], op_[o][:], AF.Sigmoid, [(OP[o], None)], [(HB[o], None)])
                STT(P, hs[o][:], hs[o][:], ssq[o][:, 0:1], hg[:, h * 192:(h + 1) * 192], ALU.mult, ALU.mult,
                    [(HS[o], None), (SSQ[o], None), (HG, None)], [(HS[o], None)])
                TT(P, "dve", yo[o][:], hs[o][:], hb[o][:], ALU.mult, [(HS[o], None), (HB[o], None)], [(YO[o], None)])
                P.dma("pool", D["Y"][t * 128:(t + 1) * 128, 1280 + h * 192:1280 + (h + 1) * 192], yo[o][:], reads=[(YO[o], None)],
                      writes=[(B["Y"], ("c", h, t))], sembuf=YO[o])
    if bg is not None:
        for _ in bg:
            pass


def post_norm_residual(C, t, pm4, PM4, x_ap, XB_key, xt, XT, rep, REP, junk, JK, ssp, SSP, dst_ap, DSTB, tmp, TMP):
    P = C.P
    for n in range(4):
        ACTF(P, junk[:, n * 512:(n + 1) * 512], pm4[n][:], AF.Square, [(PM4[n], None)], [(JK, n), (SSP, n)], accum=ssp[:, n:n + 1])
    TT(P, "dve", ssp[:, 4:5], ssp[:, 0:1], ssp[:, 1:2], ALU.add, [(SSP, 0), (SSP, 1)], [(SSP, "a")])
    TT(P, "dve", ssp[:, 5:6], ssp[:, 2:3], ssp[:, 3:4], ALU.add, [(SSP, 2), (SSP, 3)], [(SSP, "b")])
    TT(P, "dve", ssp[:, 6:7], ssp[:, 4:5], ssp[:, 5:6], ALU.add, [(SSP, "a"), (SSP, "b")], [(SSP, "c")])
    ACTF(P, ssp[:, 7:8], ssp[:, 6:7], AF.Sqrt, [(SSP, "c")], [(SSP, "r")], bias=EPS, scale=1.0 / D_)
    RECIP(P, ssp[:, 7:8], ssp[:, 7:8], [(SSP, "r")], [(SSP, "r")])
    for n in range(4):
        STT(P, tmp[:, n * 512:(n + 1) * 512], pm4[n][:], ssp[:, 7:8], rep[:, n * 512:(n + 1) * 512], ALU.mult, ALU.mult,
            [(PM4[n], None), (SSP, "r"), (REP, None)], [(TMP, n)])
    TT(P, "pool", xt[:], xt[:], tmp[:], ALU.add, [(XT, None), (TMP, None)], [(XT, None)])
    P.dma("pool", dst_ap[t * 128:(t + 1) * 128, :], xt[:], reads=[(XT, None)], writes=[(DSTB, t)], sembuf=XT)


def phase_outproj(C, l, xsrc, xdst):
    P, I, D, B, K, KB = C.P, C.I, C.D, C.B, C.K, C.KB
    wo = C.sb("o_w", [128, 16, D_], BF16); WO = Buf("o_w")
    rep = C.sb("o_rep", [128, D_], F32); REP = Buf("o_rep")
    rtmp = C.sb("o_rtmp", [128, 512], F32); RTMP = Buf("o_rtmp")
    yt = [C.sb("o_y%d" % i, [128, D_], BF16) for i in range(2)]; YT = [Buf("o_y%d" % i) for i in range(2)]
    yT = [C.sb("o_yT%d" % i, [128, 16, 128], BF16) for i in range(2)]; YTT = [Buf("o_yT%d" % i) for i in range(2)]
    xt = [C.sb("o_x%d" % i, [128, D_], F32) for i in range(2)]; XT = [Buf("o_x%d" % i) for i in range(2)]
    tmp = C.sb("o_tmp", [128, D_], F32); TMP = Buf("o_tmp")
    junk = C.sb("o_junk", [128, D_], BF16); JK = Buf("o_junk")
    ssp = [C.sb("o_ssp%d" % i, [128, 8], F32) for i in range(2)]; SSP = [Buf("o_ssp%d" % i) for i in range(2)]
    tp = C.ps("o_tp", [128, D_], BF16); TP = PB("o_tp")
    pm = [C.ps("o_pm%d" % i, [128, 512]) for i in range(4)]; PM = [PB("o_pm%d" % i) for i in range(4)]
    prep = C.ps("o_prep", [128, 512]); PREP = PB("o_prep")
    wov = D["WOUT"][l].rearrange("(kc p) n -> p kc n", p=128)
    for kc in range(16):
        P.dma("sp", wo[:, kc, :], wov[:, kc, :], reads=[(B["WOUT"], None)], writes=[(WO, kc)], sembuf=WO)
    replicate_cols(C, rep, REP, K["vec"][:, l, 2, :], 16, [(C.MODB, (l, 0, 2))], prep, PREP, rtmp, RTMP)
    for t in range(NT):
        s = t % 2
        P.dma("sp", yt[s][:], D["Y"][t * 128:(t + 1) * 128, :], reads=[(B["Y"], None)], writes=[(YT[s], None)], sembuf=YT[s])
        P.dma("sp", xt[s][:], xsrc[t * 128:(t + 1) * 128, :], reads=[(B["XR"], t)], writes=[(XT[s], None)], sembuf=XT[s])
        for c in range(16):
            TR(P, tp[:, c * 128:(c + 1) * 128], yt[s][:, c * 128:(c + 1) * 128], K["ident"][:], [(YT[s], None), (KB, "ident")], [(TP, c // 8)])
        yv = yT[s][:].rearrange("p c t -> p (c t)")
        CP(P, "act", yv[:, 0:1024], tp[:, 0:1024], [(TP, 0)], [(YTT[s], 0)])
        CP(P, "dve", yv[:, 1024:2048], tp[:, 1024:2048], [(TP, 1)], [(YTT[s], 1)])
        for n in range(4):
            for kc in range(16):
                MM(P, pm[n][:], yT[s][:, kc, :], wo[:, kc, n * 512:(n + 1) * 512], kc == 0, kc == 15, [(YTT[s], None), (WO, None)], [(PM[n], None)])
        post_norm_residual(C, t, pm, PM, None, None, xt[s], XT[s], rep, REP, junk, JK, ssp[s], SSP[s], xdst, B["XR"], tmp, TMP)


def phase_ffn(C, l, xsrc, xdst):
    P, I, D, B, K, KB = C.P, C.I, C.D, C.B, C.K, C.KB
    DSTB = B["XR"]
    xt = [C.sb("f_xt%d" % i, [128, D_], F32) for i in range(2)]; XT = [Buf("f_xt%d" % i) for i in range(2)]
    junk = C.sb("f_junk", [128, D_], BF16); JK = Buf("f_junk")
    ss = [C.sb("f_ss%d" % i, [128, 1], F32) for i in range(2)]
    rs = [C.sb("f_rs%d" % i, [128, 1], F32) for i in range(2)]; SS = [Buf("f_ss%d" % i) for i in range(2)]
    xn0 = C.sb("f_xn0", [128, D_], BF16); XN0 = Buf("f_xn0")
    xn = [xn0, xn0]; XN = [XN0, XN0]
    tp = C.ps("f_tp", [128, D_], BF16); TP = PB("f_tp")
    hT = C.sb("f_hT", [128, 16, 512], BF16); HT = Buf("f_hT")
    aT = C.sb("f_aT", [128, 44, 512], BF16); AT = Buf("f_aT")
    wg = [C.sb("f_wg%d" % i, [128, 16, 256], BF16) for i in range(2)]; WG = [Buf("f_wg%d" % i) for i in range(2)]
    wu = [C.sb("f_wu%d" % i, [128, 16, 256], BF16) for i in range(2)]; WU = [Buf("f_wu%d" % i) for i in range(2)]
    wd = [C.sb("f_wd%d" % i, [128, 4, 512], BF16) for i in range(2)]; WDB = [Buf("f_wd%d" % i) for i in range(2)]
    pg = C.ps("f_pg", [128, 512]); PG = PB("f_pg")
    pu = C.ps("f_pu", [128, 512]); PU = PB("f_pu")
    pm = [C.ps("f_pm%d" % i, [128, 512]) for i in range(4)]; PM = [PB("f_pm%d" % i) for i in range(4)]
    sg = [C.sb("f_sg%d" % i, [128, 512], F32) for i in range(2)]; SG = [Buf("f_sg%d" % i) for i in range(2)]
    rep = C.sb("f_rep", [128, D_], F32); REP = Buf("f_rep")
    fst = [C.sb("f_fst%d" % i, [128, D_], F32) for i in range(4)]; FST = [Buf("f_fst%d" % i) for i in range(4)]
    tmp = fst[0]; TMP = FST[0]
    ssp = [C.sb("f_ssp%d" % i, [128, 8], F32) for i in range(4)]; SSP = [Buf("f_ssp%d" % i) for i in range(4)]
    xr = xt; XRB = XT
    A = K["vec"][:, l, 3, :]; Sv = K["vec"][:, l, 4, :]
    VR = [(C.MODB, (l, 1, 0)), (C.MODB, (l, 1, 1))]
    replicate_cols(C, rep, REP, K["vec"][:, l, 5, :], 16, [(C.MODB, (l, 1, 2))], pg, PG, tmp, TMP)
    wgv = D["WG"][l].rearrange("(kc p) n -> p kc n", p=128)
    wuv = D["WU"][l].rearrange("(kc p) n -> p kc n", p=128)
    wdv = D["WD"][l].rearrange("(f p) n -> p f n", p=128)
    n_w = 0
    n_d = 0
    n_s = 0
    def norm_blk(blk):
        for ti in range(4):
            t = blk * 4 + ti
            s = t % 2
            norm_to_hT(C, t, ti, xsrc, B["XR"], xt[s], XT[s], ss[s], rs[s], SS[s], xn[s], XN[s], junk, JK, tp, TP, hT, HT, A, Sv, VR)

    norm_blk(0)
    for blk in range(8):
        for fs in range(22):
            wi = n_w % 2; n_w += 1
            P.dma("sp", wg[wi][:], wgv[:, :, fs * 256:(fs + 1) * 256], reads=[(B["WG"], None)], writes=[(WG[wi], None)], sembuf=WG[wi])
            P.dma("sp", wu[wi][:], wuv[:, :, fs * 256:(fs + 1) * 256], reads=[(B["WU"], None)], writes=[(WU[wi], None)], sembuf=WU[wi])
            for j in range(2):
                f = fs * 2 + j
                if f % 2 == 0:
                    pgb, PGB, pub, PUB = pg, PG, pu, PU
                else:
                    pgb, PGB, pub, PUB = pm[0], PM[0], pm[1], PM[1]
                for kc in range(16):
                    MM(P, pgb[:], wg[wi][:, kc, j * 128:(j + 1) * 128], hT[:, kc, :], kc == 0, kc == 15, [(WG[wi], None), (HT, None)], [(PGB, None)])
                for kc in range(16):
                    MM(P, pub[:], wu[wi][:, kc, j * 128:(j + 1) * 128], hT[:, kc, :], kc == 0, kc == 15, [(WU[wi], None), (HT, None)], [(PUB, None)])
                si = n_s % 2; n_s += 1
                ACTF(P, sg[si][:], pgb[:], AF.Silu, [(PGB, None)], [(SG[si], None)])
                TT(P, "dve", aT[:, f, :], sg[si][:], pub[:], ALU.mult, [(SG[si], None), (PUB, None)], [(AT, f)])
        if blk + 1 < 8:
            norm_blk(blk + 1)
        for n in range(4):
            for f0 in range(0, 44, 4):
                di = n_d % 2; n_d += 1
                P.dma("sp", wd[di][:], wdv[:, f0:f0 + 4, n * 512:(n + 1) * 512], reads=[(B["WD"], None)], writes=[(WDB[di], None)], sembuf=WDB[di])
                for fj in range(4):
                    f = f0 + fj
                    for ti in range(4):
                        MM(P, pm[ti][:], aT[:, f, ti * 128:(ti + 1) * 128], wd[di][:, fj, :], f == 0, f == 43,
                           [(AT, None), (WDB[di], None)], [(PM[ti], None)])
            for ti in range(4):
                cs = slice(n * 512, (n + 1) * 512)
                CP(P, "dve" if ti % 2 == 0 else "act", fst[ti][:, cs], pm[ti][:], [(PM[ti], None)], [(FST[ti], n)])
                ACTF(P, junk[:, cs], fst[ti][:, cs], AF.Square, [(FST[ti], n)], [(JK, n), (SSP[ti], n)], accum=ssp[ti][:, n:n + 1])
        for ti in range(4):
            t = blk * 4 + ti
            s = t % 2
            sq_, SQ_ = ssp[ti], SSP[ti]
            TT(P, "dve", sq_[:, 4:5], sq_[:, 0:1], sq_[:, 1:2], ALU.add, [(SQ_, 0), (SQ_, 1)], [(SQ_, "a")])
            TT(P, "dve", sq_[:, 5:6], sq_[:, 2:3], sq_[:, 3:4], ALU.add, [(SQ_, 2), (SQ_, 3)], [(SQ_, "b")])
            TT(P, "dve", sq_[:, 6:7], sq_[:, 4:5], sq_[:, 5:6], ALU.add, [(SQ_, "a"), (SQ_, "b")], [(SQ_, "c")])
            ACTF(P, sq_[:, 7:8], sq_[:, 6:7], AF.Sqrt, [(SQ_, "c")], [(SQ_, "r")], bias=EPS, scale=1.0 / D_)
            RECIP(P, sq_[:, 7:8], sq_[:, 7:8], [(SQ_, "r")], [(SQ_, "r")])
            STT(P, fst[ti][:], fst[ti][:], sq_[:, 7:8], rep[:], ALU.mult, ALU.mult, [(FST[ti], None), (SQ_, "r"), (REP, None)], [(FST[ti], None)])
            P.dma("sp", xr[s][:], xsrc[t * 128:(t + 1) * 128, :], reads=[(B["XR"], t)], writes=[(XRB[s], None)], sembuf=XRB[s])
            TT(P, "pool", xr[s][:], xr[s][:], fst[ti][:], ALU.add, [(XRB[s], None), (FST[ti], None)], [(XRB[s], None)])
            P.dma("pool", xdst[t * 128:(t + 1) * 128, :], xr[s][:], reads=[(XRB[s], None)], writes=[(DSTB, t)], sembuf=XRB[s])


def alibi_slopes(n):
    def pow2(m):
        start = 2.0 ** (-8.0 / m)
        return [start ** (i + 1) for i in range(m)]
    if math.log2(n).is_integer():
        s = pow2(n)
    else:
        p = 2 ** math.floor(math.log2(n))
        s = pow2(p) + pow2(2 * p)[0::2][: n - p]
    return np.array(s, dtype=np.float32)


def host_constants():
    c = {}
    c["c_ident"] = np.eye(128, dtype=np.float32)
    c["c_ones"] = np.ones((128, 128), np.float32)
    k = np.arange(128)[:, None]; m = np.arange(128)[None, :]
    c["c_U"] = (k <= m).astype(np.float32)
    c["c_L"] = (k >= m).astype(np.float32)
    R = np.zeros((64, 64), np.float32)
    for mm in range(32):
        R[mm + 32, mm] = -1.0
        R[mm, mm + 32] = 1.0
    c["c_R"] = R
    sl = alibi_slopes(12)
    E = np.zeros((128, 12, 3, 128), np.float32)
    kk = np.arange(128)[:, None]; qq = np.arange(128)[None, :]
    for d in range(3):
        dist = np.abs(qq - kk - (d - 1) * 128).astype(np.float32)
        for h in range(12):
            E[:, h, d, :] = np.where(dist <= 128, np.exp(-sl[h] * dist), 0.0)
    c["c_E"] = E.reshape(128, 12 * 384)
    inv = (1.0 / (np.float32(10000.0) ** (np.arange(0, 64, 2, dtype=np.float32) / np.float32(64)))).astype(np.float32)
    c["c_invf"] = np.concatenate([inv, inv])[:, None].astype(np.float32)
    return c


def colT(v, n):
    return np.ascontiguousarray(np.asarray(v, np.float32).reshape(n, 128).T)


def host_layout(inp):
    g = lambda k: np.asarray(inp[k])
    w_in = g("w_in")
    fm = np.zeros((L_, D_, NFM * 128), np.float32)
    def put(ch, cols):
        fm[:, :, ch * 128:ch * 128 + len(cols)] = w_in[:, :, cols]
    for c in range(6):
        put(FM_AQ + c, np.arange(c * 128, (c + 1) * 128))
    for kv in range(4):
        cols = np.arange(768 + kv * 64, 768 + (kv + 1) * 64)
        put(FM_AK + kv, np.concatenate([cols, cols]))
    for c in range(4):
        put(FM_BCQ + c, np.arange(1280 + c * 128, min(1280 + (c + 1) * 128, 1728)))
    put(FM_BCKV, np.arange(1728, 1856))
    put(FM_BKR, np.arange(1856, 1920))
    for h in range(4):
        put(FM_CQ + h, np.arange(1920 + h * 96, 1920 + (h + 1) * 96))
        put(FM_CK + h, np.arange(2304 + h * 96, 2304 + (h + 1) * 96))
    tm = np.ascontiguousarray(np.concatenate([w_in[:, :, 1024:1280], w_in[:, :, 2688:3456], w_in[:, :, 3472:4240], w_in[:, :, 3456:3472]], axis=2))
    shared = {
        "mod_w": g("mod_w"),
        "mod_bT": np.stack([colT(g("mod_b")[l], 96) for l in range(L_)]),
        "w_in_fm": fm, "w_in_tm": tm,
        "attn_sink": g("attn_sink")[:, None, :],
        "w_uq": np.concatenate([g("mla_w_uq"), np.zeros((L_, 64, 768), np.float32)], axis=1),
        "w_ukv": g("mla_w_ukv"),
        "gate_b": g("mlstm_gate_b")[:, None, :],
        "head_g": g("mlstm_head_g")[:, None, :],
        "w_out": g("w_out"), "w_gate": g("ffn_w_gate"), "w_up": g("ffn_w_up"), "w_down": g("ffn_w_down"),
    }
    for k, src in (("pre_mix_gT", "pre_mix_g"), ("post_mix_gT", "post_mix_g"), ("pre_ffn_gT", "pre_ffn_g"), ("post_ffn_gT", "post_ffn_g")):
        shared[k] = np.stack([colT(g(src)[l], 16) for l in range(L_)])
    qg = np.concatenate([g("mla_q_norm_g"), np.zeros((L_, 64), np.float32)], axis=1)
    shared["q_norm_gT"] = np.stack([colT(qg[l], 4) for l in range(L_)])
    shared["kv_norm_gT"] = np.stack([colT(g("mla_kv_norm_g")[l], 1) for l in range(L_)])
    shared.update(host_constants())
    x, c, pos = g("x"), g("c"), g("positions")
    maps = []
    for b in range(8):
        m = dict(shared)
        m["x"] = np.ascontiguousarray(x[b])
        m["cT"] = colT(c[b], 16)
        m["pos"] = np.ascontiguousarray(pos[b][None, :].astype(np.int32))
        maps.append(m)
    return maps


_CACHE = {}


def kernel(**inputs):
    if "nc" not in _CACHE:
        _CACHE["nc"] = build_program()[0]
    nc = _CACHE["nc"]
    maps = host_layout(inputs)
    res = run_bass_kernel_spmd(nc, maps, core_ids=list(range(8)))
    return np.stack([np.asarray(r["out"], dtype=np.float32) for r in res.results], axis=0)
```

```python
import numpy as np
import concourse.bass as bass
import concourse.mybir as mybir
from concourse.bass_utils import run_bass_kernel_spmd
F32 = mybir.dt.float32
BF16 = mybir.dt.bfloat16
I32 = mybir.dt.int32
AF = mybir.ActivationFunctionType
ALU = mybir.AluOpType
AX = mybir.AxisListType


class Buf:
    __slots__ = ("name", "st", "sem", "excl")

    def __init__(self, name, excl=False):
        self.name = name
        self.st = {}
        self.sem = None
        self.excl = excl


def PB(name):
    return Buf(name, excl=True)


class Op:
    __slots__ = ("eng", "fn", "idx", "waits", "is_dma", "needs_inc", "clock", "sem", "semval")

    def __init__(self, eng, fn, is_dma):
        self.eng = eng
        self.fn = fn
        self.is_dma = is_dma
        self.waits = []
        self.needs_inc = False
        self.clock = None
        self.sem = None
        self.semval = None


class Prog:
    ENGS = ("sp", "act", "dve", "pool", "pe")

    def __init__(self, nc):
        self.nc = nc
        self.ops = []
        self.known_idx = {}
        self.known_dma = {}
        self.dma_issued = {}
        self.sems = {}
        self.ctx = []
        self.free_pool = {}
        self.npool = 0
        self.phase_bufs = []
        self.phase_start = 0

    def _sem(self, key):
        s = self.sems.get(key)
        if s is None:
            cm = self.nc.semaphore("s_" + str(key))
            s = cm.__enter__()
            self.ctx.append(cm)
            self.sems[key] = s
        return s

    @staticmethod
    def _states(buf, key, create):
        st = buf.st
        if key is None:
            if create and None not in st:
                st[None] = {"w": {}, "r": {}}
            return list(st.values())
        out = []
        if None in st:
            out.append(st[None])
        if key not in st and create:
            st[key] = {"w": {}, "r": {}}
        if key in st:
            out.append(st[key])
        return out

    def _record(self, op, reads, writes):
        op.idx = len(self.ops)
        self.ops.append(op)
        ex = [rk for rk in reads if rk[0].excl]
        if ex:
            reads = [rk for rk in reads if not rk[0].excl]
            writes = list(writes) + [rk for rk in ex if rk not in writes]
        deps = []
        for (buf, key) in reads:
            for s in self._states(buf, key, True):
                deps += [(p, "RAW") for p in s["w"].values()]
        for (buf, key) in writes:
            for s in self._states(buf, key, True):
                deps += [(p, "WAW") for p in s["w"].values()]
                deps += [(p, "WAR") for p in s["r"].values()]
        for (p, kind) in deps:
            if p is op or p.idx < self.phase_start:
                continue
            if p.is_dma:
                val = self.dma_issued[p.sem]
                if op.is_dma and op.sem == p.sem:
                    val -= 16
                k = (op.eng, p.sem)
                if self.known_dma.get(k, 0) >= val:
                    continue
                self.known_dma[k] = val
                op.waits.append(("sem", p.sem, val))
            else:
                if p.eng == op.eng:
                    if op.eng == "pe":
                        continue
                k = (op.eng, p.eng)
                if self.known_idx.get(k, -1) >= p.idx:
                    continue
                self.known_idx[k] = p.idx
                p.needs_inc = True
                op.waits.append(("op", p))
        clk = ("dma", op.sem) if op.is_dma else op.eng
        for (buf, key) in reads:
            if key is None:
                if None not in buf.st:
                    buf.st[None] = {"w": {}, "r": {}}
                buf.st[None]["r"][clk] = op
            else:
                buf.st[key]["r"][clk] = op
        for (buf, key) in writes:
            if key is None:
                buf.st.clear()
                buf.st[None] = {"w": {clk: op}, "r": {}}
            else:
                buf.st[key] = {"w": {clk: op}, "r": {}}

    def op(self, eng, fn, reads=(), writes=()):
        o = Op(eng, fn, False)
        self._record(o, reads, writes)
        return o

    def dma(self, queue, out_ap, in_ap, reads=(), writes=(), sembuf=None, **kw):
        assert sembuf is not None
        qt = "sw" if queue == "pool" else "hw"
        if sembuf.sem is None:
            sembuf.sem = {}
            self.phase_bufs.append(sembuf)
        if qt not in sembuf.sem:
            fp = self.free_pool.setdefault(qt, [])
            if fp:
                sembuf.sem[qt] = fp.pop()
            else:
                sembuf.sem[qt] = "%s_%d" % (qt, self.npool)
                self.npool += 1
        o = Op(queue, lambda e: e.dma_start(out=out_ap, in_=in_ap, **kw), True)
        o.sem = sembuf.sem[qt]
        self.dma_issued[o.sem] = self.dma_issued.get(o.sem, 0) + 16
        o.semval = self.dma_issued[o.sem]
        self._record(o, reads, writes)
        return o

    def sb(self, name, shape, dtype):
        cm = self.nc.sbuf_tensor(name, list(shape), dtype)
        t = cm.__enter__()
        self.ctx.append(cm)
        return t

    def ps(self, name, shape, dtype):
        cm = self.nc.psum_tensor(name, list(shape), dtype)
        t = cm.__enter__()
        self.ctx.append(cm)
        return t

    def emit_phase(self):
        start = getattr(self, "_emitted", 0)
        ops = self.ops[start:]
        self._emitted = len(self.ops)
        if not hasattr(self, "cnt"):
            self.cnt = {e: 0 for e in self.ENGS}
            self.tot_stats = {e: 0 for e in self.ENGS}
            self.nwaits = 0
        nc = self.nc
        cnt = self.cnt
        per = {e: [o for o in ops if o.eng == e] for e in self.ENGS}
        for e in self.ENGS:
            for o in reversed(per[e]):
                if not o.is_dma:
                    o.needs_inc = True
                    break
        for o in ops:
            if (not o.is_dma) and o.needs_inc:
                cnt[o.eng] += 1
                o.clock = cnt[o.eng]
        for e in self.ENGS:
            self._sem("eng_" + e)
            self.tot_stats[e] += len(per[e])
        for o in ops:
            if o.is_dma:
                self._sem(o.sem)
            self.nwaits += len(o.waits)

        def run(e, name):
            for o in per[name]:
                for w in o.waits:
                    if w[0] == "sem":
                        e.wait_ge(self.sems[w[1]], w[2])
                    else:
                        p = w[1]
                        e.wait_ge(self.sems["eng_" + p.eng], p.clock)
                ins = o.fn(e)
                if o.is_dma:
                    ins.then_inc(self.sems[o.sem], 16)
                elif o.needs_inc:
                    ins.then_inc(self.sems["eng_" + o.eng], 1)
            for sem, tot in self.dma_issued.items():
                if sem in self.sems:
                    e.wait_ge(self.sems[sem], tot)
            for en in self.ENGS:
                if en != name and cnt[en] > 0:
                    e.wait_ge(self.sems["eng_" + en], cnt[en])

        with nc.Block() as block:
            @block.sync
            def _(e):
                run(e, "sp")

            @block.scalar
            def _(e):
                run(e, "act")

            @block.vector
            def _(e):
                run(e, "dve")

            @block.gpsimd
            def _(e):
                run(e, "pool")

            @block.tensor
            def _(e):
                run(e, "pe")
        for b in self.phase_bufs:
            for qt, sm in b.sem.items():
                self.free_pool.setdefault(qt, []).append(sm)
            b.sem = None
        self.phase_bufs = []
        self.phase_start = len(self.ops)
        last = len(self.ops)
        for a in self.ENGS:
            for b in self.ENGS:
                self.known_idx[(a, b)] = last - 1
            for sem, tot in self.dma_issued.items():
                self.known_dma[(a, sem)] = tot

    def finish(self):
        self.stats = dict(self.tot_stats)
        self.stats["waits"] = self.nwaits
        self.stats["clock"] = dict(self.cnt)
        self.stats["maxdma"] = max(self.dma_issued.values()) if self.dma_issued else 0
        self.stats["nsem"] = len(self.sems)

    def emit(self, final_wait_bufs=()):
        nc = self.nc
        cnt = {e: 0 for e in self.ENGS}
        for o in self.ops:
            if (not o.is_dma) and o.needs_inc:
                cnt[o.eng] += 1
                o.clock = cnt[o.eng]
        per = {e: [o for o in self.ops if o.eng == e] for e in self.ENGS}
        for e in self.ENGS:
            self._sem("eng_" + e)
        for o in self.ops:
            if o.is_dma:
                self._sem(o.sem)
        self.stats = {e: len(per[e]) for e in self.ENGS}
        self.stats["waits"] = sum(len(o.waits) for o in self.ops)
        self.stats["maxclock"] = dict(cnt)
        self.stats["maxdma"] = max(self.dma_issued.values()) if self.dma_issued else 0
        self.stats["nsem"] = len(self.sems)

        def run(e, name):
            for o in per[name]:
                for w in o.waits:
                    if w[0] == "sem":
                        e.wait_ge(self.sems[w[1]], w[2])
                    else:
                        p = w[1]
                        e.wait_ge(self.sems["eng_" + p.eng], p.clock)
                ins = o.fn(e)
                if o.is_dma:
                    ins.then_inc(self.sems[o.sem], 16)
                elif o.needs_inc:
                    ins.then_inc(self.sems["eng_" + o.eng], 1)
            if name == "sp":
                for sem, tot in self.dma_issued.items():
                    e.wait_ge(self.sems[sem], tot)
                for en in self.ENGS:
                    if en != "sp" and cnt[en] > 0:
                        e.wait_ge(self.sems["eng_" + en], cnt[en])

        with nc.Block() as block:
            @block.sync
            def _(e):
                run(e, "sp")

            @block.scalar
            def _(e):
                run(e, "act")

            @block.vector
            def _(e):
                run(e, "dve")

            @block.gpsimd
            def _(e):
                run(e, "pool")

            @block.tensor
            def _(e):
                run(e, "pe")

    def close(self):
        for cm in reversed(self.ctx):
            cm.__exit__(None, None, None)
        self.ctx = []


import math

S_ = 4096
D_ = 2048
NT = 32
DFF = 5632
EPS = 1e-6
L_ = 2
NFM = 24
NTM = 1808
FM_AQ, FM_AK, FM_BCQ, FM_BCKV, FM_BKR, FM_CQ, FM_CK = 0, 6, 10, 14, 15, 16, 20


def MM(P, out, lhsT, rhs, start, stop, R, W):
    return P.op("pe", lambda e: e.matmul(out, lhsT=lhsT, rhs=rhs, start=start, stop=stop), R, W)


def TR(P, out, in_, ident, R, W):
    return P.op("pe", lambda e: e.transpose(out, in_, ident), R, W)


def ACTF(P, out, in_, func, R, W, bias=None, scale=None, accum=None):
    kw = {}
    if bias is not None:
        kw["bias"] = bias
    if scale is not None:
        kw["scale"] = scale
    if accum is not None:
        kw["accum_out"] = accum
    return P.op("act", lambda e: e.activation(out=out, in_=in_, func=func, **kw), R, W)


def TS(P, eng, out, in0, s1, s2, op0, op1, R, W):
    if op1 is None:
        return P.op(eng, lambda e: e.tensor_scalar(out=out, in0=in0, scalar1=s1, scalar2=None, op0=op0), R, W)
    return P.op(eng, lambda e: e.tensor_scalar(out=out, in0=in0, scalar1=s1, scalar2=s2, op0=op0, op1=op1), R, W)


def TT(P, eng, out, in0, in1, op, R, W):
    return P.op(eng, lambda e: e.tensor_tensor(out=out, in0=in0, in1=in1, op=op), R, W)


def STT(P, out, in0, scalar, in1, op0, op1, R, W):
    return P.op("dve", lambda e: e.scalar_tensor_tensor(out=out, in0=in0, scalar=scalar, in1=in1, op0=op0, op1=op1), R, W)


def CP(P, eng, out, in_, R, W):
    if eng == "act":
        return P.op("act", lambda e: e.activation(out=out, in_=in_, func=AF.Copy), R, W)
    return P.op(eng, lambda e: e.tensor_copy(out=out, in_=in_), R, W)


def RECIP(P, out, in_, R, W):
    return P.op("dve", lambda e: e.reciprocal(out=out, in_=in_), R, W)


def MEMSET(P, eng, ap, val, R, W):
    return P.op(eng, lambda e: e.memset(ap, val), R, W)


class Ctx:
    pass


KNOB = {}


def build_program(dbg=False, phases=None, nlayers=L_):
    nc = bass.Bass("TRN2", target_bir_lowering=False)
    P = Prog(nc)
    C = Ctx()
    C.nc, C.P, C.dbg = nc, P, dbg

    def din(name, shape, dt=F32):
        return nc.dram_tensor(name, list(shape), dt, kind="ExternalInput").ap()

    def dscr(name, shape, dt):
        return nc.dram_tensor(name, list(shape), dt, kind=("ExternalOutput" if (dbg and name in dbg) else "Internal")).ap()

    I = C.I = {}
    I["x"] = din("x", [S_, D_])
    I["cT"] = din("cT", [128, 16])
    I["pos"] = din("pos", [1, S_], I32)
    I["mod_w"] = din("mod_w", [L_, D_, 6 * D_])
    I["mod_bT"] = din("mod_bT", [L_, 128, 96])
    for g in ("pre_mix_gT", "post_mix_gT", "pre_ffn_gT", "post_ffn_gT"):
        I[g] = din(g, [L_, 128, 16])
    I["w_in_fm"] = din("w_in_fm", [L_, D_, NFM * 128])
    I["w_in_tm"] = din("w_in_tm", [L_, D_, NTM])
    I["attn_sink"] = din("attn_sink", [L_, 1, 12])
    I["q_norm_gT"] = din("q_norm_gT", [L_, 128, 4])
    I["w_uq"] = din("w_uq", [L_, 512, 768])
    I["kv_norm_gT"] = din("kv_norm_gT", [L_, 128, 1])
    I["w_ukv"] = din("w_ukv", [L_, 128, 1024])
    I["gate_b"] = din("gate_b", [L_, 1, 16])
    I["head_g"] = din("head_g", [L_, 1, 768])
    I["w_out"] = din("w_out", [L_, D_, D_])
    I["w_gate"] = din("w_gate", [L_, D_, DFF])
    I["w_up"] = din("w_up", [L_, D_, DFF])
    I["w_down"] = din("w_down", [L_, DFF, D_])
    I["c_ident"] = din("c_ident", [128, 128])
    I["c_ones"] = din("c_ones", [128, 128])
    I["c_U"] = din("c_U", [128, 128])
    I["c_L"] = din("c_L", [128, 128])
    I["c_R"] = din("c_R", [64, 64])
    I["c_E"] = din("c_E", [128, 12 * 384])
    I["c_invf"] = din("c_invf", [64, 1])
    C.out = nc.dram_tensor("out", [S_, D_], F32, kind="ExternalOutput").ap()

    D = C.D = {}
    D["XR"] = dscr("XR", [S_, D_], F32)
    D["WFM"] = dscr("WFM", [L_, D_, NFM * 128], BF16)
    D["WTM"] = dscr("WTM", [L_, D_, NTM], BF16)
    D["WOUT"] = dscr("WOUTb", [L_, D_, D_], BF16)
    D["WG"] = dscr("WGb", [L_, D_, DFF], BF16)
    D["WU"] = dscr("WUb", [L_, D_, DFF], BF16)
    D["WD"] = dscr("WDb", [L_, DFF, D_], BF16)
    D["QKT"] = dscr("QKT", [NFM, 128, S_], BF16)
    D["PTM"] = dscr("PTM", [S_, NTM], BF16)
    D["GATES"] = dscr("GATES", [S_, 16], F32)
    D["Y"] = dscr("Y", [S_, D_], BF16)
    D["QN"] = dscr("QN", [4, 128, S_], BF16)
    D["QR"] = dscr("QR", [4, 64, S_], BF16)
    D["KN"] = dscr("KN", [4, 128, S_], BF16)
    D["KR"] = dscr("KR", [64, S_], BF16)
    D["VB"] = dscr("VB", [S_, 512], BF16)
    D["BREP"] = dscr("BREP", [8, 128, S_], F32)
    D["IBS"] = dscr("IBS", [128, NT * 8], F32)
    D["MODV"] = dscr("MODV", [128, L_ * 96], F32)
    B = C.B = {k: Buf("D_" + k) for k in D}
    B["CAST"] = Buf("CAST")

    def psb(name, shape, dt):
        cm = nc.sbuf_tensor(name, list(shape), dt)
        return cm.__enter__()

    K = C.K = {}
    K["ident"] = psb("k_ident", [128, 128], BF16)
    K["identf"] = psb("k_identf", [128, 128], F32)
    K["ones"] = psb("k_ones", [128, 128], F32)
    K["onesb"] = psb("k_onesb", [128, 128], BF16)
    K["U"] = psb("k_U", [128, 128], F32)
    K["Lm"] = psb("k_L", [128, 128], F32)
    K["R"] = psb("k_R", [64, 64], BF16)
    K["mod"] = psb("k_mod", [128, L_, 96], F32)
    K["vec"] = psb("k_vec", [128, L_, 6, 16], F32)
    KB = C.KB = Buf("KCONST")
    C.MODB = Buf("MODVEC")

    C.phase_ctx = []

    def begin():
        C.phase_ctx = []
        P.ctx_mark = len(P.ctx)

    C.uid = [0]

    def sb(name, shape, dt):
        C.uid[0] += 1
        name = "%s_u%d" % (name, C.uid[0])
        cm = nc.sbuf_tensor(name, list(shape), dt)
        t = cm.__enter__()
        C.phase_ctx.append(cm)
        return t

    def ps(name, shape, dt=F32):
        C.uid[0] += 1
        name = "%s_u%d" % (name, C.uid[0])
        esz = 4 if dt == F32 else 2
        n = 1
        for d in shape[1:]:
            n *= d
        per_bank = 2048 // esz
        nb = -(-n // per_bank)
        cm = nc.psum_tensor(name, [128, nb * per_bank], dt)
        t = cm.__enter__()
        C.phase_ctx.append(cm)
        v = t[0:shape[0], 0:n]
        if len(shape) == 3:
            v = v.rearrange("p (a b) -> p a b", b=shape[2])
        return v

    def end():
        P.emit_phase()
        for cm in reversed(C.phase_ctx):
            cm.__exit__(None, None, None)
        C.phase_ctx = []

    C.begin, C.end, C.sb, C.ps = begin, end, sb, ps

    want = (lambda n: True) if phases is None else (lambda n: n in phases)

    begin()
    phase_consts(C)
    late = nlayers > 1 and phases is None
    gc = phase_cast(C, [0], keys=(("WFM", "WTM") if late else ("WFM", "WTM", "WOUT", "WG", "WU", "WD"))) if want("cast") else iter(())
    gm = phase_mod(C, ([0] if late else list(range(nlayers)))) if want("mod") else iter(())
    done_c = done_m = False
    while not (done_c and done_m):
        for _ in range(2 if late else 5):
            if next(gc, "end") == "end":
                done_c = True
                break
        if next(gm, "end") == "end":
            done_m = True
    for _ in gc:
        pass
    for _ in gm:
        pass
    end()
    for l in range(nlayers):
        src = I["x"] if l == 0 else D["XR"]
        if want("inproj"):
            begin(); phase_inproj(C, l, src); end()
        if want("win"):
            bg0 = phase_mod(C, list(range(1, nlayers))) if (l == 0 and late and want("mod")) else None
            begin(); phase_window(C, l, bg0); end()
        if want("mla"):
            begin(); phase_mla_prep(C, l); end()
            bg = phase_cast(C, list(range(1, nlayers)), engs=("dve", "dve", "dve", "dve")) if (l == 0 and nlayers > 1 and want("cast")) else None
            begin(); phase_mla_attn(C, l, bg); end()
        if want("mlstm"):
            begin(); phase_mlstm_prep(C, l); end()
            bg2 = phase_cast(C, [0], engs=("pool", "pool", "pool", "pool"), keys=("WOUT", "WG", "WU", "WD")) if (l == 0 and late) else None
            begin(); phase_mlstm_attn(C, l, bg2); end()
        if want("outproj"):
            begin(); phase_outproj(C, l, src, D["XR"]); end()
        if want("ffn"):
            dst = C.out if l == nlayers - 1 else D["XR"]
            begin(); phase_ffn(C, l, D["XR"], dst); end()
    P.finish()
    P.close()
    return nc, P


def phase_consts(C):
    P, I, K, KB = C.P, C.I, C.K, C.KB
    tmp = C.sb("c_tmp", [128, 128], F32); T = Buf("c_tmp")
    tmpR = C.sb("c_tmpR", [64, 64], F32); TRb = Buf("c_tmpR")
    P.dma("sp", K["identf"][:], I["c_ident"], writes=[(KB, "identf")], sembuf=KB)
    P.dma("sp", K["ones"][:], I["c_ones"], writes=[(KB, "ones")], sembuf=KB)
    P.dma("sp", K["U"][:], I["c_U"], writes=[(KB, "U")], sembuf=KB)
    P.dma("sp", K["Lm"][:], I["c_L"], writes=[(KB, "L")], sembuf=KB)
    P.dma("sp", tmpR[:], I["c_R"], writes=[(TRb, None)], sembuf=TRb)
    CP(P, "dve", K["ident"][:], K["identf"][:], [(KB, "identf")], [(KB, "ident")])
    CP(P, "dve", K["onesb"][:], K["ones"][:], [(KB, "ones")], [(KB, "onesb")])
    CP(P, "dve", K["R"][:], tmpR[:], [(TRb, None)], [(KB, "R")])


def phase_cast(C, layers, engs=("act", "dve", "act", "dve"), keys=("WFM", "WTM", "WOUT", "WG", "WU", "WD")):
    P, I, D, B = C.P, C.I, C.D, C.B
    NB = 4
    sf = [C.sb("c_sf%d" % i, [128, 2048], F32) for i in range(NB)]; SF = [Buf("c_sf%d" % i) for i in range(NB)]
    sb_ = [C.sb("c_sb%d" % i, [128, 2048], BF16) for i in range(NB)]; SBB = [Buf("c_sb%d" % i) for i in range(NB)]
    cnt = [0]

    def cast(dst, src, rows, cols, key):
        sv = src.rearrange("(r p) n -> p r n", p=128)
        dv = dst.rearrange("(r p) n -> p r n", p=128)
        for r in range(rows // 128):
            for c0 in range(0, cols, 2048):
                w = min(2048, cols - c0)
                i = cnt[0] % NB; cnt[0] += 1
                P.dma("sp", sf[i][:, 0:w], sv[:, r, c0:c0 + w], writes=[(SF[i], None)], sembuf=SF[i])
                CP(P, engs[i], sb_[i][:, 0:w], sf[i][:, 0:w], [(SF[i], None)], [(SBB[i], None)])
                P.dma("pool", dv[:, r, c0:c0 + w], sb_[i][:, 0:w], reads=[(SBB[i], None)], writes=[(B[key], ("cast", id(dst), r, c0))], sembuf=SBB[i])
                yield
    spec = (("WFM", "w_in_fm", D_, NFM * 128), ("WTM", "w_in_tm", D_, NTM), ("WOUT", "w_out", D_, D_),
            ("WG", "w_gate", D_, DFF), ("WU", "w_up", D_, DFF), ("WD", "w_down", DFF, D_))
    for l in layers:
        for (key, src, rows, cols) in spec:
            if key in keys:
                yield from cast(D[key][l], I[src][l], rows, cols, key)


def phase_mod(C, layers):
    P, I, K = C.P, C.I, C.K
    cT = C.sb("m_cT", [128, 16], F32); CT = Buf("m_cT")
    sc = C.sb("m_silu", [128, 16, 2], F32); SC = Buf("m_silu")
    ws = [C.sb("m_w%d" % i, [128, 16, 512], F32) for i in range(2)]
    WS = [Buf("m_w%d" % i) for i in range(2)]
    mb = C.sb("m_b", [128, 96], F32); MB = Buf("m_b")
    gT = C.sb("m_g", [128, 4, 16], F32); GT = Buf("m_g")
    mps = C.ps("m_ps", [128, 96, 2]); MPS = PB("m_ps")
    MODB = C.MODB
    P.dma("sp", cT[:], I["cT"], writes=[(CT, None)], sembuf=CT)
    ACTF(P, sc[:, :, 0], cT[:], AF.Silu, [(CT, None)], [(SC, 0)])
    ACTF(P, sc[:, :, 1], cT[:], AF.Silu, [(CT, None)], [(SC, 1)])
    for l in layers:
        wv = I["mod_w"][l].rearrange("(kc p) n -> p kc n", p=128)
        P.dma("sp", mb[:], I["mod_bT"][l], writes=[(MB, None)], sembuf=MB)
        for gi, g in enumerate(("pre_mix_gT", "post_mix_gT", "pre_ffn_gT", "post_ffn_gT")):
            P.dma("sp", gT[:, gi, :], I[g][l], writes=[(GT, gi)], sembuf=GT)
        for s in range(24):
            w = ws[s % 2]
            P.dma("sp", w[:], wv[:, :, s * 512:(s + 1) * 512], writes=[(WS[s % 2], None)], sembuf=WS[s % 2])
            for j in range(4):
                col = s * 4 + j
                for kc in range(16):
                    MM(P, mps[:, col, :], w[:, kc, j * 128:(j + 1) * 128], sc[:, kc, :], kc == 0, kc == 15,
                       [(WS[s % 2], None), (SC, None)], [(MPS, None)])
            yield
        mod = K["mod"][:, l, :]
        TT(P, "dve", mod, mps[:, :, 0], mb[:], ALU.add, [(MPS, None), (MB, None)], [(MODB, (l, "mod"))])
        if C.dbg and "MODV" in C.dbg:
            P.dma("pool", C.D["MODV"][:, l * 96:(l + 1) * 96], mod, reads=[(MODB, (l, "mod"))], writes=[(C.B["MODV"], l)], sembuf=MB)
        vec = K["vec"]
        for half, (gpre, gpost) in enumerate(((0, 1), (2, 3))):
            o = half * 48
            STT(P, vec[:, l, half * 3 + 0, :], mod[:, o + 16:o + 32], 1.0, gT[:, gpre, :], ALU.add, ALU.mult,
                [(MODB, (l, "mod")), (GT, gpre)], [(MODB, (l, half, 0))])
            CP(P, "dve", vec[:, l, half * 3 + 1, :], mod[:, o:o + 16], [(MODB, (l, "mod"))], [(MODB, (l, half, 1))])
            TT(P, "dve", vec[:, l, half * 3 + 2, :], mod[:, o + 32:o + 48], gT[:, gpost, :], ALU.mult,
               [(MODB, (l, "mod")), (GT, gpost)], [(MODB, (l, half, 2))])


def replicate_cols(C, rep, REP, colvec, ncols, R, psb, PSB, tmp, TMP):
    P, K, KB = C.P, C.K, C.KB
    for c0 in range(0, ncols, 4):
        n = min(4, ncols - c0)
        for j in range(n):
            c = c0 + j
            TS(P, "dve", tmp[:, j * 128:(j + 1) * 128], K["identf"][:], colvec[:, c:c + 1], None, ALU.mult, None,
               R + [(KB, "identf")], [(TMP, j)])
            MM(P, psb[:, j * 128:(j + 1) * 128], K["ones"][:], tmp[:, j * 128:(j + 1) * 128], True, True,
               [(TMP, j), (KB, "ones")], [(PSB, None)])
        CP(P, "act", rep[:, c0 * 128:(c0 + n) * 128], psb[:, 0:n * 128], [(PSB, None)], [(REP, c0 // 4)])


def norm_to_hT(C, t, ti, src_ap, SRCB, xt, XT, ss, rs, SS, xn, XN, junk, JK, tp, TP, hTb, HTB, A, Sv, VR, keep_x=None):
    P, K, KB = C.P, C.K, C.KB
    P.dma("sp", xt[:], src_ap[t * 128:(t + 1) * 128, :], reads=[(SRCB, t)], writes=[(XT, None)], sembuf=XT)
    ACTF(P, junk[:], xt[:], AF.Square, [(XT, None)], [(JK, None), (SS, "ss")], accum=ss[:])
    ACTF(P, rs[:], ss[:], AF.Sqrt, [(SS, "ss")], [(SS, "rs")], bias=EPS, scale=1.0 / D_)
    RECIP(P, rs[:], rs[:], [(SS, "rs")], [(SS, "rs")])
    TS(P, "dve", xn[:], xt[:], rs[:, 0:1], None, ALU.mult, None, [(XT, None), (SS, "rs")], [(XN, None)])
    for c in range(16):
        TR(P, tp[:, c * 128:(c + 1) * 128], xn[:, c * 128:(c + 1) * 128], K["ident"][:], [(XN, None), (KB, "ident")], [(TP, c // 8)])
    for c in range(16):
        o = hTb[:, c, ti * 128:(ti + 1) * 128]
        i_ = tp[:, c * 128:(c + 1) * 128]
        if c < 8:
            ACTF(P, o, i_, AF.Identity, [(TP, c // 8)] + VR, [(HTB, (ti, c))], bias=Sv[:, c:c + 1], scale=A[:, c:c + 1])
        else:
            TS(P, "dve", o, i_, A[:, c:c + 1], Sv[:, c:c + 1], ALU.mult, ALU.add, [(TP, c // 8)] + VR, [(HTB, (ti, c))])


def phase_inproj(C, l, src):
    P, I, D, B, K = C.P, C.I, C.D, C.B, C.K
    SRCB = B["XR"]
    xt = [C.sb("a_xt%d" % i, [128, D_], F32) for i in range(2)]; XT = [Buf("a_xt%d" % i) for i in range(2)]
    junk = C.sb("a_junk", [128, D_], BF16); JK = Buf("a_junk")
    ss = [C.sb("a_ss%d" % i, [128, 1], F32) for i in range(2)]
    rs = [C.sb("a_rs%d" % i, [128, 1], F32) for i in range(2)]; SS = [Buf("a_ss%d" % i) for i in range(2)]
    xn = [C.sb("a_xn%d" % i, [128, D_], BF16) for i in range(2)]; XN = [Buf("a_xn%d" % i) for i in range(2)]
    tp = [C.ps("a_tp%d" % i, [128, D_], BF16) for i in range(1)]; TP = [PB("a_tp%d" % i) for i in range(1)]
    hT = [C.sb("a_hT%d" % i, [128, 16, 512], BF16) for i in range(2)]; HT = [Buf("a_hT%d" % i) for i in range(2)]
    ws = [C.sb("a_ws%d" % i, [128, 16, 512], BF16) for i in range(2)]; WS = [Buf("a_ws%d" % i) for i in range(2)]
    pm = [C.ps("a_pm%d" % i, [128, 512]) for i in range(4)]; PM = [PB("a_pm%d" % i) for i in range(4)]
    st = [C.sb("a_st%d" % i, [128, 512], BF16) for i in range(4)]; ST = [Buf("a_st%d" % i) for i in range(4)]
    sg = [C.sb("a_sg%d" % i, [128, 16], F32) for i in range(2)]; SG = [Buf("a_sg%d" % i) for i in range(2)]
    A = K["vec"][:, l, 0, :]; Sv = K["vec"][:, l, 1, :]
    VR = [(C.MODB, (l, 0, 0)), (C.MODB, (l, 0, 1))]
    wfm = D["WFM"][l].rearrange("(kc p) n -> p kc n", p=128)
    wtm = D["WTM"][l].rearrange("(kc p) n -> p kc n", p=128)
    cnt = [0, 0, 0]

    def norm_block(blk):
        for ti in range(4):
            t = blk * 4 + ti
            s = t % 2
            norm_to_hT(C, t, ti, src, SRCB, xt[s], XT[s], ss[s], rs[s], SS[s], xn[s], XN[s], junk, JK, tp[0], TP[0],
                       hT[blk % 2], HT[blk % 2], A, Sv, VR)

    def gemm_block(blk):
        hTb, HTB = hT[blk % 2], HT[blk % 2]
        for s in range(6 if KNOB.get("fm", True) else 0):
            wi = cnt[0] % 2; cnt[0] += 1
            P.dma("sp", ws[wi][:], wfm[:, :, s * 512:(s + 1) * 512], reads=[(B["WFM"], None)], writes=[(WS[wi], None)], sembuf=WS[wi])
            for j in range(4):
                ch = s * 4 + j
                pi = cnt[1] % 4; cnt[1] += 1
                for kc in range(16):
                    MM(P, pm[pi][:], ws[wi][:, kc, j * 128:(j + 1) * 128], hTb[:, kc, :], kc == 0, kc == 15,
                       [(WS[wi], None), (HTB, None)], [(PM[pi], None)])
                CP(P, "act" if pi % 2 == 0 else "dve", st[pi][:], pm[pi][:], [(PM[pi], None)], [(ST[pi], None)])
                P.dma("pool", D["QKT"][ch][:, blk * 512:(blk + 1) * 512], st[pi][:], reads=[(ST[pi], None)],
                      writes=[(B["QKT"], (ch, blk))], sembuf=ST[pi])
        for s in range(4 if KNOB.get("tm", True) else 0):
            n0 = s * 512
            ncol = min(512, NTM - n0)
            wi = cnt[0] % 2; cnt[0] += 1
            P.dma("sp", ws[wi][:, :, 0:ncol], wtm[:, :, n0:n0 + ncol], reads=[(B["WTM"], None)], writes=[(WS[wi], None)], sembuf=WS[wi])
            for ti in range(4):
                t = blk * 4 + ti
                pi = cnt[1] % 4; cnt[1] += 1
                for kc in range(16):
                    MM(P, pm[pi][:, 0:ncol], hTb[:, kc, ti * 128:(ti + 1) * 128], ws[wi][:, kc, 0:ncol], kc == 0, kc == 15,
                       [(WS[wi], None), (HTB, None)], [(PM[pi], None)])
                CP(P, "act" if pi % 2 == 0 else "dve", st[pi][:, 0:ncol], pm[pi][:, 0:ncol], [(PM[pi], None)], [(ST[pi], None)])
                P.dma("pool", D["PTM"][t * 128:(t + 1) * 128, n0:n0 + ncol], st[pi][:, 0:ncol], reads=[(ST[pi], None)],
                      writes=[(B["PTM"], (t, s))], sembuf=ST[pi])
                if s == 3:
                    gi = cnt[2] % 2; cnt[2] += 1
                    CP(P, "dve", sg[gi][:], pm[pi][:, ncol - 16:ncol], [(PM[pi], None)], [(SG[gi], None)])
                    P.dma("pool", D["GATES"][t * 128:(t + 1) * 128, :], sg[gi][:], reads=[(SG[gi], None)],
                          writes=[(B["GATES"], t)], sembuf=SG[gi])

    nblk = KNOB.get("nblk", 8)
    norm_block(0)
    for blk in range(nblk):
        if blk + 1 < nblk:
            norm_block(blk + 1)
        if KNOB.get("gemm", True):
            gemm_block(blk)


def phase_window(C, l, bg=None):
    P, I, D, B, K = C.P, C.I, C.D, C.B, C.K
    E = C.sb("w_E", [128, 12, 384], F32); EB = Buf("w_E")
    snk = C.sb("w_snk", [128, 12], F32); SK = Buf("w_snk")
    qt = [C.sb("w_q%d" % i, [128, 6, 128], BF16) for i in range(2)]; QT = [Buf("w_q%d" % i) for i in range(2)]
    kt = [C.sb("w_k%d" % i, [128, 4, 384], BF16) for i in range(2)]; KT = [Buf("w_k%d" % i) for i in range(2)]
    vt = [C.sb("w_v%d" % i, [128, 3, 4, 65], BF16) for i in range(2)]; VT = [Buf("w_v%d" % i) for i in range(2)]
    pss = [C.ps("w_ps%d" % i, [128, 512]) for i in range(2)]; PSS = [PB("w_ps%d" % i) for i in range(2)]
    acc = [C.ps("w_acc%d" % i, [128, 512]) for i in range(2)]; ACC = [PB("w_acc%d" % i) for i in range(2)]
    pe_ = [C.sb("w_pe%d" % i, [128, 384], F32) for i in range(2)]; PEB = [Buf("w_pe%d" % i) for i in range(2)]
    pT = [C.sb("w_pT%d" % i, [128, 384], BF16) for i in range(2)]; PT = [Buf("w_pT%d" % i) for i in range(2)]
    den = [C.sb("w_den%d" % i, [128, 12], F32) for i in range(2)]; DEN = [Buf("w_den%d" % i) for i in range(2)]
    ya = [C.sb("w_ya%d" % i, [128, 768], BF16) for i in range(2)]; YA = [Buf("w_ya%d" % i) for i in range(2)]
    P.dma("sp", E[:].rearrange("p h c -> p (h c)"), I["c_E"], writes=[(EB, None)], sembuf=EB)
    P.dma("sp", snk[:], I["attn_sink"][l].partition_broadcast(128), writes=[(SK, None)], sembuf=SK)
    ACTF(P, snk[:], snk[:], AF.Exp, [(SK, None)], [(SK, None)])
    for i in range(2):
        MEMSET(P, "dve", vt[i][:], 1.0, [], [(VT[i], None)])
    qk = D["QKT"].rearrange("c p t -> p c t")
    scale = 64 ** -0.5
    hc = 0
    for i in range(NT):
        if bg is not None:
            next(bg, None)
        s = i % 2
        j0, j1 = max(0, i - 1), min(NT - 1, i + 1)
        d0, d1 = j0 - (i - 1), j1 - (i - 1)
        P.dma("sp", qt[s][:], qk[:, FM_AQ:FM_AQ + 6, i * 128:(i + 1) * 128], reads=[(B["QKT"], None)], writes=[(QT[s], None)], sembuf=QT[s])
        P.dma("sp", kt[s][:, :, d0 * 128:(d1 + 1) * 128], qk[:, FM_AK:FM_AK + 4, j0 * 128:(j1 + 1) * 128], reads=[(B["QKT"], None)],
              writes=[(KT[s], None)], sembuf=KT[s])
        for d in range(d0, d1 + 1):
            j = i - 1 + d
            P.dma("sp", vt[s][:, d, :, 0:64], D["PTM"][j * 128:(j + 1) * 128, 0:256].rearrange("p (h d) -> p h d", d=64),
                  reads=[(B["PTM"], None)], writes=[(VT[s], None)], sembuf=VT[s])
        lo, hi = d0 * 128, (d1 + 1) * 128
        for hq in range(12):
            g = hq // 3; off = (hq % 2) * 64; c = hq // 2
            b = hc % 2; hc += 1
            for d in range(d0, d1 + 1):
                MM(P, pss[b][:, d * 128:(d + 1) * 128], kt[s][off:off + 64, g, d * 128:(d + 1) * 128], qt[s][off:off + 64, c, :], True, True,
                   [(KT[s], None), (QT[s], None)], [(PSS[b], None)])
            ACTF(P, pe_[b][:, lo:hi], pss[b][:, lo:hi], AF.Exp, [(PSS[b], None)], [(PEB[b], None)], scale=scale)
            TT(P, "dve", pT[b][:, lo:hi], pe_[b][:, lo:hi], E[:, hq, lo:hi], ALU.mult, [(PEB[b], None), (EB, None)], [(PT[b], None)])
            a = acc[hq // 6]; AB = ACC[hq // 6]
            co = (hq % 6) * 65
            for d in range(d0, d1 + 1):
                MM(P, a[:, co:co + 65], pT[b][:, d * 128:(d + 1) * 128], vt[s][:, d, g, :], d == d0, d == d1,
                   [(PT[b], None), (VT[s], None)], [(AB, None)])
        for hq in range(12):
            a = acc[hq // 6]; AB = ACC[hq // 6]; co = (hq % 6) * 65
            TS(P, "dve", den[s][:, hq:hq + 1], a[:, co + 64:co + 65], snk[:, hq:hq + 1], None, ALU.add, None, [(AB, None), (SK, None)], [(DEN[s], None)])
        RECIP(P, den[s][:], den[s][:], [(DEN[s], None)], [(DEN[s], None)])
        for hq in range(12):
            a = acc[hq // 6]; AB = ACC[hq // 6]; co = (hq % 6) * 65
            TS(P, "dve", ya[s][:, hq * 64:(hq + 1) * 64], a[:, co:co + 64], den[s][:, hq:hq + 1], None, ALU.mult, None,
               [(AB, None), (DEN[s], None)], [(YA[s], None)])
        P.dma("pool", D["Y"][i * 128:(i + 1) * 128, 0:768], ya[s][:], reads=[(YA[s], None)], writes=[(B["Y"], ("a", i))], sembuf=YA[s])
    if bg is not None:
        for _ in bg:
            pass


def rep_sumsq(C, sq_chunks, R, rep_ps, RPS, rstd, RSTD, n, width):
    P, K, KB = C.P, C.K, C.KB
    for i, (ap, rows) in enumerate(sq_chunks):
        MM(P, rep_ps[:, 0:width], K["onesb"][0:rows, :], ap, i == 0, i == len(sq_chunks) - 1, R + [(KB, "onesb")], [(RPS, None)])
    ACTF(P, rstd[:, 0:width], rep_ps[:, 0:width], AF.Sqrt, [(RPS, None)], [(RSTD, None)], bias=EPS, scale=1.0 / n)
    RECIP(P, rstd[:, 0:width], rstd[:, 0:width], [(RSTD, None)], [(RSTD, None)])


def phase_mla_prep(C, l):
    P, I, D, B, K, KB = C.P, C.I, C.D, C.B, C.K, C.KB
    wqf = C.sb("p_wqf", [128, 4, 768], F32); WQF = Buf("p_wqf")
    wq = C.sb("p_wq", [128, 4, 768], BF16); WQ = Buf("p_wq")
    wkf = C.sb("p_wkf", [128, 1024], F32); WKF = Buf("p_wkf")
    wk = C.sb("p_wk", [128, 1024], BF16); WK = Buf("p_wk")
    gq = C.sb("p_gq", [128, 4], F32); GQ = Buf("p_gq")
    gk = C.sb("p_gk", [128, 1], F32); GK = Buf("p_gk")
    P.dma("sp", wqf[:], I["w_uq"][l].rearrange("(kc p) n -> p kc n", p=128), writes=[(WQF, None)], sembuf=WQF)
    P.dma("sp", wkf[:], I["w_ukv"][l], writes=[(WKF, None)], sembuf=WKF)
    P.dma("sp", gq[:], I["q_norm_gT"][l], writes=[(GQ, None)], sembuf=GQ)
    P.dma("sp", gk[:], I["kv_norm_gT"][l], writes=[(GK, None)], sembuf=GK)
    for kc in range(4):
        TS(P, "dve", wq[:, kc, :], wqf[:, kc, :], gq[:, kc:kc + 1], None, ALU.mult, None, [(WQF, None), (GQ, None)], [(WQ, kc)])
    TS(P, "dve", wk[:], wkf[:], gk[:, 0:1], None, ALU.mult, None, [(WKF, None), (GK, None)], [(WK, None)])
    posi = C.sb("p_posi", [64, S_], I32); POSI = Buf("p_posi")
    ang = C.sb("p_ang", [64, S_], F32); ANG = Buf("p_ang")
    cosT = C.sb("p_cos", [64, S_], F32); COS = Buf("p_cos")
    sinT = C.sb("p_sin", [64, S_], F32); SIN = Buf("p_sin")
    invf = C.sb("p_invf", [64, 1], F32); INVF = Buf("p_invf")
    P.dma("sp", posi[:], I["pos"].partition_broadcast(64), writes=[(POSI, None)], sembuf=POSI)
    P.dma("sp", invf[:], I["c_invf"], writes=[(INVF, None)], sembuf=INVF)
    CP(P, "dve", ang[:], posi[:], [(POSI, None)], [(ANG, None)])
    TS(P, "dve", ang[:], ang[:], invf[:, 0:1], None, ALU.mult, None, [(ANG, None), (INVF, None)], [(ANG, None)])
    TWO_PI = 2.0 * math.pi
    MAGIC = 12582912.0
    TS(P, "dve", sinT[:], ang[:], 1.0 / TWO_PI, MAGIC, ALU.mult, ALU.add, [(ANG, None)], [(SIN, None)])
    TS(P, "dve", sinT[:], sinT[:], -MAGIC, None, ALU.add, None, [(SIN, None)], [(SIN, None)])
    STT(P, sinT[:], sinT[:], -TWO_PI, ang[:], ALU.mult, ALU.add, [(SIN, None), (ANG, None)], [(SIN, None)])
    TS(P, "dve", ang[:], ang[:], 0.5 * math.pi, None, ALU.add, None, [(ANG, None)], [(ANG, None)])
    TS(P, "dve", cosT[:], ang[:], 1.0 / TWO_PI, MAGIC, ALU.mult, ALU.add, [(ANG, None)], [(COS, None)])
    TS(P, "dve", cosT[:], cosT[:], -MAGIC, None, ALU.add, None, [(COS, None)], [(COS, None)])
    STT(P, cosT[:], cosT[:], -TWO_PI, ang[:], ALU.mult, ALU.add, [(COS, None), (ANG, None)], [(COS, None)])
    PI_LO = 3.1415925
    TS(P, "dve", sinT[:], sinT[:], -PI_LO, PI_LO, ALU.max, ALU.min, [(SIN, None)], [(SIN, None)])
    TS(P, "dve", cosT[:], cosT[:], -PI_LO, PI_LO, ALU.max, ALU.min, [(COS, None)], [(COS, None)])
    ACTF(P, sinT[:], sinT[:], AF.Sin, [(SIN, None)], [(SIN, None)])
    ACTF(P, cosT[:], cosT[:], AF.Sin, [(COS, None)], [(COS, None)])

    cq = [C.sb("p_cq%d" % i, [128, 4, 512], BF16) for i in range(2)]; CQ = [Buf("p_cq%d" % i) for i in range(2)]
    ckv = [C.sb("p_ckv%d" % i, [128, 512], BF16) for i in range(2)]; CKV = [Buf("p_ckv%d" % i) for i in range(2)]
    kr = [C.sb("p_kr%d" % i, [64, 512], BF16) for i in range(2)]; KRB = [Buf("p_kr%d" % i) for i in range(2)]
    sq = C.sb("p_sq", [128, 4, 512], BF16); SQ = Buf("p_sq")
    sqk = C.sb("p_sqk", [128, 512], BF16); SQK = Buf("p_sqk")
    rps = C.ps("p_rps", [128, 512]); RPS = PB("p_rps")
    rstd = C.sb("p_rstd", [128, 512], F32); RSTD = Buf("p_rstd")
    rstdk = C.sb("p_rstdk", [128, 512], F32); RSTDK = Buf("p_rstdk")
    pq = [C.ps("p_pq%d" % i, [128, 512]) for i in range(3)]; PQ = [PB("p_pq%d" % i) for i in range(3)]
    prot = C.ps("p_prot", [64, 512]); PROT = PB("p_prot")
    pv = C.ps("p_pv", [128, 512]); PV = PB("p_pv")
    ptm = C.ps("p_ptm", [128, 4, 2]); PTM_ = PB("p_ptm")
    so = [C.sb("p_so%d" % i, [128, 512], BF16) for i in range(3)]; SO = [Buf("p_so%d" % i) for i in range(3)]
    t1 = C.sb("p_t1", [64, 512], F32); T1 = Buf("p_t1")
    t2 = C.sb("p_t2", [64, 512], F32); T2 = Buf("p_t2")
    raw = C.sb("p_raw", [64, 512], BF16); RAW = Buf("p_raw")
    rtm = C.sb("p_rtm", [128, 4], F32); RTM = Buf("p_rtm")
    vo = [C.sb("p_vo%d" % i, [128, 512], BF16) for i in range(2)]; VO = [Buf("p_vo%d" % i) for i in range(2)]
    qk = D["QKT"].rearrange("c p t -> p c t")
    oc = [0]

    def rope_out(src_ps, SRC, rst, RST, blk, dst_ap, DSTB, dkey):
        cs = slice(blk * 512, (blk + 1) * 512)
        if rst is not None:
            TT(P, "dve", raw[:], src_ps, rst[0:64, :], ALU.mult, [(SRC, None), (RST, None)], [(RAW, None)])
        else:
            CP(P, "dve", raw[:], src_ps, [(SRC, None)], [(RAW, None)])
        MM(P, prot[:], K["R"][:], raw[:], True, True, [(RAW, None), (KB, "R")], [(PROT, None)])
        TT(P, "dve", t1[:], raw[:], cosT[:, cs], ALU.mult, [(RAW, None), (COS, None)], [(T1, None)])
        TT(P, "dve", t2[:], prot[:], sinT[:, cs], ALU.mult, [(PROT, None), (SIN, None)], [(T2, None)])
        o = oc[0] % 3; oc[0] += 1
        TT(P, "dve", so[o][0:64, :], t1[:], t2[:], ALU.add, [(T1, None), (T2, None)], [(SO[o], None)])
        P.dma("pool", dst_ap, so[o][0:64, :], reads=[(SO[o], None)], writes=[(DSTB, dkey)], sembuf=SO[o])

    for blk in range(8):
        s = blk % 2
        cs = slice(blk * 512, (blk + 1) * 512)
        P.dma("sp", cq[s][:], qk[:, FM_BCQ:FM_BCQ + 4, cs], reads=[(B["QKT"], None)], writes=[(CQ[s], None)], sembuf=CQ[s])
        P.dma("sp", ckv[s][:], D["QKT"][FM_BCKV][:, cs], reads=[(B["QKT"], None)], writes=[(CKV[s], None)], sembuf=CKV[s])
        P.dma("sp", kr[s][:], D["QKT"][FM_BKR][0:64, cs], reads=[(B["QKT"], None)], writes=[(KRB[s], None)], sembuf=KRB[s])
        rows = [128, 128, 128, 64]
        ACTF(P, sq[:], cq[s][:], AF.Square, [(CQ[s], None)], [(SQ, None)])
        rep_sumsq(C, [(sq[0:rows[kc], kc, :], rows[kc]) for kc in range(4)], [(SQ, None)], rps, RPS, rstd, RSTD, 448.0, 512)
        for h in range(4):
            pi = h % 3
            for kc in range(4):
                MM(P, pq[pi][:], wq[0:rows[kc], kc, h * 192:h * 192 + 128], cq[s][0:rows[kc], kc, :], kc == 0, kc == 3,
                   [(WQ, None), (CQ[s], None)], [(PQ[pi], None)])
            o = oc[0] % 3; oc[0] += 1
            TT(P, "dve", so[o][:], pq[pi][:], rstd[:], ALU.mult, [(PQ[pi], None), (RSTD, None)], [(SO[o], None)])
            P.dma("pool", D["QN"][h][:, cs], so[o][:], reads=[(SO[o], None)], writes=[(B["QN"], (h, blk))], sembuf=SO[o])
            pi = (h + 1) % 3
            for kc in range(4):
                MM(P, pq[pi][0:64, :], wq[0:rows[kc], kc, h * 192 + 128:h * 192 + 192], cq[s][0:rows[kc], kc, :], kc == 0, kc == 3,
                   [(WQ, None), (CQ[s], None)], [(PQ[pi], None)])
            rope_out(pq[pi][0:64, :], PQ[pi], rstd, RSTD, blk, D["QR"][h][:, cs], B["QR"], (h, blk))
        ACTF(P, sqk[:], ckv[s][:], AF.Square, [(CKV[s], None)], [(SQK, None)])
        rep_sumsq(C, [(sqk[:], 128)], [(SQK, None)], rps, RPS, rstdk, RSTDK, 128.0, 512)
        for h in range(4):
            pi = h % 3
            MM(P, pq[pi][:], wk[:, h * 256:h * 256 + 128], ckv[s][:], True, True, [(WK, None), (CKV[s], None)], [(PQ[pi], None)])
            o = oc[0] % 3; oc[0] += 1
            TT(P, "dve", so[o][:], pq[pi][:], rstdk[:], ALU.mult, [(PQ[pi], None), (RSTDK, None)], [(SO[o], None)])
            P.dma("pool", D["KN"][h][:, cs], so[o][:], reads=[(SO[o], None)], writes=[(B["KN"], (h, blk))], sembuf=SO[o])
        for ti in range(4):
            MM(P, ptm[:, ti, :], sqk[:, ti * 128:(ti + 1) * 128], K["onesb"][:, 0:2], True, True, [(SQK, None), (KB, "onesb")], [(PTM_, None)])
        ACTF(P, rtm[:], ptm[:, :, 0], AF.Sqrt, [(PTM_, None)], [(RTM, None)], bias=EPS, scale=1.0 / 128.0)
        RECIP(P, rtm[:], rtm[:], [(RTM, None)], [(RTM, None)])
        for ti in range(4):
            t = blk * 4 + ti
            for h in range(4):
                MM(P, pv[:, h * 128:(h + 1) * 128], ckv[s][:, ti * 128:(ti + 1) * 128], wk[:, h * 256 + 128:h * 256 + 256], True, True,
                   [(WK, None), (CKV[s], None)], [(PV, None)])
            v = t % 2
            TS(P, "dve", vo[v][:], pv[:], rtm[:, ti:ti + 1], None, ALU.mult, None, [(PV, None), (RTM, None)], [(VO[v], None)])
            P.dma("pool", D["VB"][t * 128:(t + 1) * 128, :], vo[v][:], reads=[(VO[v], None)], writes=[(B["VB"], t)], sembuf=VO[v])
        MM(P, prot[:], K["R"][:], kr[s][:], True, True, [(KRB[s], None), (KB, "R")], [(PROT, None)])
        TT(P, "dve", t1[:], kr[s][:], cosT[:, cs], ALU.mult, [(KRB[s], None), (COS, None)], [(T1, None)])
        TT(P, "dve", t2[:], prot[:], sinT[:, cs], ALU.mult, [(PROT, None), (SIN, None)], [(T2, None)])
        o = oc[0] % 3; oc[0] += 1
        TT(P, "dve", so[o][0:64, :], t1[:], t2[:], ALU.add, [(T1, None), (T2, None)], [(SO[o], None)])
        P.dma("pool", D["KR"][:, cs], so[o][0:64, :], reads=[(SO[o], None)], writes=[(B["KR"], blk)], sembuf=SO[o])


def phase_mla_attn(C, l, bg=None):
    P, I, D, B, K = C.P, C.I, C.D, C.B, C.K
    krT = C.sb("m_kr", [64, S_], BF16); KRT = Buf("m_kr")
    qn = C.sb("m_qn", [128, S_], BF16); QN = Buf("m_qn")
    qr = C.sb("m_qr", [64, S_], BF16); QR = Buf("m_qr")
    kn = C.sb("m_kn", [128, S_], BF16); KN = Buf("m_kn")
    va = C.sb("m_va", [128, NT, 129], BF16); VA = Buf("m_va")
    pss = [C.ps("m_ps%d" % i, [128, 512]) for i in range(2)]; PSS = [PB("m_ps%d" % i) for i in range(2)]
    acc = [C.ps("m_acc%d" % i, [128, 512]) for i in range(4)]; ACC = [PB("m_acc%d" % i) for i in range(4)]
    pT = [C.sb("m_pT%d" % i, [128, 512], BF16) for i in range(3)]; PT = [Buf("m_pT%d" % i) for i in range(3)]
    rd = [C.sb("m_rd%d" % i, [128, 1], F32) for i in range(2)]; RD = [Buf("m_rd%d" % i) for i in range(2)]
    yo = [C.sb("m_yo%d" % i, [128, 128], BF16) for i in range(2)]; YO = [Buf("m_yo%d" % i) for i in range(2)]
    scale = 192 ** -0.5
    P.dma("sp", krT[:], D["KR"], reads=[(B["KR"], None)], writes=[(KRT, None)], sembuf=KRT)
    MEMSET(P, "dve", va[:], 1.0, [], [(VA, "ones")])
    n = 0
    oc = 0
    for h in range(4):
        P.dma("sp", qn[:], D["QN"][h], reads=[(B["QN"], None)], writes=[(QN, None)], sembuf=QN)
        P.dma("sp", qr[:], D["QR"][h], reads=[(B["QR"], None)], writes=[(QR, None)], sembuf=QR)
        P.dma("sp", kn[:], D["KN"][h], reads=[(B["KN"], None)], writes=[(KN, None)], sembuf=KN)
        vbv = D["VB"][:, h * 128:(h + 1) * 128].rearrange("(t p) d -> p t d", p=128)
        for t0 in range(0, NT, 4):
            P.dma("sp", va[:, t0:t0 + 4, 0:128], vbv[:, t0:t0 + 4, :], reads=[(B["VB"], None), (VA, "ones")],
                  writes=[(VA, ("v", t0))], sembuf=VA)
        for qb in range(8):
            qs = slice(qb * 512, (qb + 1) * 512)
            for j in range(NT):
                ks = slice(j * 128, (j + 1) * 128)
                b = n % 2; pb = n % 3; n += 1
                if bg is not None and n % 4 == 0:
                    next(bg, None)
                MM(P, pss[b][:], kn[:, ks], qn[:, qs], True, False, [(KN, None), (QN, None)], [(PSS[b], None)])
                MM(P, pss[b][:], krT[:, ks], qr[:, qs], False, True, [(KRT, None), (QR, None)], [(PSS[b], None)])
                ACTF(P, pT[pb][:], pss[b][:], AF.Exp, [(PSS[b], None)], [(PT[pb], None)], scale=scale)
                for ii in range(4):
                    MM(P, acc[ii][:, 0:129], pT[pb][:, ii * 128:(ii + 1) * 128], va[:, j, :], j == 0, j == NT - 1,
                       [(PT[pb], None), (VA, ("v", (j // 4) * 4)), (VA, "ones")], [(ACC[ii], None)])
            for ii in range(4):
                t = qb * 4 + ii
                o = oc % 2; oc += 1
                RECIP(P, rd[o][:], acc[ii][:, 128:129], [(ACC[ii], None)], [(RD[o], None)])
                TS(P, "dve", yo[o][:], acc[ii][:, 0:128], rd[o][:, 0:1], None, ALU.mult, None, [(ACC[ii], None), (RD[o], None)], [(YO[o], None)])
                P.dma("pool", D["Y"][t * 128:(t + 1) * 128, 768 + h * 128:768 + (h + 1) * 128], yo[o][:], reads=[(YO[o], None)],
                      writes=[(B["Y"], ("b", h, t))], sembuf=YO[o])
    if bg is not None:
        for _ in bg:
            pass


def phase_mlstm_prep(C, l):
    P, I, D, B, K, KB = C.P, C.I, C.D, C.B, C.K, C.KB
    g = C.sb("g_g", [128, NT, 16], F32); G = Buf("g_g")
    gb = C.sb("g_gb", [128, 16], F32); GB = Buf("g_gb")
    lf = C.sb("g_lf", [128, NT, 8], F32); LF = Buf("g_lf")
    tot = C.sb("g_tot", [128, NT, 8], F32); TOT = Buf("g_tot")
    off = C.sb("g_off", [128, NT, 8], F32); OFF = Buf("g_off")
    cum = C.sb("g_cum", [128, NT, 8], F32); CUM = Buf("g_cum")
    ibs = C.sb("g_ibs", [128, NT, 8], F32); IBS = Buf("g_ibs")
    pt = C.ps("g_pt", [128, 256]); PTB = PB("g_pt")
    pc = C.ps("g_pc", [128, 256]); PCB = PB("g_pc")
    pc2 = C.ps("g_pc2", [128, 256]); PCB2 = PB("g_pc2")
    pr = [C.ps("g_pr%d" % i, [128, 512]) for i in range(2)]; PR = [PB("g_pr%d" % i) for i in range(2)]
    dg = [C.sb("g_dg%d" % i, [128, 512], F32) for i in range(2)]; DG = [Buf("g_dg%d" % i) for i in range(2)]
    ro = [C.sb("g_ro%d" % i, [128, 512], F32) for i in range(2)]; RO = [Buf("g_ro%d" % i) for i in range(2)]
    gv = D["GATES"].rearrange("(t p) c -> p t c", p=128)
    for t0 in range(0, NT, 4):
        P.dma("sp", g[:, t0:t0 + 4, :], gv[:, t0:t0 + 4, :], reads=[(B["GATES"], None)], writes=[(G, ("ld", t0))], sembuf=G)
    P.dma("sp", gb[:], I["gate_b"][l].partition_broadcast(128), writes=[(GB, None)], sembuf=GB)
    for t in range(NT):
        TT(P, "dve", g[:, t, :], g[:, t, :], gb[:], ALU.add, [(G, None), (GB, None)], [(G, None)])
    ACTF(P, lf[:], g[:, :, 8:16], AF.Exp, [(G, None)], [(LF, None)], scale=-1.0)
    ACTF(P, lf[:], lf[:], AF.Ln, [(LF, None)], [(LF, None)], bias=1.0)
    TS(P, "dve", lf[:], lf[:], -1.0, None, ALU.mult, None, [(LF, None)], [(LF, None)])
    lf2 = lf[:].rearrange("p t c -> p (t c)")
    MM(P, pt[:], K["ones"][:], lf2, True, True, [(LF, None), (KB, "ones")], [(PTB, None)])
    CP(P, "dve", tot[:].rearrange("p t c -> p (t c)"), pt[:], [(PTB, None)], [(TOT, None)])
    MEMSET(P, "dve", off[:], 0.0, [], [(OFF, None)])
    for t in range(1, NT):
        TT(P, "dve", off[:, t, 0:4], off[:, t - 1, 0:4], tot[:, t - 1, 0:4], ALU.add, [(OFF, None), (TOT, None)], [(OFF, None)])
    for t in range(NT - 2, -1, -1):
        TT(P, "dve", off[:, t, 4:8], off[:, t + 1, 4:8], tot[:, t + 1, 4:8], ALU.add, [(OFF, None), (TOT, None)], [(OFF, None)])
    MM(P, pc[:], K["U"][:], lf2, True, True, [(LF, None), (KB, "U")], [(PCB, None)])
    MM(P, pc2[:], K["Lm"][:], lf2, True, True, [(LF, None), (KB, "L")], [(PCB2, None)])
    pc3 = pc[:].rearrange("p (t c) -> p t c", c=8)
    pc23 = pc2[:].rearrange("p (t c) -> p t c", c=8)
    TT(P, "dve", cum[:, :, 0:4], pc3[:, :, 0:4], off[:, :, 0:4], ALU.add, [(PCB, None), (OFF, None)], [(CUM, "f")])
    TT(P, "dve", cum[:, :, 4:8], pc23[:, :, 4:8], off[:, :, 4:8], ALU.add, [(PCB2, None), (OFF, None)], [(CUM, "b")])
    TT(P, "dve", ibs[:], g[:, :, 0:8], cum[:], ALU.subtract, [(G, None), (CUM, None)], [(IBS, None)])
    P.dma("pool", D["IBS"], ibs[:].rearrange("p t c -> p (t c)"), reads=[(IBS, None)], writes=[(B["IBS"], None)], sembuf=IBS)
    n = 0
    for c in range(8):
        for t0 in range(0, NT, 4):
            b = n % 2; n += 1
            for j in range(4):
                t = t0 + j
                TS(P, "dve", dg[b][:, j * 128:(j + 1) * 128], K["identf"][:], cum[:, t, c:c + 1], None, ALU.mult, None,
                   [(CUM, None), (KB, "identf")], [(DG[b], j)])
                MM(P, pr[b][:, j * 128:(j + 1) * 128], K["ones"][:], dg[b][:, j * 128:(j + 1) * 128], True, True,
                   [(DG[b], j), (KB, "ones")], [(PR[b], None)])
            CP(P, "act", ro[b][:], pr[b][:], [(PR[b], None)], [(RO[b], None)])
            P.dma("pool", D["BREP"][c][:, t0 * 128:(t0 + 4) * 128], ro[b][:], reads=[(RO[b], None)], writes=[(B["BREP"], (c, t0))], sembuf=RO[b])


def phase_mlstm_attn(C, l, bg=None):
    P, I, D, B, K, KB = C.P, C.I, C.D, C.B, C.K, C.KB
    ibs = C.sb("s_ibs", [128, NT, 8], F32); IBS = Buf("s_ibs")
    hg = C.sb("s_hg", [128, 768], F32); HG = Buf("s_hg")
    qT = C.sb("s_qT", [96, S_], BF16); QT = Buf("s_qT")
    kT = C.sb("s_kT", [96, S_], BF16); KT = Buf("s_kT")
    va = C.sb("s_va", [128, NT, 193], BF16); VA = Buf("s_va")
    br = [C.sb("s_br%d" % i, [128, S_], F32) for i in range(2)]; BR = [Buf("s_br%d" % i) for i in range(2)]
    pss = [C.ps("s_ps%d" % i, [128, 256]) for i in range(2)]; PSS = [PB("s_ps%d" % i) for i in range(2)]
    acc = [[C.ps("s_acc%d%d" % (d, i), [128, 512]) for i in range(2)] for d in range(2)]
    ACC = [[PB("s_acc%d%d" % (d, i)) for i in range(2)] for d in range(2)]
    w = [C.sb("s_w%d" % i, [128, 256], F32) for i in range(3)]; WB = [Buf("s_w%d" % i) for i in range(3)]
    wT = [C.sb("s_wT%d" % i, [128, 256], BF16) for i in range(3)]; WT = [Buf("s_wT%d" % i) for i in range(3)]
    op_ = [C.sb("s_op%d" % i, [128, 192], BF16) for i in range(2)]; OP = [Buf("s_op%d" % i) for i in range(2)]
    dn = [C.sb("s_dn%d" % i, [128, 2], F32) for i in range(2)]; DN = [Buf("s_dn%d" % i) for i in range(2)]
    hs = [C.sb("s_hs%d" % i, [128, 192], F32) for i in range(2)]; HS = [Buf("s_hs%d" % i) for i in range(2)]
    hb = [C.sb("s_hb%d" % i, [128, 192], F32) for i in range(2)]; HB = [Buf("s_hb%d" % i) for i in range(2)]
    jk = C.sb("s_jk", [128, 192], F32); JK = Buf("s_jk")
    ssq = [C.sb("s_ssq%d" % i, [128, 1], F32) for i in range(2)]; SSQ = [Buf("s_ssq%d" % i) for i in range(2)]
    yo = [C.sb("s_yo%d" % i, [128, 192], BF16) for i in range(2)]; YO = [Buf("s_yo%d" % i) for i in range(2)]
    scale = 96 ** -0.5
    P.dma("sp", ibs[:].rearrange("p t c -> p (t c)"), D["IBS"], reads=[(B["IBS"], None)], writes=[(IBS, None)], sembuf=IBS)
    P.dma("sp", hg[:], I["head_g"][l].partition_broadcast(128), writes=[(HG, None)], sembuf=HG)
    MEMSET(P, "dve", va[:], 1.0, [], [(VA, "ones")])
    U, Lm = K["U"], K["Lm"]
    n = 0
    fc = 0
    for h in range(4):
        P.dma("sp", qT[:], D["QKT"][FM_CQ + h][0:96, :], reads=[(B["QKT"], None)], writes=[(QT, None)], sembuf=QT)
        P.dma("sp", kT[:], D["QKT"][FM_CK + h][0:96, :], reads=[(B["QKT"], None)], writes=[(KT, None)], sembuf=KT)
        cvv = D["PTM"][:, 256 + h * 192:256 + (h + 1) * 192].rearrange("(t p) d -> p t d", p=128)
        for t0 in range(0, NT, 4):
            P.dma("sp", va[:, t0:t0 + 4, 0:192], cvv[:, t0:t0 + 4, :], reads=[(B["PTM"], None), (VA, "ones")],
                  writes=[(VA, ("v", t0))], sembuf=VA)
        for d in range(2):
            P.dma("sp", br[d][:], D["BREP"][d * 4 + h], reads=[(B["BREP"], None)], writes=[(BR[d], None)], sembuf=BR[d])
        for lb in range(16):
            i0 = lb * 2
            ls = slice(lb * 256, (lb + 1) * 256)
            first = [[True, True], [True, True]]
            nexp = [[0, 0], [0, 0]]
            total = [[i0 + 1, i0 + 2], [NT - i0, NT - i0 - 1]]
            for j in range(NT):
                ks = slice(j * 128, (j + 1) * 128)
                b = n % 2; n += 1
                if bg is not None and n % 8 == 0:
                    next(bg, None)
                MM(P, pss[b][:], kT[:, ks], qT[:, ls], True, True, [(KT, None), (QT, None)], [(PSS[b], None)])
                if j < i0:
                    items = [(0, 0, 2, False)]
                elif j > i0 + 1:
                    items = [(1, 0, 2, False)]
                elif j == i0:
                    items = [(0, 0, 1, True), (1, 0, 1, True), (0, 1, 1, False)]
                else:
                    items = [(1, 0, 1, False), (0, 1, 1, True), (1, 1, 1, True)]
                for (d, a, cn, masked) in items:
                    wi = fc % 3; fc += 1
                    c = d * 4 + h
                    lsl = slice((i0 + a) * 128, (i0 + a + cn) * 128)
                    wv = w[wi][:, 0:cn * 128]
                    ACTF(P, wv, br[d][:, lsl], AF.Exp, [(BR[d], None), (IBS, None)], [(WB[wi], None)], bias=ibs[:, j, c:c + 1])
                    if masked:
                        TT(P, "dve", wv, wv, (U if d == 0 else Lm)[:], ALU.mult, [(WB[wi], None), (KB, "U"), (KB, "L")], [(WB[wi], None)])
                    STT(P, wT[wi][:, 0:cn * 128], pss[b][:, a * 128:(a + cn) * 128], scale, wv, ALU.mult, ALU.mult,
                        [(PSS[b], None), (WB[wi], None)], [(WT[wi], None)])
                    for q in range(cn):
                        ii = a + q
                        nexp[d][ii] += 1
                        MM(P, acc[d][ii][:, 0:193], wT[wi][:, q * 128:(q + 1) * 128], va[:, j, :], nexp[d][ii] == 1, nexp[d][ii] == total[d][ii],
                           [(WT[wi], None), (VA, ("v", (j // 4) * 4)), (VA, "ones")], [(ACC[d][ii], None)])
            for ii in range(2):
                t = i0 + ii
                o = t % 2
                for d in range(2):
                    a = acc[d][ii]
                    ACTF(P, dn[o][:, d:d + 1], a[:, 192:193], AF.Abs, [(ACC[d][ii], None)], [(DN[o], d)])
                TS(P, "dve", dn[o][:], dn[o][:], 1.0, None, ALU.max, None, [(DN[o], None)], [(DN[o], None)])
                RECIP(P, dn[o][:], dn[o][:], [(DN[o], None)], [(DN[o], None)])
                TS(P, "dve", hs[o][:], acc[0][ii][:, 0:192], dn[o][:, 0:1], None, ALU.mult, None, [(ACC[0][ii], None), (DN[o], None)], [(HS[o], None)])
                TS(P, "dve", hb[o][:], acc[1][ii][:, 0:192], dn[o][:, 1:2], None, ALU.mult, None, [(ACC[1][ii], None), (DN[o], None)], [(HB[o], None)])
                TT(P, "dve", hs[o][:], hs[o][:], hb[o][:], ALU.add, [(HS[o], None), (HB[o], None)], [(HS[o], None)])
                ACTF(P, jk[:], hs[o][:], AF.Square, [(HS[o], None)], [(JK, None), (SSQ[o], None)], accum=ssq[o][:])
                ACTF(P, ssq[o][:], ssq[o][:], AF.Sqrt, [(SSQ[o], None)], [(SSQ[o], None)], bias=EPS, scale=1.0 / 192.0)
                RECIP(P, ssq[o][:], ssq[o][:], [(SSQ[o], None)], [(SSQ[o], None)])
                P.dma("sp", op_[o][:], D["PTM"][t * 128:(t + 1) * 128, 1024 + h * 192:1024 + (h + 1) * 192], reads=[(B["PTM"], None)],
                      writes=[(OP[o], None)], sembuf=OP[o])
                ACTF(P, hb[o][:], op_[o][:], AF.Sigmoid, [(OP[o], None)], [(HB[o], None)])
                STT(P, hs[o][:], hs[o][:], ssq[o][:, 0:1], hg[:, h * 192:(h + 1) * 192], ALU.mult, ALU.mult,
                    [(HS[o], None), (SSQ[o], None), (HG, None)], [(HS[o], None)])
                TT(P, "dve", yo[o][:], hs[o][:], hb[o][:], ALU.mult, [(HS[o], None), (HB[o], None)], [(YO[o], None)])
                P.dma("pool", D["Y"][t * 128:(t + 1) * 128, 1280 + h * 192:1280 + (h + 1) * 192], yo[o][:], reads=[(YO[o], None)],
                      writes=[(B["Y"], ("c", h, t))], sembuf=YO[o])
    if bg is not None:
        for _ in bg:
            pass


def post_norm_residual(C, t, pm4, PM4, x_ap, XB_key, xt, XT, rep, REP, junk, JK, ssp, SSP, dst_ap, DSTB, tmp, TMP):
    P = C.P
    for n in range(4):
        ACTF(P, junk[:, n * 512:(n + 1) * 512], pm4[n][:], AF.Square, [(PM4[n], None)], [(JK, n), (SSP, n)], accum=ssp[:, n:n + 1])
    TT(P, "dve", ssp[:, 4:5], ssp[:, 0:1], ssp[:, 1:2], ALU.add, [(SSP, 0), (SSP, 1)], [(SSP, "a")])
    TT(P, "dve", ssp[:, 5:6], ssp[:, 2:3], ssp[:, 3:4], ALU.add, [(SSP, 2), (SSP, 3)], [(SSP, "b")])
    TT(P, "dve", ssp[:, 6:7], ssp[:, 4:5], ssp[:, 5:6], ALU.add, [(SSP, "a"), (SSP, "b")], [(SSP, "c")])
    ACTF(P, ssp[:, 7:8], ssp[:, 6:7], AF.Sqrt, [(SSP, "c")], [(SSP, "r")], bias=EPS, scale=1.0 / D_)
    RECIP(P, ssp[:, 7:8], ssp[:, 7:8], [(SSP, "r")], [(SSP, "r")])
    for n in range(4):
        STT(P, tmp[:, n * 512:(n + 1) * 512], pm4[n][:], ssp[:, 7:8], rep[:, n * 512:(n + 1) * 512], ALU.mult, ALU.mult,
            [(PM4[n], None), (SSP, "r"), (REP, None)], [(TMP, n)])
    TT(P, "pool", xt[:], xt[:], tmp[:], ALU.add, [(XT, None), (TMP, None)], [(XT, None)])
    P.dma("pool", dst_ap[t * 128:(t + 1) * 128, :], xt[:], reads=[(XT, None)], writes=[(DSTB, t)], sembuf=XT)


def phase_outproj(C, l, xsrc, xdst):
    P, I, D, B, K, KB = C.P, C.I, C.D, C.B, C.K, C.KB
    wo = C.sb("o_w", [128, 16, D_], BF16); WO = Buf("o_w")
    rep = C.sb("o_rep", [128, D_], F32); REP = Buf("o_rep")
    rtmp = C.sb("o_rtmp", [128, 512], F32); RTMP = Buf("o_rtmp")
    yt = [C.sb("o_y%d" % i, [128, D_], BF16) for i in range(2)]; YT = [Buf("o_y%d" % i) for i in range(2)]
    yT = [C.sb("o_yT%d" % i, [128, 16, 128], BF16) for i in range(2)]; YTT = [Buf("o_yT%d" % i) for i in range(2)]
    xt = [C.sb("o_x%d" % i, [128, D_], F32) for i in range(2)]; XT = [Buf("o_x%d" % i) for i in range(2)]
    tmp = C.sb("o_tmp", [128, D_], F32); TMP = Buf("o_tmp")
    junk = C.sb("o_junk", [128, D_], BF16); JK = Buf("o_junk")
    ssp = [C.sb("o_ssp%d" % i, [128, 8], F32) for i in range(2)]; SSP = [Buf("o_ssp%d" % i) for i in range(2)]
    tp = C.ps("o_tp", [128, D_], BF16); TP = PB("o_tp")
    pm = [C.ps("o_pm%d" % i, [128, 512]) for i in range(4)]; PM = [PB("o_pm%d" % i) for i in range(4)]
    prep = C.ps("o_prep", [128, 512]); PREP = PB("o_prep")
    wov = D["WOUT"][l].rearrange("(kc p) n -> p kc n", p=128)
    for kc in range(16):
        P.dma("sp", wo[:, kc, :], wov[:, kc, :], reads=[(B["WOUT"], None)], writes=[(WO, kc)], sembuf=WO)
    replicate_cols(C, rep, REP, K["vec"][:, l, 2, :], 16, [(C.MODB, (l, 0, 2))], prep, PREP, rtmp, RTMP)
    for t in range(NT):
        s = t % 2
        P.dma("sp", yt[s][:], D["Y"][t * 128:(t + 1) * 128, :], reads=[(B["Y"], None)], writes=[(YT[s], None)], sembuf=YT[s])
        P.dma("sp", xt[s][:], xsrc[t * 128:(t + 1) * 128, :], reads=[(B["XR"], t)], writes=[(XT[s], None)], sembuf=XT[s])
        for c in range(16):
            TR(P, tp[:, c * 128:(c + 1) * 128], yt[s][:, c * 128:(c + 1) * 128], K["ident"][:], [(YT[s], None), (KB, "ident")], [(TP, c // 8)])
        yv = yT[s][:].rearrange("p c t -> p (c t)")
        CP(P, "act", yv[:, 0:1024], tp[:, 0:1024], [(TP, 0)], [(YTT[s], 0)])
        CP(P, "dve", yv[:, 1024:2048], tp[:, 1024:2048], [(TP, 1)], [(YTT[s], 1)])
        for n in range(4):
            for kc in range(16):
                MM(P, pm[n][:], yT[s][:, kc, :], wo[:, kc, n * 512:(n + 1) * 512], kc == 0, kc == 15, [(YTT[s], None), (WO, None)], [(PM[n], None)])
        post_norm_residual(C, t, pm, PM, None, None, xt[s], XT[s], rep, REP, junk, JK, ssp[s], SSP[s], xdst, B["XR"], tmp, TMP)


def phase_ffn(C, l, xsrc, xdst):
    P, I, D, B, K, KB = C.P, C.I, C.D, C.B, C.K, C.KB
    DSTB = B["XR"]
    xt = [C.sb("f_xt%d" % i, [128, D_], F32) for i in range(2)]; XT = [Buf("f_xt%d" % i) for i in range(2)]
    junk = C.sb("f_junk", [128, D_], BF16); JK = Buf("f_junk")
    ss = [C.sb("f_ss%d" % i, [128, 1], F32) for i in range(2)]
    rs = [C.sb("f_rs%d" % i, [128, 1], F32) for i in range(2)]; SS = [Buf("f_ss%d" % i) for i in range(2)]
    xn0 = C.sb("f_xn0", [128, D_], BF16); XN0 = Buf("f_xn0")
    xn = [xn0, xn0]; XN = [XN0, XN0]
    tp = C.ps("f_tp", [128, D_], BF16); TP = PB("f_tp")
    hT = C.sb("f_hT", [128, 16, 512], BF16); HT = Buf("f_hT")
    aT = C.sb("f_aT", [128, 44, 512], BF16); AT = Buf("f_aT")
    wg = [C.sb("f_wg%d" % i, [128, 16, 256], BF16) for i in range(2)]; WG = [Buf("f_wg%d" % i) for i in range(2)]
    wu = [C.sb("f_wu%d" % i, [128, 16, 256], BF16) for i in range(2)]; WU = [Buf("f_wu%d" % i) for i in range(2)]
    wd = [C.sb("f_wd%d" % i, [128, 4, 512], BF16) for i in range(2)]; WDB = [Buf("f_wd%d" % i) for i in range(2)]
    pg = C.ps("f_pg", [128, 512]); PG = PB("f_pg")
    pu = C.ps("f_pu", [128, 512]); PU = PB("f_pu")
    pm = [C.ps("f_pm%d" % i, [128, 512]) for i in range(4)]; PM = [PB("f_pm%d" % i) for i in range(4)]
    sg = [C.sb("f_sg%d" % i, [128, 512], F32) for i in range(2)]; SG = [Buf("f_sg%d" % i) for i in range(2)]
    rep = C.sb("f_rep", [128, D_], F32); REP = Buf("f_rep")
    fst = [C.sb("f_fst%d" % i, [128, D_], F32) for i in range(4)]; FST = [Buf("f_fst%d" % i) for i in range(4)]
    tmp = fst[0]; TMP = FST[0]
    ssp = [C.sb("f_ssp%d" % i, [128, 8], F32) for i in range(4)]; SSP = [Buf("f_ssp%d" % i) for i in range(4)]
    xr = xt; XRB = XT
    A = K["vec"][:, l, 3, :]; Sv = K["vec"][:, l, 4, :]
    VR = [(C.MODB, (l, 1, 0)), (C.MODB, (l, 1, 1))]
    replicate_cols(C, rep, REP, K["vec"][:, l, 5, :], 16, [(C.MODB, (l, 1, 2))], pg, PG, tmp, TMP)
    wgv = D["WG"][l].rearrange("(kc p) n -> p kc n", p=128)
    wuv = D["WU"][l].rearrange("(kc p) n -> p kc n", p=128)
    wdv = D["WD"][l].rearrange("(f p) n -> p f n", p=128)
    n_w = 0
    n_d = 0
    n_s = 0
    def norm_blk(blk):
        for ti in range(4):
            t = blk * 4 + ti
            s = t % 2
            norm_to_hT(C, t, ti, xsrc, B["XR"], xt[s], XT[s], ss[s], rs[s], SS[s], xn[s], XN[s], junk, JK, tp, TP, hT, HT, A, Sv, VR)

    norm_blk(0)
    for blk in range(8):
        for fs in range(22):
            wi = n_w % 2; n_w += 1
            P.dma("sp", wg[wi][:], wgv[:, :, fs * 256:(fs + 1) * 256], reads=[(B["WG"], None)], writes=[(WG[wi], None)], sembuf=WG[wi])
            P.dma("sp", wu[wi][:], wuv[:, :, fs * 256:(fs + 1) * 256], reads=[(B["WU"], None)], writes=[(WU[wi], None)], sembuf=WU[wi])
            for j in range(2):
                f = fs * 2 + j
                if f % 2 == 0:
                    pgb, PGB, pub, PUB = pg, PG, pu, PU
                else:
                    pgb, PGB, pub, PUB = pm[0], PM[0], pm[1], PM[1]
                for kc in range(16):
                    MM(P, pgb[:], wg[wi][:, kc, j * 128:(j + 1) * 128], hT[:, kc, :], kc == 0, kc == 15, [(WG[wi], None), (HT, None)], [(PGB, None)])
                for kc in range(16):
                    MM(P, pub[:], wu[wi][:, kc, j * 128:(j + 1) * 128], hT[:, kc, :], kc == 0, kc == 15, [(WU[wi], None), (HT, None)], [(PUB, None)])
                si = n_s % 2; n_s += 1
                ACTF(P, sg[si][:], pgb[:], AF.Silu, [(PGB, None)], [(SG[si], None)])
                TT(P, "dve", aT[:, f, :], sg[si][:], pub[:], ALU.mult, [(SG[si], None), (PUB, None)], [(AT, f)])
        if blk + 1 < 8:
            norm_blk(blk + 1)
        for n in range(4):
            for f0 in range(0, 44, 4):
                di = n_d % 2; n_d += 1
                P.dma("sp", wd[di][:], wdv[:, f0:f0 + 4, n * 512:(n + 1) * 512], reads=[(B["WD"], None)], writes=[(WDB[di], None)], sembuf=WDB[di])
                for fj in range(4):
                    f = f0 + fj
                    for ti in range(4):
                        MM(P, pm[ti][:], aT[:, f, ti * 128:(ti + 1) * 128], wd[di][:, fj, :], f == 0, f == 43,
                           [(AT, None), (WDB[di], None)], [(PM[ti], None)])
            for ti in range(4):
                cs = slice(n * 512, (n + 1) * 512)
                CP(P, "dve" if ti % 2 == 0 else "act", fst[ti][:, cs], pm[ti][:], [(PM[ti], None)], [(FST[ti], n)])
                ACTF(P, junk[:, cs], fst[ti][:, cs], AF.Square, [(FST[ti], n)], [(JK, n), (SSP[ti], n)], accum=ssp[ti][:, n:n + 1])
        for ti in range(4):
            t = blk * 4 + ti
            s = t % 2
            sq_, SQ_ = ssp[ti], SSP[ti]
            TT(P, "dve", sq_[:, 4:5], sq_[:, 0:1], sq_[:, 1:2], ALU.add, [(SQ_, 0), (SQ_, 1)], [(SQ_, "a")])
            TT(P, "dve", sq_[:, 5:6], sq_[:, 2:3], sq_[:, 3:4], ALU.add, [(SQ_, 2), (SQ_, 3)], [(SQ_, "b")])
            TT(P, "dve", sq_[:, 6:7], sq_[:, 4:5], sq_[:, 5:6], ALU.add, [(SQ_, "a"), (SQ_, "b")], [(SQ_, "c")])
            ACTF(P, sq_[:, 7:8], sq_[:, 6:7], AF.Sqrt, [(SQ_, "c")], [(SQ_, "r")], bias=EPS, scale=1.0 / D_)
            RECIP(P, sq_[:, 7:8], sq_[:, 7:8], [(SQ_, "r")], [(SQ_, "r")])
            STT(P, fst[ti][:], fst[ti][:], sq_[:, 7:8], rep[:], ALU.mult, ALU.mult, [(FST[ti], None), (SQ_, "r"), (REP, None)], [(FST[ti], None)])
            P.dma("sp", xr[s][:], xsrc[t * 128:(t + 1) * 128, :], reads=[(B["XR"], t)], writes=[(XRB[s], None)], sembuf=XRB[s])
            TT(P, "pool", xr[s][:], xr[s][:], fst[ti][:], ALU.add, [(XRB[s], None), (FST[ti], None)], [(XRB[s], None)])
            P.dma("pool", xdst[t * 128:(t + 1) * 128, :], xr[s][:], reads=[(XRB[s], None)], writes=[(DSTB, t)], sembuf=XRB[s])


def alibi_slopes(n):
    def pow2(m):
        start = 2.0 ** (-8.0 / m)
        return [start ** (i + 1) for i in range(m)]
    if math.log2(n).is_integer():
        s = pow2(n)
    else:
        p = 2 ** math.floor(math.log2(n))
        s = pow2(p) + pow2(2 * p)[0::2][: n - p]
    return np.array(s, dtype=np.float32)


def host_constants():
    c = {}
    c["c_ident"] = np.eye(128, dtype=np.float32)
    c["c_ones"] = np.ones((128, 128), np.float32)
    k = np.arange(128)[:, None]; m = np.arange(128)[None, :]
    c["c_U"] = (k <= m).astype(np.float32)
    c["c_L"] = (k >= m).astype(np.float32)
    R = np.zeros((64, 64), np.float32)
    for mm in range(32):
        R[mm + 32, mm] = -1.0
        R[mm, mm + 32] = 1.0
    c["c_R"] = R
    sl = alibi_slopes(12)
    E = np.zeros((128, 12, 3, 128), np.float32)
    kk = np.arange(128)[:, None]; qq = np.arange(128)[None, :]
    for d in range(3):
        dist = np.abs(qq - kk - (d - 1) * 128).astype(np.float32)
        for h in range(12):
            E[:, h, d, :] = np.where(dist <= 128, np.exp(-sl[h] * dist), 0.0)
    c["c_E"] = E.reshape(128, 12 * 384)
    inv = (1.0 / (np.float32(10000.0) ** (np.arange(0, 64, 2, dtype=np.float32) / np.float32(64)))).astype(np.float32)
    c["c_invf"] = np.concatenate([inv, inv])[:, None].astype(np.float32)
    return c


def colT(v, n):
    return np.ascontiguousarray(np.asarray(v, np.float32).reshape(n, 128).T)


def host_layout(inp):
    g = lambda k: np.asarray(inp[k])
    w_in = g("w_in")
    fm = np.zeros((L_, D_, NFM * 128), np.float32)
    def put(ch, cols):
        fm[:, :, ch * 128:ch * 128 + len(cols)] = w_in[:, :, cols]
    for c in range(6):
        put(FM_AQ + c, np.arange(c * 128, (c + 1) * 128))
    for kv in range(4):
        cols = np.arange(768 + kv * 64, 768 + (kv + 1) * 64)
        put(FM_AK + kv, np.concatenate([cols, cols]))
    for c in range(4):
        put(FM_BCQ + c, np.arange(1280 + c * 128, min(1280 + (c + 1) * 128, 1728)))
    put(FM_BCKV, np.arange(1728, 1856))
    put(FM_BKR, np.arange(1856, 1920))
    for h in range(4):
        put(FM_CQ + h, np.arange(1920 + h * 96, 1920 + (h + 1) * 96))
        put(FM_CK + h, np.arange(2304 + h * 96, 2304 + (h + 1) * 96))
    tm = np.ascontiguousarray(np.concatenate([w_in[:, :, 1024:1280], w_in[:, :, 2688:3456], w_in[:, :, 3472:4240], w_in[:, :, 3456:3472]], axis=2))
    shared = {
        "mod_w": g("mod_w"),
        "mod_bT": np.stack([colT(g("mod_b")[l], 96) for l in range(L_)]),
        "w_in_fm": fm, "w_in_tm": tm,
        "attn_sink": g("attn_sink")[:, None, :],
        "w_uq": np.concatenate([g("mla_w_uq"), np.zeros((L_, 64, 768), np.float32)], axis=1),
        "w_ukv": g("mla_w_ukv"),
        "gate_b": g("mlstm_gate_b")[:, None, :],
        "head_g": g("mlstm_head_g")[:, None, :],
        "w_out": g("w_out"), "w_gate": g("ffn_w_gate"), "w_up": g("ffn_w_up"), "w_down": g("ffn_w_down"),
    }
    for k, src in (("pre_mix_gT", "pre_mix_g"), ("post_mix_gT", "post_mix_g"), ("pre_ffn_gT", "pre_ffn_g"), ("post_ffn_gT", "post_ffn_g")):
        shared[k] = np.stack([colT(g(src)[l], 16) for l in range(L_)])
    qg = np.concatenate([g("mla_q_norm_g"), np.zeros((L_, 64), np.float32)], axis=1)
    shared["q_norm_gT"] = np.stack([colT(qg[l], 4) for l in range(L_)])
    shared["kv_norm_gT"] = np.stack([colT(g("mla_kv_norm_g")[l], 1) for l in range(L_)])
    shared.update(host_constants())
    x, c, pos = g("x"), g("c"), g("positions")
    maps = []
    for b in range(8):
        m = dict(shared)
        m["x"] = np.ascontiguousarray(x[b])
        m["cT"] = colT(c[b], 16)
        m["pos"] = np.ascontiguousarray(pos[b][None, :].astype(np.int32))
        maps.append(m)
    return maps


_CACHE = {}


def kernel(**inputs):
    if "nc" not in _CACHE:
        _CACHE["nc"] = build_program()[0]
    nc = _CACHE["nc"]
    maps = host_layout(inputs)
    res = run_bass_kernel_spmd(nc, maps, core_ids=list(range(8)))
    return np.stack([np.asarray(r["out"], dtype=np.float32) for r in res.results], axis=0)
```
